# Optimizing a Trainium2 kernel written in Bass

```python
import jax
import jax.numpy as jnp
from jax import lax
import numpy as np

D_MODEL = 4096
BATCH = 2
SEQ = 8192
DEPTH = 4

A_HEADS = 4
A_DK = 128
A_DV = 256
B_HEADS = 8
B_DK = 128
B_DV = 128
C_HEADS = 4
C_DK = 128
C_DV = 256
GLA_RANK = 16
GLA_TAU = 16.0
GATE_RANK = 256
D_FF = 4 * D_MODEL
CONV_K = 5
CHUNK = 64
EPS = 1e-6

A_QK = A_HEADS * A_DK
A_V = A_HEADS * A_DV
B_QK = B_HEADS * B_DK
B_V = B_HEADS * B_DV
B_QKV = 2 * B_QK + B_V
C_QK = C_HEADS * C_DK
C_V = C_HEADS * C_DV
PROJ_SIZES = (A_QK, A_QK, A_V, A_V, 4 * A_HEADS,
              B_QKV, B_V, 4 * B_HEADS,
              C_QK, C_QK, C_V, C_V, 2 * GLA_RANK,
              GATE_RANK)
PROJ_WIDTH = sum(PROJ_SIZES)

kernel_name = 'hybrid_bidir_mlstm_gdn_gla_encoder'


def _rmsnorm(x, g):
    xf = x.astype(jnp.float32)
    y = xf * lax.rsqrt(jnp.mean(xf * xf, axis=-1, keepdims=True) + EPS)
    return (y * g.astype(jnp.float32)).astype(x.dtype)


def _head_rmsnorm(h, g):
    bsz, seq, nh, d = h.shape
    y = h * lax.rsqrt(jnp.mean(h * h, axis=-1, keepdims=True) + EPS)
    return y.reshape(bsz, seq, nh * d) * g.astype(jnp.float32)


def _l2norm(h):
    return h * lax.rsqrt(jnp.sum(h * h, axis=-1, keepdims=True) + EPS)


def _heads(a, nh):
    return a.astype(jnp.float32).reshape(a.shape[0], a.shape[1], nh, -1)


def _to_chunks(a):
    bsz, seq, nh = a.shape[:3]
    rest = a.shape[3:]
    a = a.reshape((bsz, seq // CHUNK, CHUNK, nh) + rest)
    return a.transpose((1, 0, 3, 2) + tuple(range(4, a.ndim)))


def _from_chunks(a):
    n, bsz, nh, L, d = a.shape
    return a.transpose(1, 0, 3, 2, 4).reshape(bsz, n * L, nh, d)


def _bidir(fn, shared, gates_f, gates_b):
    rev = lambda a: jnp.flip(a, axis=1)
    y_f = fn(*shared, *gates_f)
    y_b = fn(*[rev(a) for a in shared], *[rev(a) for a in gates_b])
    return y_f + rev(y_b)


def _mlstm_dir(q, k, v, i_pre, log_f):
    bsz, seq, nh, dk = q.shape
    dv = v.shape[-1]
    incl = jnp.tril(jnp.ones((CHUNK, CHUNK), dtype=bool))
    xs = (_to_chunks(q), _to_chunks(k * dk ** -0.5), _to_chunks(v), _to_chunks(i_pre), _to_chunks(log_f))

    def step(carry, inp):
        C, n, m = carry
        q_, k_, v_, i_, lf_ = inp
        F = jnp.cumsum(lf_, axis=-1)
        d_log = jnp.where(incl, F[..., :, None] - F[..., None, :] + i_[..., None, :], -jnp.inf)
        inter_log = F + m[..., None]
        m_t = jnp.maximum(inter_log, jnp.max(d_log, axis=-1))
        s = jnp.einsum('bhtd,bhsd->bhts', q_, k_) * jnp.exp(d_log - m_t[..., None])
        w_inter = jnp.exp(inter_log - m_t)
        num = jnp.einsum('bhts,bhsv->bhtv', s, v_) + w_inter[..., None] * jnp.einsum('bhtd,bhvd->bhtv', q_, C)
        den = jnp.sum(s, axis=-1) + w_inter * jnp.einsum('bhtd,bhd->bht', q_, n)
        h = num / jnp.maximum(jnp.abs(den), jnp.exp(-m_t))[..., None]
        m_new = m_t[..., -1]
        w_k = jnp.exp(F[..., -1:] - F + i_ - m_new[..., None])
        a = jnp.exp(F[..., -1] + m - m_new)
        C = a[..., None, None] * C + jnp.einsum('bhsv,bhsd->bhvd', v_ * w_k[..., None], k_)
        n = a[..., None] * n + jnp.einsum('bhs,bhsd->bhd', w_k, k_)
        return (C, n, m_new), h

    init = (jnp.zeros((bsz, nh, dv, dk), jnp.float32),
            jnp.zeros((bsz, nh, dk), jnp.float32),
            jnp.zeros((bsz, nh), jnp.float32))
    _, h = lax.scan(step, init, xs)
    return _from_chunks(h)


def _gdn_dir(q, k, v, g, beta):
    bsz, seq, nh, dk = q.shape
    dv = v.shape[-1]
    incl = jnp.tril(jnp.ones((CHUNK, CHUNK), dtype=bool))
    strict = jnp.tril(jnp.ones((CHUNK, CHUNK), dtype=bool), k=-1)
    eye = jnp.eye(CHUNK, dtype=jnp.float32)
    qc = _to_chunks(q * dk ** -0.5)
    kc = _to_chunks(k)
    vc = _to_chunks(v)
    G = jnp.cumsum(_to_chunks(g), axis=-1)
    bc = _to_chunks(beta)
    diff = G[..., :, None] - G[..., None, :]
    kk = jnp.einsum('nbhtd,nbhsd->nbhts', kc, kc)
    lower = eye + bc[..., :, None] * kk * jnp.exp(jnp.where(strict, diff, -jnp.inf))
    w = lax.linalg.triangular_solve(lower, (bc * jnp.exp(G))[..., None] * kc,
                                    left_side=True, lower=True, unit_diagonal=True)
    u = lax.linalg.triangular_solve(lower, bc[..., None] * vc,
                                    left_side=True, lower=True, unit_diagonal=True)
    a_qk = jnp.einsum('nbhtd,nbhsd->nbhts', qc, kc) * jnp.exp(jnp.where(incl, diff, -jnp.inf))

    def step(S, inp):
        q_, k_, w_, u_, a_, G_ = inp
        U = u_ - jnp.einsum('bhtd,bhdv->bhtv', w_, S)
        o = jnp.exp(G_)[..., None] * jnp.einsum('bhtd,bhdv->bhtv', q_, S) + jnp.einsum('bhts,bhsv->bhtv', a_, U)
        k_dec = k_ * jnp.exp(G_[..., -1:] - G_)[..., None]
        S = jnp.exp(G_[..., -1])[..., None, None] * S + jnp.einsum('bhsd,bhsv->bhdv', k_dec, U)
        return S, o

    s0 = jnp.zeros((bsz, nh, dk, dv), jnp.float32)
    _, o = lax.scan(step, s0, (qc, kc, w, u, a_qk, G))
    return _from_chunks(o)


def _gla_dir(q, k, v, log_a):
    bsz, seq, nh, dk = q.shape
    dv = v.shape[-1]
    incl = jnp.tril(jnp.ones((CHUNK, CHUNK), dtype=bool))[:, :, None]
    xs = (_to_chunks(q * dk ** -0.5), _to_chunks(k), _to_chunks(v), _to_chunks(log_a))

    def step(S, inp):
        q_, k_, v_, g_ = inp
        b = jnp.cumsum(g_, axis=-2)
        dec = jnp.exp(jnp.where(incl, b[..., :, None, :] - b[..., None, :, :], -jnp.inf))
        att = jnp.einsum('bhtsc,bhsc->bhts', q_[..., :, None, :] * dec, k_)
        o = jnp.einsum('bhts,bhsv->bhtv', att, v_) + jnp.einsum('bhtc,bhcv->bhtv', q_ * jnp.exp(b), S)
        b_last = b[..., -1:, :]
        S = jnp.exp(b_last[..., 0, :])[..., None] * S + jnp.einsum('bhsc,bhsv->bhcv', k_ * jnp.exp(b_last - b), v_)
        return S, o

    s0 = jnp.zeros((bsz, nh, dk, dv), jnp.float32)
    _, o = lax.scan(step, s0, xs)
    return _from_chunks(o)


def _short_conv(u, w, b):
    ch = u.shape[-1]
    y = lax.conv_general_dilated(u, w[:, None, :].astype(u.dtype), window_strides=(1,),
                                 padding=[(CONV_K // 2, CONV_K // 2)],
                                 dimension_numbers=('NWC', 'WIO', 'NWC'), feature_group_count=ch)
    return jax.nn.silu(y + b.astype(u.dtype))


def _gated_branch(y, w_b, gh, w_g, b_g, dtype):
    g = jax.nn.sigmoid((gh @ w_g).astype(jnp.float32) + b_g.astype(jnp.float32))
    return g * (y.astype(dtype) @ w_b).astype(jnp.float32)


def setup_inputs(seed: int = 0) -> dict:
    key = jax.random.key(seed)
    ks = jax.random.split(key, 26)
    nrm = lambda k, s: jax.random.normal(k, s, jnp.float32)
    lin = lambda k, s, fan_in: nrm(k, s) * fan_in ** -0.5
    f_bias = jnp.linspace(3.0, 6.0, A_HEADS, dtype=jnp.float32)
    gate_base = jnp.stack([jnp.zeros_like(f_bias), jnp.zeros_like(f_bias), f_bias, f_bias])
    dt = jnp.exp(jax.random.uniform(ks[8], (DEPTH, 2, B_HEADS), jnp.float32, np.log(1e-3), np.log(1e-1)))
    return {
        'x': nrm(ks[0], (BATCH, SEQ, D_MODEL)),
        'norm1_g': 1.0 + 0.02 * nrm(ks[1], (DEPTH, D_MODEL)),
        'w_in': lin(ks[2], (DEPTH, D_MODEL, PROJ_WIDTH), D_MODEL),
        'conv_w': lin(ks[3], (DEPTH, CONV_K, B_QKV), CONV_K),
        'conv_b': 0.01 * nrm(ks[4], (DEPTH, B_QKV)),
        'mlstm_gate_b': gate_base[None] + 0.01 * nrm(ks[5], (DEPTH, 4, A_HEADS)),
        'mlstm_norm_g': 1.0 + 0.02 * nrm(ks[6], (DEPTH, A_V)),
        'gdn_a_log': jnp.log(jax.random.uniform(ks[7], (DEPTH, 2, B_HEADS), jnp.float32, 1.0, 16.0)),
        'gdn_dt_bias': dt + jnp.log(-jnp.expm1(-dt)),
        'gdn_norm_g': 1.0 + 0.02 * nrm(ks[9], (DEPTH, B_V)),
        'gla_w_gate': lin(ks[10], (DEPTH, 2, GLA_RANK, C_QK), GLA_RANK),
        'gla_b_gate': 0.01 * nrm(ks[11], (DEPTH, 2, C_QK)),
        'gla_norm_g': 1.0 + 0.02 * nrm(ks[12], (DEPTH, C_V)),
        'w_branch_a': lin(ks[13], (DEPTH, A_V, D_MODEL), A_V),
        'w_branch_b': lin(ks[14], (DEPTH, B_V, D_MODEL), B_V),
        'w_branch_c': lin(ks[15], (DEPTH, C_V, D_MODEL), C_V),
        'w_merge_gate': lin(ks[16], (DEPTH, 3, GATE_RANK, D_MODEL), GATE_RANK),
        'b_merge_gate': 0.01 * nrm(ks[17], (DEPTH, 3, D_MODEL)),
        'w_out': lin(ks[18], (DEPTH, D_MODEL, D_MODEL), D_MODEL),
        'norm2_g': 1.0 + 0.02 * nrm(ks[19], (DEPTH, D_MODEL)),
        'w_ff1': lin(ks[20], (DEPTH, D_MODEL, D_FF), D_MODEL),
        'w_ff2': lin(ks[21], (DEPTH, D_FF, D_MODEL), D_FF),
        'final_g': 1.0 + 0.02 * nrm(ks[22], (D_MODEL,)),
    }


def reference(x, norm1_g, w_in, conv_w, conv_b, mlstm_gate_b, mlstm_norm_g,
              gdn_a_log, gdn_dt_bias, gdn_norm_g, gla_w_gate, gla_b_gate, gla_norm_g,
              w_branch_a, w_branch_b, w_branch_c, w_merge_gate, b_merge_gate, w_out,
              norm2_g, w_ff1, w_ff2, final_g):
    f32 = jnp.float32
    bsz, seq, _ = x.shape
    split_at = np.cumsum(PROJ_SIZES)[:-1].tolist()
    for l in range(DEPTH):
        xn = _rmsnorm(x, norm1_g[l])
        proj = jnp.einsum('btd,de->bte', xn, w_in[l])
        (aq, ak, av, ao, agt, bqkv, bz, bgt, cq, ck, cv, cr, clr, gh) = jnp.split(proj, split_at, axis=-1)

        agt = agt.astype(f32).reshape(bsz, seq, 4, A_HEADS) + mlstm_gate_b[l].astype(f32)
        h_a = _bidir(_mlstm_dir,
                     (_heads(aq, A_HEADS), _heads(ak, A_HEADS), _heads(av, A_HEADS)),
                     (agt[:, :, 0], jax.nn.log_sigmoid(agt[:, :, 2])),
                     (agt[:, :, 1], jax.nn.log_sigmoid(agt[:, :, 3])))
        y_a = jax.nn.sigmoid(ao.astype(f32)) * _head_rmsnorm(h_a, mlstm_norm_g[l])

        qkv = _short_conv(bqkv.astype(f32), conv_w[l], conv_b[l])
        bq, bk, bv = jnp.split(qkv, [B_QK, 2 * B_QK], axis=-1)
        bq = _l2norm(_heads(bq, B_HEADS))
        bk = _l2norm(_heads(bk, B_HEADS))
        bv = _heads(bv, B_HEADS)
        bgt = bgt.astype(f32).reshape(bsz, seq, 4, B_HEADS)
        a_rate = jnp.exp(gdn_a_log[l].astype(f32))
        dtb = gdn_dt_bias[l].astype(f32)
        dec_f = -a_rate[0] * jax.nn.softplus(bgt[:, :, 0] + dtb[0])
        dec_b = -a_rate[1] * jax.nn.softplus(bgt[:, :, 1] + dtb[1])
        h_b = _bidir(_gdn_dir, (bq, bk, bv),
                     (dec_f, jax.nn.sigmoid(bgt[:, :, 2])),
                     (dec_b, jax.nn.sigmoid(bgt[:, :, 3])))
        y_b = _head_rmsnorm(h_b, gdn_norm_g[l]) * jax.nn.silu(bz.astype(f32))

        clr = clr.astype(f32).reshape(bsz, seq, 2, GLA_RANK)
        lg = jax.nn.log_sigmoid(jnp.einsum('btnr,nrc->btnc', clr, gla_w_gate[l].astype(f32))
                                + gla_b_gate[l].astype(f32)) / GLA_TAU
        lg_f = lg[:, :, 0].reshape(bsz, seq, C_HEADS, C_DK)
        lg_b = lg[:, :, 1].reshape(bsz, seq, C_HEADS, C_DK)
        h_c = _bidir(_gla_dir, (_heads(cq, C_HEADS), _heads(ck, C_HEADS), _heads(cv, C_HEADS)),
                     (lg_f,), (lg_b,))
        y_c = _head_rmsnorm(h_c, gla_norm_g[l]) * jax.nn.silu(cr.astype(f32))

        mix = (_gated_branch(y_a, w_branch_a[l], gh, w_merge_gate[l, 0], b_merge_gate[l, 0], x.dtype)
               + _gated_branch(y_b, w_branch_b[l], gh, w_merge_gate[l, 1], b_merge_gate[l, 1], x.dtype)
               + _gated_branch(y_c, w_branch_c[l], gh, w_merge_gate[l, 2], b_merge_gate[l, 2], x.dtype))
        x = x + jnp.einsum('btd,de->bte', mix.astype(x.dtype), w_out[l])

        xn = _rmsnorm(x, norm2_g[l])
        hid = jnp.square(jax.nn.relu(jnp.einsum('btd,df->btf', xn, w_ff1[l])))
        x = x + jnp.einsum('btf,fd->btd', hid, w_ff2[l])
    return _rmsnorm(x, final_g)
```

```python
import numpy as np
from contextlib import ExitStack
import concourse.bass as bass
import concourse.mybir as mybir
from concourse.bass_utils import run_bass_kernel_spmd

F32 = mybir.dt.float32
BF16 = mybir.dt.bfloat16
ALU = mybir.AluOpType
AF = mybir.ActivationFunctionType

ENGS = ("pe", "dve", "act", "pool", "sp")
NDSEM = 6
NEG = -30000.0
EPS = 1e-6
NCORES = 8


class Res:
    __slots__ = ("w", "r")

    def __init__(self):
        self.w = None
        self.r = {}


class T:
    def __init__(self, h, nparts=1):
        self.h = h
        self.parts = [Res() for _ in range(nparts)]

    def __getitem__(self, idx):
        return self.h[idx]

    def ap(self):
        return self.h.ap() if hasattr(self.h, "ap") else self.h[:]

    def res(self, i=None):
        if i is None:
            return self.parts
        if isinstance(i, (list, tuple, range)):
            return [self.parts[j] for j in i]
        return [self.parts[i]]


class Prog:
    def __init__(self, nc, es):
        self.nc = nc
        self.es0 = es
        self.es = es
        self.ops = {e: [] for e in ENGS}
        self.cnt = {e: 0 for e in ENGS}
        self.sem = {e: es.enter_context(nc.semaphore("s_" + e)) for e in ENGS}
        self.dsem = {}
        self.dcnt = {}
        self.drr = {}
        for q in ("sp", "pool", "cc"):
            self.dsem[q] = [es.enter_context(nc.semaphore("d_%s%d" % (q, i))) for i in range(NDSEM)]
            self.dcnt[q] = [0] * NDSEM
            self.drr[q] = 0
        self.seen = {e: {} for e in ENGS}
        self.psum_rr = 0
        self.nins = 0

    def sb(self, name, shape, dtype, nparts=1):
        self.nins += 0
        self._uid = getattr(self, "_uid", 0) + 1
        return T(self.es.enter_context(self.nc.sbuf_tensor("%s_u%d" % (name, self._uid), list(shape), dtype)), nparts)

    def dram(self, name, shape, dtype, nparts=1):
        return T(self.nc.dram_tensor(name, list(shape), dtype, kind="Internal"), nparts)

    def _semh(self, key):
        if isinstance(key, str):
            return self.sem[key]
        return self.dsem[key[0]][key[1]]

    def _deps(self, eng, reads, writes):
        need = {}
        for r in reads:
            if r.w is not None and need.get(r.w[0], 0) < r.w[1]:
                need[r.w[0]] = r.w[1]
        for w in writes:
            if w.w is not None and need.get(w.w[0], 0) < w.w[1]:
                need[w.w[0]] = w.w[1]
            for k, v in w.r.items():
                if need.get(k, 0) < v:
                    need[k] = v
        waits = []
        seen = self.seen[eng]
        for k, v in need.items():
            if seen.get(k, 0) < v:
                seen[k] = v
                waits.append((k, v))
        return waits

    def _mark(self, key, v, reads, writes):
        for r in reads:
            r.r[key] = v
        for w in writes:
            w.w = (key, v)
            w.r = {}

    def op(self, eng, fn, reads=(), writes=()):
        waits = self._deps(eng, reads, writes)
        if eng == "pe":
            waits = [(k, v) for (k, v) in waits if k != "pe"]
        self.cnt[eng] += 1
        v = self.cnt[eng]
        self.ops[eng].append((waits, fn, (eng, 1)))
        self._mark(eng, v, reads, writes)
        self.nins += 1

    def _async(self, q, cls, fn, reads, writes, inc):
        i = self.drr[cls]
        self.drr[cls] = (i + 1) % NDSEM
        key = (cls, i)
        waits = self._deps(q, reads, writes)
        prev = self.dcnt[cls][i]
        if prev and self.seen[q].get(key, 0) < prev:
            self.seen[q][key] = prev
            waits.append((key, prev))
        self.dcnt[cls][i] += inc
        v = self.dcnt[cls][i]
        self.ops[q].append((waits, fn, (key, inc)))
        self._mark(key, v, reads, writes)
        self.nins += 1

    def dma(self, out_ap, in_ap, reads=(), writes=(), q="sp", slow=False):
        cls = "sp" if q == "sp" else "pool"
        if slow:
            self._async(q, cls, lambda e, o=out_ap, a=in_ap: e.dma_start(out=o, in_=a, allow_slow_non_contiguous=True), reads, writes, 16)
        else:
            self._async(q, cls, lambda e, o=out_ap, a=in_ap: e.dma_start(out=o, in_=a), reads, writes, 16)

    def coll(self, kind, out_ap, in_ap, groups, reads=(), writes=()):
        self._async("pool", "cc",
                    lambda e, o=out_ap, a=in_ap: e.collective_compute(kind, ALU.bypass, replica_groups=groups,
                                                                      ins=[a], outs=[o]),
                    reads, writes, 1)

    def barrier(self, full=False):
        tgt = {e: self.cnt[e] for e in ENGS if self.cnt[e]}
        for cls in (("sp", "pool", "cc") if full else ("sp",)):
            for i in range(NDSEM):
                if self.dcnt[cls][i]:
                    tgt[(cls, i)] = self.dcnt[cls][i]
        for e in ENGS:
            waits = []
            for k, v in tgt.items():
                if self.seen[e].get(k, 0) < v:
                    self.seen[e][k] = v
                    waits.append((k, v))
            self.ops[e].append((waits, None, None))

    def flush(self):
        nc = self.nc
        ops = self.ops
        self.ops = {e: [] for e in ENGS}
        with nc.Block() as block:
            def run(e, h):
                for waits, fn, inc in ops[e]:
                    for k, v in waits:
                        h.wait_ge(self._semh(k), v)
                    if fn is not None:
                        fn(h).then_inc(self._semh(inc[0]), inc[1])

            block.tensor(lambda h: run("pe", h))
            block.vector(lambda h: run("dve", h))
            block.scalar(lambda h: run("act", h))
            block.gpsimd(lambda h: run("pool", h))
            block.sync(lambda h: run("sp", h))

    class _Phase:
        def __init__(self, P):
            self.P = P

        def __enter__(self):
            self.es = ExitStack()
            self.es.__enter__()
            self.P.es = self.es
            return self.P

        def __exit__(self, *a):
            self.P.barrier()
            self.P.flush()
            self.P.es = self.P.es0
            return self.es.__exit__(*a)

    def phase(self):
        return Prog._Phase(self)


A_HEADS, A_DK, A_DV = 4, 128, 256
B_HEADS, B_DK, B_DV = 8, 128, 128
C_HEADS, C_DK, C_DV = 4, 128, 256
GLA_RANK, GLA_TAU, GATE_RANK, CONV_K = 16, 16.0, 256, 5
A_QK, A_V = 512, 1024
B_QK, B_V, B_QKV = 1024, 1024, 3072
C_QK, C_V = 512, 1024
PROJ_SIZES = (A_QK, A_QK, A_V, A_V, 16, B_QKV, B_V, 32, C_QK, C_QK, C_V, C_V, 32, GATE_RANK)
OFF = np.concatenate([[0], np.cumsum(PROJ_SIZES)]).tolist()
(O_AQ, O_AK, O_AV, O_AO, O_AGT, O_BQKV, O_BZ, O_BGT, O_CQ, O_CK, O_CV, O_CR, O_CLR, O_GH) = OFF[:14]
NFM = 11
NTM = 5
NCOLS = NFM * 128 + NTM * 256 + 256


def my_cols(j):
    r = lambda a, n: list(range(a, a + n))
    cols = []
    cols += r(O_AQ + j * 128, 128) + r(O_AK + j * 128, 128)
    for part in range(3):
        for hh in (2 * j, 2 * j + 1):
            cols += r(O_BQKV + part * 1024 + hh * 128, 128)
    cols += r(O_CQ + j * 128, 128) + r(O_CK + j * 128, 128)
    small = [-1] * 128
    for g in range(4):
        small[g] = O_AGT + g * 4 + j
    k = 4
    for g in range(4):
        for hh in (2 * j, 2 * j + 1):
            small[k] = O_BGT + g * 8 + hh
            k += 1
    for i in range(16):
        small[32 + i] = O_CLR + i
        small[64 + i] = O_CLR + 16 + i
    cols += small
    cols += r(O_AV + j * 256, 256) + r(O_AO + j * 256, 256)
    cols += r(O_BZ + 2 * j * 128, 256)
    cols += r(O_CV + j * 256, 256) + r(O_CR + j * 256, 256)
    cols += r(O_GH, 256)
    assert len(cols) == NCOLS
    return cols


def block_widths():
    return [128] * NFM + [256] * NTM + [128, 128]


def tile_major(W):
    K, N = W.shape
    return np.ascontiguousarray(W.reshape(K // 128, 128, N // 128, 128).transpose(2, 1, 0, 3)).reshape(-1)


def piece_plan(E):
    P = max(1, -(-E // (8 * 131072)))
    while E % (8 * P) != 0:
        P += 1
    pe = E // (8 * P)
    b = 1
    for cand in (2048, 1024, 512, 256, 128, 64, 661, 1):
        if pe % cand == 0:
            b = cand
            break
    return P, pe, pe // b, b


class Cfg:
    def __init__(self, D, FF, S, L):
        self.D, self.FF, self.S, self.L = D, FF, S, L
        self.KT = D // 128
        self.FT = FF // 128
        self.TL = S // 4
        self.NCH = S // 128
        self.TT = min(256, self.TL)
        self.PT = 256
        self.YP = min(512, S)
        self.big = [("wa", 1024, D), ("wb", 1024, D), ("wc", 1024, D), ("wg", 768, D),
                    ("wo", D, D), ("w1", D, FF), ("w2", FF, D)]


def sp_layout(cfg):
    L, KT = cfg.L, cfg.KT
    o = {}
    n = 0
    for name, w in [("g1", L * KT), ("g2", L * KT), ("gf", KT), ("bm", L * 3 * KT), ("convw", L * 6 * 5),
                    ("convb", L * 6), ("agb", L * 4), ("ang", L * 256), ("gdn_alog", L * 4), ("gdn_dtb", L * 4),
                    ("bng", L * 256), ("glaw", L * 2 * 128), ("glab", L * 2), ("cng", L * 256)]:
        o[name] = n
        n += w
    o["_n"] = n
    return o


def make_consts():
    c = np.zeros((128, 8, 128), np.float32)
    s = np.arange(128)[:, None]
    t = np.arange(128)[None, :]
    c[:, 0] = np.eye(128)
    c[:, 1] = 1.0
    c[:, 2] = np.where(s <= t, 0, NEG)
    c[:, 3] = np.where(s < t, 0, NEG)
    c[:, 4] = np.where(s >= t, 0, NEG)
    c[:, 5] = np.where(s > t, 0, NEG)
    c[:, 6] = (s <= t)
    c[:, 7] = (s >= t)
    return c.reshape(128, 1024)


def prep_inputs(inputs, cfg):
    D, FF, S, L, KT = cfg.D, cfg.FF, cfg.S, cfg.L, cfg.KT
    f = lambda k: np.asarray(inputs[k], dtype=np.float32)
    x = f("x")
    spo = sp_layout(cfg)
    bigsrc = {"wa": f("w_branch_a"), "wb": f("w_branch_b"), "wc": f("w_branch_c"),
              "wg": f("w_merge_gate").reshape(L, 768, D), "wo": f("w_out"), "w1": f("w_ff1"), "w2": f("w_ff2")}
    w_in = f("w_in")
    consts = make_consts()
    shards = {}
    for name, K_, N_ in cfg.big:
        P_, pe, a, b = piece_plan(K_ * N_)
        for l in range(L):
            if name == "wg":
                flat = np.concatenate([tile_major(bigsrc[name][l][jb * 256:(jb + 1) * 256]) for jb in range(3)])
            else:
                flat = tile_major(bigsrc[name][l])
            shards[(name, l)] = flat.reshape(P_, 8, pe)
    in_maps = []
    bw = block_widths()
    for c in range(NCORES):
        b_, j = c // 4, c % 4
        m = {}
        m["x_own"] = np.ascontiguousarray(x[b_, j * cfg.TL:(j + 1) * cfg.TL, :])
        m["consts"] = consts
        rm = np.zeros((128, 4), np.float32)
        rm[:, j] = 1.0
        m["rmask"] = rm
        cols = np.array(my_cols(j))
        for l in range(L):
            wsel = np.where(cols[None, :] >= 0, w_in[l][:, np.maximum(cols, 0)], 0.0).astype(np.float32)
            blocks = []
            c0 = 0
            for w_ in bw:
                blk = wsel[:, c0:c0 + w_]
                blocks.append(np.ascontiguousarray(blk.reshape(KT, 128, w_).transpose(1, 0, 2)).reshape(-1))
                c0 += w_
            m["win_%d" % l] = np.concatenate(blocks).reshape(D, NCOLS)
            for name, K_, N_ in cfg.big:
                P_, pe, a, bb = piece_plan(K_ * N_)
                m["%s_%d" % (name, l)] = np.ascontiguousarray(shards[(name, l)][:, c, :]).reshape(P_ * a, bb)
        sp = np.zeros((128, spo["_n"]), np.float32)
        tm = lambda v: v.reshape(KT, 128).T
        for l in range(L):
            sp[:, spo["g1"] + l * KT: spo["g1"] + (l + 1) * KT] = tm(f("norm1_g")[l])
            sp[:, spo["g2"] + l * KT: spo["g2"] + (l + 1) * KT] = tm(f("norm2_g")[l])
            for jb in range(3):
                o = spo["bm"] + (l * 3 + jb) * KT
                sp[:, o:o + KT] = tm(f("b_merge_gate")[l, jb])
            for blk in range(6):
                part, hh = blk // 2, 2 * j + blk % 2
                ch = part * 1024 + hh * 128
                o = spo["convw"] + (l * 6 + blk) * 5
                sp[:, o:o + 5] = f("conv_w")[l][:, ch:ch + 128].T
                sp[:, spo["convb"] + l * 6 + blk] = f("conv_b")[l][ch:ch + 128]
            sp[:, spo["agb"] + l * 4: spo["agb"] + l * 4 + 4] = f("mlstm_gate_b")[l][:, j][None, :]
            sp[:, spo["ang"] + l * 256: spo["ang"] + (l + 1) * 256] = f("mlstm_norm_g")[l][j * 256:(j + 1) * 256][None, :]
            for d_ in range(2):
                for hh in range(2):
                    sp[:, spo["gdn_alog"] + l * 4 + d_ * 2 + hh] = f("gdn_a_log")[l, d_, 2 * j + hh]
                    sp[:, spo["gdn_dtb"] + l * 4 + d_ * 2 + hh] = f("gdn_dt_bias")[l, d_, 2 * j + hh]
            sp[:, spo["bng"] + l * 256: spo["bng"] + (l + 1) * 256] = f("gdn_norm_g")[l][2 * j * 128:(2 * j + 2) * 128][None, :]
            for d_ in range(2):
                o = spo["glaw"] + (l * 2 + d_) * 128
                sp[0:16, o:o + 128] = f("gla_w_gate")[l, d_][:, j * 128:(j + 1) * 128]
                sp[:, spo["glab"] + l * 2 + d_] = f("gla_b_gate")[l, d_][j * 128:(j + 1) * 128]
            sp[:, spo["cng"] + l * 256: spo["cng"] + (l + 1) * 256] = f("gla_norm_g")[l][j * 256:(j + 1) * 256][None, :]
        sp[:, spo["gf"]: spo["gf"] + KT] = tm(f("final_g"))
        m["sp"] = sp
        in_maps.append(m)
    return in_maps


class Ctx:
    pass


def flat_blocks(t, nblk_elems):
    a = t.h.ap()
    fl = a.rearrange("r b -> (r b)")
    return fl.rearrange("(n p f) -> n p f", p=128, f=nblk_elems // 128)


def build(cfg, debug=()):
    D, FF, S, L, KT, FT, TL, NCH, TT, PT = cfg.D, cfg.FF, cfg.S, cfg.L, cfg.KT, cfg.FT, cfg.TL, cfg.NCH, cfg.TT, cfg.PT
    nc = bass.Bass("TRN2", target_bir_lowering=False)
    K = Ctx()
    K.cfg, K.nc = cfg, nc
    spo = sp_layout(cfg)
    K.spo = spo
    ext = lambda name, shape, dt=F32: T(nc.dram_tensor(name, list(shape), dt, kind="ExternalInput"))
    K.x_own = ext("x_own", [TL, D])
    K.consts_d = ext("consts", [128, 1024])
    K.rmask_d = ext("rmask", [128, 4])
    K.sp_d = ext("sp", [128, spo["_n"]])
    K.win_d = [ext("win_%d" % l, [D, NCOLS]) for l in range(L)]
    K.big_d = {}
    for name, K_, N_ in cfg.big:
        P_, pe, a, b = piece_plan(K_ * N_)
        for l in range(L):
            K.big_d[(name, l)] = ext("%s_%d" % (name, l), [P_ * a, b])
    K.out = T(nc.dram_tensor("out", [TL, D], F32, kind="ExternalOutput"))
    K.stub_y = ext("stub_y", [S // cfg.YP * 768, cfg.YP], BF16) if "stub_y" in debug else None
    K.debug = debug
    K.which = "abc"
    K.post = True
    K.post_lvl = 9
    for d_ in debug:
        if d_.startswith("postlvl="):
            K.post_lvl = int(d_[8:])
    for d_ in debug:
        if d_.startswith("which="):
            K.which = d_[6:]
        if d_ == "nopost":
            K.post = False
    K.dbg_out = []
    with ExitStack() as es:
        P = Prog(nc, es)
        K.P = P
        K.ps = [T(es.enter_context(nc.psum_tensor("ps%d" % i, [128, 512], F32))) for i in range(7)]
        K.psb = T(es.enter_context(nc.psum_tensor("psb", [128, 1024], BF16)), nparts=4)
        K.ps_rr = 0
        K.psb_rr = 0
        K.cst = P.sb("cst", [128, 1024], F32)
        K.spt = P.sb("spt", [128, spo["_n"]], F32)
        K.rmask = P.sb("rmaskt", [128, 4], F32)
        K.identb = P.sb("identb", [128, 128], BF16)
        P.dma(K.cst[:], K.consts_d[:], K.consts_d.res(), K.cst.res())
        P.dma(K.spt[:], K.sp_d[:], K.sp_d.res(), K.spt.res())
        P.dma(K.rmask[:], K.rmask_d[:], K.rmask_d.res(), K.rmask.res())
        P.op("dve", lambda e: e.tensor_copy(K.identb[:], K.cst[:, 0:128]), K.cst.res(), K.identb.res())
        K.xres = P.dram("xres", [D, TL], F32, nparts=TL // TT)
        K.xnp = P.dram("xnp", [TL // 128 * 128, KT * 128], BF16, nparts=TL // 128)
        K.xng = P.dram("xng", [TL // 128 * 4 * 128, KT * 128], BF16, nparts=TL // 128)
        K.ghT = P.dram("ghT", [256, TL], BF16)
        K.wfull = {}
        K.winb = []
        for l in range(L):
            K.winb.append(P.dram("winb_%d" % l, [D, NCOLS], BF16))
            for name, K_, N_ in cfg.big:
                P_, pe, a, b = piece_plan(K_ * N_)
                K.wfull[(name, l)] = P.dram("wf_%s_%d" % (name, l), [P_ * 8 * a, b], BF16)
                K.wfull[(name, l)].tmp = P.dram("wsb_%s_%d" % (name, l), [P_ * a, b], BF16)
                K.wfull[(name, l)].half = P.dram("wsh_%s_%d" % (name, l), [P_ * 4 * a, b], BF16, nparts=P_)
        alloc_mixer_dram(K)
        if "dumpH" in debug:
            K.dbg_out += [("H0", K.H[0]), ("H1", K.H[1]), ("fm32", K.fm32), ("fm16", K.fm16)]
        if "dumpY" in debug:
            K.dbg_out += [("yp", K.yp)]
        phase_weights(K, 0)
        phase_x0(K)
        for l in range(L):
            phase_a(K, l)
            if l + 1 < L:
                phase_weights(K, l + 1)
            phase_b(K, l)
            phase_dense(K, l, final=(l == L - 1))
        for name, t in K.dbg_out:
            o = T(nc.dram_tensor("dbg_" + name, list(t.h.shape), t.h.dtype, kind="ExternalOutput"))
            P.dma(o.h.ap(), t.h.ap(), t.res(), o.res(), q="pool")
        P.barrier(full=True)
        P.flush()
    return nc


def psum(K):
    i = K.ps_rr
    K.ps_rr = (i + 1) % len(K.ps)
    return K.ps[i]


def phase_weights(K, l):
    P, cfg = K.P, K.cfg
    g4 = [[0, 1, 2, 3], [4, 5, 6, 7]]
    g2 = [[0, 4], [1, 5], [2, 6], [3, 7]]
    rows = cfg.D
    step = max(1, rows // 8)
    for r0 in range(0, rows, step):
        P.dma(K.winb[l][r0:r0 + step, :], K.win_d[l][r0:r0 + step, :], K.win_d[l].res(), K.winb[l].res(), q="pool")
    for name, K_, N_ in cfg.big:
        P_, pe, a, b = piece_plan(K_ * N_)
        src, full = K.big_d[(name, l)], K.wfull[(name, l)]
        tmp, half = full.tmp, full.half
        nrow = P_ * a
        step = max(a, (nrow // 8 // a) * a) if nrow >= 8 * a else nrow
        for r0 in range(0, nrow, step):
            r1 = min(nrow, r0 + step)
            P.dma(tmp[r0:r1, :], src[r0:r1, :], src.res(), tmp.res(), q="pool")
        for p in range(P_):
            P.coll("AllGather", half[p * 4 * a:(p + 1) * 4 * a, :], tmp[p * a:(p + 1) * a, :], g4, tmp.res(), half.res(p))
        for p in range(P_):
            P.coll("AllGather", full[p * 8 * a:(p + 1) * 8 * a, :], half[p * 4 * a:(p + 1) * 4 * a, :], g2, half.res(p), full.res())


def phase_x0(K):
    P, cfg = K.P, K.cfg
    D, KT, TL = cfg.D, cfg.KT, cfg.TL
    xr = K.xres.h.ap().rearrange("(k p) t -> p k t", p=128)
    with P.phase():
        xt = [P.sb("x0_in%d" % i, [128, D], F32) for i in range(2)]
        xo = [P.sb("x0_out%d" % i, [128, KT, 128], F32) for i in range(2)]
        ident = K.cst[:, 0:128]
        for tb in range(TL // 128):
            a, o = xt[tb % 2], xo[tb % 2]
            P.dma(a[:], K.x_own[tb * 128:(tb + 1) * 128, :], K.x_own.res(), a.res())
            for g in range(KT // 4):
                ps = psum(K)
                for i in range(4):
                    kt = g * 4 + i
                    P.op("pe", lambda e, ps=ps, i=i, kt=kt, a=a: e.transpose(ps[:, i * 128:(i + 1) * 128], a[:, kt * 128:(kt + 1) * 128], ident),
                         a.res() + K.cst.res(), ps.res())
                P.op("act", lambda e, ps=ps, g=g, o=o: e.copy(o[:, g * 4:(g + 1) * 4, :], ps[:, :]), ps.res(), o.res())
            P.dma(xr[:, :, tb * 128:(tb + 1) * 128], o[:], o.res(), K.xres.res((tb * 128) // cfg.TT))


def rms_stats(K, xt, nkt, ntok, sq, rstd, tagres):
    P = K.P
    ones = K.cst[:, 128:256]
    ps = psum(K)
    G = 4
    for g in range(nkt // G):
        s = sq[g % 2]
        P.op("act", lambda e, s=s, g=g: e.activation(s[:], xt[:, g * G:(g + 1) * G, :], AF.Square), xt.res(), s.res())
        for i in range(G):
            kt = g * G + i
            P.op("pe", lambda e, s=s, i=i, kt=kt: e.matmul(ps[:, 0:ntok], ones, s[:, i, :], start=(kt == 0), stop=(kt == nkt - 1)),
                 s.res() + K.cst.res(), ps.res())
    P.op("act", lambda e: e.activation(rstd[:], ps[:, 0:ntok], AF.Sqrt, bias=K.epsD[:, 0:1], scale=1.0 / (nkt * 128)),
         ps.res() + K.epst.res(), rstd.res())
    P.op("dve", lambda e: e.reciprocal(rstd[:], rstd[:]), rstd.res(), rstd.res())


def phase_a(K, l):
    P, cfg, spo = K.P, K.cfg, K.spo
    D, KT, TL = cfg.D, cfg.KT, cfg.TL
    xr = K.xres.h.ap().rearrange("(k p) t -> p k t", p=128)
    groups4 = [[0, 1, 2, 3], [4, 5, 6, 7]]
    gho = (NFM * 128 + NTM * 256) * D
    wfl = K.winb[l].h.ap().rearrange("r b -> (r b)")
    with P.phase():
        make_eps(K)
        xt = [P.sb("a_x%d" % i, [128, KT, 128], F32) for i in range(2)]
        xn = [P.sb("a_xn%d" % i, [128, KT, 128], BF16) for i in range(2)]
        sq = [P.sb("a_sq%d" % i, [128, 4, 128], F32) for i in range(2)]
        rstd = [P.sb("a_rstd%d" % i, [128, 128], F32) for i in range(2)]
        wgh = P.sb("a_wgh", [128, 2, KT, 128], BF16)
        gho_sb = [P.sb("a_gho%d" % i, [128, 2, 128], BF16) for i in range(2)]
        for m in range(2):
            src = wfl[gho + m * D * 128: gho + (m + 1) * D * 128].rearrange("(p f) -> p f", p=128)
            P.dma(wgh[:, m, :, :], src, K.winb[l].res(), wgh.res())
        g1 = K.spt
        ght = K.ghT.h.ap().rearrange("(m p) t -> p m t", p=128)
        for pc in range(TL // 128):
            a, n, r, go = xt[pc % 2], xn[pc % 2], rstd[pc % 2], gho_sb[pc % 2]
            P.dma(a[:], xr[:, :, pc * 128:(pc + 1) * 128], K.xres.res((pc * 128) // cfg.TT), a.res())
            rms_stats(K, a, KT, 128, sq, r, None)
            for kt in range(KT):
                c = spo["g1"] + l * KT + kt
                P.op("dve", lambda e, kt=kt, c=c, a=a, n=n, r=r: e.scalar_tensor_tensor(n[:, kt, :], a[:, kt, :], g1[:, c:c + 1], r[:], ALU.mult, ALU.mult),
                     a.res() + r.res() + K.spt.res(), n.res())
            P.dma(K.xnp[pc * 128:(pc + 1) * 128, :], n[:].rearrange("p k t -> p (k t)"), n.res(), K.xnp.res(pc))
            for m in range(2):
                ps = psum(K)
                for kt in range(KT):
                    P.op("pe", lambda e, ps=ps, m=m, kt=kt, n=n: e.matmul(ps[:, 0:128], wgh[:, m, kt, :], n[:, kt, :], start=(kt == 0), stop=(kt == KT - 1)),
                         wgh.res() + n.res(), ps.res())
                P.op("act", lambda e, ps=ps, m=m, go=go: e.copy(go[:, m, :], ps[:, 0:128]), ps.res(), go.res())
            P.dma(ght[:, :, pc * 128:(pc + 1) * 128], go[:], go.res(), K.ghT.res())
            P.coll("AllGather", K.xng[pc * 512:(pc + 1) * 512, :], K.xnp[pc * 128:(pc + 1) * 128, :], groups4,
                   K.xnp.res(pc), K.xng.res(pc))


def make_eps(K):
    P = K.P
    K.epst = P.sb("epst", [128, 2], F32)
    K.epsD = K.epst
    P.op("dve", lambda e: e.memset(K.epst[:], EPS), (), K.epst.res())


def phase_dense(K, l, final):
    P, cfg, spo = K.P, K.cfg, K.spo
    D, FF, KT, FT, TL, TT, YP = cfg.D, cfg.FF, cfg.KT, cfg.FT, cfg.TL, cfg.TT, cfg.YP
    xr = K.xres.h.ap().rearrange("(k p) t -> p k t", p=128)
    ght = K.ghT.h.ap().rearrange("(m p) t -> p m t", p=128)
    ygv = K.yg.h.ap().rearrange("(q c p) t -> q p c t", c=6, p=128)
    wbr = [flat_blocks(K.wfull[(n, l)], 8 * 128 * 128) for n in ("wa", "wb", "wc")]
    wgfl = K.wfull[("wg", l)].h.ap().rearrange("r b -> (r b)")
    wo = flat_blocks(K.wfull[("wo", l)], D * 128)
    w1 = flat_blocks(K.wfull[("w1", l)], D * 128)
    w2 = flat_blocks(K.wfull[("w2", l)], FF * 128)
    FSUB = min(FT, 32)
    with P.phase():
        make_eps(K)
        xT = P.sb("d_xT", [128, KT, TT], F32, nparts=KT)
        act = P.sb("d_act", [128, KT, TT], BF16, nparts=KT)
        hT = P.sb("d_hT", [128, FT, TT], BF16, nparts=FT)
        yT = [P.sb("d_yT%d" % j, [128, 6, TT], BF16) for j in range(4)]
        ycand = [P.sb("d_yc%d" % i, [128, 6, TT], BF16) for i in range(2)]
        ghs = P.sb("d_gh", [128, 2, TT], BF16)
        NW = 4
        wp = [P.sb("d_w%d" % i, [128, 4096], BF16) for i in range(NW)]
        K.wrr = 0
        sq = [P.sb("d_sq%d" % i, [128, 4, TT], F32) for i in range(2)]
        rstd = P.sb("d_rstd", [128, TT], F32)
        gs = [P.sb("d_gs%d" % i, [128, TT], F32) for i in range(2)]
        tmp = [P.sb("d_tmp%d" % i, [128, TT], F32) for i in range(2)]
        acc = P.sb("d_acc", [128, TT], F32)
        if final:
            otv = hT.h[:].rearrange("p f t -> p (f t)").bitcast(F32)

        def wnext():
            w = wp[K.wrr]
            K.wrr = (K.wrr + 1) % NW
            return w

        for tt in range(TL // TT):
            t0 = tt * TT
            P.dma(xT[:], xr[:, :, t0:t0 + TT], K.xres.res(tt), xT.res())
            P.dma(ghs[:], ght[:, :, t0:t0 + TT], K.ghT.res(), ghs.res())
            ci = 0
            for j in range(4):
                for r in range(4):
                    g0 = r * TL + t0
                    tb, off = g0 // YP, g0 % YP
                    yc = ycand[ci % 2]
                    ci += 1
                    P.dma(yc[:], ygv[tb * 4 + j][:, :, off:off + TT], K.yg.res(tb), yc.res())
                    if r == 0:
                        P.op("dve", lambda e, j=j, yc=yc, r=r: e.tensor_scalar(yT[j][:], yc[:], K.rmask[:, r:r + 1], None, ALU.mult),
                             yc.res() + K.rmask.res(), yT[j].res())
                    else:
                        P.op("dve", lambda e, j=j, yc=yc, r=r: e.scalar_tensor_tensor(yT[j][:], yc[:], K.rmask[:, r:r + 1], yT[j][:], ALU.mult, ALU.add),
                             yc.res() + K.rmask.res() + yT[j].res(), yT[j].res())
            for dt in range(KT):
                w = wnext()
                for jb in range(3):
                    o = jb * 256 * D + dt * 256 * 128
                    P.dma(w[:, jb * 256:(jb + 1) * 256], wgfl[o:o + 256 * 128].rearrange("(p f) -> p f", p=128),
                          K.wfull[("wg", l)].res(), w.res())
                    P.dma(w[:, 768 + jb * 1024: 768 + (jb + 1) * 1024], wbr[jb][dt], K.wfull[(("wa", "wb", "wc")[jb], l)].res(), w.res())
                for jb in range(3):
                    psg = psum(K)
                    for rt in range(2):
                        c0 = jb * 256 + rt * 128
                        P.op("pe", lambda e, psg=psg, w=w, c0=c0, rt=rt: e.matmul(psg[:, 0:TT], w[:, c0:c0 + 128], ghs[:, rt, :], start=(rt == 0), stop=(rt == 1)),
                             w.res() + ghs.res(), psg.res())
                    g = gs[jb % 2]
                    bc = spo["bm"] + (l * 3 + jb) * KT + dt
                    P.op("act", lambda e, psg=psg, g=g, bc=bc: e.activation(g[:], psg[:, 0:TT], AF.Sigmoid, bias=K.spt[:, bc:bc + 1]),
                         psg.res() + K.spt.res(), g.res())
                    psb = psum(K)
                    for ct in range(8):
                        c0 = 768 + jb * 1024 + ct * 128
                        P.op("pe", lambda e, psb=psb, w=w, c0=c0, ct=ct, jb=jb: e.matmul(psb[:, 0:TT], w[:, c0:c0 + 128], yT[ct // 2][:, 2 * jb + ct % 2, :], start=(ct == 0), stop=(ct == 7)),
                             w.res() + yT[ct // 2].res(), psb.res())
                    if jb == 0:
                        P.op("dve", lambda e, psb=psb, g=g: e.tensor_tensor(acc[:], psb[:, 0:TT], g[:], ALU.mult), psb.res() + g.res(), acc.res())
                    else:
                        tm_ = tmp[jb % 2]
                        P.op("dve", lambda e, psb=psb, g=g, tm_=tm_: e.tensor_tensor(tm_[:], psb[:, 0:TT], g[:], ALU.mult), psb.res() + g.res(), tm_.res())
                        if jb == 1:
                            P.op("dve", lambda e, tm_=tm_: e.tensor_tensor(acc[:], acc[:], tm_[:], ALU.add), acc.res() + tm_.res(), acc.res())
                        else:
                            P.op("dve", lambda e, tm_=tm_, dt=dt: e.tensor_tensor(act[:, dt, :], acc[:], tm_[:], ALU.add), acc.res() + tm_.res(), act.res(dt))
            for et in range(KT):
                w = wnext()
                P.dma(w[:, 0:KT * 128], wo[et], K.wfull[("wo", l)].res(), w.res())
                ps = psum(K)
                for dt in range(KT):
                    P.op("pe", lambda e, ps=ps, w=w, dt=dt: e.matmul(ps[:, 0:TT], w[:, dt * 128:(dt + 1) * 128], act[:, dt, :], start=(dt == 0), stop=(dt == KT - 1)),
                         w.res() + act.res(dt), ps.res())
                P.op("dve", lambda e, ps=ps, et=et: e.tensor_tensor(xT[:, et, :], xT[:, et, :], ps[:, 0:TT], ALU.add), ps.res() + xT.res(et), xT.res(et))
            rms_stats(K, xT, KT, TT, sq, rstd, None)
            for kt in range(KT):
                c = spo["g2"] + l * KT + kt
                P.op("dve", lambda e, kt=kt, c=c: e.scalar_tensor_tensor(act[:, kt, :], xT[:, kt, :], K.spt[:, c:c + 1], rstd[:], ALU.mult, ALU.mult),
                     xT.res(kt) + rstd.res() + K.spt.res(), act.res(kt))
            for ft in range(FT):
                w = wnext()
                P.dma(w[:, 0:KT * 128], w1[ft], K.wfull[("w1", l)].res(), w.res())
                ps = psum(K)
                for kt in range(KT):
                    P.op("pe", lambda e, ps=ps, w=w, kt=kt: e.matmul(ps[:, 0:TT], w[:, kt * 128:(kt + 1) * 128], act[:, kt, :], start=(kt == 0), stop=(kt == KT - 1)),
                         w.res() + act.res(kt), ps.res())
                sv = tmp[ft % 2]
                P.op("act", lambda e, ps=ps, sv=sv: e.activation(sv[:], ps[:, 0:TT], AF.Square), ps.res(), sv.res())
                P.op("dve", lambda e, ps=ps, sv=sv, ft=ft: e.scalar_tensor_tensor(hT[:, ft, :], ps[:, 0:TT], 0.0, sv[:], ALU.is_gt, ALU.mult),
                     ps.res() + sv.res(), hT.res(ft))
            for dt in range(KT):
                ps = psum(K)
                for sub in range(FT // FSUB):
                    w = wnext()
                    P.dma(w[:, 0:FSUB * 128], w2[dt][:, sub * FSUB * 128:(sub + 1) * FSUB * 128], K.wfull[("w2", l)].res(), w.res())
                    for fi in range(FSUB):
                        ft = sub * FSUB + fi
                        P.op("pe", lambda e, ps=ps, w=w, fi=fi, ft=ft: e.matmul(ps[:, 0:TT], w[:, fi * 128:(fi + 1) * 128], hT[:, ft, :], start=(ft == 0), stop=(ft == FT - 1)),
                             w.res() + hT.res(ft), ps.res())
                P.op("dve", lambda e, ps=ps, dt=dt: e.tensor_tensor(xT[:, dt, :], xT[:, dt, :], ps[:, 0:TT], ALU.add), ps.res() + xT.res(dt), xT.res(dt))
            if not final:
                P.dma(xr[:, :, t0:t0 + TT], xT[:], xT.res(), K.xres.res(tt))
            else:
                rms_stats(K, xT, KT, TT, sq, rstd, None)
                for kt in range(KT):
                    c = spo["gf"] + kt
                    P.op("dve", lambda e, kt=kt, c=c: e.scalar_tensor_tensor(xT[:, kt, :], xT[:, kt, :], K.spt[:, c:c + 1], rstd[:], ALU.mult, ALU.mult),
                         xT.res(kt) + rstd.res() + K.spt.res(), xT.res(kt))
                ident = K.cst[:, 0:128]
                for s_ in range(TT // 128):
                    o_ = otv[:, (s_ % 2) * D:(s_ % 2 + 1) * D]
                    for g in range(KT // 4):
                        ps = psum(K)
                        for i in range(4):
                            kt = g * 4 + i
                            P.op("pe", lambda e, ps=ps, i=i, kt=kt, s_=s_: e.transpose(ps[:, i * 128:(i + 1) * 128], xT[:, kt, s_ * 128:(s_ + 1) * 128], ident),
                                 xT.res(kt) + K.cst.res(), ps.res())
                        P.op("act", lambda e, ps=ps, g=g, o_=o_: e.copy(o_[:, g * 512:(g + 1) * 512], ps[:, :]), ps.res(), hT.res())
                    P.dma(K.out[t0 + s_ * 128: t0 + (s_ + 1) * 128, :], o_, hT.res(), K.out.res())


def alloc_mixer_dram(K):
    P, cfg = K.P, K.cfg
    S, YP = cfg.S, cfg.YP
    npc = S // YP
    K.fm32 = P.dram("fm32", [9 * 128, S], F32, nparts=9)
    K.fm16 = P.dram("fm16", [2 * 128, S], BF16, nparts=2)
    K.tm = {"av": P.dram("tm_av", [S, 256], BF16), "ao": P.dram("tm_ao", [S, 256], F32),
            "bz": P.dram("tm_bz", [S, 256], F32), "cv": P.dram("tm_cv", [S, 256], BF16),
            "cr": P.dram("tm_cr", [S, 256], F32)}
    K.yp = P.dram("yp", [npc * 6 * 128, YP], BF16, nparts=npc)
    K.yg = P.dram("yg", [npc * 4 * 6 * 128, YP], BF16, nparts=npc)
    K.H = [P.dram("H%d" % d, [S, 768], F32, nparts=4) for d in range(2)]
    K.gl = {}


def b_proj(K, l):
    P, cfg = K.P, K.cfg
    D, KT, S, TL, PT = cfg.D, cfg.KT, cfg.S, cfg.TL, cfg.PT
    wfl = K.winb[l].h.ap().rearrange("r b -> (r b)")
    bw = block_widths()
    boff = np.concatenate([[0], np.cumsum(bw)]).tolist()
    tmnames = ["av", "ao", "bz", "cv", "cr"]
    with P.phase():
        xn = [P.sb("p_xn%d" % i, [128, KT, PT], BF16) for i in range(2)]
        wq = [P.sb("p_w%d" % i, [128, KT * 256], BF16) for i in range(3)]
        st32 = [P.sb("p_s32_%d" % i, [128, 256], F32) for i in range(3)]
        st16 = [P.sb("p_s16_%d" % i, [128, 256], BF16) for i in range(3)]
        wr = 0
        sr = 0
        for ti in range(S // PT):
            g0 = ti * PT
            r, w0 = g0 // TL, g0 % TL
            x = xn[ti % 2]
            for i in range(PT // 128):
                pc = (w0 + i * 128) // 128
                P.dma(x[:, :, i * 128:(i + 1) * 128], K.xng[pc * 512 + r * 128: pc * 512 + (r + 1) * 128, :].rearrange("p (k t) -> p k t", t=128),
                      K.xng.res(pc), x.res())
            for bi in range(NFM):
                w = wq[wr % 3]
                wr += 1
                o = boff[bi] * D
                P.dma(w[:, 0:KT * 128], wfl[o:o + D * 128].rearrange("(p f) -> p f", p=128), K.winb[l].res(), w.res())
                ps = psum(K)
                for kt in range(KT):
                    P.op("pe", lambda e, ps=ps, w=w, kt=kt, x=x: e.matmul(ps[:, 0:PT], w[:, kt * 128:(kt + 1) * 128], x[:, kt, :], start=(kt == 0), stop=(kt == KT - 1)),
                         w.res() + x.res(), ps.res())
                if bi < 2:
                    st = st16[sr % 3]
                    sr += 1
                    P.op("act", lambda e, ps=ps, st=st, bi=bi: e.activation(st[:, 0:PT], ps[:, 0:PT], AF.Copy, scale=(1.0 if bi == 0 else A_DK ** -0.5)), ps.res(), st.res())
                    P.dma(K.fm16[bi * 128:(bi + 1) * 128, g0:g0 + PT], st[:, 0:PT], st.res(), K.fm16.res(bi))
                else:
                    st = st32[sr % 3]
                    sr += 1
                    P.op("act", lambda e, ps=ps, st=st: e.copy(st[:, 0:PT], ps[:, 0:PT]), ps.res(), st.res())
                    P.dma(K.fm32[(bi - 2) * 128:(bi - 1) * 128, g0:g0 + PT], st[:, 0:PT], st.res(), K.fm32.res(bi - 2))
            for bi in range(NTM):
                w = wq[wr % 3]
                wr += 1
                o = boff[NFM + bi] * D
                P.dma(w[:, 0:KT * 256], wfl[o:o + D * 256].rearrange("(p f) -> p f", p=128), K.winb[l].res(), w.res())
                dst = K.tm[tmnames[bi]]
                is16 = tmnames[bi] in ("av", "cv")
                for sub in range(PT // 128):
                    ps = psum(K)
                    for kt in range(KT):
                        P.op("pe", lambda e, ps=ps, w=w, kt=kt, x=x, sub=sub: e.matmul(ps[:, 0:256], x[:, kt, sub * 128:(sub + 1) * 128], w[:, kt * 256:(kt + 1) * 256], start=(kt == 0), stop=(kt == KT - 1)),
                             w.res() + x.res(), ps.res())
                    st = (st16 if is16 else st32)[sr % 3]
                    sr += 1
                    P.op("dve", lambda e, ps=ps, st=st: e.tensor_copy(st[:, 0:256], ps[:, 0:256]), ps.res(), st.res())
                    P.dma(dst[g0 + sub * 128: g0 + (sub + 1) * 128, :], st[:, 0:256], st.res(), dst.res())


def b_ygather(K, l):
    P, cfg = K.P, K.cfg
    npc = cfg.S // cfg.YP
    groups4 = [[0, 1, 2, 3], [4, 5, 6, 7]]
    for tb in range(npc):
        P.coll("AllGather", K.yg[tb * 4 * 768:(tb + 1) * 4 * 768, :], K.yp[tb * 768:(tb + 1) * 768, :], groups4,
               K.yp.res(tb), K.yg.res(tb))


def phase_b(K, l):
    b_proj(K, l)
    if K.stub_y is not None:
        P = K.P
        P.dma(K.yp[:, :], K.stub_y[:, :], K.stub_y.res(), K.yp.res(), q="pool")
    else:
        b_mixers(K, l)
    b_ygather(K, l)


def run_cfg(inputs, cfg, debug=(), extra=None):
    in_maps = prep_inputs(inputs, cfg)
    if extra is not None:
        for c in range(NCORES):
            in_maps[c].update(extra[c])
    nc = build(cfg, debug=debug)
    res = run_bass_kernel_spmd(nc, in_maps, core_ids=list(range(NCORES)))
    out = np.zeros((2, cfg.S, cfg.D), np.float32)
    for c in range(NCORES):
        b_, j = c // 4, c % 4
        out[b_, j * cfg.TL:(j + 1) * cfg.TL, :] = res.results[c]["out"]
    return out, res


def kernel(**inputs):
    x = np.asarray(inputs["x"])
    L = int(np.asarray(inputs["w_in"]).shape[0])
    cfg = Cfg(int(x.shape[2]), int(np.asarray(inputs["w_ff1"]).shape[2]), int(x.shape[1]), L)
    out, _ = run_cfg(inputs, cfg)
    return out.astype(np.float32)


def tr_bf(K, dst_ap, dst_res, src_ap, src_res, eng="act", scale_ap=None):
    P = K.P
    i = K.psb_rr
    K.psb_rr = (i + 1) % 4
    pv = K.psb[:, i * 256:i * 256 + 128]
    P.op("pe", lambda e: e.transpose(pv, src_ap, K.identb[:]), list(src_res) + K.identb.res(), K.psb.res(i))
    if scale_ap is not None:
        P.op("dve", lambda e: e.tensor_scalar(dst_ap, pv, scale_ap[0], None, ALU.mult), K.psb.res(i) + list(scale_ap[1]), dst_res)
    elif eng == "act":
        P.op("act", lambda e: e.copy(dst_ap, pv), K.psb.res(i), dst_res)
    else:
        P.op("dve", lambda e: e.tensor_copy(dst_ap, pv), K.psb.res(i), dst_res)


def gla_prep(K, l):
    P, cfg, spo = K.P, K.cfg, K.spo
    S, PT = cfg.S, cfg.PT
    K.glaB = [P.dram("glaB%d_%d" % (d, l), [128, S + 1], F32, nparts=S // PT) for d in range(2)]
    if "dumpG" in K.debug and l == 0:
        K.dbg_out += [("glaB0", K.glaB[0]), ("glaB1", K.glaB[1])]
    with P.phase():
        negb = P.sb("gp_negb", [128, 2], F32)
        zc = P.sb("gp_z", [128, 1], F32)
        P.op("dve", lambda e: e.memset(zc[:], 0.0), (), zc.res())
        P.op("dve", lambda e: e.tensor_scalar(negb[:], K.spt[:, spo["glab"] + l * 2: spo["glab"] + l * 2 + 2], -1.0, None, ALU.mult), K.spt.res(), negb.res())
        clr = [P.sb("gp_clr%d" % i, [16, PT], F32) for i in range(2)]
        ex = [P.sb("gp_e%d" % i, [128, PT], F32) for i in range(2)]
        bt = [P.sb("gp_b%d" % i, [128, PT], F32) for i in range(3)]
        n = 0
        for d in range(2):
            P.dma(K.glaB[d][:, (0 if d == 0 else S):(1 if d == 0 else S + 1)], zc[:], zc.res(), K.glaB[d].res(0 if d == 0 else S // PT - 1), slow=True)
            prev = None
            order = range(S // PT) if d == 0 else range(S // PT - 1, -1, -1)
            wg = K.spt[0:16, spo["glaw"] + (l * 2 + d) * 128: spo["glaw"] + (l * 2 + d + 1) * 128]
            for ti in order:
                g0 = ti * PT
                c_, e_, b_ = clr[n % 2], ex[n % 2], bt[n % 3]
                n += 1
                r0 = 8 * 128 + 32 + 32 * d
                P.dma(c_[:], K.fm32[r0:r0 + 16, g0:g0 + PT], K.fm32.res(8), c_.res())
                ps = psum(K)
                P.op("pe", lambda e, ps=ps, c_=c_, wg=wg: e.matmul(ps[:, 0:PT], wg, c_[:], start=True, stop=True), c_.res() + K.spt.res(), ps.res())
                P.op("act", lambda e, ps=ps, e_=e_, d=d: e.activation(e_[:], ps[:, 0:PT], AF.Exp, bias=negb[:, d:d + 1], scale=-1.0), ps.res() + negb.res(), e_.res())
                P.op("act", lambda e, e_=e_: e.activation(e_[:], e_[:], AF.Ln, bias=K.cst[:, 128:129], scale=1.0), e_.res() + K.cst.res(), e_.res())
                P.op("dve", lambda e, e_=e_: e.tensor_scalar(e_[:], e_[:], 1.0 / GLA_TAU, None, ALU.mult), e_.res(), e_.res())
                if d == 0:
                    init = 0.0 if prev is None else prev[:, PT - 1:PT]
                    P.op("dve", lambda e, b_=b_, e_=e_, init=init: e.tensor_tensor_scan(b_[:], e_[:], e_[:], init, ALU.add, ALU.bypass),
                         e_.res() + (prev.res() if prev is not None else []), b_.res())
                    P.dma(K.glaB[d][:, 1 + g0:1 + g0 + PT], b_[:], b_.res(), K.glaB[d].res(ti))
                else:
                    init = 0.0 if prev is None else prev[:, 0:1]
                    P.op("dve", lambda e, b_=b_, e_=e_, init=init: e.tensor_tensor_scan(b_[:, ::-1], e_[:, ::-1], e_[:, ::-1], init, ALU.add, ALU.bypass),
                         e_.res() + (prev.res() if prev is not None else []), b_.res())
                    P.dma(K.glaB[d][:, g0:g0 + PT], b_[:], b_.res(), K.glaB[d].res(ti))
                prev = b_


def gla_stream(K, l, d):
    P, cfg = K.P, K.cfg
    S, NCH, PT = cfg.S, cfg.NCH, cfg.PT
    tg = "gl%d" % d
    st = P.sb(tg + "_st", [128, 256], F32)
    stb = P.sb(tg + "_stb", [128, 256], BF16)
    P.op("dve", lambda e: e.memset(st[:], 0.0), (), st.res())
    P.op("dve", lambda e: e.memset(stb[:], 0.0), (), stb.res())
    nb = 2
    bs = [P.sb(tg + "_bs%d" % i, [128, 129], F32) for i in range(nb)]
    qk = [P.sb(tg + "_qk%d" % i, [128, 2, 128], F32) for i in range(nb)]
    v = [P.sb(tg + "_v%d" % i, [128, 256], BF16) for i in range(nb)]
    E1 = [P.sb(tg + "_E1%d" % i, [128, 128], F32) for i in range(nb)]
    E2 = [P.sb(tg + "_E2%d" % i, [128, 128], F32) for i in range(nb)]
    qd = [P.sb(tg + "_qd%d" % i, [128, 128], BF16) for i in range(nb)]
    kd = [P.sb(tg + "_kd%d" % i, [128, 128], BF16) for i in range(nb)]
    ktm = [P.sb(tg + "_kt%d" % i, [128, 128], BF16) for i in range(nb)]
    stm = [P.sb(tg + "_sm%d" % i, [128, 128], BF16) for i in range(nb)]
    o = [P.sb(tg + "_o%d" % i, [128, 256], F32) for i in range(nb)]
    tS = P.sb(tg + "_tS", [128, 256], F32)
    mask = K.cst[:, (6 + d) * 128:(7 + d) * 128]
    order = range(NCH) if d == 0 else range(NCH - 1, -1, -1)
    n = 0
    for c in order:
        cs = c * 128
        i = n % nb
        n += 1
        P.dma(qk[i][:, 0, :], K.fm32[6 * 128:7 * 128, cs:cs + 128], K.fm32.res(6), qk[i].res())
        P.dma(qk[i][:, 1, :], K.fm32[7 * 128:8 * 128, cs:cs + 128], K.fm32.res(7), qk[i].res())
        P.dma(bs[i][:], K.glaB[d][:, cs:cs + 129], K.glaB[d].res(), bs[i].res())
        P.dma(v[i][:], K.tm["cv"][cs:cs + 128, :], K.tm["cv"].res(), v[i].res())
        if d == 0:
            bcur, bref, edge = bs[i][:, 1:129], bs[i][:, 0:1], 127
        else:
            bcur, bref, edge = bs[i][:, 0:128], bs[i][:, 128:129], 0
        P.op("act", lambda e, i=i, bcur=bcur, bref=bref: e.activation(E1[i][:], bcur, AF.Exp, bias=bref, scale=-1.0), bs[i].res(), E1[i].res())
        P.op("dve", lambda e, i=i: e.reciprocal(E2[i][:], E1[i][:]), E1[i].res(), E2[i].res())
        P.op("dve", lambda e, i=i: e.scalar_tensor_tensor(qd[i][:], qk[i][:, 0, :], C_DK ** -0.5, E1[i][:], ALU.mult, ALU.mult), qk[i].res() + E1[i].res(), qd[i].res())
        P.op("dve", lambda e, i=i: e.tensor_tensor(kd[i][:], qk[i][:, 1, :], E2[i][:], ALU.mult), qk[i].res() + E2[i].res(), kd[i].res())
        ps1 = psum(K)
        P.op("pe", lambda e, i=i, ps1=ps1: e.matmul(ps1[:, 0:128], kd[i][:], qd[i][:], start=True, stop=True), kd[i].res() + qd[i].res(), ps1.res())
        P.op("dve", lambda e, i=i, ps1=ps1: e.tensor_tensor(stm[i][:], ps1[:, 0:128], mask, ALU.mult), ps1.res() + K.cst.res(), stm[i].res())
        tr_bf(K, ktm[i][:], ktm[i].res(), kd[i][:], kd[i].res())
        ps2 = psum(K)
        P.op("pe", lambda e, i=i, ps2=ps2: e.matmul(ps2[:, 0:256], stm[i][:], v[i][:], start=True, stop=False), stm[i].res() + v[i].res(), ps2.res())
        P.op("pe", lambda e, i=i, ps2=ps2: e.matmul(ps2[:, 0:256], qd[i][:], stb[:], start=False, stop=True), qd[i].res() + stb.res(), ps2.res())
        P.op("act", lambda e, i=i, ps2=ps2: e.copy(o[i][:], ps2[:, 0:256]), ps2.res(), o[i].res())
        P.dma(K.H[d][cs:cs + 128, 512:768], o[i][:], o[i].res(), K.H[d].res(3))
        ps3 = psum(K)
        P.op("pe", lambda e, i=i, ps3=ps3: e.matmul(ps3[:, 0:256], ktm[i][:], v[i][:], start=True, stop=True), ktm[i].res() + v[i].res(), ps3.res())
        P.op("dve", lambda e, ps3=ps3: e.tensor_tensor(tS[:], st[:], ps3[:, 0:256], ALU.add), st.res() + ps3.res(), tS.res())
        P.op("dve", lambda e, i=i, edge=edge: e.tensor_scalar(st[:], tS[:], E1[i][:, edge:edge + 1], None, ALU.mult), tS.res() + E1[i].res(), st.res())
        P.op("act", lambda e: e.copy(stb[:], st[:]), st.res(), stb.res())
        yield


def mlstm_prep(K, l):
    P, cfg, spo = K.P, K.cfg, K.spo
    S, NCH = cfg.S, cfg.NCH
    RS = min(S, 1024)
    K.mlG = [P.dram("mlG%d_%d" % (d, l), [3, S], F32) for d in range(2)]
    with P.phase():
        nbias = P.sb("mp_nb", [1, 4], F32)
        P.op("dve", lambda e: e.tensor_scalar(nbias[:], K.spt[0:1, spo["agb"] + l * 4: spo["agb"] + l * 4 + 4], -1.0, None, ALU.mult), K.spt.res(), nbias.res())
        zr = P.sb("mp_z", [1, RS], F32)
        P.op("dve", lambda e: e.memset(zr[:], 0.0), (), zr.res())
        names = ["fr", "ir", "lf", "F", "m", "a", "u", "em"]
        tsets = [{nm: P.sb("mp_%s_%d" % (nm, i_), [1, RS], F32) for nm in names} for i_ in range(2)]
        for d in range(2):
            prevF = prevm = None
            segs = range(S // RS) if d == 0 else range(S // RS - 1, -1, -1)
            for si, sg in enumerate(segs):
                t = tsets[si % 2]
                g0 = sg * RS
                rb = 8 * 128
                P.dma(t["ir"][:], K.fm32[rb + d:rb + d + 1, g0:g0 + RS], K.fm32.res(8), t["ir"].res())
                P.dma(t["fr"][:], K.fm32[rb + 2 + d:rb + 3 + d, g0:g0 + RS], K.fm32.res(8), t["fr"].res())
                P.op("act", lambda e, t=t, d=d: e.activation(t["fr"][:], t["fr"][:], AF.Exp, bias=nbias[:, 2 + d:3 + d], scale=-1.0), t["fr"].res() + nbias.res(), t["fr"].res())
                P.op("act", lambda e, t=t: e.activation(t["fr"][:], t["fr"][:], AF.Ln, bias=K.cst[0:1, 128:129], scale=1.0), t["fr"].res() + K.cst.res(), t["fr"].res())
                P.op("dve", lambda e, t=t: e.tensor_scalar(t["lf"][:], t["fr"][:], -1.0, None, ALU.mult), t["fr"].res(), t["lf"].res())
                P.op("dve", lambda e, t=t, d=d: e.tensor_scalar(t["ir"][:], t["ir"][:], K.spt[0:1, spo["agb"] + l * 4 + d: spo["agb"] + l * 4 + d + 1], None, ALU.add), t["ir"].res() + K.spt.res(), t["ir"].res())
                if d == 0:
                    iF = 0.0 if prevF is None else prevF[:, RS - 1:RS]
                    im = 0.0 if prevm is None else prevm[:, RS - 1:RS]
                    vw = lambda ap: ap[:]
                else:
                    iF = 0.0 if prevF is None else prevF[:, 0:1]
                    im = 0.0 if prevm is None else prevm[:, 0:1]
                    vw = lambda ap: ap[:, ::-1]
                dep = (prevF.res() if prevF is not None else []) + (prevm.res() if prevm is not None else [])
                P.op("dve", lambda e, t=t, iF=iF, vw=vw: e.tensor_tensor_scan(vw(t["F"]), vw(t["lf"]), vw(zr), iF, ALU.add, ALU.add), t["lf"].res() + zr.res() + dep, t["F"].res())
                P.op("dve", lambda e, t=t, im=im, vw=vw: e.tensor_tensor_scan(vw(t["m"]), vw(t["lf"]), vw(t["ir"]), im, ALU.add, ALU.max), t["lf"].res() + t["ir"].res() + dep, t["m"].res())
                P.op("dve", lambda e, t=t: e.tensor_tensor(t["a"][:], t["F"][:], t["m"][:], ALU.subtract), t["F"].res() + t["m"].res(), t["a"].res())
                P.op("dve", lambda e, t=t: e.tensor_tensor(t["u"][:], t["ir"][:], t["F"][:], ALU.subtract), t["F"].res() + t["ir"].res(), t["u"].res())
                P.op("act", lambda e, t=t: e.activation(t["em"][:], t["m"][:], AF.Exp, scale=-1.0), t["m"].res(), t["em"].res())
                for ri, nm in enumerate(("a", "u", "em")):
                    P.dma(K.mlG[d][ri:ri + 1, g0:g0 + RS], t[nm][:], t[nm].res(), K.mlG[d].res())
                prevF, prevm = t["F"], t["m"]


def col_from_rows(K, dst, dram_row_ap, dram_res, nch, tmp):
    P = K.P
    P.dma(tmp[0:nch, :], dram_row_ap.rearrange("o (c t) -> (o c) t", t=128), dram_res, tmp.res())
    ps = psum(K)
    P.op("pe", lambda e: e.transpose(ps[:, 0:nch], tmp[0:nch, :], K.cst[0:nch, 0:nch]), tmp.res() + K.cst.res(), ps.res())
    P.op("act", lambda e: e.copy(dst, ps[:, 0:nch]), ps.res(), [])


def mlstm_stream(K, l, d):
    P, cfg = K.P, K.cfg
    S, NCH = cfg.S, cfg.NCH
    tg = "ml%d" % d
    st = P.sb(tg + "_st", [128, 257], F32)
    stb = P.sb(tg + "_stb", [128, 257], BF16)
    P.op("dve", lambda e: e.memset(st[:], 0.0), (), st.res())
    P.op("dve", lambda e: e.memset(stb[:], 0.0), (), stb.res())
    cols = P.sb(tg + "_cols", [128, 2, NCH], F32)
    cmt = P.sb(tg + "_cmt", [128, 128], F32)
    for ri in range(2):
        P.dma(cmt[0:NCH, :], K.mlG[d][1 + ri:2 + ri, :].rearrange("o (c t) -> (o c) t", t=128), K.mlG[d].res(), cmt.res())
        ps = psum(K)
        P.op("pe", lambda e, ps=ps: e.transpose(ps[:, 0:NCH], cmt[0:NCH, :], K.cst[0:NCH, 0:NCH]), cmt.res() + K.cst.res(), ps.res())
        P.op("act", lambda e, ps=ps, ri=ri: e.copy(cols[:, ri, :], ps[:, 0:NCH]), ps.res(), cols.res())
    nb = 2
    q = [P.sb(tg + "_q%d" % i, [128, 128], BF16) for i in range(nb)]
    k = [P.sb(tg + "_k%d" % i, [128, 128], BF16) for i in range(nb)]
    va = [P.sb(tg + "_va%d" % i, [128, 257], BF16) for i in range(nb)]
    ar = [P.sb(tg + "_ar%d" % i, [1, 128], F32) for i in range(nb)]
    W = [P.sb(tg + "_W%d" % i, [128, 128], F32) for i in range(nb)]
    Wi = [P.sb(tg + "_Wi%d" % i, [128, 128], F32) for i in range(nb)]
    Dm = [P.sb(tg + "_Dm%d" % i, [128, 128], BF16) for i in range(nb)]
    qd = [P.sb(tg + "_qd%d" % i, [128, 128], BF16) for i in range(nb)]
    kw = [P.sb(tg + "_kw%d" % i, [128, 128], BF16) for i in range(nb)]
    h = [P.sb(tg + "_h%d" % i, [128, 256], F32) for i in range(nb)]
    dn = [P.sb(tg + "_dn%d" % i, [128, 2], F32) for i in range(nb)]
    negab = [P.sb(tg + "_na%d" % i, [128, 1], F32) for i in range(2)]
    for i in range(nb):
        P.op("dve", lambda e, i=i: e.memset(va[i][:, 256:257], 1.0), (), va[i].res())
    P.op("dve", lambda e: e.memset(negab[0][:], 0.0), (), negab[0].res())
    ones_row = K.cst[0:1, 128:256]
    ident = K.cst[:, 0:128]
    negm = K.cst[:, (2 + 2 * d) * 128:(3 + 2 * d) * 128]
    edge = 127 if d == 0 else 0
    order = range(NCH) if d == 0 else range(NCH - 1, -1, -1)
    n = 0
    for c in order:
        cs = c * 128
        i = n % nb
        na_in, na_out = negab[n % 2], negab[(n + 1) % 2]
        n += 1
        P.dma(q[i][:], K.fm16[0:128, cs:cs + 128], K.fm16.res(0), q[i].res())
        P.dma(k[i][:], K.fm16[128:256, cs:cs + 128], K.fm16.res(1), k[i].res())
        P.dma(va[i][:, 0:256], K.tm["av"][cs:cs + 128, :], K.tm["av"].res(), va[i].res())
        P.dma(ar[i][:], K.mlG[d][0:1, cs:cs + 128], K.mlG[d].res(), ar[i].res())
        ps1 = psum(K)
        P.op("pe", lambda e, i=i, ps1=ps1: e.matmul(ps1[:, 0:128], ones_row, ar[i][:], start=True, stop=False), ar[i].res() + K.cst.res(), ps1.res())
        P.op("pe", lambda e, ps1=ps1: e.matmul(ps1[:, 0:128], ident, negm, start=False, stop=True), K.cst.res(), ps1.res())
        P.op("act", lambda e, i=i, ps1=ps1, c=c: e.activation(W[i][:], ps1[:, 0:128], AF.Exp, bias=cols[:, 0, c:c + 1]), ps1.res() + cols.res(), W[i].res())
        ps1b = psum(K)
        P.op("pe", lambda e, i=i, ps1b=ps1b: e.matmul(ps1b[:, 0:128], ones_row, ar[i][:], start=True, stop=True), ar[i].res() + K.cst.res(), ps1b.res())
        P.op("act", lambda e, i=i, ps1b=ps1b, na_in=na_in: e.activation(Wi[i][:], ps1b[:, 0:128], AF.Exp, bias=na_in[:, 0:1]), ps1b.res() + na_in.res(), Wi[i].res())
        P.op("dve", lambda e, ps1b=ps1b, na_out=na_out: e.tensor_scalar(na_out[:], ps1b[:, edge:edge + 1], -1.0, None, ALU.mult), ps1b.res(), na_out.res())
        ps2 = psum(K)
        P.op("pe", lambda e, i=i, ps2=ps2: e.matmul(ps2[:, 0:128], k[i][:], q[i][:], start=True, stop=True), k[i].res() + q[i].res(), ps2.res())
        P.op("dve", lambda e, i=i, ps2=ps2: e.tensor_tensor(Dm[i][:], ps2[:, 0:128], W[i][:], ALU.mult), ps2.res() + W[i].res(), Dm[i].res())
        P.op("dve", lambda e, i=i: e.tensor_tensor(qd[i][:], q[i][:], Wi[i][:], ALU.mult), q[i].res() + Wi[i].res(), qd[i].res())
        tr_bf(K, kw[i][:], kw[i].res(), k[i][:], k[i].res(), scale_ap=(W[i][:, edge:edge + 1], W[i].res()))
        ps3 = psum(K)
        P.op("pe", lambda e, i=i, ps3=ps3: e.matmul(ps3[:, 0:257], Dm[i][:], va[i][:], start=True, stop=False), Dm[i].res() + va[i].res(), ps3.res())
        P.op("pe", lambda e, i=i, ps3=ps3: e.matmul(ps3[:, 0:257], qd[i][:], stb[:], start=False, stop=True), qd[i].res() + stb.res(), ps3.res())
        P.op("act", lambda e, i=i, ps3=ps3: e.activation(dn[i][:, 0:1], ps3[:, 256:257], AF.Abs), ps3.res(), dn[i].res())
        P.op("dve", lambda e, i=i, c=c: e.tensor_tensor(dn[i][:, 0:1], dn[i][:, 0:1], cols[:, 1, c:c + 1], ALU.max), dn[i].res() + cols.res(), dn[i].res())
        P.op("dve", lambda e, i=i: e.reciprocal(dn[i][:, 1:2], dn[i][:, 0:1]), dn[i].res(), dn[i].res())
        P.op("act", lambda e, i=i, ps3=ps3: e.activation(h[i][:], ps3[:, 0:256], AF.Copy, scale=dn[i][:, 1:2]), ps3.res() + dn[i].res(), h[i].res())
        P.dma(K.H[d][cs:cs + 128, 0:256], h[i][:], h[i].res(), K.H[d].res(0))
        ps4 = psum(K)
        P.op("pe", lambda e, i=i, ps4=ps4: e.matmul(ps4[:, 0:257], kw[i][:], va[i][:], start=True, stop=True), kw[i].res() + va[i].res(), ps4.res())
        P.op("dve", lambda e, i=i, ps4=ps4: e.scalar_tensor_tensor(st[:], st[:], Wi[i][:, edge:edge + 1], ps4[:, 0:257], ALU.mult, ALU.add), st.res() + Wi[i].res() + ps4.res(), st.res())
        P.op("act", lambda e: e.copy(stb[:], st[:]), st.res(), stb.res())
        yield


def b_post(K, l):
    P, cfg, spo = K.P, K.cfg, K.spo
    S, NCH, YP = cfg.S, cfg.NCH, cfg.YP
    ypv = K.yp.h.ap().rearrange("(q c p) t -> q p c t", c=6, p=128)
    segs = [(0, 256, 0), (256, 128, 1), (384, 128, 2), (512, 256, 3)]
    with P.phase():
        make_eps(K)
        nb = 2
        hf = [P.sb("po_hf%d" % i, [128, 768], F32) for i in range(nb)]
        hb = [P.sb("po_hb%d" % i, [128, 768], F32) for i in range(nb)]
        gt = [P.sb("po_g%d" % i, [128, 768], F32) for i in range(nb)]
        junk = P.sb("po_junk", [128, 768], F32)
        ss = [P.sb("po_ss%d" % i, [128, 4], F32) for i in range(nb)]
        t1 = [P.sb("po_t1%d" % i, [128, 768], F32) for i in range(nb)]
        yb = [P.sb("po_y%d" % i, [128, 768], F32) for i in range(nb)]
        yt = [P.sb("po_yt%d" % i, [128, 6, 128], BF16) for i in range(nb)]
        ng = P.sb("po_ng", [128, 768], F32)
        P.op("dve", lambda e: e.tensor_copy(ng[:, 0:256], K.spt[:, spo["ang"] + l * 256: spo["ang"] + (l + 1) * 256]), K.spt.res(), ng.res())
        P.op("dve", lambda e: e.tensor_copy(ng[:, 256:512], K.spt[:, spo["bng"] + l * 256: spo["bng"] + (l + 1) * 256]), K.spt.res(), ng.res())
        P.op("dve", lambda e: e.tensor_copy(ng[:, 512:768], K.spt[:, spo["cng"] + l * 256: spo["cng"] + (l + 1) * 256]), K.spt.res(), ng.res())
        for c in range(NCH):
            cs = c * 128
            i = c % nb
            P.dma(hf[i][:], K.H[0][cs:cs + 128, :], K.H[0].res(), hf[i].res())
            P.dma(hb[i][:], K.H[1][cs:cs + 128, :], K.H[1].res(), hb[i].res())
            P.dma(gt[i][:, 0:256], K.tm["ao"][cs:cs + 128, :], K.tm["ao"].res(), gt[i].res())
            P.dma(gt[i][:, 256:512], K.tm["bz"][cs:cs + 128, :], K.tm["bz"].res(), gt[i].res())
            P.dma(gt[i][:, 512:768], K.tm["cr"][cs:cs + 128, :], K.tm["cr"].res(), gt[i].res())
            P.op("dve", lambda e, i=i: e.tensor_tensor(hf[i][:], hf[i][:], hb[i][:], ALU.add), hf[i].res() + hb[i].res(), hf[i].res())
            if K.post_lvl < 2:
                continue
            for si, (c0, w, _) in enumerate(segs):
                P.op("act", lambda e, i=i, c0=c0, w=w: e.activation(junk[:, c0:c0 + w], hf[i][:, c0:c0 + w], AF.Square), hf[i].res(), junk.res())
                P.op("dve", lambda e, i=i, c0=c0, w=w, si=si: e.reduce_sum(ss[i][:, si:si + 1], junk[:, c0:c0 + w], mybir.AxisListType.X), junk.res(), ss[i].res())
            for si, (c0, w, _) in enumerate(segs):
                P.op("act", lambda e, i=i, si=si, w=w: e.activation(ss[i][:, si:si + 1], ss[i][:, si:si + 1], AF.Sqrt, bias=K.epst[:, 0:1], scale=1.0 / w),
                     ss[i].res() + K.epst.res(), ss[i].res())
            P.op("dve", lambda e, i=i: e.reciprocal(ss[i][:], ss[i][:]), ss[i].res(), ss[i].res())
            for si, (c0, w, _) in enumerate(segs):
                P.op("dve", lambda e, i=i, c0=c0, w=w, si=si: e.scalar_tensor_tensor(t1[i][:, c0:c0 + w], hf[i][:, c0:c0 + w], ss[i][:, si:si + 1], ng[:, c0:c0 + w], ALU.mult, ALU.mult),
                     hf[i].res() + ss[i].res() + ng.res(), t1[i].res())
            if K.post_lvl < 3:
                continue
            P.op("act", lambda e, i=i: e.activation(gt[i][:, 0:256], gt[i][:, 0:256], AF.Sigmoid), gt[i].res(), gt[i].res())
            P.op("act", lambda e, i=i: e.activation(gt[i][:, 256:768], gt[i][:, 256:768], AF.Silu), gt[i].res(), gt[i].res())
            P.op("dve", lambda e, i=i: e.tensor_tensor(yb[i][:], t1[i][:], gt[i][:], ALU.mult), t1[i].res() + gt[i].res(), yb[i].res())
            if K.post_lvl < 4:
                continue
            for g in range(2):
                ps = psum(K)
                for k_ in range(3):
                    ct = g * 3 + k_
                    P.op("pe", lambda e, ps=ps, k_=k_, ct=ct, i=i: e.transpose(ps[:, k_ * 128:(k_ + 1) * 128], yb[i][:, ct * 128:(ct + 1) * 128], K.cst[:, 0:128]),
                         yb[i].res() + K.cst.res(), ps.res())
                if g == 0:
                    P.op("act", lambda e, ps=ps, i=i, g=g: e.copy(yt[i][:, g * 3:(g + 1) * 3, :], ps[:, 0:384]), ps.res(), yt[i].res())
                else:
                    P.op("dve", lambda e, ps=ps, i=i, g=g: e.tensor_copy(yt[i][:, g * 3:(g + 1) * 3, :], ps[:, 0:384]), ps.res(), yt[i].res())
            tb, off = cs // YP, cs % YP
            if "post_nodma" not in K.debug:
                P.dma(ypv[tb][:, :, off:off + 128], yt[i][:], yt[i].res(), K.yp.res(tb))


def b_mixers(K, l):
    P = K.P
    which = K.which
    if "c" in which:
        gla_prep(K, l)
    if "a" in which:
        mlstm_prep(K, l)
    if "b" in which:
        gdn_prep(K, l)
    with P.phase():
        streams = []
        if "c" in which:
            streams += [gla_stream(K, l, 0), gla_stream(K, l, 1)]
        if "a" in which:
            streams += [mlstm_stream(K, l, 0), mlstm_stream(K, l, 1)]
        if "b" in which:
            streams += [gdn_stream(K, l, hh, d) for hh in range(2) for d in range(2)]
        live = list(streams)
        while live:
            nxt = []
            for g in live:
                try:
                    next(g)
                    nxt.append(g)
                except StopIteration:
                    pass
            live = nxt
    if K.post:
        b_post(K, l)


def gdn_prep(K, l):
    P, cfg, spo = K.P, K.cfg, K.spo
    S, NCH, PT = cfg.S, cfg.NCH, cfg.PT
    if not hasattr(K, "gq"):
        K.gq = [P.dram("gdn_q%d" % h, [128, S], BF16) for h in range(2)]
        K.gk = [P.dram("gdn_k%d" % h, [128, S], BF16) for h in range(2)]
        K.gktm = [P.dram("gdn_ktm%d" % h, [S, 128], BF16) for h in range(2)]
        K.gvtm = [P.dram("gdn_vtm%d" % h, [S, 128], F32) for h in range(2)]
        K.grow = [[P.dram("gdn_row%d%d" % (h, d), [2, S], F32) for d in range(2)] for h in range(2)]
        K.gcol = [[P.dram("gdn_col%d%d" % (h, d), [128, 5 * NCH], F32) for d in range(2)] for h in range(2)]
        mk = lambda nm: [[P.dram("gdn_%s%d%d" % (nm, h, d), [S, 128], BF16, nparts=NCH) for d in range(2)] for h in range(2)]
        K.gTT, K.gAQ, K.gQE, K.gKD = mk("tt"), mk("aq"), mk("qe"), mk("kd")
    ones = K.cst[:, 128:256]
    with P.phase():
        make_eps(K)
        xh = [P.sb("g1_xh%d" % i, [128, PT + 4], F32) for i in range(2)]
        acc = [P.sb("g1_acc%d" % i, [128, PT], F32) for i in range(2)]
        sq = [P.sb("g1_sq%d" % i, [128, PT], F32) for i in range(2)]
        rn = [P.sb("g1_rn%d" % i, [128, PT], F32) for i in range(2)]
        ob = [P.sb("g1_ob%d" % i, [128, PT], BF16) for i in range(2)]
        tmo = [P.sb("g1_tm%d" % i, [128, 128], BF16) for i in range(2)]
        tvo = [P.sb("g1_tv%d" % i, [128, 128], F32) for i in range(2)]
        n = 0
        for ti in range(S // PT):
            g0 = ti * PT
            lo, hi = max(0, g0 - 2), min(S, g0 + PT + 2)
            for hh in range(2):
                for part in range(3):
                    blk = part * 2 + hh
                    x_, a_, s_, r_, o_ = xh[n % 2], acc[n % 2], sq[n % 2], rn[n % 2], ob[n % 2]
                    n += 1
                    if g0 == 0:
                        P.op("dve", lambda e, x_=x_: e.memset(x_[:, 0:2], 0.0), (), x_.res())
                    if g0 + PT == S:
                        P.op("dve", lambda e, x_=x_: e.memset(x_[:, PT + 2:PT + 4], 0.0), (), x_.res())
                    P.dma(x_[:, lo - (g0 - 2): hi - (g0 - 2)], K.fm32[blk * 128:(blk + 1) * 128, lo:hi], K.fm32.res(blk), x_.res())
                    cw = spo["convw"] + (l * 6 + blk) * 5
                    P.op("dve", lambda e, x_=x_, a_=a_, cw=cw: e.tensor_scalar(a_[:], x_[:, 0:PT], K.spt[:, cw:cw + 1], None, ALU.mult), x_.res() + K.spt.res(), a_.res())
                    for tap in range(1, 5):
                        P.op("dve", lambda e, x_=x_, a_=a_, cw=cw, tap=tap: e.scalar_tensor_tensor(a_[:], x_[:, tap:tap + PT], K.spt[:, cw + tap:cw + tap + 1], a_[:], ALU.mult, ALU.add),
                             x_.res() + K.spt.res() + a_.res(), a_.res())
                    cb = spo["convb"] + l * 6 + blk
                    P.op("act", lambda e, a_=a_, cb=cb: e.activation(a_[:], a_[:], AF.Silu, bias=K.spt[:, cb:cb + 1]), a_.res() + K.spt.res(), a_.res())
                    if part < 2:
                        P.op("act", lambda e, a_=a_, s_=s_: e.activation(s_[:], a_[:], AF.Square), a_.res(), s_.res())
                        ps = psum(K)
                        P.op("pe", lambda e, ps=ps, s_=s_: e.matmul(ps[:, 0:PT], ones, s_[:], start=True, stop=True), s_.res() + K.cst.res(), ps.res())
                        P.op("act", lambda e, ps=ps, r_=r_: e.activation(r_[:], ps[:, 0:PT], AF.Sqrt, bias=K.epst[:, 0:1], scale=1.0), ps.res() + K.epst.res(), r_.res())
                        P.op("dve", lambda e, r_=r_: e.reciprocal(r_[:], r_[:]), r_.res(), r_.res())
                        sc = B_DK ** -0.5 if part == 0 else 1.0
                        P.op("dve", lambda e, a_=a_, r_=r_, o_=o_, sc=sc: e.scalar_tensor_tensor(o_[:], a_[:], sc, r_[:], ALU.mult, ALU.mult), a_.res() + r_.res(), o_.res())
                        dst = (K.gq if part == 0 else K.gk)[hh]
                        P.dma(dst[:, g0:g0 + PT], o_[:], o_.res(), dst.res())
                        if part == 1:
                            for sub in range(PT // 128):
                                t_ = tmo[sub % 2]
                                tr_bf(K, t_[:], t_.res(), o_[:, sub * 128:(sub + 1) * 128], o_.res())
                                P.dma(K.gktm[hh][g0 + sub * 128: g0 + (sub + 1) * 128, :], t_[:], t_.res(), K.gktm[hh].res())
                    else:
                        for sub in range(PT // 128):
                            t_ = tvo[sub % 2]
                            ps = psum(K)
                            P.op("pe", lambda e, ps=ps, a_=a_, sub=sub: e.transpose(ps[:, 0:128], a_[:, sub * 128:(sub + 1) * 128], K.cst[:, 0:128]), a_.res() + K.cst.res(), ps.res())
                            P.op("act", lambda e, ps=ps, t_=t_: e.copy(t_[:], ps[:, 0:128]), ps.res(), t_.res())
                            P.dma(K.gvtm[hh][g0 + sub * 128: g0 + (sub + 1) * 128, :], t_[:], t_.res(), K.gvtm[hh].res())
    with P.phase():
        for hh in range(2):
            for d in range(2):
                tg = "g2_%d%d" % (hh, d)
                xg = P.sb(tg + "xg", [NCH, 128], F32)
                xb = P.sb(tg + "xb", [NCH, 128], F32)
                gp = P.sb(tg + "gp", [NCH, 128], F32)
                m5 = P.sb(tg + "m5", [NCH, 5, 128], F32)
                row = P.sb(tg + "row", [NCH, 2, 128], F32)
                sc_ = P.sb(tg + "sc", [128, 4], F32)
                colt = P.sb(tg + "col", [128, 5, NCH], F32)
                rb = 8 * 128 + 4
                P.dma(xg[:], K.fm32[rb + d * 2 + hh: rb + d * 2 + hh + 1, :].rearrange("o (c t) -> (o c) t", t=128), K.fm32.res(8), xg.res())
                P.dma(xb[:], K.fm32[rb + 4 + d * 2 + hh: rb + 4 + d * 2 + hh + 1, :].rearrange("o (c t) -> (o c) t", t=128), K.fm32.res(8), xb.res())
                ca = spo["gdn_alog"] + l * 4 + d * 2 + hh
                cd = spo["gdn_dtb"] + l * 4 + d * 2 + hh
                P.op("act", lambda e, sc_=sc_, ca=ca: e.activation(sc_[:, 0:1], K.spt[:, ca:ca + 1], AF.Exp), K.spt.res(), sc_.res())
                P.op("act", lambda e, xg=xg, cd=cd: e.activation(xg[:], xg[:], AF.Exp, bias=K.spt[0:NCH, cd:cd + 1]), xg.res() + K.spt.res(), xg.res())
                P.op("act", lambda e, xg=xg: e.activation(xg[:], xg[:], AF.Ln, bias=K.cst[0:NCH, 128:129]), xg.res() + K.cst.res(), xg.res())
                P.op("dve", lambda e, xg=xg, sc_=sc_: e.tensor_scalar(xg[:], xg[:], sc_[0:NCH, 0:1], None, ALU.mult), xg.res() + sc_.res(), xg.res())
                vw = (lambda ap: ap) if d == 0 else (lambda ap: ap[:, ::-1])
                P.op("dve", lambda e, xg=xg, gp=gp, vw=vw: e.tensor_tensor_scan(vw(gp[:, :]), vw(xg[:, :]), vw(xg[:, :]), 0.0, ALU.add, ALU.bypass), xg.res(), gp.res())
                P.op("act", lambda e, xb=xb: e.activation(xb[:], xb[:], AF.Exp, scale=-1.0), xb.res(), xb.res())
                P.op("act", lambda e, xb=xb: e.activation(xb[:], xb[:], AF.Ln, bias=K.cst[0:NCH, 128:129]), xb.res() + K.cst.res(), xb.res())
                P.op("dve", lambda e, gp=gp, row=row: e.tensor_scalar(row[:, 0, :], gp[:], -1.0, None, ALU.mult), gp.res(), row.res())
                P.op("dve", lambda e, gp=gp, xb=xb, row=row: e.scalar_tensor_tensor(row[:, 1, :], gp[:], -1.0, xb[:], ALU.mult, ALU.subtract), gp.res() + xb.res(), row.res())
                for r_ in range(2):
                    P.dma(K.grow[hh][d][r_:r_ + 1, :].rearrange("o (c t) -> (o c) t", t=128), row[:, r_, :], row.res(), K.grow[hh][d].res())
                edge = 127 if d == 0 else 0
                P.op("dve", lambda e, gp=gp, m5=m5: e.tensor_copy(m5[:, 0, :], gp[:]), gp.res(), m5.res())
                P.op("act", lambda e, gp=gp, m5=m5: e.activation(m5[:, 1, :], gp[:], AF.Exp, scale=-1.0), gp.res(), m5.res())
                P.op("dve", lambda e, m5=m5: e.tensor_scalar(m5[:, 1, :], m5[:, 1, :], -1.0, None, ALU.mult), m5.res(), m5.res())
                P.op("act", lambda e, xb=xb, m5=m5: e.activation(m5[:, 2, :], xb[:], AF.Exp, scale=-1.0), xb.res(), m5.res())
                P.op("dve", lambda e, gp=gp, sc_=sc_, edge=edge: e.tensor_scalar(sc_[0:NCH, 1:2], gp[:, edge:edge + 1], -1.0, None, ALU.mult), gp.res(), sc_.res())
                P.op("act", lambda e, gp=gp, m5=m5, sc_=sc_: e.activation(m5[:, 3, :], gp[:], AF.Exp, bias=sc_[0:NCH, 1:2]), gp.res() + sc_.res(), m5.res())
                P.op("act", lambda e, gp=gp, m5=m5, edge=edge: e.activation(m5[:, 4, :], gp[:, edge:edge + 1].to_broadcast([NCH, 128]), AF.Exp, scale=-1.0), gp.res(), m5.res())
                for q_ in range(5):
                    ps = psum(K)
                    P.op("pe", lambda e, ps=ps, m5=m5, q_=q_: e.transpose(ps[:, 0:NCH], m5[:, q_, :], K.cst[0:NCH, 0:NCH]), m5.res() + K.cst.res(), ps.res())
                    P.op("act", lambda e, ps=ps, colt=colt, q_=q_: e.copy(colt[:, q_, :], ps[:, 0:NCH]), ps.res(), colt.res())
                P.dma(K.gcol[hh][d][:, :], colt[:].rearrange("p a c -> p (a c)"), colt.res(), K.gcol[hh][d].res())
    with P.phase():
        streams = [gdn_solve_stream(K, l, hh, d) for hh in range(2) for d in range(2)]
        live = list(streams)
        while live:
            nxt = []
            for g in live:
                try:
                    next(g)
                    nxt.append(g)
                except StopIteration:
                    pass
            live = nxt


def gdn_solve_stream(K, l, hh, d):
    P, cfg = K.P, K.cfg
    S, NCH = cfg.S, cfg.NCH
    tg = "gs%d%d" % (hh, d)
    colt = P.sb(tg + "col", [128, 5, NCH], F32)
    P.dma(colt[:].rearrange("p a c -> p (a c)"), K.gcol[hh][d][:, :], K.gcol[hh][d].res(), colt.res())
    nb = 2
    kT = [P.sb(tg + "kT%d" % i, [128, 128], BF16) for i in range(nb)]
    qT = [P.sb(tg + "qT%d" % i, [128, 128], BF16) for i in range(nb)]
    ktm = [P.sb(tg + "ktm%d" % i, [128, 128], BF16) for i in range(nb)]
    rows = [P.sb(tg + "rw%d" % i, [1, 2, 128], F32) for i in range(nb)]
    EA = P.sb(tg + "EA", [128, 128], F32)
    EQ = P.sb(tg + "EQ", [128, 128], F32)
    EG = P.sb(tg + "EG", [128, 128], F32)
    Nm = P.sb(tg + "N", [128, 128], F32)
    Pm = [P.sb(tg + "P%d" % i, [128, 128], F32) for i in range(2)]
    Qm = [P.sb(tg + "Q%d" % i, [128, 128], F32) for i in range(2)]
    Xm = [P.sb(tg + "X%d" % i, [128, 128], F32) for i in range(2)]
    obuf = [P.sb(tg + "o%d" % i, [128, 4, 128], BF16) for i in range(nb)]
    ones_row = K.cst[0:1, 128:256]
    ident = K.cst[:, 0:128]
    neg_incl = K.cst[:, (2 + 2 * d) * 128:(3 + 2 * d) * 128]
    neg_strict = K.cst[:, (3 + 2 * d) * 128:(4 + 2 * d) * 128]
    n = 0
    for c in range(NCH):
        cs = c * 128
        i = n % nb
        n += 1
        P.dma(kT[i][:], K.gk[hh][:, cs:cs + 128], K.gk[hh].res(), kT[i].res())
        P.dma(qT[i][:], K.gq[hh][:, cs:cs + 128], K.gq[hh].res(), qT[i].res())
        P.dma(ktm[i][:], K.gktm[hh][cs:cs + 128, :], K.gktm[hh].res(), ktm[i].res())
        P.dma(rows[i][:], K.grow[hh][d][:, cs:cs + 128].rearrange("(o r) t -> o r t", o=1), K.grow[hh][d].res(), rows[i].res())
        gcol = colt[:, 0, c:c + 1]
        psA = psum(K)
        P.op("pe", lambda e, i=i, psA=psA: e.matmul(psA[:, 0:128], ones_row, rows[i][:, 1, :], start=True, stop=False), rows[i].res() + K.cst.res(), psA.res())
        P.op("pe", lambda e, psA=psA: e.matmul(psA[:, 0:128], ident, neg_strict, start=False, stop=True), K.cst.res(), psA.res())
        P.op("act", lambda e, psA=psA, gcol=gcol: e.activation(EA[:], psA[:, 0:128], AF.Exp, bias=gcol), psA.res() + colt.res(), EA.res())
        psK = psum(K)
        P.op("pe", lambda e, i=i, psK=psK: e.matmul(psK[:, 0:128], kT[i][:], kT[i][:], start=True, stop=True), kT[i].res(), psK.res())
        P.op("dve", lambda e, psK=psK: e.tensor_tensor(Nm[:], psK[:, 0:128], EA[:], ALU.mult), psK.res() + EA.res(), Nm.res())
        psQ = psum(K)
        P.op("pe", lambda e, i=i, psQ=psQ: e.matmul(psQ[:, 0:128], ones_row, rows[i][:, 0, :], start=True, stop=False), rows[i].res() + K.cst.res(), psQ.res())
        P.op("pe", lambda e, psQ=psQ: e.matmul(psQ[:, 0:128], ident, neg_incl, start=False, stop=True), K.cst.res(), psQ.res())
        P.op("act", lambda e, psQ=psQ, gcol=gcol: e.activation(EQ[:], psQ[:, 0:128], AF.Exp, bias=gcol), psQ.res() + colt.res(), EQ.res())
        psKQ = psum(K)
        P.op("pe", lambda e, i=i, psKQ=psKQ: e.matmul(psKQ[:, 0:128], kT[i][:], qT[i][:], start=True, stop=True), kT[i].res() + qT[i].res(), psKQ.res())
        P.op("dve", lambda e, i=i, psKQ=psKQ: e.tensor_tensor(obuf[i][:, 1, :], psKQ[:, 0:128], EQ[:], ALU.mult), psKQ.res() + EQ.res(), obuf[i].res())
        psG = psum(K)
        P.op("pe", lambda e, i=i, psG=psG: e.matmul(psG[:, 0:128], ones_row, rows[i][:, 0, :], start=True, stop=True), rows[i].res() + K.cst.res(), psG.res())
        P.op("act", lambda e, psG=psG: e.activation(EG[:], psG[:, 0:128], AF.Exp), psG.res(), EG.res())
        P.op("dve", lambda e, i=i: e.tensor_tensor(obuf[i][:, 2, :], qT[i][:], EG[:], ALU.mult), qT[i].res() + EG.res(), obuf[i].res())
        P.op("dve", lambda e, i=i, c=c: e.tensor_scalar(obuf[i][:, 3, :], ktm[i][:], colt[:, 3, c:c + 1], None, ALU.mult), ktm[i].res() + colt.res(), obuf[i].res())
        ps = psum(K)
        P.op("pe", lambda e, ps=ps: e.transpose(ps[:, 0:128], Nm[:], ident), Nm.res() + K.cst.res(), ps.res())
        P.op("act", lambda e, ps=ps: e.copy(Qm[0][:], ps[:, 0:128]), ps.res(), Qm[0].res())
        P.op("dve", lambda e: e.tensor_tensor(Xm[0][:], ident, Nm[:], ALU.subtract), Nm.res() + K.cst.res(), Xm[0].res())
        Pc, Qc, Xc = Nm, Qm[0], Xm[0]
        for k in range(1, 7):
            Qn = Qm[k % 2]
            psq = psum(K)
            P.op("pe", lambda e, psq=psq, Pc=Pc, Qc=Qc: e.matmul(psq[:, 0:128], Pc[:], Qc[:], start=True, stop=True), Pc.res() + Qc.res(), psq.res())
            if k < 6:
                Pn = Pm[k % 2]
                psp = psum(K)
                P.op("pe", lambda e, psp=psp, Pc=Pc, Qc=Qc: e.matmul(psp[:, 0:128], Qc[:], Pc[:], start=True, stop=True), Pc.res() + Qc.res(), psp.res())
            P.op("act", lambda e, psq=psq, Qn=Qn: e.copy(Qn[:], psq[:, 0:128]), psq.res(), Qn.res())
            if k < 6:
                P.op("act", lambda e, psp=psp, Pn=Pn: e.copy(Pn[:], psp[:, 0:128]), psp.res(), Pn.res())
            Xn = Xm[k % 2]
            psx = psum(K)
            P.op("pe", lambda e, psx=psx, Qn=Qn, Xc=Xc: e.matmul(psx[:, 0:128], Qn[:], Xc[:], start=True, stop=True), Qn.res() + Xc.res(), psx.res())
            P.op("dve", lambda e, psx=psx, Xn=Xn, Xc=Xc: e.tensor_tensor(Xn[:], psx[:, 0:128], Xc[:], ALU.add), psx.res() + Xc.res(), Xn.res())
            Qc, Xc = Qn, Xn
            if k < 6:
                Pc = Pn
        P.op("dve", lambda e, i=i, Xc=Xc, c=c: e.tensor_scalar(obuf[i][:, 0, :], Xc[:], colt[:, 2, c:c + 1], None, ALU.mult), Xc.res() + colt.res(), obuf[i].res())
        for q_, dst in enumerate((K.gTT, K.gAQ, K.gQE, K.gKD)):
            P.dma(dst[hh][d][cs:cs + 128, :], obuf[i][:, q_, :], obuf[i].res(), dst[hh][d].res(c))
        yield


def gdn_stream(K, l, hh, d):
    P, cfg = K.P, K.cfg
    S, NCH = cfg.S, cfg.NCH
    tg = "gr%d%d" % (hh, d)
    colt = P.sb(tg + "col", [128, 5, NCH], F32)
    P.dma(colt[:].rearrange("p a c -> p (a c)"), K.gcol[hh][d][:, :], K.gcol[hh][d].res(), colt.res())
    st = P.sb(tg + "st", [128, 128], F32)
    stb = P.sb(tg + "stb", [128, 128], BF16)
    P.op("dve", lambda e: e.memset(st[:], 0.0), (), st.res())
    P.op("dve", lambda e: e.memset(stb[:], 0.0), (), stb.res())
    nb = 2
    kT = [P.sb(tg + "kT%d" % i, [128, 128], BF16) for i in range(nb)]
    mats = [P.sb(tg + "m%d" % i, [128, 4, 128], BF16) for i in range(nb)]
    v = [P.sb(tg + "v%d" % i, [128, 128], F32) for i in range(nb)]
    Rb = [P.sb(tg + "R%d" % i, [128, 128], BF16) for i in range(nb)]
    Ub = [P.sb(tg + "U%d" % i, [128, 128], BF16) for i in range(nb)]
    o = [P.sb(tg + "o%d" % i, [128, 128], F32) for i in range(nb)]
    order = range(NCH) if d == 0 else range(NCH - 1, -1, -1)
    hc0 = 256 + hh * 128
    n = 0
    for c in order:
        cs = c * 128
        i = n % nb
        n += 1
        P.dma(kT[i][:], K.gk[hh][:, cs:cs + 128], K.gk[hh].res(), kT[i].res())
        for q_, src in enumerate((K.gTT, K.gAQ, K.gQE, K.gKD)):
            P.dma(mats[i][:, q_, :], src[hh][d][cs:cs + 128, :], src[hh][d].res(c), mats[i].res())
        P.dma(v[i][:], K.gvtm[hh][cs:cs + 128, :], K.gvtm[hh].res(), v[i].res())
        ps1 = psum(K)
        P.op("pe", lambda e, i=i, ps1=ps1: e.matmul(ps1[:, 0:128], kT[i][:], stb[:], start=True, stop=True), kT[i].res() + stb.res(), ps1.res())
        P.op("dve", lambda e, i=i, ps1=ps1, c=c: e.scalar_tensor_tensor(Rb[i][:], ps1[:, 0:128], colt[:, 1, c:c + 1], v[i][:], ALU.mult, ALU.add), ps1.res() + colt.res() + v[i].res(), Rb[i].res())
        ps2 = psum(K)
        P.op("pe", lambda e, i=i, ps2=ps2: e.matmul(ps2[:, 0:128], mats[i][:, 0, :], Rb[i][:], start=True, stop=True), mats[i].res() + Rb[i].res(), ps2.res())
        P.op("act", lambda e, i=i, ps2=ps2: e.copy(Ub[i][:], ps2[:, 0:128]), ps2.res(), Ub[i].res())
        ps3 = psum(K)
        P.op("pe", lambda e, i=i, ps3=ps3: e.matmul(ps3[:, 0:128], mats[i][:, 1, :], Ub[i][:], start=True, stop=False), mats[i].res() + Ub[i].res(), ps3.res())
        P.op("pe", lambda e, i=i, ps3=ps3: e.matmul(ps3[:, 0:128], mats[i][:, 2, :], stb[:], start=False, stop=True), mats[i].res() + stb.res(), ps3.res())
        P.op("act", lambda e, i=i, ps3=ps3: e.copy(o[i][:], ps3[:, 0:128]), ps3.res(), o[i].res())
        P.dma(K.H[d][cs:cs + 128, hc0:hc0 + 128], o[i][:], o[i].res(), K.H[d].res(1 + hh))
        ps4 = psum(K)
        P.op("pe", lambda e, i=i, ps4=ps4: e.matmul(ps4[:, 0:128], mats[i][:, 3, :], Ub[i][:], start=True, stop=True), mats[i].res() + Ub[i].res(), ps4.res())
        P.op("dve", lambda e, ps4=ps4, c=c: e.scalar_tensor_tensor(st[:], st[:], colt[:, 4, c:c + 1], ps4[:, 0:128], ALU.mult, ALU.add), st.res() + colt.res() + ps4.res(), st.res())
        P.op("act", lambda e: e.copy(stb[:], st[:]), st.res(), stb.res())
        yield
```

```python
import numpy as np
from contextlib import ExitStack
import concourse.bass as bass
import concourse.mybir as mybir
from concourse.bass_utils import run_bass_kernel_spmd

F32 = mybir.dt.float32
BF16 = mybir.dt.bfloat16
ALU = mybir.AluOpType
AF = mybir.ActivationFunctionType

ENGS = ("pe", "dve", "act", "pool", "sp")
NDSEM = 6
NEG = -30000.0
EPS = 1e-6
NCORES = 8


class Res:
    __slots__ = ("w", "r")

    def __init__(self):
        self.w = None
        self.r = {}


class T:
    def __init__(self, h, nparts=1):
        self.h = h
        self.parts = [Res() for _ in range(nparts)]

    def __getitem__(self, idx):
        return self.h[idx]

    def ap(self):
        return self.h.ap() if hasattr(self.h, "ap") else self.h[:]

    def res(self, i=None):
        if i is None:
            return self.parts
        if isinstance(i, (list, tuple, range)):
            return [self.parts[j] for j in i]
        return [self.parts[i]]


class Prog:
    def __init__(self, nc, es):
        self.nc = nc
        self.es0 = es
        self.es = es
        self.ops = {e: [] for e in ENGS}
        self.cnt = {e: 0 for e in ENGS}
        self.sem = {e: es.enter_context(nc.semaphore("s_" + e)) for e in ENGS}
        self.dsem = {}
        self.dcnt = {}
        self.drr = {}
        for q in ("sp", "pool", "cc", "act"):
            self.dsem[q] = [es.enter_context(nc.semaphore("d_%s%d" % (q, i))) for i in range(NDSEM)]
            self.dcnt[q] = [0] * NDSEM
            self.drr[q] = 0
        self.seen = {e: {} for e in ENGS}
        self.psum_rr = 0
        self.nins = 0

    def sb(self, name, shape, dtype, nparts=1):
        self.nins += 0
        self._uid = getattr(self, "_uid", 0) + 1
        return T(self.es.enter_context(self.nc.sbuf_tensor("%s_u%d" % (name, self._uid), list(shape), dtype)), nparts)

    def dram(self, name, shape, dtype, nparts=1):
        return T(self.nc.dram_tensor(name, list(shape), dtype, kind="Internal"), nparts)

    def _semh(self, key):
        if isinstance(key, str):
            return self.sem[key]
        return self.dsem[key[0]][key[1]]

    def _deps(self, eng, reads, writes):
        need = {}
        for r in reads:
            if r.w is not None and need.get(r.w[0], 0) < r.w[1]:
                need[r.w[0]] = r.w[1]
        for w in writes:
            if w.w is not None and need.get(w.w[0], 0) < w.w[1]:
                need[w.w[0]] = w.w[1]
            for k, v in w.r.items():
                if need.get(k, 0) < v:
                    need[k] = v
        waits = []
        seen = self.seen[eng]
        for k, v in need.items():
            if seen.get(k, 0) < v:
                seen[k] = v
                waits.append((k, v))
        return waits

    def _mark(self, key, v, reads, writes):
        for r in reads:
            r.r[key] = v
        for w in writes:
            w.w = (key, v)
            w.r = {}

    def op(self, eng, fn, reads=(), writes=()):
        waits = self._deps(eng, reads, writes)
        if eng == "pe":
            waits = [(k, v) for (k, v) in waits if k != "pe"]
        self.cnt[eng] += 1
        v = self.cnt[eng]
        self.ops[eng].append((waits, fn, (eng, 1)))
        self._mark(eng, v, reads, writes)
        self.nins += 1

    def _async(self, q, cls, fn, reads, writes, inc):
        i = self.drr[cls]
        self.drr[cls] = (i + 1) % NDSEM
        key = (cls, i)
        waits = self._deps(q, reads, writes)
        prev = self.dcnt[cls][i]
        if prev and self.seen[q].get(key, 0) < prev:
            self.seen[q][key] = prev
            waits.append((key, prev))
        self.dcnt[cls][i] += inc
        v = self.dcnt[cls][i]
        self.ops[q].append((waits, fn, (key, inc)))
        self._mark(key, v, reads, writes)
        self.nins += 1

    def dma(self, out_ap, in_ap, reads=(), writes=(), q="sp", slow=False):
        cls = q if q in ("sp", "act") else "pool"
        if slow:
            self._async(q, cls, lambda e, o=out_ap, a=in_ap: e.dma_start(out=o, in_=a, allow_slow_non_contiguous=True), reads, writes, 16)
        else:
            self._async(q, cls, lambda e, o=out_ap, a=in_ap: e.dma_start(out=o, in_=a), reads, writes, 16)

    def coll(self, kind, out_ap, in_ap, groups, reads=(), writes=()):
        self._async("pool", "cc",
                    lambda e, o=out_ap, a=in_ap: e.collective_compute(kind, ALU.bypass, replica_groups=groups,
                                                                      ins=[a], outs=[o]),
                    reads, writes, 1)

    def barrier(self, full=False):
        tgt = {e: self.cnt[e] for e in ENGS if self.cnt[e]}
        for cls in (("sp", "act", "pool", "cc") if full else ("sp", "act")):
            for i in range(NDSEM):
                if self.dcnt[cls][i]:
                    tgt[(cls, i)] = self.dcnt[cls][i]
        for e in ENGS:
            waits = []
            for k, v in tgt.items():
                if self.seen[e].get(k, 0) < v:
                    self.seen[e][k] = v
                    waits.append((k, v))
            self.ops[e].append((waits, None, None))

    def flush(self):
        nc = self.nc
        ops = self.ops
        self.ops = {e: [] for e in ENGS}
        with nc.Block() as block:
            def run(e, h):
                for waits, fn, inc in ops[e]:
                    for k, v in waits:
                        h.wait_ge(self._semh(k), v)
                    if fn is not None:
                        fn(h).then_inc(self._semh(inc[0]), inc[1])

            block.tensor(lambda h: run("pe", h))
            block.vector(lambda h: run("dve", h))
            block.scalar(lambda h: run("act", h))
            block.gpsimd(lambda h: run("pool", h))
            block.sync(lambda h: run("sp", h))

    class _Phase:
        def __init__(self, P):
            self.P = P

        def __enter__(self):
            self.es = ExitStack()
            self.es.__enter__()
            self.P.es = self.es
            return self.P

        def __exit__(self, *a):
            self.P.barrier()
            self.P.flush()
            self.P.es = self.P.es0
            return self.es.__exit__(*a)

    def phase(self):
        return Prog._Phase(self)


A_HEADS, A_DK, A_DV = 4, 128, 256
B_HEADS, B_DK, B_DV = 8, 128, 128
C_HEADS, C_DK, C_DV = 4, 128, 256
GLA_RANK, GLA_TAU, GATE_RANK, CONV_K = 16, 16.0, 256, 5
A_QK, A_V = 512, 1024
B_QK, B_V, B_QKV = 1024, 1024, 3072
C_QK, C_V = 512, 1024
PROJ_SIZES = (A_QK, A_QK, A_V, A_V, 16, B_QKV, B_V, 32, C_QK, C_QK, C_V, C_V, 32, GATE_RANK)
OFF = np.concatenate([[0], np.cumsum(PROJ_SIZES)]).tolist()
(O_AQ, O_AK, O_AV, O_AO, O_AGT, O_BQKV, O_BZ, O_BGT, O_CQ, O_CK, O_CV, O_CR, O_CLR, O_GH) = OFF[:14]
NFM = 11
NTM = 5
NCOLS = NFM * 128 + NTM * 256 + 256


def my_cols(j):
    r = lambda a, n: list(range(a, a + n))
    cols = []
    cols += r(O_AQ + j * 128, 128) + r(O_AK + j * 128, 128)
    for part in range(3):
        for hh in (2 * j, 2 * j + 1):
            cols += r(O_BQKV + part * 1024 + hh * 128, 128)
    cols += r(O_CQ + j * 128, 128) + r(O_CK + j * 128, 128)
    small = [-1] * 128
    for g in range(4):
        small[g] = O_AGT + g * 4 + j
    k = 4
    for g in range(4):
        for hh in (2 * j, 2 * j + 1):
            small[k] = O_BGT + g * 8 + hh
            k += 1
    for i in range(16):
        small[32 + i] = O_CLR + i
        small[64 + i] = O_CLR + 16 + i
    cols += small
    cols += r(O_AV + j * 256, 256) + r(O_AO + j * 256, 256)
    cols += r(O_BZ + 2 * j * 128, 256)
    cols += r(O_CV + j * 256, 256) + r(O_CR + j * 256, 256)
    cols += r(O_GH, 256)
    assert len(cols) == NCOLS
    return cols


def block_widths():
    return [128] * NFM + [256] * NTM + [128, 128]


def tile_major(W):
    K, N = W.shape
    return np.ascontiguousarray(W.reshape(K // 128, 128, N // 128, 128).transpose(2, 1, 0, 3)).reshape(-1)


def piece_plan(E):
    P = max(1, -(-E // (8 * 131072)))
    while E % (8 * P) != 0:
        P += 1
    pe = E // (8 * P)
    b = 1
    for cand in (2048, 1024, 512, 256, 128, 64, 661, 1):
        if pe % cand == 0:
            b = cand
            break
    return P, pe, pe // b, b


class Cfg:
    def __init__(self, D, FF, S, L):
        self.D, self.FF, self.S, self.L = D, FF, S, L
        self.KT = D // 128
        self.FT = FF // 128
        self.TL = S // 4
        self.NCH = S // 128
        self.TT = min(256, self.TL)
        self.PT = 256
        self.YP = min(512, S)
        self.big = [("wa", 1024, D), ("wb", 1024, D), ("wc", 1024, D), ("wg", 768, D),
                    ("wo", D, D), ("w1", D, FF), ("w2", FF, D)]


def sp_layout(cfg):
    L, KT = cfg.L, cfg.KT
    o = {}
    n = 0
    for name, w in [("g1", L * KT), ("g2", L * KT), ("gf", KT), ("bm", L * 3 * KT), ("convw", L * 6 * 5),
                    ("convb", L * 6), ("agb", L * 4), ("ang", L * 256), ("gdn_alog", L * 4), ("gdn_dtb", L * 4),
                    ("bng", L * 256), ("glaw", L * 2 * 128), ("glab", L * 2), ("cng", L * 256)]:
        o[name] = n
        n += w
    o["_n"] = n
    return o


def make_consts():
    c = np.zeros((128, 8, 128), np.float32)
    s = np.arange(128)[:, None]
    t = np.arange(128)[None, :]
    c[:, 0] = np.eye(128)
    c[:, 1] = 1.0
    c[:, 2] = np.where(s <= t, 0, NEG)
    c[:, 3] = np.where(s < t, 0, NEG)
    c[:, 4] = np.where(s >= t, 0, NEG)
    c[:, 5] = np.where(s > t, 0, NEG)
    c[:, 6] = (s <= t)
    c[:, 7] = (s >= t)
    return c.reshape(128, 1024)


def prep_inputs(inputs, cfg):
    D, FF, S, L, KT = cfg.D, cfg.FF, cfg.S, cfg.L, cfg.KT
    f = lambda k: np.asarray(inputs[k], dtype=np.float32)
    x = f("x")
    spo = sp_layout(cfg)
    bigsrc = {"wa": f("w_branch_a"), "wb": f("w_branch_b"), "wc": f("w_branch_c"),
              "wg": f("w_merge_gate").reshape(L, 768, D), "wo": f("w_out"), "w1": f("w_ff1"), "w2": f("w_ff2")}
    w_in = f("w_in")
    consts = make_consts()
    shards = {}
    for name, K_, N_ in cfg.big:
        P_, pe, a, b = piece_plan(K_ * N_)
        for l in range(L):
            if name == "wg":
                flat = np.concatenate([tile_major(bigsrc[name][l][jb * 256:(jb + 1) * 256]) for jb in range(3)])
            else:
                flat = tile_major(bigsrc[name][l])
            shards[(name, l)] = flat.reshape(P_, 8, pe)
    in_maps = []
    bw = block_widths()
    for c in range(NCORES):
        b_, j = c // 4, c % 4
        m = {}
        m["x_own"] = np.ascontiguousarray(x[b_, j * cfg.TL:(j + 1) * cfg.TL, :])
        m["consts"] = consts
        rm = np.zeros((128, 4), np.float32)
        rm[:, j] = 1.0
        m["rmask"] = rm
        cols = np.array(my_cols(j))
        for l in range(L):
            wsel = np.where(cols[None, :] >= 0, w_in[l][:, np.maximum(cols, 0)], 0.0).astype(np.float32)
            blocks = []
            c0 = 0
            for w_ in bw:
                blk = wsel[:, c0:c0 + w_]
                blocks.append(np.ascontiguousarray(blk.reshape(KT, 128, w_).transpose(1, 0, 2)).reshape(-1))
                c0 += w_
            m["win_%d" % l] = np.concatenate(blocks).reshape(D, NCOLS)
            for name, K_, N_ in cfg.big:
                P_, pe, a, bb = piece_plan(K_ * N_)
                m["%s_%d" % (name, l)] = np.ascontiguousarray(shards[(name, l)][:, c, :]).reshape(P_ * a, bb)
        sp = np.zeros((128, spo["_n"]), np.float32)
        tm = lambda v: v.reshape(KT, 128).T
        for l in range(L):
            sp[:, spo["g1"] + l * KT: spo["g1"] + (l + 1) * KT] = tm(f("norm1_g")[l])
            sp[:, spo["g2"] + l * KT: spo["g2"] + (l + 1) * KT] = tm(f("norm2_g")[l])
            for jb in range(3):
                o = spo["bm"] + (l * 3 + jb) * KT
                sp[:, o:o + KT] = tm(f("b_merge_gate")[l, jb])
            for blk in range(6):
                part, hh = blk // 2, 2 * j + blk % 2
                ch = part * 1024 + hh * 128
                o = spo["convw"] + (l * 6 + blk) * 5
                sp[:, o:o + 5] = f("conv_w")[l][:, ch:ch + 128].T
                sp[:, spo["convb"] + l * 6 + blk] = f("conv_b")[l][ch:ch + 128]
            sp[:, spo["agb"] + l * 4: spo["agb"] + l * 4 + 4] = f("mlstm_gate_b")[l][:, j][None, :]
            sp[:, spo["ang"] + l * 256: spo["ang"] + (l + 1) * 256] = f("mlstm_norm_g")[l][j * 256:(j + 1) * 256][None, :]
            for d_ in range(2):
                for hh in range(2):
                    sp[:, spo["gdn_alog"] + l * 4 + d_ * 2 + hh] = f("gdn_a_log")[l, d_, 2 * j + hh]
                    sp[:, spo["gdn_dtb"] + l * 4 + d_ * 2 + hh] = f("gdn_dt_bias")[l, d_, 2 * j + hh]
            sp[:, spo["bng"] + l * 256: spo["bng"] + (l + 1) * 256] = f("gdn_norm_g")[l][2 * j * 128:(2 * j + 2) * 128][None, :]
            for d_ in range(2):
                o = spo["glaw"] + (l * 2 + d_) * 128
                sp[0:16, o:o + 128] = f("gla_w_gate")[l, d_][:, j * 128:(j + 1) * 128]
                sp[:, spo["glab"] + l * 2 + d_] = f("gla_b_gate")[l, d_][j * 128:(j + 1) * 128]
            sp[:, spo["cng"] + l * 256: spo["cng"] + (l + 1) * 256] = f("gla_norm_g")[l][j * 256:(j + 1) * 256][None, :]
        sp[:, spo["gf"]: spo["gf"] + KT] = tm(f("final_g"))
        m["sp"] = sp
        in_maps.append(m)
    return in_maps


class Ctx:
    pass


def flat_blocks(t, nblk_elems):
    a = t.h.ap()
    fl = a.rearrange("r b -> (r b)")
    return fl.rearrange("(n p f) -> n p f", p=128, f=nblk_elems // 128)


def build(cfg, debug=()):
    D, FF, S, L, KT, FT, TL, NCH, TT, PT = cfg.D, cfg.FF, cfg.S, cfg.L, cfg.KT, cfg.FT, cfg.TL, cfg.NCH, cfg.TT, cfg.PT
    nc = bass.Bass("TRN2", target_bir_lowering=False)
    K = Ctx()
    K.cfg, K.nc = cfg, nc
    spo = sp_layout(cfg)
    K.spo = spo
    ext = lambda name, shape, dt=F32: T(nc.dram_tensor(name, list(shape), dt, kind="ExternalInput"))
    K.x_own = ext("x_own", [TL, D])
    K.consts_d = ext("consts", [128, 1024])
    K.rmask_d = ext("rmask", [128, 4])
    K.sp_d = ext("sp", [128, spo["_n"]])
    K.win_d = [ext("win_%d" % l, [D, NCOLS]) for l in range(L)]
    K.big_d = {}
    for name, K_, N_ in cfg.big:
        P_, pe, a, b = piece_plan(K_ * N_)
        for l in range(L):
            K.big_d[(name, l)] = ext("%s_%d" % (name, l), [P_ * a, b])
    K.out = T(nc.dram_tensor("out", [TL, D], F32, kind="ExternalOutput"))
    K.stub_y = ext("stub_y", [S // cfg.YP * 768, cfg.YP], BF16) if "stub_y" in debug else None
    K.debug = debug
    K.which = "abc"
    K.post = True
    K.post_lvl = 9
    for d_ in debug:
        if d_.startswith("postlvl="):
            K.post_lvl = int(d_[8:])
    for d_ in debug:
        if d_.startswith("which="):
            K.which = d_[6:]
        if d_ == "nopost":
            K.post = False
    K.dbg_out = []
    with ExitStack() as es:
        P = Prog(nc, es)
        K.P = P
        K.ps = [T(es.enter_context(nc.psum_tensor("ps%d" % i, [128, 512], F32))) for i in range(7)]
        K.psb = T(es.enter_context(nc.psum_tensor("psb", [128, 1024], BF16)), nparts=4)
        K.ps_rr = 0
        K.psb_rr = 0
        K.cst = P.sb("cst", [128, 1024], F32)
        K.spt = P.sb("spt", [128, spo["_n"]], F32)
        K.rmask = P.sb("rmaskt", [128, 4], F32)
        K.identb = P.sb("identb", [128, 128], BF16)
        P.dma(K.cst[:], K.consts_d[:], K.consts_d.res(), K.cst.res())
        P.dma(K.spt[:], K.sp_d[:], K.sp_d.res(), K.spt.res())
        P.dma(K.rmask[:], K.rmask_d[:], K.rmask_d.res(), K.rmask.res())
        P.op("dve", lambda e: e.tensor_copy(K.identb[:], K.cst[:, 0:128]), K.cst.res(), K.identb.res())
        K.xres = P.dram("xres", [D, TL], F32, nparts=TL // TT)
        K.xnp = P.dram("xnp", [TL // 128 * 128, KT * 128], BF16, nparts=TL // 128)
        K.xng = P.dram("xng", [TL // 128 * 4 * 128, KT * 128], BF16, nparts=TL // 128)
        K.ghT = P.dram("ghT", [256, TL], BF16)
        K.wfull = {}
        K.winb = []
        for l in range(L):
            K.winb.append(P.dram("winb_%d" % l, [D, NCOLS], BF16))
            for name, K_, N_ in cfg.big:
                P_, pe, a, b = piece_plan(K_ * N_)
                K.wfull[(name, l)] = P.dram("wf_%s_%d" % (name, l), [P_ * 8 * a, b], BF16)
                K.wfull[(name, l)].tmp = P.dram("wsb_%s_%d" % (name, l), [P_ * a, b], BF16)
                K.wfull[(name, l)].half = P.dram("wsh_%s_%d" % (name, l), [P_ * 4 * a, b], BF16, nparts=P_)
        alloc_mixer_dram(K)
        if "dumpH" in debug:
            K.dbg_out += [("H0", K.H[0]), ("H1", K.H[1]), ("fm32", K.fm32), ("fm16", K.fm16)]
        if "dumpY" in debug:
            K.dbg_out += [("yp", K.yp)]
        cast_win(K, 0)
        phase_x0(K)
        for l in range(L):
            phase_a(K, l)
            if l == 0:
                phase_weights(K, 0)
            phase_b(K, l)
            if l + 1 < L:
                cast_win(K, l + 1)
                phase_weights(K, l + 1)
            phase_dense(K, l, final=(l == L - 1))
        for name, t in K.dbg_out:
            o = T(nc.dram_tensor("dbg_" + name, list(t.h.shape), t.h.dtype, kind="ExternalOutput"))
            P.dma(o.h.ap(), t.h.ap(), t.res(), o.res(), q="pool")
        P.barrier(full=True)
        P.flush()
    return nc


def psum(K):
    i = K.ps_rr
    K.ps_rr = (i + 1) % len(K.ps)
    return K.ps[i]


def cast_win(K, l):
    P, cfg = K.P, K.cfg
    rows = cfg.D
    step = max(1, rows // 8)
    for r0 in range(0, rows, step):
        P.dma(K.winb[l][r0:r0 + step, :], K.win_d[l][r0:r0 + step, :], K.win_d[l].res(), K.winb[l].res(), q="pool")


def phase_weights(K, l):
    P, cfg = K.P, K.cfg
    g4 = [[0, 1, 2, 3], [4, 5, 6, 7]]
    g2 = [[0, 4], [1, 5], [2, 6], [3, 7]]
    for name, K_, N_ in cfg.big:
        P_, pe, a, b = piece_plan(K_ * N_)
        src, full = K.big_d[(name, l)], K.wfull[(name, l)]
        tmp, half = full.tmp, full.half
        nrow = P_ * a
        step = max(a, (nrow // 8 // a) * a) if nrow >= 8 * a else nrow
        for r0 in range(0, nrow, step):
            r1 = min(nrow, r0 + step)
            P.dma(tmp[r0:r1, :], src[r0:r1, :], src.res(), tmp.res(), q="pool")
        for p in range(P_):
            P.coll("AllGather", half[p * 4 * a:(p + 1) * 4 * a, :], tmp[p * a:(p + 1) * a, :], g4, tmp.res(), half.res(p))
        for p in range(P_):
            P.coll("AllGather", full[p * 8 * a:(p + 1) * 8 * a, :], half[p * 4 * a:(p + 1) * 4 * a, :], g2, half.res(p), full.res())


def phase_x0(K):
    P, cfg = K.P, K.cfg
    D, KT, TL = cfg.D, cfg.KT, cfg.TL
    xr = K.xres.h.ap().rearrange("(k p) t -> p k t", p=128)
    with P.phase():
        xt = [P.sb("x0_in%d" % i, [128, D], F32) for i in range(2)]
        xo = [P.sb("x0_out%d" % i, [128, KT, 128], F32) for i in range(2)]
        ident = K.cst[:, 0:128]
        for tb in range(TL // 128):
            a, o = xt[tb % 2], xo[tb % 2]
            P.dma(a[:], K.x_own[tb * 128:(tb + 1) * 128, :], K.x_own.res(), a.res())
            for g in range(KT // 4):
                ps = psum(K)
                for i in range(4):
                    kt = g * 4 + i
                    P.op("pe", lambda e, ps=ps, i=i, kt=kt, a=a: e.transpose(ps[:, i * 128:(i + 1) * 128], a[:, kt * 128:(kt + 1) * 128], ident),
                         a.res() + K.cst.res(), ps.res())
                P.op("act", lambda e, ps=ps, g=g, o=o: e.copy(o[:, g * 4:(g + 1) * 4, :], ps[:, :]), ps.res(), o.res())
            P.dma(xr[:, :, tb * 128:(tb + 1) * 128], o[:], o.res(), K.xres.res((tb * 128) // cfg.TT))


def rms_stats(K, xt, nkt, ntok, sq, rstd, tagres):
    P = K.P
    ones = K.cst[:, 128:256]
    ps = psum(K)
    G = 4
    for g in range(nkt // G):
        s = sq[g % 2]
        P.op("act", lambda e, s=s, g=g: e.activation(s[:], xt[:, g * G:(g + 1) * G, :], AF.Square), xt.res(), s.res())
        for i in range(G):
            kt = g * G + i
            P.op("pe", lambda e, s=s, i=i, kt=kt: e.matmul(ps[:, 0:ntok], ones, s[:, i, :], start=(kt == 0), stop=(kt == nkt - 1)),
                 s.res() + K.cst.res(), ps.res())
    P.op("act", lambda e: e.activation(rstd[:], ps[:, 0:ntok], AF.Sqrt, bias=K.epsD[:, 0:1], scale=1.0 / (nkt * 128)),
         ps.res() + K.epst.res(), rstd.res())
    P.op("dve", lambda e: e.reciprocal(rstd[:], rstd[:]), rstd.res(), rstd.res())


def phase_a(K, l):
    P, cfg, spo = K.P, K.cfg, K.spo
    D, KT, TL = cfg.D, cfg.KT, cfg.TL
    xr = K.xres.h.ap().rearrange("(k p) t -> p k t", p=128)
    groups4 = [[0, 1, 2, 3], [4, 5, 6, 7]]
    gho = (NFM * 128 + NTM * 256) * D
    wfl = K.winb[l].h.ap().rearrange("r b -> (r b)")
    with P.phase():
        make_eps(K)
        xt = [P.sb("a_x%d" % i, [128, KT, 128], F32) for i in range(2)]
        xn = [P.sb("a_xn%d" % i, [128, KT, 128], BF16) for i in range(2)]
        sq = [P.sb("a_sq%d" % i, [128, 4, 128], F32) for i in range(2)]
        rstd = [P.sb("a_rstd%d" % i, [128, 128], F32) for i in range(2)]
        wgh = P.sb("a_wgh", [128, 2, KT, 128], BF16)
        gho_sb = [P.sb("a_gho%d" % i, [128, 2, 128], BF16) for i in range(2)]
        for m in range(2):
            src = wfl[gho + m * D * 128: gho + (m + 1) * D * 128].rearrange("(p f) -> p f", p=128)
            P.dma(wgh[:, m, :, :], src, K.winb[l].res(), wgh.res())
        g1 = K.spt
        ght = K.ghT.h.ap().rearrange("(m p) t -> p m t", p=128)
        for pc in range(TL // 128):
            a, n, r, go = xt[pc % 2], xn[pc % 2], rstd[pc % 2], gho_sb[pc % 2]
            P.dma(a[:], xr[:, :, pc * 128:(pc + 1) * 128], K.xres.res((pc * 128) // cfg.TT), a.res())
            rms_stats(K, a, KT, 128, sq, r, None)
            for kt in range(KT):
                c = spo["g1"] + l * KT + kt
                P.op("dve", lambda e, kt=kt, c=c, a=a, n=n, r=r: e.scalar_tensor_tensor(n[:, kt, :], a[:, kt, :], g1[:, c:c + 1], r[:], ALU.mult, ALU.mult),
                     a.res() + r.res() + K.spt.res(), n.res())
            P.dma(K.xnp[pc * 128:(pc + 1) * 128, :], n[:].rearrange("p k t -> p (k t)"), n.res(), K.xnp.res(pc))
            for m in range(2):
                ps = psum(K)
                for kt in range(KT):
                    P.op("pe", lambda e, ps=ps, m=m, kt=kt, n=n: e.matmul(ps[:, 0:128], wgh[:, m, kt, :], n[:, kt, :], start=(kt == 0), stop=(kt == KT - 1)),
                         wgh.res() + n.res(), ps.res())
                P.op("act", lambda e, ps=ps, m=m, go=go: e.copy(go[:, m, :], ps[:, 0:128]), ps.res(), go.res())
            P.dma(ght[:, :, pc * 128:(pc + 1) * 128], go[:], go.res(), K.ghT.res())
            P.coll("AllGather", K.xng[pc * 512:(pc + 1) * 512, :], K.xnp[pc * 128:(pc + 1) * 128, :], groups4,
                   K.xnp.res(pc), K.xng.res(pc))


def make_eps(K):
    P = K.P
    K.epst = P.sb("epst", [128, 2], F32)
    K.epsD = K.epst
    P.op("dve", lambda e: e.memset(K.epst[:], EPS), (), K.epst.res())


def phase_dense(K, l, final):
    P, cfg, spo = K.P, K.cfg, K.spo
    D, FF, KT, FT, TL, TT, YP = cfg.D, cfg.FF, cfg.KT, cfg.FT, cfg.TL, cfg.TT, cfg.YP
    xr = K.xres.h.ap().rearrange("(k p) t -> p k t", p=128)
    ght = K.ghT.h.ap().rearrange("(m p) t -> p m t", p=128)
    ygv = K.yg.h.ap().rearrange("(q c p) t -> q p c t", c=6, p=128)
    wbr = [flat_blocks(K.wfull[(n, l)], 8 * 128 * 128) for n in ("wa", "wb", "wc")]
    wgfl = K.wfull[("wg", l)].h.ap().rearrange("r b -> (r b)")
    wo = flat_blocks(K.wfull[("wo", l)], D * 128)
    w1 = flat_blocks(K.wfull[("w1", l)], D * 128)
    w2 = flat_blocks(K.wfull[("w2", l)], FF * 128)
    FSUB = min(FT, 32)
    with P.phase():
        make_eps(K)
        xT = P.sb("d_xT", [128, KT, TT], F32, nparts=KT)
        act = P.sb("d_act", [128, KT, TT], BF16, nparts=KT)
        hT = P.sb("d_hT", [128, FT, TT], BF16, nparts=FT)
        yT = [P.sb("d_yT%d" % j, [128, 6, TT], BF16) for j in range(4)]
        ycand = [P.sb("d_yc%d" % i, [128, 6, TT], BF16) for i in range(2)]
        ghs = P.sb("d_gh", [128, 2, TT], BF16)
        NW = 4
        wp = [P.sb("d_w%d" % i, [128, 4096], BF16) for i in range(NW)]
        K.wrr = 0
        sq = [P.sb("d_sq%d" % i, [128, 4, TT], F32) for i in range(2)]
        rstd = P.sb("d_rstd", [128, TT], F32)
        gs = [P.sb("d_gs%d" % i, [128, TT], F32) for i in range(2)]
        tmp = [P.sb("d_tmp%d" % i, [128, TT], F32) for i in range(2)]
        acc = P.sb("d_acc", [128, TT], F32)
        if final:
            otv = hT.h[:].rearrange("p f t -> p (f t)").bitcast(F32)

        def wnext():
            w = wp[K.wrr]
            K.wrr = (K.wrr + 1) % NW
            return w

        for tt in range(TL // TT):
            t0 = tt * TT
            P.dma(xT[:], xr[:, :, t0:t0 + TT], K.xres.res(tt), xT.res())
            P.dma(ghs[:], ght[:, :, t0:t0 + TT], K.ghT.res(), ghs.res())
            ci = 0
            for j in range(4):
                for r in range(4):
                    g0 = r * TL + t0
                    tb, off = g0 // YP, g0 % YP
                    yc = ycand[ci % 2]
                    ci += 1
                    P.dma(yc[:], ygv[tb * 4 + j][:, :, off:off + TT], K.yg.res(tb), yc.res())
                    if r == 0:
                        P.op("dve", lambda e, j=j, yc=yc, r=r: e.tensor_scalar(yT[j][:], yc[:], K.rmask[:, r:r + 1], None, ALU.mult),
                             yc.res() + K.rmask.res(), yT[j].res())
                    else:
                        P.op("dve", lambda e, j=j, yc=yc, r=r: e.scalar_tensor_tensor(yT[j][:], yc[:], K.rmask[:, r:r + 1], yT[j][:], ALU.mult, ALU.add),
                             yc.res() + K.rmask.res() + yT[j].res(), yT[j].res())
            for dt in range(KT):
                w = wnext()
                for jb in range(3):
                    o = jb * 256 * D + dt * 256 * 128
                    P.dma(w[:, jb * 256:(jb + 1) * 256], wgfl[o:o + 256 * 128].rearrange("(p f) -> p f", p=128),
                          K.wfull[("wg", l)].res(), w.res())
                    P.dma(w[:, 768 + jb * 1024: 768 + (jb + 1) * 1024], wbr[jb][dt], K.wfull[(("wa", "wb", "wc")[jb], l)].res(), w.res())
                for jb in range(3):
                    psg = psum(K)
                    for rt in range(2):
                        c0 = jb * 256 + rt * 128
                        P.op("pe", lambda e, psg=psg, w=w, c0=c0, rt=rt: e.matmul(psg[:, 0:TT], w[:, c0:c0 + 128], ghs[:, rt, :], start=(rt == 0), stop=(rt == 1)),
                             w.res() + ghs.res(), psg.res())
                    g = gs[jb % 2]
                    bc = spo["bm"] + (l * 3 + jb) * KT + dt
                    P.op("act", lambda e, psg=psg, g=g, bc=bc: e.activation(g[:], psg[:, 0:TT], AF.Sigmoid, bias=K.spt[:, bc:bc + 1]),
                         psg.res() + K.spt.res(), g.res())
                    psb = psum(K)
                    for ct in range(8):
                        c0 = 768 + jb * 1024 + ct * 128
                        P.op("pe", lambda e, psb=psb, w=w, c0=c0, ct=ct, jb=jb: e.matmul(psb[:, 0:TT], w[:, c0:c0 + 128], yT[ct // 2][:, 2 * jb + ct % 2, :], start=(ct == 0), stop=(ct == 7)),
                             w.res() + yT[ct // 2].res(), psb.res())
                    if jb == 0:
                        P.op("dve", lambda e, psb=psb, g=g: e.tensor_tensor(acc[:], psb[:, 0:TT], g[:], ALU.mult), psb.res() + g.res(), acc.res())
                    else:
                        tm_ = tmp[jb % 2]
                        P.op("dve", lambda e, psb=psb, g=g, tm_=tm_: e.tensor_tensor(tm_[:], psb[:, 0:TT], g[:], ALU.mult), psb.res() + g.res(), tm_.res())
                        if jb == 1:
                            P.op("dve", lambda e, tm_=tm_: e.tensor_tensor(acc[:], acc[:], tm_[:], ALU.add), acc.res() + tm_.res(), acc.res())
                        else:
                            P.op("dve", lambda e, tm_=tm_, dt=dt: e.tensor_tensor(act[:, dt, :], acc[:], tm_[:], ALU.add), acc.res() + tm_.res(), act.res(dt))
            for et in range(KT):
                w = wnext()
                P.dma(w[:, 0:KT * 128], wo[et], K.wfull[("wo", l)].res(), w.res())
                ps = psum(K)
                for dt in range(KT):
                    P.op("pe", lambda e, ps=ps, w=w, dt=dt: e.matmul(ps[:, 0:TT], w[:, dt * 128:(dt + 1) * 128], act[:, dt, :], start=(dt == 0), stop=(dt == KT - 1)),
                         w.res() + act.res(dt), ps.res())
                P.op("dve", lambda e, ps=ps, et=et: e.tensor_tensor(xT[:, et, :], xT[:, et, :], ps[:, 0:TT], ALU.add), ps.res() + xT.res(et), xT.res(et))
            rms_stats(K, xT, KT, TT, sq, rstd, None)
            for kt in range(KT):
                c = spo["g2"] + l * KT + kt
                P.op("dve", lambda e, kt=kt, c=c: e.scalar_tensor_tensor(act[:, kt, :], xT[:, kt, :], K.spt[:, c:c + 1], rstd[:], ALU.mult, ALU.mult),
                     xT.res(kt) + rstd.res() + K.spt.res(), act.res(kt))
            for ft in range(FT):
                w = wnext()
                P.dma(w[:, 0:KT * 128], w1[ft], K.wfull[("w1", l)].res(), w.res())
                ps = psum(K)
                for kt in range(KT):
                    P.op("pe", lambda e, ps=ps, w=w, kt=kt: e.matmul(ps[:, 0:TT], w[:, kt * 128:(kt + 1) * 128], act[:, kt, :], start=(kt == 0), stop=(kt == KT - 1)),
                         w.res() + act.res(kt), ps.res())
                sv = tmp[ft % 2]
                P.op("act", lambda e, ps=ps, sv=sv: e.activation(sv[:], ps[:, 0:TT], AF.Square), ps.res(), sv.res())
                P.op("dve", lambda e, ps=ps, sv=sv, ft=ft: e.scalar_tensor_tensor(hT[:, ft, :], ps[:, 0:TT], 0.0, sv[:], ALU.is_gt, ALU.mult),
                     ps.res() + sv.res(), hT.res(ft))
            for dt in range(KT):
                ps = psum(K)
                for sub in range(FT // FSUB):
                    w = wnext()
                    P.dma(w[:, 0:FSUB * 128], w2[dt][:, sub * FSUB * 128:(sub + 1) * FSUB * 128], K.wfull[("w2", l)].res(), w.res())
                    for fi in range(FSUB):
                        ft = sub * FSUB + fi
                        P.op("pe", lambda e, ps=ps, w=w, fi=fi, ft=ft: e.matmul(ps[:, 0:TT], w[:, fi * 128:(fi + 1) * 128], hT[:, ft, :], start=(ft == 0), stop=(ft == FT - 1)),
                             w.res() + hT.res(ft), ps.res())
                P.op("dve", lambda e, ps=ps, dt=dt: e.tensor_tensor(xT[:, dt, :], xT[:, dt, :], ps[:, 0:TT], ALU.add), ps.res() + xT.res(dt), xT.res(dt))
            if not final:
                P.dma(xr[:, :, t0:t0 + TT], xT[:], xT.res(), K.xres.res(tt))
            else:
                rms_stats(K, xT, KT, TT, sq, rstd, None)
                for kt in range(KT):
                    c = spo["gf"] + kt
                    P.op("dve", lambda e, kt=kt, c=c: e.scalar_tensor_tensor(xT[:, kt, :], xT[:, kt, :], K.spt[:, c:c + 1], rstd[:], ALU.mult, ALU.mult),
                         xT.res(kt) + rstd.res() + K.spt.res(), xT.res(kt))
                ident = K.cst[:, 0:128]
                for s_ in range(TT // 128):
                    o_ = otv[:, (s_ % 2) * D:(s_ % 2 + 1) * D]
                    for g in range(KT // 4):
                        ps = psum(K)
                        for i in range(4):
                            kt = g * 4 + i
                            P.op("pe", lambda e, ps=ps, i=i, kt=kt, s_=s_: e.transpose(ps[:, i * 128:(i + 1) * 128], xT[:, kt, s_ * 128:(s_ + 1) * 128], ident),
                                 xT.res(kt) + K.cst.res(), ps.res())
                        P.op("act", lambda e, ps=ps, g=g, o_=o_: e.copy(o_[:, g * 512:(g + 1) * 512], ps[:, :]), ps.res(), hT.res())
                    P.dma(K.out[t0 + s_ * 128: t0 + (s_ + 1) * 128, :], o_, hT.res(), K.out.res())


def alloc_mixer_dram(K):
    P, cfg = K.P, K.cfg
    S, YP = cfg.S, cfg.YP
    npc = S // YP
    K.fm32 = P.dram("fm32", [9 * 128, S], F32, nparts=9)
    K.fm16 = P.dram("fm16", [2 * 128, S], BF16, nparts=2)
    K.tm = {"av": P.dram("tm_av", [S, 256], BF16), "ao": P.dram("tm_ao", [S, 256], F32),
            "bz": P.dram("tm_bz", [S, 256], F32), "cv": P.dram("tm_cv", [S, 256], BF16),
            "cr": P.dram("tm_cr", [S, 256], F32)}
    K.yp = P.dram("yp", [npc * 6 * 128, YP], BF16, nparts=npc)
    K.yg = P.dram("yg", [npc * 4 * 6 * 128, YP], BF16, nparts=npc)
    K.H = [P.dram("H%d" % d, [S, 768], F32, nparts=4) for d in range(2)]
    K.gl = {}


def b_proj(K, l):
    P, cfg = K.P, K.cfg
    D, KT, S, TL, PT = cfg.D, cfg.KT, cfg.S, cfg.TL, cfg.PT
    wfl = K.winb[l].h.ap().rearrange("r b -> (r b)")
    bw = block_widths()
    boff = np.concatenate([[0], np.cumsum(bw)]).tolist()
    tmnames = ["av", "ao", "bz", "cv", "cr"]
    NB = NFM + NTM
    with P.phase():
        xn = [P.sb("p_xn%d" % i, [128, KT, PT], BF16) for i in range(2)]
        NWQ = 4
        wq = [P.sb("p_w%d" % i, [128, KT * 256], BF16) for i in range(NWQ)]
        st32 = [P.sb("p_s32_%d" % i, [128, 512], F32) for i in range(3)]
        st16 = [P.sb("p_s16_%d" % i, [128, 512], BF16) for i in range(3)]
        ntile = S // PT
        jobs = [(ti, bi) for ti in range(ntile) for bi in range(NB)]

        def load_w(k):
            ti, bi = jobs[k]
            w = wq[k % NWQ]
            wd = 128 if bi < NFM else 256
            o = boff[bi] * D
            P.dma(w[:, 0:KT * wd], wfl[o:o + D * wd].rearrange("(p f) -> p f", p=128), K.winb[l].res(), w.res())

        def load_x(ti):
            g0 = ti * PT
            r, w0 = g0 // TL, g0 % TL
            x = xn[ti % 2]
            for i in range(PT // 128):
                pc = (w0 + i * 128) // 128
                P.dma(x[:, :, i * 128:(i + 1) * 128], K.xng[pc * 512 + r * 128: pc * 512 + (r + 1) * 128, :].rearrange("p (k t) -> p k t", t=128),
                      K.xng.res(pc), x.res())

        load_x(0)
        for k in range(min(NWQ - 1, len(jobs))):
            load_w(k)
        sr = 0
        for k, (ti, bi) in enumerate(jobs):
            g0 = ti * PT
            x = xn[ti % 2]
            w = wq[k % NWQ]
            if bi == 0 and ti + 1 < ntile:
                load_x(ti + 1)
            if k + NWQ - 1 < len(jobs):
                load_w(k + NWQ - 1)
            if bi < NFM:
                ps = psum(K)
                for kt in range(KT):
                    P.op("pe", lambda e, ps=ps, w=w, kt=kt, x=x: e.matmul(ps[:, 0:PT], w[:, kt * 128:(kt + 1) * 128], x[:, kt, :], start=(kt == 0), stop=(kt == KT - 1)),
                         w.res() + x.res(), ps.res())
                if bi < 2:
                    st = st16[sr % 3]
                    sr += 1
                    P.op("act", lambda e, ps=ps, st=st, bi=bi: e.activation(st[:, 0:PT], ps[:, 0:PT], AF.Copy, scale=(1.0 if bi == 0 else A_DK ** -0.5)), ps.res(), st.res())
                    P.dma(K.fm16[bi * 128:(bi + 1) * 128, g0:g0 + PT], st[:, 0:PT], st.res(), K.fm16.res(bi), q="act")
                else:
                    st = st32[sr % 3]
                    sr += 1
                    P.op("act", lambda e, ps=ps, st=st: e.copy(st[:, 0:PT], ps[:, 0:PT]), ps.res(), st.res())
                    P.dma(K.fm32[(bi - 2) * 128:(bi - 1) * 128, g0:g0 + PT], st[:, 0:PT], st.res(), K.fm32.res(bi - 2), q="act")
            else:
                nm = tmnames[bi - NFM]
                dst = K.tm[nm]
                is16 = nm in ("av", "cv")
                for sub in range(PT // 128):
                    ps = psum(K)
                    for kt in range(KT):
                        P.op("pe", lambda e, ps=ps, w=w, kt=kt, x=x, sub=sub: e.matmul(ps[:, 0:256], x[:, kt, sub * 128:(sub + 1) * 128], w[:, kt * 256:(kt + 1) * 256], start=(kt == 0), stop=(kt == KT - 1)),
                             w.res() + x.res(), ps.res())
                    st = (st16 if is16 else st32)[sr % 3]
                    sr += 1
                    P.op("dve", lambda e, ps=ps, st=st: e.tensor_copy(st[:, 0:256], ps[:, 0:256]), ps.res(), st.res())
                    P.dma(dst[g0 + sub * 128: g0 + (sub + 1) * 128, :], st[:, 0:256], st.res(), dst.res(), q="act")


def b_ygather(K, l):
    P, cfg = K.P, K.cfg
    npc = cfg.S // cfg.YP
    groups4 = [[0, 1, 2, 3], [4, 5, 6, 7]]
    for tb in range(npc):
        P.coll("AllGather", K.yg[tb * 4 * 768:(tb + 1) * 4 * 768, :], K.yp[tb * 768:(tb + 1) * 768, :], groups4,
               K.yp.res(tb), K.yg.res(tb))


def phase_b(K, l):
    b_proj(K, l)
    if K.stub_y is not None:
        P = K.P
        P.dma(K.yp[:, :], K.stub_y[:, :], K.stub_y.res(), K.yp.res(), q="pool")
    else:
        b_mixers(K, l)
    b_ygather(K, l)


def run_cfg(inputs, cfg, debug=(), extra=None):
    in_maps = prep_inputs(inputs, cfg)
    if extra is not None:
        for c in range(NCORES):
            in_maps[c].update(extra[c])
    nc = build(cfg, debug=debug)
    res = run_bass_kernel_spmd(nc, in_maps, core_ids=list(range(NCORES)))
    out = np.zeros((2, cfg.S, cfg.D), np.float32)
    for c in range(NCORES):
        b_, j = c // 4, c % 4
        out[b_, j * cfg.TL:(j + 1) * cfg.TL, :] = res.results[c]["out"]
    return out, res


def kernel(**inputs):
    x = np.asarray(inputs["x"])
    L = int(np.asarray(inputs["w_in"]).shape[0])
    cfg = Cfg(int(x.shape[2]), int(np.asarray(inputs["w_ff1"]).shape[2]), int(x.shape[1]), L)
    out, _ = run_cfg(inputs, cfg)
    return out.astype(np.float32)


def tr_bf(K, dst_ap, dst_res, src_ap, src_res, eng="act", scale_ap=None):
    P = K.P
    i = K.psb_rr
    K.psb_rr = (i + 1) % 4
    pv = K.psb[:, i * 256:i * 256 + 128]
    P.op("pe", lambda e: e.transpose(pv, src_ap, K.identb[:]), list(src_res) + K.identb.res(), K.psb.res(i))
    if scale_ap is not None:
        P.op("dve", lambda e: e.tensor_scalar(dst_ap, pv, scale_ap[0], None, ALU.mult), K.psb.res(i) + list(scale_ap[1]), dst_res)
    elif eng == "act":
        P.op("act", lambda e: e.copy(dst_ap, pv), K.psb.res(i), dst_res)
    else:
        P.op("dve", lambda e: e.tensor_copy(dst_ap, pv), K.psb.res(i), dst_res)


def gla_prep(K, l):
    P, cfg, spo = K.P, K.cfg, K.spo
    S, PT = cfg.S, cfg.PT
    K.glaB = [P.dram("glaB%d_%d" % (d, l), [128, S + 1], F32, nparts=S // PT) for d in range(2)]
    if "dumpG" in K.debug and l == 0:
        K.dbg_out += [("glaB0", K.glaB[0]), ("glaB1", K.glaB[1])]
    with P.phase():
        negb = P.sb("gp_negb", [128, 2], F32)
        zc = P.sb("gp_z", [128, 1], F32)
        P.op("dve", lambda e: e.memset(zc[:], 0.0), (), zc.res())
        P.op("dve", lambda e: e.tensor_scalar(negb[:], K.spt[:, spo["glab"] + l * 2: spo["glab"] + l * 2 + 2], -1.0, None, ALU.mult), K.spt.res(), negb.res())
        clr = [P.sb("gp_clr%d" % i, [16, PT], F32) for i in range(2)]
        ex = [P.sb("gp_e%d" % i, [128, PT], F32) for i in range(2)]
        bt = [P.sb("gp_b%d" % i, [128, PT], F32) for i in range(3)]
        n = 0
        for d in range(2):
            P.dma(K.glaB[d][:, (0 if d == 0 else S):(1 if d == 0 else S + 1)], zc[:], zc.res(), K.glaB[d].res(0 if d == 0 else S // PT - 1), slow=True)
            prev = None
            order = range(S // PT) if d == 0 else range(S // PT - 1, -1, -1)
            wg = K.spt[0:16, spo["glaw"] + (l * 2 + d) * 128: spo["glaw"] + (l * 2 + d + 1) * 128]
            for ti in order:
                g0 = ti * PT
                c_, e_, b_ = clr[n % 2], ex[n % 2], bt[n % 3]
                n += 1
                r0 = 8 * 128 + 32 + 32 * d
                P.dma(c_[:], K.fm32[r0:r0 + 16, g0:g0 + PT], K.fm32.res(8), c_.res())
                ps = psum(K)
                P.op("pe", lambda e, ps=ps, c_=c_, wg=wg: e.matmul(ps[:, 0:PT], wg, c_[:], start=True, stop=True), c_.res() + K.spt.res(), ps.res())
                P.op("act", lambda e, ps=ps, e_=e_, d=d: e.activation(e_[:], ps[:, 0:PT], AF.Exp, bias=negb[:, d:d + 1], scale=-1.0), ps.res() + negb.res(), e_.res())
                P.op("act", lambda e, e_=e_: e.activation(e_[:], e_[:], AF.Ln, bias=K.cst[:, 128:129], scale=1.0), e_.res() + K.cst.res(), e_.res())
                P.op("dve", lambda e, e_=e_: e.tensor_scalar(e_[:], e_[:], 1.0 / GLA_TAU, None, ALU.mult), e_.res(), e_.res())
                if d == 0:
                    init = 0.0 if prev is None else prev[:, PT - 1:PT]
                    P.op("dve", lambda e, b_=b_, e_=e_, init=init: e.tensor_tensor_scan(b_[:], e_[:], e_[:], init, ALU.add, ALU.bypass),
                         e_.res() + (prev.res() if prev is not None else []), b_.res())
                    P.dma(K.glaB[d][:, 1 + g0:1 + g0 + PT], b_[:], b_.res(), K.glaB[d].res(ti))
                else:
                    init = 0.0 if prev is None else prev[:, 0:1]
                    P.op("dve", lambda e, b_=b_, e_=e_, init=init: e.tensor_tensor_scan(b_[:, ::-1], e_[:, ::-1], e_[:, ::-1], init, ALU.add, ALU.bypass),
                         e_.res() + (prev.res() if prev is not None else []), b_.res())
                    P.dma(K.glaB[d][:, g0:g0 + PT], b_[:], b_.res(), K.glaB[d].res(ti))
                prev = b_


def gla_stream(K, l, d):
    P, cfg = K.P, K.cfg
    S, NCH, PT = cfg.S, cfg.NCH, cfg.PT
    tg = "gl%d" % d
    st = P.sb(tg + "_st", [128, 256], F32)
    stb = P.sb(tg + "_stb", [128, 256], BF16)
    P.op("dve", lambda e: e.memset(st[:], 0.0), (), st.res())
    P.op("dve", lambda e: e.memset(stb[:], 0.0), (), stb.res())
    nb = 2
    bs = [P.sb(tg + "_bs%d" % i, [128, 129], F32) for i in range(nb)]
    qk = [P.sb(tg + "_qk%d" % i, [128, 2, 128], F32) for i in range(nb)]
    v = [P.sb(tg + "_v%d" % i, [128, 256], BF16) for i in range(nb)]
    E1 = [P.sb(tg + "_E1%d" % i, [128, 128], F32) for i in range(nb)]
    E2 = [P.sb(tg + "_E2%d" % i, [128, 128], F32) for i in range(nb)]
    qd = [P.sb(tg + "_qd%d" % i, [128, 128], BF16) for i in range(nb)]
    kd = [P.sb(tg + "_kd%d" % i, [128, 128], BF16) for i in range(nb)]
    ktm = [P.sb(tg + "_kt%d" % i, [128, 128], BF16) for i in range(nb)]
    stm = [P.sb(tg + "_sm%d" % i, [128, 128], BF16) for i in range(nb)]
    o = [P.sb(tg + "_o%d" % i, [128, 256], F32) for i in range(nb)]
    tS = P.sb(tg + "_tS", [128, 256], F32)
    mask = K.cst[:, (6 + d) * 128:(7 + d) * 128]
    order = range(NCH) if d == 0 else range(NCH - 1, -1, -1)
    n = 0
    for c in order:
        cs = c * 128
        i = n % nb
        n += 1
        P.dma(qk[i][:, 0, :], K.fm32[6 * 128:7 * 128, cs:cs + 128], K.fm32.res(6), qk[i].res())
        P.dma(qk[i][:, 1, :], K.fm32[7 * 128:8 * 128, cs:cs + 128], K.fm32.res(7), qk[i].res())
        P.dma(bs[i][:], K.glaB[d][:, cs:cs + 129], K.glaB[d].res(), bs[i].res())
        P.dma(v[i][:], K.tm["cv"][cs:cs + 128, :], K.tm["cv"].res(), v[i].res())
        if d == 0:
            bcur, bref, edge = bs[i][:, 1:129], bs[i][:, 0:1], 127
        else:
            bcur, bref, edge = bs[i][:, 0:128], bs[i][:, 128:129], 0
        P.op("act", lambda e, i=i, bcur=bcur, bref=bref: e.activation(E1[i][:], bcur, AF.Exp, bias=bref, scale=-1.0), bs[i].res(), E1[i].res())
        P.op("dve", lambda e, i=i: e.reciprocal(E2[i][:], E1[i][:]), E1[i].res(), E2[i].res())
        P.op("dve", lambda e, i=i: e.scalar_tensor_tensor(qd[i][:], qk[i][:, 0, :], C_DK ** -0.5, E1[i][:], ALU.mult, ALU.mult), qk[i].res() + E1[i].res(), qd[i].res())
        P.op("dve", lambda e, i=i: e.tensor_tensor(kd[i][:], qk[i][:, 1, :], E2[i][:], ALU.mult), qk[i].res() + E2[i].res(), kd[i].res())
        ps1 = psum(K)
        P.op("pe", lambda e, i=i, ps1=ps1: e.matmul(ps1[:, 0:128], kd[i][:], qd[i][:], start=True, stop=True), kd[i].res() + qd[i].res(), ps1.res())
        P.op("dve", lambda e, i=i, ps1=ps1: e.tensor_tensor(stm[i][:], ps1[:, 0:128], mask, ALU.mult), ps1.res() + K.cst.res(), stm[i].res())
        tr_bf(K, ktm[i][:], ktm[i].res(), kd[i][:], kd[i].res())
        ps2 = psum(K)
        P.op("pe", lambda e, i=i, ps2=ps2: e.matmul(ps2[:, 0:256], stm[i][:], v[i][:], start=True, stop=False), stm[i].res() + v[i].res(), ps2.res())
        P.op("pe", lambda e, i=i, ps2=ps2: e.matmul(ps2[:, 0:256], qd[i][:], stb[:], start=False, stop=True), qd[i].res() + stb.res(), ps2.res())
        P.op("act", lambda e, i=i, ps2=ps2: e.copy(o[i][:], ps2[:, 0:256]), ps2.res(), o[i].res())
        P.dma(K.H[d][cs:cs + 128, 512:768], o[i][:], o[i].res(), K.H[d].res(3))
        ps3 = psum(K)
        P.op("pe", lambda e, i=i, ps3=ps3: e.matmul(ps3[:, 0:256], ktm[i][:], v[i][:], start=True, stop=True), ktm[i].res() + v[i].res(), ps3.res())
        P.op("dve", lambda e, ps3=ps3: e.tensor_tensor(tS[:], st[:], ps3[:, 0:256], ALU.add), st.res() + ps3.res(), tS.res())
        P.op("dve", lambda e, i=i, edge=edge: e.tensor_scalar(st[:], tS[:], E1[i][:, edge:edge + 1], None, ALU.mult), tS.res() + E1[i].res(), st.res())
        P.op("act", lambda e: e.copy(stb[:], st[:]), st.res(), stb.res())
        yield


def mlstm_prep(K, l):
    P, cfg, spo = K.P, K.cfg, K.spo
    S, NCH = cfg.S, cfg.NCH
    RS = min(S, 1024)
    K.mlG = [P.dram("mlG%d_%d" % (d, l), [3, S], F32) for d in range(2)]
    with P.phase():
        nbias = P.sb("mp_nb", [1, 4], F32)
        P.op("dve", lambda e: e.tensor_scalar(nbias[:], K.spt[0:1, spo["agb"] + l * 4: spo["agb"] + l * 4 + 4], -1.0, None, ALU.mult), K.spt.res(), nbias.res())
        zr = P.sb("mp_z", [1, RS], F32)
        P.op("dve", lambda e: e.memset(zr[:], 0.0), (), zr.res())
        names = ["fr", "ir", "lf", "F", "m", "a", "u", "em"]
        tsets = [{nm: P.sb("mp_%s_%d" % (nm, i_), [1, RS], F32) for nm in names} for i_ in range(2)]
        for d in range(2):
            prevF = prevm = None
            segs = range(S // RS) if d == 0 else range(S // RS - 1, -1, -1)
            for si, sg in enumerate(segs):
                t = tsets[si % 2]
                g0 = sg * RS
                rb = 8 * 128
                P.dma(t["ir"][:], K.fm32[rb + d:rb + d + 1, g0:g0 + RS], K.fm32.res(8), t["ir"].res())
                P.dma(t["fr"][:], K.fm32[rb + 2 + d:rb + 3 + d, g0:g0 + RS], K.fm32.res(8), t["fr"].res())
                P.op("act", lambda e, t=t, d=d: e.activation(t["fr"][:], t["fr"][:], AF.Exp, bias=nbias[:, 2 + d:3 + d], scale=-1.0), t["fr"].res() + nbias.res(), t["fr"].res())
                P.op("act", lambda e, t=t: e.activation(t["fr"][:], t["fr"][:], AF.Ln, bias=K.cst[0:1, 128:129], scale=1.0), t["fr"].res() + K.cst.res(), t["fr"].res())
                P.op("dve", lambda e, t=t: e.tensor_scalar(t["lf"][:], t["fr"][:], -1.0, None, ALU.mult), t["fr"].res(), t["lf"].res())
                P.op("dve", lambda e, t=t, d=d: e.tensor_scalar(t["ir"][:], t["ir"][:], K.spt[0:1, spo["agb"] + l * 4 + d: spo["agb"] + l * 4 + d + 1], None, ALU.add), t["ir"].res() + K.spt.res(), t["ir"].res())
                if d == 0:
                    iF = 0.0 if prevF is None else prevF[:, RS - 1:RS]
                    im = 0.0 if prevm is None else prevm[:, RS - 1:RS]
                    vw = lambda ap: ap[:]
                else:
                    iF = 0.0 if prevF is None else prevF[:, 0:1]
                    im = 0.0 if prevm is None else prevm[:, 0:1]
                    vw = lambda ap: ap[:, ::-1]
                dep = (prevF.res() if prevF is not None else []) + (prevm.res() if prevm is not None else [])
                P.op("dve", lambda e, t=t, iF=iF, vw=vw: e.tensor_tensor_scan(vw(t["F"]), vw(t["lf"]), vw(zr), iF, ALU.add, ALU.add), t["lf"].res() + zr.res() + dep, t["F"].res())
                P.op("dve", lambda e, t=t, im=im, vw=vw: e.tensor_tensor_scan(vw(t["m"]), vw(t["lf"]), vw(t["ir"]), im, ALU.add, ALU.max), t["lf"].res() + t["ir"].res() + dep, t["m"].res())
                P.op("dve", lambda e, t=t: e.tensor_tensor(t["a"][:], t["F"][:], t["m"][:], ALU.subtract), t["F"].res() + t["m"].res(), t["a"].res())
                P.op("dve", lambda e, t=t: e.tensor_tensor(t["u"][:], t["ir"][:], t["F"][:], ALU.subtract), t["F"].res() + t["ir"].res(), t["u"].res())
                P.op("act", lambda e, t=t: e.activation(t["em"][:], t["m"][:], AF.Exp, scale=-1.0), t["m"].res(), t["em"].res())
                for ri, nm in enumerate(("a", "u", "em")):
                    P.dma(K.mlG[d][ri:ri + 1, g0:g0 + RS], t[nm][:], t[nm].res(), K.mlG[d].res())
                prevF, prevm = t["F"], t["m"]


def col_from_rows(K, dst, dram_row_ap, dram_res, nch, tmp):
    P = K.P
    P.dma(tmp[0:nch, :], dram_row_ap.rearrange("o (c t) -> (o c) t", t=128), dram_res, tmp.res())
    ps = psum(K)
    P.op("pe", lambda e: e.transpose(ps[:, 0:nch], tmp[0:nch, :], K.cst[0:nch, 0:nch]), tmp.res() + K.cst.res(), ps.res())
    P.op("act", lambda e: e.copy(dst, ps[:, 0:nch]), ps.res(), [])


def mlstm_stream(K, l, d):
    P, cfg = K.P, K.cfg
    S, NCH = cfg.S, cfg.NCH
    tg = "ml%d" % d
    st = P.sb(tg + "_st", [128, 257], F32)
    stb = P.sb(tg + "_stb", [128, 257], BF16)
    P.op("dve", lambda e: e.memset(st[:], 0.0), (), st.res())
    P.op("dve", lambda e: e.memset(stb[:], 0.0), (), stb.res())
    cols = P.sb(tg + "_cols", [128, 2, NCH], F32)
    cmt = P.sb(tg + "_cmt", [128, 128], F32)
    for ri in range(2):
        P.dma(cmt[0:NCH, :], K.mlG[d][1 + ri:2 + ri, :].rearrange("o (c t) -> (o c) t", t=128), K.mlG[d].res(), cmt.res())
        ps = psum(K)
        P.op("pe", lambda e, ps=ps: e.transpose(ps[:, 0:NCH], cmt[0:NCH, :], K.cst[0:NCH, 0:NCH]), cmt.res() + K.cst.res(), ps.res())
        P.op("act", lambda e, ps=ps, ri=ri: e.copy(cols[:, ri, :], ps[:, 0:NCH]), ps.res(), cols.res())
    nb = 2
    q = [P.sb(tg + "_q%d" % i, [128, 128], BF16) for i in range(nb)]
    k = [P.sb(tg + "_k%d" % i, [128, 128], BF16) for i in range(nb)]
    va = [P.sb(tg + "_va%d" % i, [128, 257], BF16) for i in range(nb)]
    ar = [P.sb(tg + "_ar%d" % i, [1, 128], F32) for i in range(nb)]
    W = [P.sb(tg + "_W%d" % i, [128, 128], F32) for i in range(nb)]
    Wi = [P.sb(tg + "_Wi%d" % i, [128, 128], F32) for i in range(nb)]
    Dm = [P.sb(tg + "_Dm%d" % i, [128, 128], BF16) for i in range(nb)]
    qd = [P.sb(tg + "_qd%d" % i, [128, 128], BF16) for i in range(nb)]
    kw = [P.sb(tg + "_kw%d" % i, [128, 128], BF16) for i in range(nb)]
    h = [P.sb(tg + "_h%d" % i, [128, 256], F32) for i in range(nb)]
    dn = [P.sb(tg + "_dn%d" % i, [128, 2], F32) for i in range(nb)]
    negab = [P.sb(tg + "_na%d" % i, [128, 1], F32) for i in range(2)]
    for i in range(nb):
        P.op("dve", lambda e, i=i: e.memset(va[i][:, 256:257], 1.0), (), va[i].res())
    P.op("dve", lambda e: e.memset(negab[0][:], 0.0), (), negab[0].res())
    ones_row = K.cst[0:1, 128:256]
    ident = K.cst[:, 0:128]
    negm = K.cst[:, (2 + 2 * d) * 128:(3 + 2 * d) * 128]
    edge = 127 if d == 0 else 0
    order = range(NCH) if d == 0 else range(NCH - 1, -1, -1)
    n = 0
    for c in order:
        cs = c * 128
        i = n % nb
        na_in, na_out = negab[n % 2], negab[(n + 1) % 2]
        n += 1
        P.dma(q[i][:], K.fm16[0:128, cs:cs + 128], K.fm16.res(0), q[i].res())
        P.dma(k[i][:], K.fm16[128:256, cs:cs + 128], K.fm16.res(1), k[i].res())
        P.dma(va[i][:, 0:256], K.tm["av"][cs:cs + 128, :], K.tm["av"].res(), va[i].res())
        P.dma(ar[i][:], K.mlG[d][0:1, cs:cs + 128], K.mlG[d].res(), ar[i].res())
        ps1 = psum(K)
        P.op("pe", lambda e, i=i, ps1=ps1: e.matmul(ps1[:, 0:128], ones_row, ar[i][:], start=True, stop=False), ar[i].res() + K.cst.res(), ps1.res())
        P.op("pe", lambda e, ps1=ps1: e.matmul(ps1[:, 0:128], ident, negm, start=False, stop=True), K.cst.res(), ps1.res())
        P.op("act", lambda e, i=i, ps1=ps1, c=c: e.activation(W[i][:], ps1[:, 0:128], AF.Exp, bias=cols[:, 0, c:c + 1]), ps1.res() + cols.res(), W[i].res())
        ps1b = psum(K)
        P.op("pe", lambda e, i=i, ps1b=ps1b: e.matmul(ps1b[:, 0:128], ones_row, ar[i][:], start=True, stop=True), ar[i].res() + K.cst.res(), ps1b.res())
        P.op("act", lambda e, i=i, ps1b=ps1b, na_in=na_in: e.activation(Wi[i][:], ps1b[:, 0:128], AF.Exp, bias=na_in[:, 0:1]), ps1b.res() + na_in.res(), Wi[i].res())
        P.op("dve", lambda e, ps1b=ps1b, na_out=na_out: e.tensor_scalar(na_out[:], ps1b[:, edge:edge + 1], -1.0, None, ALU.mult), ps1b.res(), na_out.res())
        ps2 = psum(K)
        P.op("pe", lambda e, i=i, ps2=ps2: e.matmul(ps2[:, 0:128], k[i][:], q[i][:], start=True, stop=True), k[i].res() + q[i].res(), ps2.res())
        P.op("dve", lambda e, i=i, ps2=ps2: e.tensor_tensor(Dm[i][:], ps2[:, 0:128], W[i][:], ALU.mult), ps2.res() + W[i].res(), Dm[i].res())
        P.op("dve", lambda e, i=i: e.tensor_tensor(qd[i][:], q[i][:], Wi[i][:], ALU.mult), q[i].res() + Wi[i].res(), qd[i].res())
        tr_bf(K, kw[i][:], kw[i].res(), k[i][:], k[i].res(), scale_ap=(W[i][:, edge:edge + 1], W[i].res()))
        ps3 = psum(K)
        P.op("pe", lambda e, i=i, ps3=ps3: e.matmul(ps3[:, 0:257], Dm[i][:], va[i][:], start=True, stop=False), Dm[i].res() + va[i].res(), ps3.res())
        P.op("pe", lambda e, i=i, ps3=ps3: e.matmul(ps3[:, 0:257], qd[i][:], stb[:], start=False, stop=True), qd[i].res() + stb.res(), ps3.res())
        P.op("act", lambda e, i=i, ps3=ps3: e.activation(dn[i][:, 0:1], ps3[:, 256:257], AF.Abs), ps3.res(), dn[i].res())
        P.op("dve", lambda e, i=i, c=c: e.tensor_tensor(dn[i][:, 0:1], dn[i][:, 0:1], cols[:, 1, c:c + 1], ALU.max), dn[i].res() + cols.res(), dn[i].res())
        P.op("dve", lambda e, i=i: e.reciprocal(dn[i][:, 1:2], dn[i][:, 0:1]), dn[i].res(), dn[i].res())
        P.op("act", lambda e, i=i, ps3=ps3: e.activation(h[i][:], ps3[:, 0:256], AF.Copy, scale=dn[i][:, 1:2]), ps3.res() + dn[i].res(), h[i].res())
        P.dma(K.H[d][cs:cs + 128, 0:256], h[i][:], h[i].res(), K.H[d].res(0))
        ps4 = psum(K)
        P.op("pe", lambda e, i=i, ps4=ps4: e.matmul(ps4[:, 0:257], kw[i][:], va[i][:], start=True, stop=True), kw[i].res() + va[i].res(), ps4.res())
        P.op("dve", lambda e, i=i, ps4=ps4: e.scalar_tensor_tensor(st[:], st[:], Wi[i][:, edge:edge + 1], ps4[:, 0:257], ALU.mult, ALU.add), st.res() + Wi[i].res() + ps4.res(), st.res())
        P.op("act", lambda e: e.copy(stb[:], st[:]), st.res(), stb.res())
        yield


def b_post(K, l):
    P, cfg, spo = K.P, K.cfg, K.spo
    S, NCH, YP = cfg.S, cfg.NCH, cfg.YP
    ypv = K.yp.h.ap().rearrange("(q c p) t -> q p c t", c=6, p=128)
    segs = [(0, 256, 0), (256, 128, 1), (384, 128, 2), (512, 256, 3)]
    with P.phase():
        make_eps(K)
        nb = 2
        hf = [P.sb("po_hf%d" % i, [128, 768], F32) for i in range(nb)]
        hb = [P.sb("po_hb%d" % i, [128, 768], F32) for i in range(nb)]
        gt = [P.sb("po_g%d" % i, [128, 768], F32) for i in range(nb)]
        junk = P.sb("po_junk", [128, 768], F32)
        ss = [P.sb("po_ss%d" % i, [128, 4], F32) for i in range(nb)]
        t1 = [P.sb("po_t1%d" % i, [128, 768], F32) for i in range(nb)]
        yb = [P.sb("po_y%d" % i, [128, 768], F32) for i in range(nb)]
        yt = [P.sb("po_yt%d" % i, [128, 6, 128], BF16) for i in range(nb)]
        ng = P.sb("po_ng", [128, 768], F32)
        P.op("dve", lambda e: e.tensor_copy(ng[:, 0:256], K.spt[:, spo["ang"] + l * 256: spo["ang"] + (l + 1) * 256]), K.spt.res(), ng.res())
        P.op("dve", lambda e: e.tensor_copy(ng[:, 256:512], K.spt[:, spo["bng"] + l * 256: spo["bng"] + (l + 1) * 256]), K.spt.res(), ng.res())
        P.op("dve", lambda e: e.tensor_copy(ng[:, 512:768], K.spt[:, spo["cng"] + l * 256: spo["cng"] + (l + 1) * 256]), K.spt.res(), ng.res())
        for c in range(NCH):
            cs = c * 128
            i = c % nb
            P.dma(hf[i][:], K.H[0][cs:cs + 128, :], K.H[0].res(), hf[i].res())
            P.dma(hb[i][:], K.H[1][cs:cs + 128, :], K.H[1].res(), hb[i].res())
            P.dma(gt[i][:, 0:256], K.tm["ao"][cs:cs + 128, :], K.tm["ao"].res(), gt[i].res())
            P.dma(gt[i][:, 256:512], K.tm["bz"][cs:cs + 128, :], K.tm["bz"].res(), gt[i].res())
            P.dma(gt[i][:, 512:768], K.tm["cr"][cs:cs + 128, :], K.tm["cr"].res(), gt[i].res())
            P.op("dve", lambda e, i=i: e.tensor_tensor(hf[i][:], hf[i][:], hb[i][:], ALU.add), hf[i].res() + hb[i].res(), hf[i].res())
            if K.post_lvl < 2:
                continue
            for si, (c0, w, _) in enumerate(segs):
                P.op("act", lambda e, i=i, c0=c0, w=w: e.activation(junk[:, c0:c0 + w], hf[i][:, c0:c0 + w], AF.Square), hf[i].res(), junk.res())
                P.op("dve", lambda e, i=i, c0=c0, w=w, si=si: e.reduce_sum(ss[i][:, si:si + 1], junk[:, c0:c0 + w], mybir.AxisListType.X), junk.res(), ss[i].res())
            for si, (c0, w, _) in enumerate(segs):
                P.op("act", lambda e, i=i, si=si, w=w: e.activation(ss[i][:, si:si + 1], ss[i][:, si:si + 1], AF.Sqrt, bias=K.epst[:, 0:1], scale=1.0 / w),
                     ss[i].res() + K.epst.res(), ss[i].res())
            P.op("dve", lambda e, i=i: e.reciprocal(ss[i][:], ss[i][:]), ss[i].res(), ss[i].res())
            for si, (c0, w, _) in enumerate(segs):
                P.op("dve", lambda e, i=i, c0=c0, w=w, si=si: e.scalar_tensor_tensor(t1[i][:, c0:c0 + w], hf[i][:, c0:c0 + w], ss[i][:, si:si + 1], ng[:, c0:c0 + w], ALU.mult, ALU.mult),
                     hf[i].res() + ss[i].res() + ng.res(), t1[i].res())
            if K.post_lvl < 3:
                continue
            P.op("act", lambda e, i=i: e.activation(gt[i][:, 0:256], gt[i][:, 0:256], AF.Sigmoid), gt[i].res(), gt[i].res())
            P.op("act", lambda e, i=i: e.activation(gt[i][:, 256:768], gt[i][:, 256:768], AF.Silu), gt[i].res(), gt[i].res())
            P.op("dve", lambda e, i=i: e.tensor_tensor(yb[i][:], t1[i][:], gt[i][:], ALU.mult), t1[i].res() + gt[i].res(), yb[i].res())
            if K.post_lvl < 4:
                continue
            for g in range(2):
                ps = psum(K)
                for k_ in range(3):
                    ct = g * 3 + k_
                    P.op("pe", lambda e, ps=ps, k_=k_, ct=ct, i=i: e.transpose(ps[:, k_ * 128:(k_ + 1) * 128], yb[i][:, ct * 128:(ct + 1) * 128], K.cst[:, 0:128]),
                         yb[i].res() + K.cst.res(), ps.res())
                if g == 0:
                    P.op("act", lambda e, ps=ps, i=i, g=g: e.copy(yt[i][:, g * 3:(g + 1) * 3, :], ps[:, 0:384]), ps.res(), yt[i].res())
                else:
                    P.op("dve", lambda e, ps=ps, i=i, g=g: e.tensor_copy(yt[i][:, g * 3:(g + 1) * 3, :], ps[:, 0:384]), ps.res(), yt[i].res())
            tb, off = cs // YP, cs % YP
            if "post_nodma" not in K.debug:
                P.dma(ypv[tb][:, :, off:off + 128], yt[i][:], yt[i].res(), K.yp.res(tb))


def b_mixers(K, l):
    P = K.P
    which = K.which
    if "c" in which:
        gla_prep(K, l)
    if "a" in which:
        mlstm_prep(K, l)
    if "b" in which:
        gdn_prep(K, l)
    with P.phase():
        streams = []
        if "c" in which:
            streams += [gla_stream(K, l, 0), gla_stream(K, l, 1)]
        if "a" in which:
            streams += [mlstm_stream(K, l, 0), mlstm_stream(K, l, 1)]
        if "b" in which:
            streams += [gdn_stream(K, l, hh, d) for hh in range(2) for d in range(2)]
        live = list(streams)
        while live:
            nxt = []
            for g in live:
                try:
                    next(g)
                    nxt.append(g)
                except StopIteration:
                    pass
            live = nxt
    if K.post:
        b_post(K, l)


def gdn_prep(K, l):
    P, cfg, spo = K.P, K.cfg, K.spo
    S, NCH, PT = cfg.S, cfg.NCH, cfg.PT
    if not hasattr(K, "gq"):
        K.gq = [P.dram("gdn_q%d" % h, [128, S], BF16) for h in range(2)]
        K.gk = [P.dram("gdn_k%d" % h, [128, S], BF16) for h in range(2)]
        K.gktm = [P.dram("gdn_ktm%d" % h, [S, 128], BF16) for h in range(2)]
        K.gvtm = [P.dram("gdn_vtm%d" % h, [S, 128], F32) for h in range(2)]
        K.grow = [[P.dram("gdn_row%d%d" % (h, d), [2, S], F32) for d in range(2)] for h in range(2)]
        K.gcol = [[P.dram("gdn_col%d%d" % (h, d), [128, 5 * NCH], F32) for d in range(2)] for h in range(2)]
        mk = lambda nm: [[P.dram("gdn_%s%d%d" % (nm, h, d), [S, 128], BF16, nparts=NCH) for d in range(2)] for h in range(2)]
        K.gTT, K.gAQ, K.gQE, K.gKD = mk("tt"), mk("aq"), mk("qe"), mk("kd")
    ones = K.cst[:, 128:256]
    with P.phase():
        make_eps(K)
        xh = [P.sb("g1_xh%d" % i, [128, PT + 4], F32) for i in range(2)]
        acc = [P.sb("g1_acc%d" % i, [128, PT], F32) for i in range(2)]
        sq = [P.sb("g1_sq%d" % i, [128, PT], F32) for i in range(2)]
        rn = [P.sb("g1_rn%d" % i, [128, PT], F32) for i in range(2)]
        ob = [P.sb("g1_ob%d" % i, [128, PT], BF16) for i in range(2)]
        tmo = [P.sb("g1_tm%d" % i, [128, 128], BF16) for i in range(2)]
        tvo = [P.sb("g1_tv%d" % i, [128, 128], F32) for i in range(2)]
        n = 0
        for ti in range(S // PT):
            g0 = ti * PT
            lo, hi = max(0, g0 - 2), min(S, g0 + PT + 2)
            for hh in range(2):
                for part in range(3):
                    blk = part * 2 + hh
                    x_, a_, s_, r_, o_ = xh[n % 2], acc[n % 2], sq[n % 2], rn[n % 2], ob[n % 2]
                    n += 1
                    if g0 == 0:
                        P.op("dve", lambda e, x_=x_: e.memset(x_[:, 0:2], 0.0), (), x_.res())
                    if g0 + PT == S:
                        P.op("dve", lambda e, x_=x_: e.memset(x_[:, PT + 2:PT + 4], 0.0), (), x_.res())
                    P.dma(x_[:, lo - (g0 - 2): hi - (g0 - 2)], K.fm32[blk * 128:(blk + 1) * 128, lo:hi], K.fm32.res(blk), x_.res())
                    cw = spo["convw"] + (l * 6 + blk) * 5
                    P.op("dve", lambda e, x_=x_, a_=a_, cw=cw: e.tensor_scalar(a_[:], x_[:, 0:PT], K.spt[:, cw:cw + 1], None, ALU.mult), x_.res() + K.spt.res(), a_.res())
                    for tap in range(1, 5):
                        P.op("dve", lambda e, x_=x_, a_=a_, cw=cw, tap=tap: e.scalar_tensor_tensor(a_[:], x_[:, tap:tap + PT], K.spt[:, cw + tap:cw + tap + 1], a_[:], ALU.mult, ALU.add),
                             x_.res() + K.spt.res() + a_.res(), a_.res())
                    cb = spo["convb"] + l * 6 + blk
                    P.op("act", lambda e, a_=a_, cb=cb: e.activation(a_[:], a_[:], AF.Silu, bias=K.spt[:, cb:cb + 1]), a_.res() + K.spt.res(), a_.res())
                    if part < 2:
                        P.op("act", lambda e, a_=a_, s_=s_: e.activation(s_[:], a_[:], AF.Square), a_.res(), s_.res())
                        ps = psum(K)
                        P.op("pe", lambda e, ps=ps, s_=s_: e.matmul(ps[:, 0:PT], ones, s_[:], start=True, stop=True), s_.res() + K.cst.res(), ps.res())
                        P.op("act", lambda e, ps=ps, r_=r_: e.activation(r_[:], ps[:, 0:PT], AF.Sqrt, bias=K.epst[:, 0:1], scale=1.0), ps.res() + K.epst.res(), r_.res())
                        P.op("dve", lambda e, r_=r_: e.reciprocal(r_[:], r_[:]), r_.res(), r_.res())
                        sc = B_DK ** -0.5 if part == 0 else 1.0
                        P.op("dve", lambda e, a_=a_, r_=r_, o_=o_, sc=sc: e.scalar_tensor_tensor(o_[:], a_[:], sc, r_[:], ALU.mult, ALU.mult), a_.res() + r_.res(), o_.res())
                        dst = (K.gq if part == 0 else K.gk)[hh]
                        P.dma(dst[:, g0:g0 + PT], o_[:], o_.res(), dst.res())
                        if part == 1:
                            for sub in range(PT // 128):
                                t_ = tmo[sub % 2]
                                tr_bf(K, t_[:], t_.res(), o_[:, sub * 128:(sub + 1) * 128], o_.res())
                                P.dma(K.gktm[hh][g0 + sub * 128: g0 + (sub + 1) * 128, :], t_[:], t_.res(), K.gktm[hh].res())
                    else:
                        for sub in range(PT // 128):
                            t_ = tvo[sub % 2]
                            ps = psum(K)
                            P.op("pe", lambda e, ps=ps, a_=a_, sub=sub: e.transpose(ps[:, 0:128], a_[:, sub * 128:(sub + 1) * 128], K.cst[:, 0:128]), a_.res() + K.cst.res(), ps.res())
                            P.op("act", lambda e, ps=ps, t_=t_: e.copy(t_[:], ps[:, 0:128]), ps.res(), t_.res())
                            P.dma(K.gvtm[hh][g0 + sub * 128: g0 + (sub + 1) * 128, :], t_[:], t_.res(), K.gvtm[hh].res())
    with P.phase():
        for hh in range(2):
            for d in range(2):
                tg = "g2_%d%d" % (hh, d)
                xg = P.sb(tg + "xg", [NCH, 128], F32)
                xb = P.sb(tg + "xb", [NCH, 128], F32)
                gp = P.sb(tg + "gp", [NCH, 128], F32)
                m5 = P.sb(tg + "m5", [NCH, 5, 128], F32)
                row = P.sb(tg + "row", [NCH, 2, 128], F32)
                sc_ = P.sb(tg + "sc", [128, 4], F32)
                colt = P.sb(tg + "col", [128, 5, NCH], F32)
                rb = 8 * 128 + 4
                P.dma(xg[:], K.fm32[rb + d * 2 + hh: rb + d * 2 + hh + 1, :].rearrange("o (c t) -> (o c) t", t=128), K.fm32.res(8), xg.res())
                P.dma(xb[:], K.fm32[rb + 4 + d * 2 + hh: rb + 4 + d * 2 + hh + 1, :].rearrange("o (c t) -> (o c) t", t=128), K.fm32.res(8), xb.res())
                ca = spo["gdn_alog"] + l * 4 + d * 2 + hh
                cd = spo["gdn_dtb"] + l * 4 + d * 2 + hh
                P.op("act", lambda e, sc_=sc_, ca=ca: e.activation(sc_[:, 0:1], K.spt[:, ca:ca + 1], AF.Exp), K.spt.res(), sc_.res())
                P.op("act", lambda e, xg=xg, cd=cd: e.activation(xg[:], xg[:], AF.Exp, bias=K.spt[0:NCH, cd:cd + 1]), xg.res() + K.spt.res(), xg.res())
                P.op("act", lambda e, xg=xg: e.activation(xg[:], xg[:], AF.Ln, bias=K.cst[0:NCH, 128:129]), xg.res() + K.cst.res(), xg.res())
                P.op("dve", lambda e, xg=xg, sc_=sc_: e.tensor_scalar(xg[:], xg[:], sc_[0:NCH, 0:1], None, ALU.mult), xg.res() + sc_.res(), xg.res())
                vw = (lambda ap: ap) if d == 0 else (lambda ap: ap[:, ::-1])
                P.op("dve", lambda e, xg=xg, gp=gp, vw=vw: e.tensor_tensor_scan(vw(gp[:, :]), vw(xg[:, :]), vw(xg[:, :]), 0.0, ALU.add, ALU.bypass), xg.res(), gp.res())
                P.op("act", lambda e, xb=xb: e.activation(xb[:], xb[:], AF.Exp, scale=-1.0), xb.res(), xb.res())
                P.op("act", lambda e, xb=xb: e.activation(xb[:], xb[:], AF.Ln, bias=K.cst[0:NCH, 128:129]), xb.res() + K.cst.res(), xb.res())
                P.op("dve", lambda e, gp=gp, row=row: e.tensor_scalar(row[:, 0, :], gp[:], -1.0, None, ALU.mult), gp.res(), row.res())
                P.op("dve", lambda e, gp=gp, xb=xb, row=row: e.scalar_tensor_tensor(row[:, 1, :], gp[:], -1.0, xb[:], ALU.mult, ALU.subtract), gp.res() + xb.res(), row.res())
                for r_ in range(2):
                    P.dma(K.grow[hh][d][r_:r_ + 1, :].rearrange("o (c t) -> (o c) t", t=128), row[:, r_, :], row.res(), K.grow[hh][d].res())
                edge = 127 if d == 0 else 0
                P.op("dve", lambda e, gp=gp, m5=m5: e.tensor_copy(m5[:, 0, :], gp[:]), gp.res(), m5.res())
                P.op("act", lambda e, gp=gp, m5=m5: e.activation(m5[:, 1, :], gp[:], AF.Exp, scale=-1.0), gp.res(), m5.res())
                P.op("dve", lambda e, m5=m5: e.tensor_scalar(m5[:, 1, :], m5[:, 1, :], -1.0, None, ALU.mult), m5.res(), m5.res())
                P.op("act", lambda e, xb=xb, m5=m5: e.activation(m5[:, 2, :], xb[:], AF.Exp, scale=-1.0), xb.res(), m5.res())
                P.op("dve", lambda e, gp=gp, sc_=sc_, edge=edge: e.tensor_scalar(sc_[0:NCH, 1:2], gp[:, edge:edge + 1], -1.0, None, ALU.mult), gp.res(), sc_.res())
                P.op("act", lambda e, gp=gp, m5=m5, sc_=sc_: e.activation(m5[:, 3, :], gp[:], AF.Exp, bias=sc_[0:NCH, 1:2]), gp.res() + sc_.res(), m5.res())
                P.op("act", lambda e, gp=gp, m5=m5, edge=edge: e.activation(m5[:, 4, :], gp[:, edge:edge + 1].to_broadcast([NCH, 128]), AF.Exp, scale=-1.0), gp.res(), m5.res())
                for q_ in range(5):
                    ps = psum(K)
                    P.op("pe", lambda e, ps=ps, m5=m5, q_=q_: e.transpose(ps[:, 0:NCH], m5[:, q_, :], K.cst[0:NCH, 0:NCH]), m5.res() + K.cst.res(), ps.res())
                    P.op("act", lambda e, ps=ps, colt=colt, q_=q_: e.copy(colt[:, q_, :], ps[:, 0:NCH]), ps.res(), colt.res())
                P.dma(K.gcol[hh][d][:, :], colt[:].rearrange("p a c -> p (a c)"), colt.res(), K.gcol[hh][d].res())
    with P.phase():
        streams = [gdn_solve_stream(K, l, hh, d) for hh in range(2) for d in range(2)]
        live = list(streams)
        while live:
            nxt = []
            for g in live:
                try:
                    next(g)
                    nxt.append(g)
                except StopIteration:
                    pass
            live = nxt


def gdn_solve_stream(K, l, hh, d):
    P, cfg = K.P, K.cfg
    S, NCH = cfg.S, cfg.NCH
    tg = "gs%d%d" % (hh, d)
    colt = P.sb(tg + "col", [128, 5, NCH], F32)
    P.dma(colt[:].rearrange("p a c -> p (a c)"), K.gcol[hh][d][:, :], K.gcol[hh][d].res(), colt.res())
    nb = 2
    kT = [P.sb(tg + "kT%d" % i, [128, 128], BF16) for i in range(nb)]
    qT = [P.sb(tg + "qT%d" % i, [128, 128], BF16) for i in range(nb)]
    ktm = [P.sb(tg + "ktm%d" % i, [128, 128], BF16) for i in range(nb)]
    rows = [P.sb(tg + "rw%d" % i, [1, 2, 128], F32) for i in range(nb)]
    EA = P.sb(tg + "EA", [128, 128], F32)
    EQ = P.sb(tg + "EQ", [128, 128], F32)
    EG = P.sb(tg + "EG", [128, 128], F32)
    Nm = P.sb(tg + "N", [128, 128], F32)
    Pm = [P.sb(tg + "P%d" % i, [128, 128], F32) for i in range(2)]
    Qm = [P.sb(tg + "Q%d" % i, [128, 128], F32) for i in range(2)]
    Xm = [P.sb(tg + "X%d" % i, [128, 128], F32) for i in range(2)]
    obuf = [P.sb(tg + "o%d" % i, [128, 4, 128], BF16) for i in range(nb)]
    ones_row = K.cst[0:1, 128:256]
    ident = K.cst[:, 0:128]
    neg_incl = K.cst[:, (2 + 2 * d) * 128:(3 + 2 * d) * 128]
    neg_strict = K.cst[:, (3 + 2 * d) * 128:(4 + 2 * d) * 128]
    n = 0
    for c in range(NCH):
        cs = c * 128
        i = n % nb
        n += 1
        P.dma(kT[i][:], K.gk[hh][:, cs:cs + 128], K.gk[hh].res(), kT[i].res())
        P.dma(qT[i][:], K.gq[hh][:, cs:cs + 128], K.gq[hh].res(), qT[i].res())
        P.dma(ktm[i][:], K.gktm[hh][cs:cs + 128, :], K.gktm[hh].res(), ktm[i].res())
        P.dma(rows[i][:], K.grow[hh][d][:, cs:cs + 128].rearrange("(o r) t -> o r t", o=1), K.grow[hh][d].res(), rows[i].res())
        gcol = colt[:, 0, c:c + 1]
        psA = psum(K)
        P.op("pe", lambda e, i=i, psA=psA: e.matmul(psA[:, 0:128], ones_row, rows[i][:, 1, :], start=True, stop=False), rows[i].res() + K.cst.res(), psA.res())
        P.op("pe", lambda e, psA=psA: e.matmul(psA[:, 0:128], ident, neg_strict, start=False, stop=True), K.cst.res(), psA.res())
        P.op("act", lambda e, psA=psA, gcol=gcol: e.activation(EA[:], psA[:, 0:128], AF.Exp, bias=gcol), psA.res() + colt.res(), EA.res())
        psK = psum(K)
        P.op("pe", lambda e, i=i, psK=psK: e.matmul(psK[:, 0:128], kT[i][:], kT[i][:], start=True, stop=True), kT[i].res(), psK.res())
        P.op("dve", lambda e, psK=psK: e.tensor_tensor(Nm[:], psK[:, 0:128], EA[:], ALU.mult), psK.res() + EA.res(), Nm.res())
        psQ = psum(K)
        P.op("pe", lambda e, i=i, psQ=psQ: e.matmul(psQ[:, 0:128], ones_row, rows[i][:, 0, :], start=True, stop=False), rows[i].res() + K.cst.res(), psQ.res())
        P.op("pe", lambda e, psQ=psQ: e.matmul(psQ[:, 0:128], ident, neg_incl, start=False, stop=True), K.cst.res(), psQ.res())
        P.op("act", lambda e, psQ=psQ, gcol=gcol: e.activation(EQ[:], psQ[:, 0:128], AF.Exp, bias=gcol), psQ.res() + colt.res(), EQ.res())
        psKQ = psum(K)
        P.op("pe", lambda e, i=i, psKQ=psKQ: e.matmul(psKQ[:, 0:128], kT[i][:], qT[i][:], start=True, stop=True), kT[i].res() + qT[i].res(), psKQ.res())
        P.op("dve", lambda e, i=i, psKQ=psKQ: e.tensor_tensor(obuf[i][:, 1, :], psKQ[:, 0:128], EQ[:], ALU.mult), psKQ.res() + EQ.res(), obuf[i].res())
        psG = psum(K)
        P.op("pe", lambda e, i=i, psG=psG: e.matmul(psG[:, 0:128], ones_row, rows[i][:, 0, :], start=True, stop=True), rows[i].res() + K.cst.res(), psG.res())
        P.op("act", lambda e, psG=psG: e.activation(EG[:], psG[:, 0:128], AF.Exp), psG.res(), EG.res())
        P.op("dve", lambda e, i=i: e.tensor_tensor(obuf[i][:, 2, :], qT[i][:], EG[:], ALU.mult), qT[i].res() + EG.res(), obuf[i].res())
        P.op("dve", lambda e, i=i, c=c: e.tensor_scalar(obuf[i][:, 3, :], ktm[i][:], colt[:, 3, c:c + 1], None, ALU.mult), ktm[i].res() + colt.res(), obuf[i].res())
        ps = psum(K)
        P.op("pe", lambda e, ps=ps: e.transpose(ps[:, 0:128], Nm[:], ident), Nm.res() + K.cst.res(), ps.res())
        P.op("act", lambda e, ps=ps: e.copy(Qm[0][:], ps[:, 0:128]), ps.res(), Qm[0].res())
        P.op("dve", lambda e: e.tensor_tensor(Xm[0][:], ident, Nm[:], ALU.subtract), Nm.res() + K.cst.res(), Xm[0].res())
        Pc, Qc, Xc = Nm, Qm[0], Xm[0]
        for k in range(1, 7):
            Qn = Qm[k % 2]
            psq = psum(K)
            P.op("pe", lambda e, psq=psq, Pc=Pc, Qc=Qc: e.matmul(psq[:, 0:128], Pc[:], Qc[:], start=True, stop=True), Pc.res() + Qc.res(), psq.res())
            if k < 6:
                Pn = Pm[k % 2]
                psp = psum(K)
                P.op("pe", lambda e, psp=psp, Pc=Pc, Qc=Qc: e.matmul(psp[:, 0:128], Qc[:], Pc[:], start=True, stop=True), Pc.res() + Qc.res(), psp.res())
            P.op("act", lambda e, psq=psq, Qn=Qn: e.copy(Qn[:], psq[:, 0:128]), psq.res(), Qn.res())
            if k < 6:
                P.op("act", lambda e, psp=psp, Pn=Pn: e.copy(Pn[:], psp[:, 0:128]), psp.res(), Pn.res())
            Xn = Xm[k % 2]
            psx = psum(K)
            P.op("pe", lambda e, psx=psx, Qn=Qn, Xc=Xc: e.matmul(psx[:, 0:128], Qn[:], Xc[:], start=True, stop=True), Qn.res() + Xc.res(), psx.res())
            P.op("dve", lambda e, psx=psx, Xn=Xn, Xc=Xc: e.tensor_tensor(Xn[:], psx[:, 0:128], Xc[:], ALU.add), psx.res() + Xc.res(), Xn.res())
            Qc, Xc = Qn, Xn
            if k < 6:
                Pc = Pn
        P.op("dve", lambda e, i=i, Xc=Xc, c=c: e.tensor_scalar(obuf[i][:, 0, :], Xc[:], colt[:, 2, c:c + 1], None, ALU.mult), Xc.res() + colt.res(), obuf[i].res())
        for q_, dst in enumerate((K.gTT, K.gAQ, K.gQE, K.gKD)):
            P.dma(dst[hh][d][cs:cs + 128, :], obuf[i][:, q_, :], obuf[i].res(), dst[hh][d].res(c))
        yield


def gdn_stream(K, l, hh, d):
    P, cfg = K.P, K.cfg
    S, NCH = cfg.S, cfg.NCH
    tg = "gr%d%d" % (hh, d)
    colt = P.sb(tg + "col", [128, 5, NCH], F32)
    P.dma(colt[:].rearrange("p a c -> p (a c)"), K.gcol[hh][d][:, :], K.gcol[hh][d].res(), colt.res())
    st = P.sb(tg + "st", [128, 128], F32)
    stb = P.sb(tg + "stb", [128, 128], BF16)
    P.op("dve", lambda e: e.memset(st[:], 0.0), (), st.res())
    P.op("dve", lambda e: e.memset(stb[:], 0.0), (), stb.res())
    nb = 2
    kT = [P.sb(tg + "kT%d" % i, [128, 128], BF16) for i in range(nb)]
    mats = [P.sb(tg + "m%d" % i, [128, 4, 128], BF16) for i in range(nb)]
    v = [P.sb(tg + "v%d" % i, [128, 128], F32) for i in range(nb)]
    Rb = [P.sb(tg + "R%d" % i, [128, 128], BF16) for i in range(nb)]
    Ub = [P.sb(tg + "U%d" % i, [128, 128], BF16) for i in range(nb)]
    o = [P.sb(tg + "o%d" % i, [128, 128], F32) for i in range(nb)]
    order = range(NCH) if d == 0 else range(NCH - 1, -1, -1)
    hc0 = 256 + hh * 128
    n = 0
    for c in order:
        cs = c * 128
        i = n % nb
        n += 1
        P.dma(kT[i][:], K.gk[hh][:, cs:cs + 128], K.gk[hh].res(), kT[i].res())
        for q_, src in enumerate((K.gTT, K.gAQ, K.gQE, K.gKD)):
            P.dma(mats[i][:, q_, :], src[hh][d][cs:cs + 128, :], src[hh][d].res(c), mats[i].res())
        P.dma(v[i][:], K.gvtm[hh][cs:cs + 128, :], K.gvtm[hh].res(), v[i].res())
        ps1 = psum(K)
        P.op("pe", lambda e, i=i, ps1=ps1: e.matmul(ps1[:, 0:128], kT[i][:], stb[:], start=True, stop=True), kT[i].res() + stb.res(), ps1.res())
        P.op("dve", lambda e, i=i, ps1=ps1, c=c: e.scalar_tensor_tensor(Rb[i][:], ps1[:, 0:128], colt[:, 1, c:c + 1], v[i][:], ALU.mult, ALU.add), ps1.res() + colt.res() + v[i].res(), Rb[i].res())
        ps2 = psum(K)
        P.op("pe", lambda e, i=i, ps2=ps2: e.matmul(ps2[:, 0:128], mats[i][:, 0, :], Rb[i][:], start=True, stop=True), mats[i].res() + Rb[i].res(), ps2.res())
        P.op("act", lambda e, i=i, ps2=ps2: e.copy(Ub[i][:], ps2[:, 0:128]), ps2.res(), Ub[i].res())
        ps3 = psum(K)
        P.op("pe", lambda e, i=i, ps3=ps3: e.matmul(ps3[:, 0:128], mats[i][:, 1, :], Ub[i][:], start=True, stop=False), mats[i].res() + Ub[i].res(), ps3.res())
        P.op("pe", lambda e, i=i, ps3=ps3: e.matmul(ps3[:, 0:128], mats[i][:, 2, :], stb[:], start=False, stop=True), mats[i].res() + stb.res(), ps3.res())
        P.op("act", lambda e, i=i, ps3=ps3: e.copy(o[i][:], ps3[:, 0:128]), ps3.res(), o[i].res())
        P.dma(K.H[d][cs:cs + 128, hc0:hc0 + 128], o[i][:], o[i].res(), K.H[d].res(1 + hh))
        ps4 = psum(K)
        P.op("pe", lambda e, i=i, ps4=ps4: e.matmul(ps4[:, 0:128], mats[i][:, 3, :], Ub[i][:], start=True, stop=True), mats[i].res() + Ub[i].res(), ps4.res())
        P.op("dve", lambda e, ps4=ps4, c=c: e.scalar_tensor_tensor(st[:], st[:], colt[:, 4, c:c + 1], ps4[:, 0:128], ALU.mult, ALU.add), st.res() + colt.res() + ps4.res(), st.res())
        P.op("act", lambda e: e.copy(stb[:], st[:]), st.res(), stb.res())
        yield
```

```python
import numpy as np
from contextlib import ExitStack
import concourse.bass as bass
import concourse.mybir as mybir
from concourse.bass_utils import run_bass_kernel_spmd

F32 = mybir.dt.float32
BF16 = mybir.dt.bfloat16
ALU = mybir.AluOpType
AF = mybir.ActivationFunctionType

ENGS = ("pe", "dve", "act", "pool", "sp")
NDSEM = 6
NEG = -30000.0
EPS = 1e-6
NCORES = 8


class Res:
    __slots__ = ("w", "r")

    def __init__(self):
        self.w = None
        self.r = {}


class T:
    def __init__(self, h, nparts=1):
        self.h = h
        self.parts = [Res() for _ in range(nparts)]

    def __getitem__(self, idx):
        return self.h[idx]

    def ap(self):
        return self.h.ap() if hasattr(self.h, "ap") else self.h[:]

    def res(self, i=None):
        if i is None:
            return self.parts
        if isinstance(i, (list, tuple, range)):
            return [self.parts[j] for j in i]
        return [self.parts[i]]


class Prog:
    def __init__(self, nc, es):
        self.nc = nc
        self.es0 = es
        self.es = es
        self.ops = {e: [] for e in ENGS}
        self.cnt = {e: 0 for e in ENGS}
        self.sem = {e: es.enter_context(nc.semaphore("s_" + e)) for e in ENGS}
        self.dsem = {}
        self.dcnt = {}
        self.drr = {}
        for q in ("sp", "pool", "cc", "act"):
            self.dsem[q] = [es.enter_context(nc.semaphore("d_%s%d" % (q, i))) for i in range(NDSEM)]
            self.dcnt[q] = [0] * NDSEM
            self.drr[q] = 0
        self.seen = {e: {} for e in ENGS}
        self.psum_rr = 0
        self.nins = 0

    def sb(self, name, shape, dtype, nparts=1):
        self.nins += 0
        self._uid = getattr(self, "_uid", 0) + 1
        return T(self.es.enter_context(self.nc.sbuf_tensor("%s_u%d" % (name, self._uid), list(shape), dtype)), nparts)

    def dram(self, name, shape, dtype, nparts=1):
        return T(self.nc.dram_tensor(name, list(shape), dtype, kind="Internal"), nparts)

    def _semh(self, key):
        if isinstance(key, str):
            return self.sem[key]
        return self.dsem[key[0]][key[1]]

    def _deps(self, eng, reads, writes):
        need = {}
        for r in reads:
            if r.w is not None and need.get(r.w[0], 0) < r.w[1]:
                need[r.w[0]] = r.w[1]
        for w in writes:
            if w.w is not None and need.get(w.w[0], 0) < w.w[1]:
                need[w.w[0]] = w.w[1]
            for k, v in w.r.items():
                if need.get(k, 0) < v:
                    need[k] = v
        waits = []
        seen = self.seen[eng]
        for k, v in need.items():
            if seen.get(k, 0) < v:
                seen[k] = v
                waits.append((k, v))
        return waits

    def _mark(self, key, v, reads, writes):
        for r in reads:
            r.r[key] = v
        for w in writes:
            w.w = (key, v)
            w.r = {}

    def op(self, eng, fn, reads=(), writes=()):
        waits = self._deps(eng, reads, writes)
        if eng == "pe":
            waits = [(k, v) for (k, v) in waits if k != "pe"]
        self.cnt[eng] += 1
        v = self.cnt[eng]
        self.ops[eng].append((waits, fn, (eng, 1)))
        self._mark(eng, v, reads, writes)
        self.nins += 1

    def _async(self, q, cls, fn, reads, writes, inc):
        i = self.drr[cls]
        self.drr[cls] = (i + 1) % NDSEM
        key = (cls, i)
        waits = self._deps(q, reads, writes)
        prev = self.dcnt[cls][i]
        if prev and self.seen[q].get(key, 0) < prev:
            self.seen[q][key] = prev
            waits.append((key, prev))
        self.dcnt[cls][i] += inc
        v = self.dcnt[cls][i]
        self.ops[q].append((waits, fn, (key, inc)))
        self._mark(key, v, reads, writes)
        self.nins += 1

    def dma(self, out_ap, in_ap, reads=(), writes=(), q="sp", slow=False):
        cls = q if q in ("sp", "act") else "pool"
        if slow:
            self._async(q, cls, lambda e, o=out_ap, a=in_ap: e.dma_start(out=o, in_=a, allow_slow_non_contiguous=True), reads, writes, 16)
        else:
            self._async(q, cls, lambda e, o=out_ap, a=in_ap: e.dma_start(out=o, in_=a), reads, writes, 16)

    def coll(self, kind, out_ap, in_ap, groups, reads=(), writes=()):
        self._async("pool", "cc",
                    lambda e, o=out_ap, a=in_ap: e.collective_compute(kind, ALU.bypass, replica_groups=groups,
                                                                      ins=[a], outs=[o]),
                    reads, writes, 1)

    def barrier(self, full=False):
        tgt = {e: self.cnt[e] for e in ENGS if self.cnt[e]}
        for cls in (("sp", "act", "pool", "cc") if full else ("sp", "act")):
            for i in range(NDSEM):
                if self.dcnt[cls][i]:
                    tgt[(cls, i)] = self.dcnt[cls][i]
        for e in ENGS:
            waits = []
            for k, v in tgt.items():
                if self.seen[e].get(k, 0) < v:
                    self.seen[e][k] = v
                    waits.append((k, v))
            self.ops[e].append((waits, None, None))

    def flush(self):
        nc = self.nc
        ops = self.ops
        self.ops = {e: [] for e in ENGS}
        with nc.Block() as block:
            def run(e, h):
                for waits, fn, inc in ops[e]:
                    for k, v in waits:
                        h.wait_ge(self._semh(k), v)
                    if fn is not None:
                        fn(h).then_inc(self._semh(inc[0]), inc[1])

            block.tensor(lambda h: run("pe", h))
            block.vector(lambda h: run("dve", h))
            block.scalar(lambda h: run("act", h))
            block.gpsimd(lambda h: run("pool", h))
            block.sync(lambda h: run("sp", h))

    class _Phase:
        def __init__(self, P):
            self.P = P

        def __enter__(self):
            self.es = ExitStack()
            self.es.__enter__()
            self.P.es = self.es
            return self.P

        def __exit__(self, *a):
            self.P.barrier()
            self.P.flush()
            self.P.es = self.P.es0
            return self.es.__exit__(*a)

    def phase(self):
        return Prog._Phase(self)


A_HEADS, A_DK, A_DV = 4, 128, 256
B_HEADS, B_DK, B_DV = 8, 128, 128
C_HEADS, C_DK, C_DV = 4, 128, 256
GLA_RANK, GLA_TAU, GATE_RANK, CONV_K = 16, 16.0, 256, 5
A_QK, A_V = 512, 1024
B_QK, B_V, B_QKV = 1024, 1024, 3072
C_QK, C_V = 512, 1024
PROJ_SIZES = (A_QK, A_QK, A_V, A_V, 16, B_QKV, B_V, 32, C_QK, C_QK, C_V, C_V, 32, GATE_RANK)
OFF = np.concatenate([[0], np.cumsum(PROJ_SIZES)]).tolist()
(O_AQ, O_AK, O_AV, O_AO, O_AGT, O_BQKV, O_BZ, O_BGT, O_CQ, O_CK, O_CV, O_CR, O_CLR, O_GH) = OFF[:14]
NFM = 11
NTM = 5
NCOLS = NFM * 128 + NTM * 256 + 256


def my_cols(j):
    r = lambda a, n: list(range(a, a + n))
    cols = []
    cols += r(O_AQ + j * 128, 128) + r(O_AK + j * 128, 128)
    for part in range(3):
        for hh in (2 * j, 2 * j + 1):
            cols += r(O_BQKV + part * 1024 + hh * 128, 128)
    cols += r(O_CQ + j * 128, 128) + r(O_CK + j * 128, 128)
    small = [-1] * 128
    for g in range(4):
        small[g] = O_AGT + g * 4 + j
    k = 4
    for g in range(4):
        for hh in (2 * j, 2 * j + 1):
            small[k] = O_BGT + g * 8 + hh
            k += 1
    for i in range(16):
        small[32 + i] = O_CLR + i
        small[64 + i] = O_CLR + 16 + i
    cols += small
    cols += r(O_AV + j * 256, 256) + r(O_AO + j * 256, 256)
    cols += r(O_BZ + 2 * j * 128, 256)
    cols += r(O_CV + j * 256, 256) + r(O_CR + j * 256, 256)
    cols += r(O_GH, 256)
    assert len(cols) == NCOLS
    return cols


def block_widths():
    return [128] * NFM + [256] * NTM + [128, 128]


def tile_major(W):
    K, N = W.shape
    return np.ascontiguousarray(W.reshape(K // 128, 128, N // 128, 128).transpose(2, 1, 0, 3)).reshape(-1)


def piece_plan(E):
    P = max(1, -(-E // (8 * 131072)))
    while E % (8 * P) != 0:
        P += 1
    pe = E // (8 * P)
    b = 1
    for cand in (2048, 1024, 512, 256, 128, 64, 661, 1):
        if pe % cand == 0:
            b = cand
            break
    return P, pe, pe // b, b


class Cfg:
    def __init__(self, D, FF, S, L):
        self.D, self.FF, self.S, self.L = D, FF, S, L
        self.KT = D // 128
        self.FT = FF // 128
        self.TL = S // 4
        self.NCH = S // 128
        self.TT = min(256, self.TL)
        self.PT = 256
        self.PTP = min(512, self.TL)
        self.YP = min(512, S)
        self.big = [("wa", 1024, D), ("wb", 1024, D), ("wc", 1024, D), ("wg", 768, D),
                    ("wo", D, D), ("w1", D, FF), ("w2", FF, D)]


def sp_layout(cfg):
    L, KT = cfg.L, cfg.KT
    o = {}
    n = 0
    for name, w in [("g1", L * KT), ("g2", L * KT), ("gf", KT), ("bm", L * 3 * KT), ("convw", L * 6 * 5),
                    ("convb", L * 6), ("agb", L * 4), ("ang", L * 256), ("gdn_alog", L * 4), ("gdn_dtb", L * 4),
                    ("bng", L * 256), ("glaw", L * 2 * 128), ("glab", L * 2), ("cng", L * 256)]:
        o[name] = n
        n += w
    o["_n"] = n
    return o


def make_consts():
    c = np.zeros((128, 8, 128), np.float32)
    s = np.arange(128)[:, None]
    t = np.arange(128)[None, :]
    c[:, 0] = np.eye(128)
    c[:, 1] = 1.0
    c[:, 2] = np.where(s <= t, 0, NEG)
    c[:, 3] = np.where(s < t, 0, NEG)
    c[:, 4] = np.where(s >= t, 0, NEG)
    c[:, 5] = np.where(s > t, 0, NEG)
    c[:, 6] = (s <= t)
    c[:, 7] = (s >= t)
    return c.reshape(128, 1024)


def prep_inputs(inputs, cfg):
    D, FF, S, L, KT = cfg.D, cfg.FF, cfg.S, cfg.L, cfg.KT
    f = lambda k: np.asarray(inputs[k], dtype=np.float32)
    x = f("x")
    spo = sp_layout(cfg)
    bigsrc = {"wa": f("w_branch_a"), "wb": f("w_branch_b"), "wc": f("w_branch_c"),
              "wg": f("w_merge_gate").reshape(L, 768, D), "wo": f("w_out"), "w1": f("w_ff1"), "w2": f("w_ff2")}
    w_in = f("w_in")
    consts = make_consts()
    shards = {}
    for name, K_, N_ in cfg.big:
        P_, pe, a, b = piece_plan(K_ * N_)
        for l in range(L):
            if name == "wg":
                flat = np.concatenate([tile_major(bigsrc[name][l][jb * 256:(jb + 1) * 256]) for jb in range(3)])
            else:
                flat = tile_major(bigsrc[name][l])
            shards[(name, l)] = flat.reshape(P_, 8, pe)
    in_maps = []
    bw = block_widths()
    for c in range(NCORES):
        b_, j = c // 4, c % 4
        m = {}
        m["x_own"] = np.ascontiguousarray(x[b_, j * cfg.TL:(j + 1) * cfg.TL, :])
        m["consts"] = consts
        rm = np.zeros((128, 4), np.float32)
        rm[:, j] = 1.0
        m["rmask"] = rm
        cols = np.array(my_cols(j))
        for l in range(L):
            wsel = np.where(cols[None, :] >= 0, w_in[l][:, np.maximum(cols, 0)], 0.0).astype(np.float32)
            blocks = []
            c0 = 0
            for w_ in bw:
                blk = wsel[:, c0:c0 + w_]
                blocks.append(np.ascontiguousarray(blk.reshape(KT, 128, w_).transpose(1, 0, 2)).reshape(-1))
                c0 += w_
            m["win_%d" % l] = np.concatenate(blocks).reshape(D, NCOLS)
            for name, K_, N_ in cfg.big:
                P_, pe, a, bb = piece_plan(K_ * N_)
                m["%s_%d" % (name, l)] = np.ascontiguousarray(shards[(name, l)][:, c, :]).reshape(P_ * a, bb)
        sp = np.zeros((128, spo["_n"]), np.float32)
        tm = lambda v: v.reshape(KT, 128).T
        for l in range(L):
            sp[:, spo["g1"] + l * KT: spo["g1"] + (l + 1) * KT] = tm(f("norm1_g")[l])
            sp[:, spo["g2"] + l * KT: spo["g2"] + (l + 1) * KT] = tm(f("norm2_g")[l])
            for jb in range(3):
                o = spo["bm"] + (l * 3 + jb) * KT
                sp[:, o:o + KT] = tm(f("b_merge_gate")[l, jb])
            for blk in range(6):
                part, hh = blk // 2, 2 * j + blk % 2
                ch = part * 1024 + hh * 128
                o = spo["convw"] + (l * 6 + blk) * 5
                sp[:, o:o + 5] = f("conv_w")[l][:, ch:ch + 128].T
                sp[:, spo["convb"] + l * 6 + blk] = f("conv_b")[l][ch:ch + 128]
            sp[:, spo["agb"] + l * 4: spo["agb"] + l * 4 + 4] = f("mlstm_gate_b")[l][:, j][None, :]
            sp[:, spo["ang"] + l * 256: spo["ang"] + (l + 1) * 256] = f("mlstm_norm_g")[l][j * 256:(j + 1) * 256][None, :]
            for d_ in range(2):
                for hh in range(2):
                    sp[:, spo["gdn_alog"] + l * 4 + d_ * 2 + hh] = f("gdn_a_log")[l, d_, 2 * j + hh]
                    sp[:, spo["gdn_dtb"] + l * 4 + d_ * 2 + hh] = f("gdn_dt_bias")[l, d_, 2 * j + hh]
            sp[:, spo["bng"] + l * 256: spo["bng"] + (l + 1) * 256] = f("gdn_norm_g")[l][2 * j * 128:(2 * j + 2) * 128][None, :]
            for d_ in range(2):
                o = spo["glaw"] + (l * 2 + d_) * 128
                sp[0:16, o:o + 128] = f("gla_w_gate")[l, d_][:, j * 128:(j + 1) * 128]
                sp[:, spo["glab"] + l * 2 + d_] = f("gla_b_gate")[l, d_][j * 128:(j + 1) * 128]
            sp[:, spo["cng"] + l * 256: spo["cng"] + (l + 1) * 256] = f("gla_norm_g")[l][j * 256:(j + 1) * 256][None, :]
        sp[:, spo["gf"]: spo["gf"] + KT] = tm(f("final_g"))
        m["sp"] = sp
        in_maps.append(m)
    return in_maps


class Ctx:
    pass


def flat_blocks(t, nblk_elems):
    a = t.h.ap()
    fl = a.rearrange("r b -> (r b)")
    return fl.rearrange("(n p f) -> n p f", p=128, f=nblk_elems // 128)


def build(cfg, debug=()):
    D, FF, S, L, KT, FT, TL, NCH, TT, PT = cfg.D, cfg.FF, cfg.S, cfg.L, cfg.KT, cfg.FT, cfg.TL, cfg.NCH, cfg.TT, cfg.PT
    nc = bass.Bass("TRN2", target_bir_lowering=False)
    K = Ctx()
    K.cfg, K.nc = cfg, nc
    spo = sp_layout(cfg)
    K.spo = spo
    ext = lambda name, shape, dt=F32: T(nc.dram_tensor(name, list(shape), dt, kind="ExternalInput"))
    K.x_own = ext("x_own", [TL, D])
    K.consts_d = ext("consts", [128, 1024])
    K.rmask_d = ext("rmask", [128, 4])
    K.sp_d = ext("sp", [128, spo["_n"]])
    K.win_d = [ext("win_%d" % l, [D, NCOLS]) for l in range(L)]
    K.big_d = {}
    for name, K_, N_ in cfg.big:
        P_, pe, a, b = piece_plan(K_ * N_)
        for l in range(L):
            K.big_d[(name, l)] = ext("%s_%d" % (name, l), [P_ * a, b])
    K.out = T(nc.dram_tensor("out", [TL, D], F32, kind="ExternalOutput"))
    K.stub_y = ext("stub_y", [S // cfg.YP * 768, cfg.YP], BF16) if "stub_y" in debug else None
    K.debug = debug
    K.which = "abc"
    K.post = True
    K.post_lvl = 9
    for d_ in debug:
        if d_.startswith("postlvl="):
            K.post_lvl = int(d_[8:])
    for d_ in debug:
        if d_.startswith("which="):
            K.which = d_[6:]
        if d_ == "nopost":
            K.post = False
    K.dbg_out = []
    with ExitStack() as es:
        P = Prog(nc, es)
        K.P = P
        K.ps = [T(es.enter_context(nc.psum_tensor("ps%d" % i, [128, 512], F32))) for i in range(7)]
        K.psb = T(es.enter_context(nc.psum_tensor("psb", [128, 1024], BF16)), nparts=4)
        K.ps_rr = 0
        K.psb_rr = 0
        K.cst = P.sb("cst", [128, 1024], F32)
        K.spt = P.sb("spt", [128, spo["_n"]], F32)
        K.rmask = P.sb("rmaskt", [128, 4], F32)
        K.identb = P.sb("identb", [128, 128], BF16)
        P.dma(K.cst[:], K.consts_d[:], K.consts_d.res(), K.cst.res())
        P.dma(K.spt[:], K.sp_d[:], K.sp_d.res(), K.spt.res())
        P.dma(K.rmask[:], K.rmask_d[:], K.rmask_d.res(), K.rmask.res())
        P.op("dve", lambda e: e.tensor_copy(K.identb[:], K.cst[:, 0:128]), K.cst.res(), K.identb.res())
        K.xres = P.dram("xres", [D, TL], F32, nparts=TL // TT)
        K.xnp = P.dram("xnp", [TL // 128 * 128, KT * 128], BF16, nparts=TL // 128)
        K.xng = P.dram("xng", [TL // 128 * 4 * 128, KT * 128], BF16, nparts=TL // 128)
        K.ghT = P.dram("ghT", [256, TL], BF16)
        K.wfull = {}
        K.winb = []
        for l in range(L):
            K.winb.append(P.dram("winb_%d" % l, [D, NCOLS], BF16))
            for name, K_, N_ in cfg.big:
                P_, pe, a, b = piece_plan(K_ * N_)
                K.wfull[(name, l)] = P.dram("wf_%s_%d" % (name, l), [P_ * 8 * a, b], BF16)
                K.wfull[(name, l)].tmp = P.dram("wsb_%s_%d" % (name, l), [P_ * a, b], BF16)
                K.wfull[(name, l)].half = P.dram("wsh_%s_%d" % (name, l), [P_ * 4 * a, b], BF16, nparts=P_)
        alloc_mixer_dram(K)
        if "dumpH" in debug:
            K.dbg_out += [("H0", K.H[0]), ("H1", K.H[1]), ("fm32", K.fm32), ("fm16", K.fm16)]
        if "dumpY" in debug:
            K.dbg_out += [("yp", K.yp)]
        cast_win(K, 0)
        phase_x0(K)
        for l in range(L):
            phase_a(K, l)
            if l == 0:
                phase_weights(K, 0)
            phase_b(K, l)
            if l + 1 < L:
                cast_win(K, l + 1)
                phase_weights(K, l + 1)
            phase_dense(K, l, final=(l == L - 1))
        for name, t in K.dbg_out:
            o = T(nc.dram_tensor("dbg_" + name, list(t.h.shape), t.h.dtype, kind="ExternalOutput"))
            P.dma(o.h.ap(), t.h.ap(), t.res(), o.res(), q="pool")
        P.barrier(full=True)
        P.flush()
    return nc


def psum(K):
    i = K.ps_rr
    K.ps_rr = (i + 1) % len(K.ps)
    return K.ps[i]


def cast_win(K, l):
    P, cfg = K.P, K.cfg
    rows = cfg.D
    step = max(1, rows // 8)
    for r0 in range(0, rows, step):
        P.dma(K.winb[l][r0:r0 + step, :], K.win_d[l][r0:r0 + step, :], K.win_d[l].res(), K.winb[l].res(), q="pool")


def phase_weights(K, l):
    P, cfg = K.P, K.cfg
    g4 = [[0, 1, 2, 3], [4, 5, 6, 7]]
    g2 = [[0, 4], [1, 5], [2, 6], [3, 7]]
    for name, K_, N_ in cfg.big:
        P_, pe, a, b = piece_plan(K_ * N_)
        src, full = K.big_d[(name, l)], K.wfull[(name, l)]
        tmp, half = full.tmp, full.half
        nrow = P_ * a
        step = max(a, (nrow // 8 // a) * a) if nrow >= 8 * a else nrow
        for r0 in range(0, nrow, step):
            r1 = min(nrow, r0 + step)
            P.dma(tmp[r0:r1, :], src[r0:r1, :], src.res(), tmp.res(), q="pool")
        for p in range(P_):
            P.coll("AllGather", half[p * 4 * a:(p + 1) * 4 * a, :], tmp[p * a:(p + 1) * a, :], g4, tmp.res(), half.res(p))
        for p in range(P_):
            P.coll("AllGather", full[p * 8 * a:(p + 1) * 8 * a, :], half[p * 4 * a:(p + 1) * 4 * a, :], g2, half.res(p), full.res())


def phase_x0(K):
    P, cfg = K.P, K.cfg
    D, KT, TL = cfg.D, cfg.KT, cfg.TL
    xr = K.xres.h.ap().rearrange("(k p) t -> p k t", p=128)
    with P.phase():
        xt = [P.sb("x0_in%d" % i, [128, D], F32) for i in range(2)]
        xo = [P.sb("x0_out%d" % i, [128, KT, 128], F32) for i in range(2)]
        ident = K.cst[:, 0:128]
        for tb in range(TL // 128):
            a, o = xt[tb % 2], xo[tb % 2]
            P.dma(a[:], K.x_own[tb * 128:(tb + 1) * 128, :], K.x_own.res(), a.res())
            for g in range(KT // 4):
                ps = psum(K)
                for i in range(4):
                    kt = g * 4 + i
                    P.op("pe", lambda e, ps=ps, i=i, kt=kt, a=a: e.transpose(ps[:, i * 128:(i + 1) * 128], a[:, kt * 128:(kt + 1) * 128], ident),
                         a.res() + K.cst.res(), ps.res())
                P.op("act", lambda e, ps=ps, g=g, o=o: e.copy(o[:, g * 4:(g + 1) * 4, :], ps[:, :]), ps.res(), o.res())
            P.dma(xr[:, :, tb * 128:(tb + 1) * 128], o[:], o.res(), K.xres.res((tb * 128) // cfg.TT))


def rms_stats(K, xt, nkt, ntok, sq, rstd, tagres):
    P = K.P
    ones = K.cst[:, 128:256]
    ps = psum(K)
    G = 4
    for g in range(nkt // G):
        s = sq[g % 2]
        P.op("act", lambda e, s=s, g=g: e.activation(s[:], xt[:, g * G:(g + 1) * G, :], AF.Square), xt.res(), s.res())
        for i in range(G):
            kt = g * G + i
            P.op("pe", lambda e, s=s, i=i, kt=kt: e.matmul(ps[:, 0:ntok], ones, s[:, i, :], start=(kt == 0), stop=(kt == nkt - 1)),
                 s.res() + K.cst.res(), ps.res())
    P.op("act", lambda e: e.activation(rstd[:], ps[:, 0:ntok], AF.Sqrt, bias=K.epsD[:, 0:1], scale=1.0 / (nkt * 128)),
         ps.res() + K.epst.res(), rstd.res())
    P.op("dve", lambda e: e.reciprocal(rstd[:], rstd[:]), rstd.res(), rstd.res())


def phase_a(K, l):
    P, cfg, spo = K.P, K.cfg, K.spo
    D, KT, TL = cfg.D, cfg.KT, cfg.TL
    xr = K.xres.h.ap().rearrange("(k p) t -> p k t", p=128)
    groups4 = [[0, 1, 2, 3], [4, 5, 6, 7]]
    gho = (NFM * 128 + NTM * 256) * D
    wfl = K.winb[l].h.ap().rearrange("r b -> (r b)")
    with P.phase():
        make_eps(K)
        xt = [P.sb("a_x%d" % i, [128, KT, 128], F32) for i in range(2)]
        xn = [P.sb("a_xn%d" % i, [128, KT, 128], BF16) for i in range(2)]
        sq = [P.sb("a_sq%d" % i, [128, 4, 128], F32) for i in range(2)]
        rstd = [P.sb("a_rstd%d" % i, [128, 128], F32) for i in range(2)]
        wgh = P.sb("a_wgh", [128, 2, KT, 128], BF16)
        gho_sb = [P.sb("a_gho%d" % i, [128, 2, 128], BF16) for i in range(2)]
        for m in range(2):
            src = wfl[gho + m * D * 128: gho + (m + 1) * D * 128].rearrange("(p f) -> p f", p=128)
            P.dma(wgh[:, m, :, :], src, K.winb[l].res(), wgh.res())
        g1 = K.spt
        ght = K.ghT.h.ap().rearrange("(m p) t -> p m t", p=128)
        for pc in range(TL // 128):
            a, n, r, go = xt[pc % 2], xn[pc % 2], rstd[pc % 2], gho_sb[pc % 2]
            P.dma(a[:], xr[:, :, pc * 128:(pc + 1) * 128], K.xres.res((pc * 128) // cfg.TT), a.res())
            rms_stats(K, a, KT, 128, sq, r, None)
            for kt in range(KT):
                c = spo["g1"] + l * KT + kt
                P.op("dve", lambda e, kt=kt, c=c, a=a, n=n, r=r: e.scalar_tensor_tensor(n[:, kt, :], a[:, kt, :], g1[:, c:c + 1], r[:], ALU.mult, ALU.mult),
                     a.res() + r.res() + K.spt.res(), n.res())
            P.dma(K.xnp[pc * 128:(pc + 1) * 128, :], n[:].rearrange("p k t -> p (k t)"), n.res(), K.xnp.res(pc))
            for m in range(2):
                ps = psum(K)
                for kt in range(KT):
                    P.op("pe", lambda e, ps=ps, m=m, kt=kt, n=n: e.matmul(ps[:, 0:128], wgh[:, m, kt, :], n[:, kt, :], start=(kt == 0), stop=(kt == KT - 1)),
                         wgh.res() + n.res(), ps.res())
                P.op("act", lambda e, ps=ps, m=m, go=go: e.copy(go[:, m, :], ps[:, 0:128]), ps.res(), go.res())
            P.dma(ght[:, :, pc * 128:(pc + 1) * 128], go[:], go.res(), K.ghT.res())
            P.coll("AllGather", K.xng[pc * 512:(pc + 1) * 512, :], K.xnp[pc * 128:(pc + 1) * 128, :], groups4,
                   K.xnp.res(pc), K.xng.res(pc))


def make_eps(K):
    P = K.P
    K.epst = P.sb("epst", [128, 2], F32)
    K.epsD = K.epst
    P.op("dve", lambda e: e.memset(K.epst[:], EPS), (), K.epst.res())


def phase_dense(K, l, final):
    P, cfg, spo = K.P, K.cfg, K.spo
    D, FF, KT, FT, TL, TT, YP = cfg.D, cfg.FF, cfg.KT, cfg.FT, cfg.TL, cfg.TT, cfg.YP
    xr = K.xres.h.ap().rearrange("(k p) t -> p k t", p=128)
    ght = K.ghT.h.ap().rearrange("(m p) t -> p m t", p=128)
    ygv = K.yg.h.ap().rearrange("(q c p) t -> q p c t", c=6, p=128)
    wbr = [flat_blocks(K.wfull[(n, l)], 8 * 128 * 128) for n in ("wa", "wb", "wc")]
    wgfl = K.wfull[("wg", l)].h.ap().rearrange("r b -> (r b)")
    wo = flat_blocks(K.wfull[("wo", l)], D * 128)
    w1 = flat_blocks(K.wfull[("w1", l)], D * 128)
    w2 = flat_blocks(K.wfull[("w2", l)], FF * 128)
    FSUB = min(FT, 32)
    with P.phase():
        make_eps(K)
        xT = P.sb("d_xT", [128, KT, TT], F32, nparts=KT)
        act = P.sb("d_act", [128, KT, TT], BF16, nparts=KT)
        hT = P.sb("d_hT", [128, FT, TT], BF16, nparts=FT)
        yT = [P.sb("d_yT%d" % j, [128, 6, TT], BF16) for j in range(4)]
        ycand = [P.sb("d_yc%d" % i, [128, 6, TT], BF16) for i in range(2)]
        ghs = P.sb("d_gh", [128, 2, TT], BF16)
        NW = 4
        wp = [P.sb("d_w%d" % i, [128, 4096], BF16) for i in range(NW)]
        K.wrr = 0
        sq = [P.sb("d_sq%d" % i, [128, 4, TT], F32) for i in range(2)]
        rstd = P.sb("d_rstd", [128, TT], F32)
        gs = [P.sb("d_gs%d" % i, [128, TT], F32) for i in range(2)]
        tmp = [P.sb("d_tmp%d" % i, [128, TT], F32) for i in range(2)]
        acc = P.sb("d_acc", [128, TT], F32)
        if final:
            otv = hT.h[:].rearrange("p f t -> p (f t)").bitcast(F32)

        def wnext():
            w = wp[K.wrr]
            K.wrr = (K.wrr + 1) % NW
            return w

        for tt in range(TL // TT):
            t0 = tt * TT
            P.dma(xT[:], xr[:, :, t0:t0 + TT], K.xres.res(tt), xT.res())
            P.dma(ghs[:], ght[:, :, t0:t0 + TT], K.ghT.res(), ghs.res())
            ci = 0
            for j in range(4):
                for r in range(4):
                    g0 = r * TL + t0
                    tb, off = g0 // YP, g0 % YP
                    yc = ycand[ci % 2]
                    ci += 1
                    P.dma(yc[:], ygv[tb * 4 + j][:, :, off:off + TT], K.yg.res(tb), yc.res())
                    if r == 0:
                        P.op("dve", lambda e, j=j, yc=yc, r=r: e.tensor_scalar(yT[j][:], yc[:], K.rmask[:, r:r + 1], None, ALU.mult),
                             yc.res() + K.rmask.res(), yT[j].res())
                    else:
                        P.op("dve", lambda e, j=j, yc=yc, r=r: e.scalar_tensor_tensor(yT[j][:], yc[:], K.rmask[:, r:r + 1], yT[j][:], ALU.mult, ALU.add),
                             yc.res() + K.rmask.res() + yT[j].res(), yT[j].res())
            for dt in range(KT):
                w = wnext()
                for jb in range(3):
                    o = jb * 256 * D + dt * 256 * 128
                    P.dma(w[:, jb * 256:(jb + 1) * 256], wgfl[o:o + 256 * 128].rearrange("(p f) -> p f", p=128),
                          K.wfull[("wg", l)].res(), w.res())
                    P.dma(w[:, 768 + jb * 1024: 768 + (jb + 1) * 1024], wbr[jb][dt], K.wfull[(("wa", "wb", "wc")[jb], l)].res(), w.res())
                for jb in range(3):
                    psg = psum(K)
                    for rt in range(2):
                        c0 = jb * 256 + rt * 128
                        P.op("pe", lambda e, psg=psg, w=w, c0=c0, rt=rt: e.matmul(psg[:, 0:TT], w[:, c0:c0 + 128], ghs[:, rt, :], start=(rt == 0), stop=(rt == 1)),
                             w.res() + ghs.res(), psg.res())
                    g = gs[jb % 2]
                    bc = spo["bm"] + (l * 3 + jb) * KT + dt
                    P.op("act", lambda e, psg=psg, g=g, bc=bc: e.activation(g[:], psg[:, 0:TT], AF.Sigmoid, bias=K.spt[:, bc:bc + 1]),
                         psg.res() + K.spt.res(), g.res())
                    psb = psum(K)
                    for ct in range(8):
                        c0 = 768 + jb * 1024 + ct * 128
                        P.op("pe", lambda e, psb=psb, w=w, c0=c0, ct=ct, jb=jb: e.matmul(psb[:, 0:TT], w[:, c0:c0 + 128], yT[ct // 2][:, 2 * jb + ct % 2, :], start=(ct == 0), stop=(ct == 7)),
                             w.res() + yT[ct // 2].res(), psb.res())
                    if jb == 0:
                        P.op("dve", lambda e, psb=psb, g=g: e.tensor_tensor(acc[:], psb[:, 0:TT], g[:], ALU.mult), psb.res() + g.res(), acc.res())
                    else:
                        tm_ = tmp[jb % 2]
                        P.op("dve", lambda e, psb=psb, g=g, tm_=tm_: e.tensor_tensor(tm_[:], psb[:, 0:TT], g[:], ALU.mult), psb.res() + g.res(), tm_.res())
                        if jb == 1:
                            P.op("dve", lambda e, tm_=tm_: e.tensor_tensor(acc[:], acc[:], tm_[:], ALU.add), acc.res() + tm_.res(), acc.res())
                        else:
                            P.op("dve", lambda e, tm_=tm_, dt=dt: e.tensor_tensor(act[:, dt, :], acc[:], tm_[:], ALU.add), acc.res() + tm_.res(), act.res(dt))
            for et in range(KT):
                w = wnext()
                P.dma(w[:, 0:KT * 128], wo[et], K.wfull[("wo", l)].res(), w.res())
                ps = psum(K)
                for dt in range(KT):
                    P.op("pe", lambda e, ps=ps, w=w, dt=dt: e.matmul(ps[:, 0:TT], w[:, dt * 128:(dt + 1) * 128], act[:, dt, :], start=(dt == 0), stop=(dt == KT - 1)),
                         w.res() + act.res(dt), ps.res())
                P.op("dve", lambda e, ps=ps, et=et: e.tensor_tensor(xT[:, et, :], xT[:, et, :], ps[:, 0:TT], ALU.add), ps.res() + xT.res(et), xT.res(et))
            rms_stats(K, xT, KT, TT, sq, rstd, None)
            for kt in range(KT):
                c = spo["g2"] + l * KT + kt
                P.op("dve", lambda e, kt=kt, c=c: e.scalar_tensor_tensor(act[:, kt, :], xT[:, kt, :], K.spt[:, c:c + 1], rstd[:], ALU.mult, ALU.mult),
                     xT.res(kt) + rstd.res() + K.spt.res(), act.res(kt))
            for ft in range(FT):
                w = wnext()
                P.dma(w[:, 0:KT * 128], w1[ft], K.wfull[("w1", l)].res(), w.res())
                ps = psum(K)
                for kt in range(KT):
                    P.op("pe", lambda e, ps=ps, w=w, kt=kt: e.matmul(ps[:, 0:TT], w[:, kt * 128:(kt + 1) * 128], act[:, kt, :], start=(kt == 0), stop=(kt == KT - 1)),
                         w.res() + act.res(kt), ps.res())
                sv = tmp[ft % 2]
                P.op("act", lambda e, ps=ps, sv=sv: e.activation(sv[:], ps[:, 0:TT], AF.Square), ps.res(), sv.res())
                P.op("dve", lambda e, ps=ps, sv=sv, ft=ft: e.scalar_tensor_tensor(hT[:, ft, :], ps[:, 0:TT], 0.0, sv[:], ALU.is_gt, ALU.mult),
                     ps.res() + sv.res(), hT.res(ft))
            for dt in range(KT):
                ps = psum(K)
                for sub in range(FT // FSUB):
                    w = wnext()
                    P.dma(w[:, 0:FSUB * 128], w2[dt][:, sub * FSUB * 128:(sub + 1) * FSUB * 128], K.wfull[("w2", l)].res(), w.res())
                    for fi in range(FSUB):
                        ft = sub * FSUB + fi
                        P.op("pe", lambda e, ps=ps, w=w, fi=fi, ft=ft: e.matmul(ps[:, 0:TT], w[:, fi * 128:(fi + 1) * 128], hT[:, ft, :], start=(ft == 0), stop=(ft == FT - 1)),
                             w.res() + hT.res(ft), ps.res())
                P.op("dve", lambda e, ps=ps, dt=dt: e.tensor_tensor(xT[:, dt, :], xT[:, dt, :], ps[:, 0:TT], ALU.add), ps.res() + xT.res(dt), xT.res(dt))
            if not final:
                P.dma(xr[:, :, t0:t0 + TT], xT[:], xT.res(), K.xres.res(tt))
            else:
                rms_stats(K, xT, KT, TT, sq, rstd, None)
                for kt in range(KT):
                    c = spo["gf"] + kt
                    P.op("dve", lambda e, kt=kt, c=c: e.scalar_tensor_tensor(xT[:, kt, :], xT[:, kt, :], K.spt[:, c:c + 1], rstd[:], ALU.mult, ALU.mult),
                         xT.res(kt) + rstd.res() + K.spt.res(), xT.res(kt))
                ident = K.cst[:, 0:128]
                for s_ in range(TT // 128):
                    o_ = otv[:, (s_ % 2) * D:(s_ % 2 + 1) * D]
                    for g in range(KT // 4):
                        ps = psum(K)
                        for i in range(4):
                            kt = g * 4 + i
                            P.op("pe", lambda e, ps=ps, i=i, kt=kt, s_=s_: e.transpose(ps[:, i * 128:(i + 1) * 128], xT[:, kt, s_ * 128:(s_ + 1) * 128], ident),
                                 xT.res(kt) + K.cst.res(), ps.res())
                        P.op("act", lambda e, ps=ps, g=g, o_=o_: e.copy(o_[:, g * 512:(g + 1) * 512], ps[:, :]), ps.res(), hT.res())
                    P.dma(K.out[t0 + s_ * 128: t0 + (s_ + 1) * 128, :], o_, hT.res(), K.out.res())


def alloc_mixer_dram(K):
    P, cfg = K.P, K.cfg
    S, YP = cfg.S, cfg.YP
    npc = S // YP
    K.fm32 = P.dram("fm32", [9 * 128, S], F32, nparts=9)
    K.fm16 = P.dram("fm16", [2 * 128, S], BF16, nparts=2)
    K.tm = {"av": P.dram("tm_av", [S, 256], BF16), "ao": P.dram("tm_ao", [S, 256], F32),
            "bz": P.dram("tm_bz", [S, 256], F32), "cv": P.dram("tm_cv", [S, 256], BF16),
            "cr": P.dram("tm_cr", [S, 256], F32)}
    K.yp = P.dram("yp", [npc * 6 * 128, YP], BF16, nparts=npc)
    K.yg = P.dram("yg", [npc * 4 * 6 * 128, YP], BF16, nparts=npc)
    K.H = [P.dram("H%d" % d, [S, 768], F32, nparts=4) for d in range(2)]
    K.gl = {}


def b_proj(K, l):
    P, cfg = K.P, K.cfg
    D, KT, S, TL, PT = cfg.D, cfg.KT, cfg.S, cfg.TL, cfg.PTP
    wfl = K.winb[l].h.ap().rearrange("r b -> (r b)")
    bw = block_widths()
    boff = np.concatenate([[0], np.cumsum(bw)]).tolist()
    tmnames = ["av", "ao", "bz", "cv", "cr"]
    NB = NFM + NTM
    with P.phase():
        xn = [P.sb("p_xn%d" % i, [128, KT, PT], BF16) for i in range(2)]
        NWQ = 4
        wq = [P.sb("p_w%d" % i, [128, KT * 256], BF16) for i in range(NWQ)]
        st32 = [P.sb("p_s32_%d" % i, [128, 512], F32) for i in range(3)]
        st16 = [P.sb("p_s16_%d" % i, [128, 512], BF16) for i in range(3)]
        ntile = S // PT
        jobs = [(ti, bi) for ti in range(ntile) for bi in range(NB)]

        def load_w(k):
            ti, bi = jobs[k]
            w = wq[k % NWQ]
            wd = 128 if bi < NFM else 256
            o = boff[bi] * D
            P.dma(w[:, 0:KT * wd], wfl[o:o + D * wd].rearrange("(p f) -> p f", p=128), K.winb[l].res(), w.res())

        def load_x(ti):
            g0 = ti * PT
            r, w0 = g0 // TL, g0 % TL
            x = xn[ti % 2]
            for i in range(PT // 128):
                pc = (w0 + i * 128) // 128
                P.dma(x[:, :, i * 128:(i + 1) * 128], K.xng[pc * 512 + r * 128: pc * 512 + (r + 1) * 128, :].rearrange("p (k t) -> p k t", t=128),
                      K.xng.res(pc), x.res())

        load_x(0)
        for k in range(min(NWQ - 1, len(jobs))):
            load_w(k)
        sr = 0
        for k, (ti, bi) in enumerate(jobs):
            g0 = ti * PT
            x = xn[ti % 2]
            w = wq[k % NWQ]
            if bi == 0 and ti + 1 < ntile:
                load_x(ti + 1)
            if k + NWQ - 1 < len(jobs):
                load_w(k + NWQ - 1)
            if bi < NFM:
                ps = psum(K)
                for kt in range(KT):
                    P.op("pe", lambda e, ps=ps, w=w, kt=kt, x=x: e.matmul(ps[:, 0:PT], w[:, kt * 128:(kt + 1) * 128], x[:, kt, :], start=(kt == 0), stop=(kt == KT - 1)),
                         w.res() + x.res(), ps.res())
                if bi < 2:
                    st = st16[sr % 3]
                    sr += 1
                    P.op("act", lambda e, ps=ps, st=st, bi=bi: e.activation(st[:, 0:PT], ps[:, 0:PT], AF.Copy, scale=(1.0 if bi == 0 else A_DK ** -0.5)), ps.res(), st.res())
                    P.dma(K.fm16[bi * 128:(bi + 1) * 128, g0:g0 + PT], st[:, 0:PT], st.res(), K.fm16.res(bi), q="act")
                else:
                    st = st32[sr % 3]
                    sr += 1
                    P.op("act", lambda e, ps=ps, st=st: e.copy(st[:, 0:PT], ps[:, 0:PT]), ps.res(), st.res())
                    P.dma(K.fm32[(bi - 2) * 128:(bi - 1) * 128, g0:g0 + PT], st[:, 0:PT], st.res(), K.fm32.res(bi - 2), q="act")
            else:
                nm = tmnames[bi - NFM]
                dst = K.tm[nm]
                is16 = nm in ("av", "cv")
                for sub in range(PT // 128):
                    ps = psum(K)
                    for kt in range(KT):
                        P.op("pe", lambda e, ps=ps, w=w, kt=kt, x=x, sub=sub: e.matmul(ps[:, 0:256], x[:, kt, sub * 128:(sub + 1) * 128], w[:, kt * 256:(kt + 1) * 256], start=(kt == 0), stop=(kt == KT - 1)),
                             w.res() + x.res(), ps.res())
                    st = (st16 if is16 else st32)[sr % 3]
                    sr += 1
                    P.op("dve", lambda e, ps=ps, st=st: e.tensor_copy(st[:, 0:256], ps[:, 0:256]), ps.res(), st.res())
                    P.dma(dst[g0 + sub * 128: g0 + (sub + 1) * 128, :], st[:, 0:256], st.res(), dst.res(), q="act")


def b_ygather(K, l):
    P, cfg = K.P, K.cfg
    npc = cfg.S // cfg.YP
    groups4 = [[0, 1, 2, 3], [4, 5, 6, 7]]
    for tb in range(npc):
        P.coll("AllGather", K.yg[tb * 4 * 768:(tb + 1) * 4 * 768, :], K.yp[tb * 768:(tb + 1) * 768, :], groups4,
               K.yp.res(tb), K.yg.res(tb))


def phase_b(K, l):
    b_proj(K, l)
    if K.stub_y is not None:
        P = K.P
        P.dma(K.yp[:, :], K.stub_y[:, :], K.stub_y.res(), K.yp.res(), q="pool")
    else:
        b_mixers(K, l)
    b_ygather(K, l)


def run_cfg(inputs, cfg, debug=(), extra=None):
    in_maps = prep_inputs(inputs, cfg)
    if extra is not None:
        for c in range(NCORES):
            in_maps[c].update(extra[c])
    nc = build(cfg, debug=debug)
    res = run_bass_kernel_spmd(nc, in_maps, core_ids=list(range(NCORES)))
    out = np.zeros((2, cfg.S, cfg.D), np.float32)
    for c in range(NCORES):
        b_, j = c // 4, c % 4
        out[b_, j * cfg.TL:(j + 1) * cfg.TL, :] = res.results[c]["out"]
    return out, res


def kernel(**inputs):
    x = np.asarray(inputs["x"])
    L = int(np.asarray(inputs["w_in"]).shape[0])
    cfg = Cfg(int(x.shape[2]), int(np.asarray(inputs["w_ff1"]).shape[2]), int(x.shape[1]), L)
    out, _ = run_cfg(inputs, cfg)
    return out.astype(np.float32)


def tr_bf(K, dst_ap, dst_res, src_ap, src_res, eng="act", scale_ap=None):
    P = K.P
    i = K.psb_rr
    K.psb_rr = (i + 1) % 4
    pv = K.psb[:, i * 256:i * 256 + 128]
    P.op("pe", lambda e: e.transpose(pv, src_ap, K.identb[:]), list(src_res) + K.identb.res(), K.psb.res(i))
    if scale_ap is not None:
        P.op("dve", lambda e: e.tensor_scalar(dst_ap, pv, scale_ap[0], None, ALU.mult), K.psb.res(i) + list(scale_ap[1]), dst_res)
    elif eng == "act":
        P.op("act", lambda e: e.copy(dst_ap, pv), K.psb.res(i), dst_res)
    else:
        P.op("dve", lambda e: e.tensor_copy(dst_ap, pv), K.psb.res(i), dst_res)


def gla_prep(K, l):
    P, cfg, spo = K.P, K.cfg, K.spo
    S, PT = cfg.S, cfg.PT
    K.glaB = [P.dram("glaB%d_%d" % (d, l), [128, S + 1], F32, nparts=S // PT) for d in range(2)]
    if "dumpG" in K.debug and l == 0:
        K.dbg_out += [("glaB0", K.glaB[0]), ("glaB1", K.glaB[1])]
    with P.phase():
        negb = P.sb("gp_negb", [128, 2], F32)
        zc = P.sb("gp_z", [128, 1], F32)
        P.op("dve", lambda e: e.memset(zc[:], 0.0), (), zc.res())
        P.op("dve", lambda e: e.tensor_scalar(negb[:], K.spt[:, spo["glab"] + l * 2: spo["glab"] + l * 2 + 2], -1.0, None, ALU.mult), K.spt.res(), negb.res())
        clr = [P.sb("gp_clr%d" % i, [16, PT], F32) for i in range(2)]
        ex = [P.sb("gp_e%d" % i, [128, PT], F32) for i in range(2)]
        bt = [P.sb("gp_b%d" % i, [128, PT], F32) for i in range(3)]
        n = 0
        for d in range(2):
            P.dma(K.glaB[d][:, (0 if d == 0 else S):(1 if d == 0 else S + 1)], zc[:], zc.res(), K.glaB[d].res(0 if d == 0 else S // PT - 1), slow=True)
            prev = None
            order = range(S // PT) if d == 0 else range(S // PT - 1, -1, -1)
            wg = K.spt[0:16, spo["glaw"] + (l * 2 + d) * 128: spo["glaw"] + (l * 2 + d + 1) * 128]
            for ti in order:
                g0 = ti * PT
                c_, e_, b_ = clr[n % 2], ex[n % 2], bt[n % 3]
                n += 1
                r0 = 8 * 128 + 32 + 32 * d
                P.dma(c_[:], K.fm32[r0:r0 + 16, g0:g0 + PT], K.fm32.res(8), c_.res())
                ps = psum(K)
                P.op("pe", lambda e, ps=ps, c_=c_, wg=wg: e.matmul(ps[:, 0:PT], wg, c_[:], start=True, stop=True), c_.res() + K.spt.res(), ps.res())
                P.op("act", lambda e, ps=ps, e_=e_, d=d: e.activation(e_[:], ps[:, 0:PT], AF.Exp, bias=negb[:, d:d + 1], scale=-1.0), ps.res() + negb.res(), e_.res())
                P.op("act", lambda e, e_=e_: e.activation(e_[:], e_[:], AF.Ln, bias=K.cst[:, 128:129], scale=1.0), e_.res() + K.cst.res(), e_.res())
                P.op("dve", lambda e, e_=e_: e.tensor_scalar(e_[:], e_[:], 1.0 / GLA_TAU, None, ALU.mult), e_.res(), e_.res())
                if d == 0:
                    init = 0.0 if prev is None else prev[:, PT - 1:PT]
                    P.op("dve", lambda e, b_=b_, e_=e_, init=init: e.tensor_tensor_scan(b_[:], e_[:], e_[:], init, ALU.add, ALU.bypass),
                         e_.res() + (prev.res() if prev is not None else []), b_.res())
                    P.dma(K.glaB[d][:, 1 + g0:1 + g0 + PT], b_[:], b_.res(), K.glaB[d].res(ti))
                else:
                    init = 0.0 if prev is None else prev[:, 0:1]
                    P.op("dve", lambda e, b_=b_, e_=e_, init=init: e.tensor_tensor_scan(b_[:, ::-1], e_[:, ::-1], e_[:, ::-1], init, ALU.add, ALU.bypass),
                         e_.res() + (prev.res() if prev is not None else []), b_.res())
                    P.dma(K.glaB[d][:, g0:g0 + PT], b_[:], b_.res(), K.glaB[d].res(ti))
                prev = b_


def gla_stream(K, l, d):
    P, cfg = K.P, K.cfg
    S, NCH, PT = cfg.S, cfg.NCH, cfg.PT
    tg = "gl%d" % d
    st = P.sb(tg + "_st", [128, 256], F32)
    stb = P.sb(tg + "_stb", [128, 256], BF16)
    P.op("dve", lambda e: e.memset(st[:], 0.0), (), st.res())
    P.op("dve", lambda e: e.memset(stb[:], 0.0), (), stb.res())
    nb = 2
    bs = [P.sb(tg + "_bs%d" % i, [128, 129], F32) for i in range(nb)]
    qk = [P.sb(tg + "_qk%d" % i, [128, 2, 128], F32) for i in range(nb)]
    v = [P.sb(tg + "_v%d" % i, [128, 256], BF16) for i in range(nb)]
    E1 = [P.sb(tg + "_E1%d" % i, [128, 128], F32) for i in range(nb)]
    E2 = [P.sb(tg + "_E2%d" % i, [128, 128], F32) for i in range(nb)]
    qd = [P.sb(tg + "_qd%d" % i, [128, 128], BF16) for i in range(nb)]
    kd = [P.sb(tg + "_kd%d" % i, [128, 128], BF16) for i in range(nb)]
    ktm = [P.sb(tg + "_kt%d" % i, [128, 128], BF16) for i in range(nb)]
    stm = [P.sb(tg + "_sm%d" % i, [128, 128], BF16) for i in range(nb)]
    o = [P.sb(tg + "_o%d" % i, [128, 256], F32) for i in range(nb)]
    tS = P.sb(tg + "_tS", [128, 256], F32)
    mask = K.cst[:, (6 + d) * 128:(7 + d) * 128]
    order = range(NCH) if d == 0 else range(NCH - 1, -1, -1)
    n = 0
    for c in order:
        cs = c * 128
        i = n % nb
        n += 1
        P.dma(qk[i][:, 0, :], K.fm32[6 * 128:7 * 128, cs:cs + 128], K.fm32.res(6), qk[i].res())
        P.dma(qk[i][:, 1, :], K.fm32[7 * 128:8 * 128, cs:cs + 128], K.fm32.res(7), qk[i].res())
        P.dma(bs[i][:], K.glaB[d][:, cs:cs + 129], K.glaB[d].res(), bs[i].res())
        P.dma(v[i][:], K.tm["cv"][cs:cs + 128, :], K.tm["cv"].res(), v[i].res())
        if d == 0:
            bcur, bref, edge = bs[i][:, 1:129], bs[i][:, 0:1], 127
        else:
            bcur, bref, edge = bs[i][:, 0:128], bs[i][:, 128:129], 0
        P.op("act", lambda e, i=i, bcur=bcur, bref=bref: e.activation(E1[i][:], bcur, AF.Exp, bias=bref, scale=-1.0), bs[i].res(), E1[i].res())
        P.op("dve", lambda e, i=i: e.reciprocal(E2[i][:], E1[i][:]), E1[i].res(), E2[i].res())
        P.op("dve", lambda e, i=i: e.scalar_tensor_tensor(qd[i][:], qk[i][:, 0, :], C_DK ** -0.5, E1[i][:], ALU.mult, ALU.mult), qk[i].res() + E1[i].res(), qd[i].res())
        P.op("dve", lambda e, i=i: e.tensor_tensor(kd[i][:], qk[i][:, 1, :], E2[i][:], ALU.mult), qk[i].res() + E2[i].res(), kd[i].res())
        ps1 = psum(K)
        P.op("pe", lambda e, i=i, ps1=ps1: e.matmul(ps1[:, 0:128], kd[i][:], qd[i][:], start=True, stop=True), kd[i].res() + qd[i].res(), ps1.res())
        P.op("dve", lambda e, i=i, ps1=ps1: e.tensor_tensor(stm[i][:], ps1[:, 0:128], mask, ALU.mult), ps1.res() + K.cst.res(), stm[i].res())
        tr_bf(K, ktm[i][:], ktm[i].res(), kd[i][:], kd[i].res())
        ps2 = psum(K)
        P.op("pe", lambda e, i=i, ps2=ps2: e.matmul(ps2[:, 0:256], stm[i][:], v[i][:], start=True, stop=False), stm[i].res() + v[i].res(), ps2.res())
        P.op("pe", lambda e, i=i, ps2=ps2: e.matmul(ps2[:, 0:256], qd[i][:], stb[:], start=False, stop=True), qd[i].res() + stb.res(), ps2.res())
        P.op("act", lambda e, i=i, ps2=ps2: e.copy(o[i][:], ps2[:, 0:256]), ps2.res(), o[i].res())
        P.dma(K.H[d][cs:cs + 128, 512:768], o[i][:], o[i].res(), K.H[d].res(3))
        ps3 = psum(K)
        P.op("pe", lambda e, i=i, ps3=ps3: e.matmul(ps3[:, 0:256], ktm[i][:], v[i][:], start=True, stop=True), ktm[i].res() + v[i].res(), ps3.res())
        P.op("dve", lambda e, ps3=ps3: e.tensor_tensor(tS[:], st[:], ps3[:, 0:256], ALU.add), st.res() + ps3.res(), tS.res())
        P.op("dve", lambda e, i=i, edge=edge: e.tensor_scalar(st[:], tS[:], E1[i][:, edge:edge + 1], None, ALU.mult), tS.res() + E1[i].res(), st.res())
        P.op("act", lambda e: e.copy(stb[:], st[:]), st.res(), stb.res())
        yield


def mlstm_prep(K, l):
    P, cfg, spo = K.P, K.cfg, K.spo
    S, NCH = cfg.S, cfg.NCH
    RS = min(S, 1024)
    K.mlG = [P.dram("mlG%d_%d" % (d, l), [3, S], F32) for d in range(2)]
    with P.phase():
        nbias = P.sb("mp_nb", [1, 4], F32)
        P.op("dve", lambda e: e.tensor_scalar(nbias[:], K.spt[0:1, spo["agb"] + l * 4: spo["agb"] + l * 4 + 4], -1.0, None, ALU.mult), K.spt.res(), nbias.res())
        zr = P.sb("mp_z", [1, RS], F32)
        P.op("dve", lambda e: e.memset(zr[:], 0.0), (), zr.res())
        names = ["fr", "ir", "lf", "F", "m", "a", "u", "em"]
        tsets = [{nm: P.sb("mp_%s_%d" % (nm, i_), [1, RS], F32) for nm in names} for i_ in range(2)]
        for d in range(2):
            prevF = prevm = None
            segs = range(S // RS) if d == 0 else range(S // RS - 1, -1, -1)
            for si, sg in enumerate(segs):
                t = tsets[si % 2]
                g0 = sg * RS
                rb = 8 * 128
                P.dma(t["ir"][:], K.fm32[rb + d:rb + d + 1, g0:g0 + RS], K.fm32.res(8), t["ir"].res())
                P.dma(t["fr"][:], K.fm32[rb + 2 + d:rb + 3 + d, g0:g0 + RS], K.fm32.res(8), t["fr"].res())
                P.op("act", lambda e, t=t, d=d: e.activation(t["fr"][:], t["fr"][:], AF.Exp, bias=nbias[:, 2 + d:3 + d], scale=-1.0), t["fr"].res() + nbias.res(), t["fr"].res())
                P.op("act", lambda e, t=t: e.activation(t["fr"][:], t["fr"][:], AF.Ln, bias=K.cst[0:1, 128:129], scale=1.0), t["fr"].res() + K.cst.res(), t["fr"].res())
                P.op("dve", lambda e, t=t: e.tensor_scalar(t["lf"][:], t["fr"][:], -1.0, None, ALU.mult), t["fr"].res(), t["lf"].res())
                P.op("dve", lambda e, t=t, d=d: e.tensor_scalar(t["ir"][:], t["ir"][:], K.spt[0:1, spo["agb"] + l * 4 + d: spo["agb"] + l * 4 + d + 1], None, ALU.add), t["ir"].res() + K.spt.res(), t["ir"].res())
                if d == 0:
                    iF = 0.0 if prevF is None else prevF[:, RS - 1:RS]
                    im = 0.0 if prevm is None else prevm[:, RS - 1:RS]
                    vw = lambda ap: ap[:]
                else:
                    iF = 0.0 if prevF is None else prevF[:, 0:1]
                    im = 0.0 if prevm is None else prevm[:, 0:1]
                    vw = lambda ap: ap[:, ::-1]
                dep = (prevF.res() if prevF is not None else []) + (prevm.res() if prevm is not None else [])
                P.op("dve", lambda e, t=t, iF=iF, vw=vw: e.tensor_tensor_scan(vw(t["F"]), vw(t["lf"]), vw(zr), iF, ALU.add, ALU.add), t["lf"].res() + zr.res() + dep, t["F"].res())
                P.op("dve", lambda e, t=t, im=im, vw=vw: e.tensor_tensor_scan(vw(t["m"]), vw(t["lf"]), vw(t["ir"]), im, ALU.add, ALU.max), t["lf"].res() + t["ir"].res() + dep, t["m"].res())
                P.op("dve", lambda e, t=t: e.tensor_tensor(t["a"][:], t["F"][:], t["m"][:], ALU.subtract), t["F"].res() + t["m"].res(), t["a"].res())
                P.op("dve", lambda e, t=t: e.tensor_tensor(t["u"][:], t["ir"][:], t["F"][:], ALU.subtract), t["F"].res() + t["ir"].res(), t["u"].res())
                P.op("act", lambda e, t=t: e.activation(t["em"][:], t["m"][:], AF.Exp, scale=-1.0), t["m"].res(), t["em"].res())
                for ri, nm in enumerate(("a", "u", "em")):
                    P.dma(K.mlG[d][ri:ri + 1, g0:g0 + RS], t[nm][:], t[nm].res(), K.mlG[d].res())
                prevF, prevm = t["F"], t["m"]


def col_from_rows(K, dst, dram_row_ap, dram_res, nch, tmp):
    P = K.P
    P.dma(tmp[0:nch, :], dram_row_ap.rearrange("o (c t) -> (o c) t", t=128), dram_res, tmp.res())
    ps = psum(K)
    P.op("pe", lambda e: e.transpose(ps[:, 0:nch], tmp[0:nch, :], K.cst[0:nch, 0:nch]), tmp.res() + K.cst.res(), ps.res())
    P.op("act", lambda e: e.copy(dst, ps[:, 0:nch]), ps.res(), [])


def mlstm_stream(K, l, d):
    P, cfg = K.P, K.cfg
    S, NCH = cfg.S, cfg.NCH
    tg = "ml%d" % d
    st = P.sb(tg + "_st", [128, 257], F32)
    stb = P.sb(tg + "_stb", [128, 257], BF16)
    P.op("dve", lambda e: e.memset(st[:], 0.0), (), st.res())
    P.op("dve", lambda e: e.memset(stb[:], 0.0), (), stb.res())
    cols = P.sb(tg + "_cols", [128, 2, NCH], F32)
    cmt = P.sb(tg + "_cmt", [128, 128], F32)
    for ri in range(2):
        P.dma(cmt[0:NCH, :], K.mlG[d][1 + ri:2 + ri, :].rearrange("o (c t) -> (o c) t", t=128), K.mlG[d].res(), cmt.res())
        ps = psum(K)
        P.op("pe", lambda e, ps=ps: e.transpose(ps[:, 0:NCH], cmt[0:NCH, :], K.cst[0:NCH, 0:NCH]), cmt.res() + K.cst.res(), ps.res())
        P.op("act", lambda e, ps=ps, ri=ri: e.copy(cols[:, ri, :], ps[:, 0:NCH]), ps.res(), cols.res())
    nb = 2
    q = [P.sb(tg + "_q%d" % i, [128, 128], BF16) for i in range(nb)]
    k = [P.sb(tg + "_k%d" % i, [128, 128], BF16) for i in range(nb)]
    va = [P.sb(tg + "_va%d" % i, [128, 257], BF16) for i in range(nb)]
    ar = [P.sb(tg + "_ar%d" % i, [1, 128], F32) for i in range(nb)]
    W = [P.sb(tg + "_W%d" % i, [128, 128], F32) for i in range(nb)]
    Wi = [P.sb(tg + "_Wi%d" % i, [128, 128], F32) for i in range(nb)]
    Dm = [P.sb(tg + "_Dm%d" % i, [128, 128], BF16) for i in range(nb)]
    qd = [P.sb(tg + "_qd%d" % i, [128, 128], BF16) for i in range(nb)]
    kw = [P.sb(tg + "_kw%d" % i, [128, 128], BF16) for i in range(nb)]
    h = [P.sb(tg + "_h%d" % i, [128, 256], F32) for i in range(nb)]
    dn = [P.sb(tg + "_dn%d" % i, [128, 2], F32) for i in range(nb)]
    negab = [P.sb(tg + "_na%d" % i, [128, 1], F32) for i in range(2)]
    for i in range(nb):
        P.op("dve", lambda e, i=i: e.memset(va[i][:, 256:257], 1.0), (), va[i].res())
    P.op("dve", lambda e: e.memset(negab[0][:], 0.0), (), negab[0].res())
    ones_row = K.cst[0:1, 128:256]
    ident = K.cst[:, 0:128]
    negm = K.cst[:, (2 + 2 * d) * 128:(3 + 2 * d) * 128]
    edge = 127 if d == 0 else 0
    order = range(NCH) if d == 0 else range(NCH - 1, -1, -1)
    n = 0
    for c in order:
        cs = c * 128
        i = n % nb
        na_in, na_out = negab[n % 2], negab[(n + 1) % 2]
        n += 1
        P.dma(q[i][:], K.fm16[0:128, cs:cs + 128], K.fm16.res(0), q[i].res())
        P.dma(k[i][:], K.fm16[128:256, cs:cs + 128], K.fm16.res(1), k[i].res())
        P.dma(va[i][:, 0:256], K.tm["av"][cs:cs + 128, :], K.tm["av"].res(), va[i].res())
        P.dma(ar[i][:], K.mlG[d][0:1, cs:cs + 128], K.mlG[d].res(), ar[i].res())
        ps1 = psum(K)
        P.op("pe", lambda e, i=i, ps1=ps1: e.matmul(ps1[:, 0:128], ones_row, ar[i][:], start=True, stop=False), ar[i].res() + K.cst.res(), ps1.res())
        P.op("pe", lambda e, ps1=ps1: e.matmul(ps1[:, 0:128], ident, negm, start=False, stop=True), K.cst.res(), ps1.res())
        P.op("act", lambda e, i=i, ps1=ps1, c=c: e.activation(W[i][:], ps1[:, 0:128], AF.Exp, bias=cols[:, 0, c:c + 1]), ps1.res() + cols.res(), W[i].res())
        ps1b = psum(K)
        P.op("pe", lambda e, i=i, ps1b=ps1b: e.matmul(ps1b[:, 0:128], ones_row, ar[i][:], start=True, stop=True), ar[i].res() + K.cst.res(), ps1b.res())
        P.op("act", lambda e, i=i, ps1b=ps1b, na_in=na_in: e.activation(Wi[i][:], ps1b[:, 0:128], AF.Exp, bias=na_in[:, 0:1]), ps1b.res() + na_in.res(), Wi[i].res())
        P.op("dve", lambda e, ps1b=ps1b, na_out=na_out: e.tensor_scalar(na_out[:], ps1b[:, edge:edge + 1], -1.0, None, ALU.mult), ps1b.res(), na_out.res())
        ps2 = psum(K)
        P.op("pe", lambda e, i=i, ps2=ps2: e.matmul(ps2[:, 0:128], k[i][:], q[i][:], start=True, stop=True), k[i].res() + q[i].res(), ps2.res())
        P.op("dve", lambda e, i=i, ps2=ps2: e.tensor_tensor(Dm[i][:], ps2[:, 0:128], W[i][:], ALU.mult), ps2.res() + W[i].res(), Dm[i].res())
        P.op("dve", lambda e, i=i: e.tensor_tensor(qd[i][:], q[i][:], Wi[i][:], ALU.mult), q[i].res() + Wi[i].res(), qd[i].res())
        tr_bf(K, kw[i][:], kw[i].res(), k[i][:], k[i].res(), scale_ap=(W[i][:, edge:edge + 1], W[i].res()))
        ps3 = psum(K)
        P.op("pe", lambda e, i=i, ps3=ps3: e.matmul(ps3[:, 0:257], Dm[i][:], va[i][:], start=True, stop=False), Dm[i].res() + va[i].res(), ps3.res())
        P.op("pe", lambda e, i=i, ps3=ps3: e.matmul(ps3[:, 0:257], qd[i][:], stb[:], start=False, stop=True), qd[i].res() + stb.res(), ps3.res())
        P.op("act", lambda e, i=i, ps3=ps3: e.activation(dn[i][:, 0:1], ps3[:, 256:257], AF.Abs), ps3.res(), dn[i].res())
        P.op("dve", lambda e, i=i, c=c: e.tensor_tensor(dn[i][:, 0:1], dn[i][:, 0:1], cols[:, 1, c:c + 1], ALU.max), dn[i].res() + cols.res(), dn[i].res())
        P.op("dve", lambda e, i=i: e.reciprocal(dn[i][:, 1:2], dn[i][:, 0:1]), dn[i].res(), dn[i].res())
        P.op("act", lambda e, i=i, ps3=ps3: e.activation(h[i][:], ps3[:, 0:256], AF.Copy, scale=dn[i][:, 1:2]), ps3.res() + dn[i].res(), h[i].res())
        P.dma(K.H[d][cs:cs + 128, 0:256], h[i][:], h[i].res(), K.H[d].res(0))
        ps4 = psum(K)
        P.op("pe", lambda e, i=i, ps4=ps4: e.matmul(ps4[:, 0:257], kw[i][:], va[i][:], start=True, stop=True), kw[i].res() + va[i].res(), ps4.res())
        P.op("dve", lambda e, i=i, ps4=ps4: e.scalar_tensor_tensor(st[:], st[:], Wi[i][:, edge:edge + 1], ps4[:, 0:257], ALU.mult, ALU.add), st.res() + Wi[i].res() + ps4.res(), st.res())
        P.op("act", lambda e: e.copy(stb[:], st[:]), st.res(), stb.res())
        yield


def b_post(K, l):
    P, cfg, spo = K.P, K.cfg, K.spo
    S, NCH, YP = cfg.S, cfg.NCH, cfg.YP
    ypv = K.yp.h.ap().rearrange("(q c p) t -> q p c t", c=6, p=128)
    segs = [(0, 256, 0), (256, 128, 1), (384, 128, 2), (512, 256, 3)]
    with P.phase():
        make_eps(K)
        nb = 2
        hf = [P.sb("po_hf%d" % i, [128, 768], F32) for i in range(nb)]
        hb = [P.sb("po_hb%d" % i, [128, 768], F32) for i in range(nb)]
        gt = [P.sb("po_g%d" % i, [128, 768], F32) for i in range(nb)]
        junk = P.sb("po_junk", [128, 768], F32)
        ss = [P.sb("po_ss%d" % i, [128, 4], F32) for i in range(nb)]
        t1 = [P.sb("po_t1%d" % i, [128, 768], F32) for i in range(nb)]
        yb = [P.sb("po_y%d" % i, [128, 768], F32) for i in range(nb)]
        yt = [P.sb("po_yt%d" % i, [128, 6, 128], BF16) for i in range(nb)]
        ng = P.sb("po_ng", [128, 768], F32)
        P.op("dve", lambda e: e.tensor_copy(ng[:, 0:256], K.spt[:, spo["ang"] + l * 256: spo["ang"] + (l + 1) * 256]), K.spt.res(), ng.res())
        P.op("dve", lambda e: e.tensor_copy(ng[:, 256:512], K.spt[:, spo["bng"] + l * 256: spo["bng"] + (l + 1) * 256]), K.spt.res(), ng.res())
        P.op("dve", lambda e: e.tensor_copy(ng[:, 512:768], K.spt[:, spo["cng"] + l * 256: spo["cng"] + (l + 1) * 256]), K.spt.res(), ng.res())
        for c in range(NCH):
            cs = c * 128
            i = c % nb
            P.dma(hf[i][:], K.H[0][cs:cs + 128, :], K.H[0].res(), hf[i].res())
            P.dma(hb[i][:], K.H[1][cs:cs + 128, :], K.H[1].res(), hb[i].res())
            P.dma(gt[i][:, 0:256], K.tm["ao"][cs:cs + 128, :], K.tm["ao"].res(), gt[i].res())
            P.dma(gt[i][:, 256:512], K.tm["bz"][cs:cs + 128, :], K.tm["bz"].res(), gt[i].res())
            P.dma(gt[i][:, 512:768], K.tm["cr"][cs:cs + 128, :], K.tm["cr"].res(), gt[i].res())
            P.op("dve", lambda e, i=i: e.tensor_tensor(hf[i][:], hf[i][:], hb[i][:], ALU.add), hf[i].res() + hb[i].res(), hf[i].res())
            if K.post_lvl < 2:
                continue
            for si, (c0, w, _) in enumerate(segs):
                P.op("act", lambda e, i=i, c0=c0, w=w: e.activation(junk[:, c0:c0 + w], hf[i][:, c0:c0 + w], AF.Square), hf[i].res(), junk.res())
                P.op("dve", lambda e, i=i, c0=c0, w=w, si=si: e.reduce_sum(ss[i][:, si:si + 1], junk[:, c0:c0 + w], mybir.AxisListType.X), junk.res(), ss[i].res())
            for si, (c0, w, _) in enumerate(segs):
                P.op("act", lambda e, i=i, si=si, w=w: e.activation(ss[i][:, si:si + 1], ss[i][:, si:si + 1], AF.Sqrt, bias=K.epst[:, 0:1], scale=1.0 / w),
                     ss[i].res() + K.epst.res(), ss[i].res())
            P.op("dve", lambda e, i=i: e.reciprocal(ss[i][:], ss[i][:]), ss[i].res(), ss[i].res())
            for si, (c0, w, _) in enumerate(segs):
                P.op("dve", lambda e, i=i, c0=c0, w=w, si=si: e.scalar_tensor_tensor(t1[i][:, c0:c0 + w], hf[i][:, c0:c0 + w], ss[i][:, si:si + 1], ng[:, c0:c0 + w], ALU.mult, ALU.mult),
                     hf[i].res() + ss[i].res() + ng.res(), t1[i].res())
            if K.post_lvl < 3:
                continue
            P.op("act", lambda e, i=i: e.activation(gt[i][:, 0:256], gt[i][:, 0:256], AF.Sigmoid), gt[i].res(), gt[i].res())
            P.op("act", lambda e, i=i: e.activation(gt[i][:, 256:768], gt[i][:, 256:768], AF.Silu), gt[i].res(), gt[i].res())
            P.op("dve", lambda e, i=i: e.tensor_tensor(yb[i][:], t1[i][:], gt[i][:], ALU.mult), t1[i].res() + gt[i].res(), yb[i].res())
            if K.post_lvl < 4:
                continue
            for g in range(2):
                ps = psum(K)
                for k_ in range(3):
                    ct = g * 3 + k_
                    P.op("pe", lambda e, ps=ps, k_=k_, ct=ct, i=i: e.transpose(ps[:, k_ * 128:(k_ + 1) * 128], yb[i][:, ct * 128:(ct + 1) * 128], K.cst[:, 0:128]),
                         yb[i].res() + K.cst.res(), ps.res())
                if g == 0:
                    P.op("act", lambda e, ps=ps, i=i, g=g: e.copy(yt[i][:, g * 3:(g + 1) * 3, :], ps[:, 0:384]), ps.res(), yt[i].res())
                else:
                    P.op("dve", lambda e, ps=ps, i=i, g=g: e.tensor_copy(yt[i][:, g * 3:(g + 1) * 3, :], ps[:, 0:384]), ps.res(), yt[i].res())
            tb, off = cs // YP, cs % YP
            if "post_nodma" not in K.debug:
                P.dma(ypv[tb][:, :, off:off + 128], yt[i][:], yt[i].res(), K.yp.res(tb))


def b_mixers(K, l):
    P = K.P
    which = K.which
    if "c" in which:
        gla_prep(K, l)
    if "a" in which:
        mlstm_prep(K, l)
    if "b" in which:
        gdn_prep(K, l)
    with P.phase():
        streams = []
        if "c" in which:
            streams += [gla_stream(K, l, 0), gla_stream(K, l, 1)]
        if "a" in which:
            streams += [mlstm_stream(K, l, 0), mlstm_stream(K, l, 1)]
        if "b" in which:
            streams += [gdn_stream(K, l, hh, d) for hh in range(2) for d in range(2)]
        live = list(streams)
        while live:
            nxt = []
            for g in live:
                try:
                    next(g)
                    nxt.append(g)
                except StopIteration:
                    pass
            live = nxt
    if K.post:
        b_post(K, l)


def gdn_prep(K, l):
    P, cfg, spo = K.P, K.cfg, K.spo
    S, NCH, PT = cfg.S, cfg.NCH, cfg.PT
    if not hasattr(K, "gq"):
        K.gq = [P.dram("gdn_q%d" % h, [128, S], BF16) for h in range(2)]
        K.gk = [P.dram("gdn_k%d" % h, [128, S], BF16) for h in range(2)]
        K.gktm = [P.dram("gdn_ktm%d" % h, [S, 128], BF16) for h in range(2)]
        K.gvtm = [P.dram("gdn_vtm%d" % h, [S, 128], F32) for h in range(2)]
        K.grow = [[P.dram("gdn_row%d%d" % (h, d), [2, S], F32) for d in range(2)] for h in range(2)]
        K.gcol = [[P.dram("gdn_col%d%d" % (h, d), [128, 5 * NCH], F32) for d in range(2)] for h in range(2)]
        mk = lambda nm: [[P.dram("gdn_%s%d%d" % (nm, h, d), [S, 128], BF16, nparts=NCH) for d in range(2)] for h in range(2)]
        K.gTT, K.gAQ, K.gQE, K.gKD = mk("tt"), mk("aq"), mk("qe"), mk("kd")
    ones = K.cst[:, 128:256]
    with P.phase():
        make_eps(K)
        xh = [P.sb("g1_xh%d" % i, [128, PT + 4], F32) for i in range(2)]
        acc = [P.sb("g1_acc%d" % i, [128, PT], F32) for i in range(2)]
        sq = [P.sb("g1_sq%d" % i, [128, PT], F32) for i in range(2)]
        rn = [P.sb("g1_rn%d" % i, [128, PT], F32) for i in range(2)]
        ob = [P.sb("g1_ob%d" % i, [128, PT], BF16) for i in range(2)]
        tmo = [P.sb("g1_tm%d" % i, [128, 128], BF16) for i in range(2)]
        tvo = [P.sb("g1_tv%d" % i, [128, 128], F32) for i in range(2)]
        n = 0
        for ti in range(S // PT):
            g0 = ti * PT
            lo, hi = max(0, g0 - 2), min(S, g0 + PT + 2)
            for hh in range(2):
                for part in range(3):
                    blk = part * 2 + hh
                    x_, a_, s_, r_, o_ = xh[n % 2], acc[n % 2], sq[n % 2], rn[n % 2], ob[n % 2]
                    n += 1
                    if g0 == 0:
                        P.op("dve", lambda e, x_=x_: e.memset(x_[:, 0:2], 0.0), (), x_.res())
                    if g0 + PT == S:
                        P.op("dve", lambda e, x_=x_: e.memset(x_[:, PT + 2:PT + 4], 0.0), (), x_.res())
                    P.dma(x_[:, lo - (g0 - 2): hi - (g0 - 2)], K.fm32[blk * 128:(blk + 1) * 128, lo:hi], K.fm32.res(blk), x_.res())
                    cw = spo["convw"] + (l * 6 + blk) * 5
                    P.op("dve", lambda e, x_=x_, a_=a_, cw=cw: e.tensor_scalar(a_[:], x_[:, 0:PT], K.spt[:, cw:cw + 1], None, ALU.mult), x_.res() + K.spt.res(), a_.res())
                    for tap in range(1, 5):
                        P.op("dve", lambda e, x_=x_, a_=a_, cw=cw, tap=tap: e.scalar_tensor_tensor(a_[:], x_[:, tap:tap + PT], K.spt[:, cw + tap:cw + tap + 1], a_[:], ALU.mult, ALU.add),
                             x_.res() + K.spt.res() + a_.res(), a_.res())
                    cb = spo["convb"] + l * 6 + blk
                    P.op("act", lambda e, a_=a_, cb=cb: e.activation(a_[:], a_[:], AF.Silu, bias=K.spt[:, cb:cb + 1]), a_.res() + K.spt.res(), a_.res())
                    if part < 2:
                        P.op("act", lambda e, a_=a_, s_=s_: e.activation(s_[:], a_[:], AF.Square), a_.res(), s_.res())
                        ps = psum(K)
                        P.op("pe", lambda e, ps=ps, s_=s_: e.matmul(ps[:, 0:PT], ones, s_[:], start=True, stop=True), s_.res() + K.cst.res(), ps.res())
                        P.op("act", lambda e, ps=ps, r_=r_: e.activation(r_[:], ps[:, 0:PT], AF.Sqrt, bias=K.epst[:, 0:1], scale=1.0), ps.res() + K.epst.res(), r_.res())
                        P.op("dve", lambda e, r_=r_: e.reciprocal(r_[:], r_[:]), r_.res(), r_.res())
                        sc = B_DK ** -0.5 if part == 0 else 1.0
                        P.op("dve", lambda e, a_=a_, r_=r_, o_=o_, sc=sc: e.scalar_tensor_tensor(o_[:], a_[:], sc, r_[:], ALU.mult, ALU.mult), a_.res() + r_.res(), o_.res())
                        dst = (K.gq if part == 0 else K.gk)[hh]
                        P.dma(dst[:, g0:g0 + PT], o_[:], o_.res(), dst.res())
                        if part == 1:
                            for sub in range(PT // 128):
                                t_ = tmo[sub % 2]
                                tr_bf(K, t_[:], t_.res(), o_[:, sub * 128:(sub + 1) * 128], o_.res())
                                P.dma(K.gktm[hh][g0 + sub * 128: g0 + (sub + 1) * 128, :], t_[:], t_.res(), K.gktm[hh].res())
                    else:
                        for sub in range(PT // 128):
                            t_ = tvo[sub % 2]
                            ps = psum(K)
                            P.op("pe", lambda e, ps=ps, a_=a_, sub=sub: e.transpose(ps[:, 0:128], a_[:, sub * 128:(sub + 1) * 128], K.cst[:, 0:128]), a_.res() + K.cst.res(), ps.res())
                            P.op("act", lambda e, ps=ps, t_=t_: e.copy(t_[:], ps[:, 0:128]), ps.res(), t_.res())
                            P.dma(K.gvtm[hh][g0 + sub * 128: g0 + (sub + 1) * 128, :], t_[:], t_.res(), K.gvtm[hh].res())
    with P.phase():
        for hh in range(2):
            for d in range(2):
                tg = "g2_%d%d" % (hh, d)
                xg = P.sb(tg + "xg", [NCH, 128], F32)
                xb = P.sb(tg + "xb", [NCH, 128], F32)
                gp = P.sb(tg + "gp", [NCH, 128], F32)
                m5 = P.sb(tg + "m5", [NCH, 5, 128], F32)
                row = P.sb(tg + "row", [NCH, 2, 128], F32)
                sc_ = P.sb(tg + "sc", [128, 4], F32)
                colt = P.sb(tg + "col", [128, 5, NCH], F32)
                rb = 8 * 128 + 4
                P.dma(xg[:], K.fm32[rb + d * 2 + hh: rb + d * 2 + hh + 1, :].rearrange("o (c t) -> (o c) t", t=128), K.fm32.res(8), xg.res())
                P.dma(xb[:], K.fm32[rb + 4 + d * 2 + hh: rb + 4 + d * 2 + hh + 1, :].rearrange("o (c t) -> (o c) t", t=128), K.fm32.res(8), xb.res())
                ca = spo["gdn_alog"] + l * 4 + d * 2 + hh
                cd = spo["gdn_dtb"] + l * 4 + d * 2 + hh
                P.op("act", lambda e, sc_=sc_, ca=ca: e.activation(sc_[:, 0:1], K.spt[:, ca:ca + 1], AF.Exp), K.spt.res(), sc_.res())
                P.op("act", lambda e, xg=xg, cd=cd: e.activation(xg[:], xg[:], AF.Exp, bias=K.spt[0:NCH, cd:cd + 1]), xg.res() + K.spt.res(), xg.res())
                P.op("act", lambda e, xg=xg: e.activation(xg[:], xg[:], AF.Ln, bias=K.cst[0:NCH, 128:129]), xg.res() + K.cst.res(), xg.res())
                P.op("dve", lambda e, xg=xg, sc_=sc_: e.tensor_scalar(xg[:], xg[:], sc_[0:NCH, 0:1], None, ALU.mult), xg.res() + sc_.res(), xg.res())
                vw = (lambda ap: ap) if d == 0 else (lambda ap: ap[:, ::-1])
                P.op("dve", lambda e, xg=xg, gp=gp, vw=vw: e.tensor_tensor_scan(vw(gp[:, :]), vw(xg[:, :]), vw(xg[:, :]), 0.0, ALU.add, ALU.bypass), xg.res(), gp.res())
                P.op("act", lambda e, xb=xb: e.activation(xb[:], xb[:], AF.Exp, scale=-1.0), xb.res(), xb.res())
                P.op("act", lambda e, xb=xb: e.activation(xb[:], xb[:], AF.Ln, bias=K.cst[0:NCH, 128:129]), xb.res() + K.cst.res(), xb.res())
                P.op("dve", lambda e, gp=gp, row=row: e.tensor_scalar(row[:, 0, :], gp[:], -1.0, None, ALU.mult), gp.res(), row.res())
                P.op("dve", lambda e, gp=gp, xb=xb, row=row: e.scalar_tensor_tensor(row[:, 1, :], gp[:], -1.0, xb[:], ALU.mult, ALU.subtract), gp.res() + xb.res(), row.res())
                for r_ in range(2):
                    P.dma(K.grow[hh][d][r_:r_ + 1, :].rearrange("o (c t) -> (o c) t", t=128), row[:, r_, :], row.res(), K.grow[hh][d].res())
                edge = 127 if d == 0 else 0
                P.op("dve", lambda e, gp=gp, m5=m5: e.tensor_copy(m5[:, 0, :], gp[:]), gp.res(), m5.res())
                P.op("act", lambda e, gp=gp, m5=m5: e.activation(m5[:, 1, :], gp[:], AF.Exp, scale=-1.0), gp.res(), m5.res())
                P.op("dve", lambda e, m5=m5: e.tensor_scalar(m5[:, 1, :], m5[:, 1, :], -1.0, None, ALU.mult), m5.res(), m5.res())
                P.op("act", lambda e, xb=xb, m5=m5: e.activation(m5[:, 2, :], xb[:], AF.Exp, scale=-1.0), xb.res(), m5.res())
                P.op("dve", lambda e, gp=gp, sc_=sc_, edge=edge: e.tensor_scalar(sc_[0:NCH, 1:2], gp[:, edge:edge + 1], -1.0, None, ALU.mult), gp.res(), sc_.res())
                P.op("act", lambda e, gp=gp, m5=m5, sc_=sc_: e.activation(m5[:, 3, :], gp[:], AF.Exp, bias=sc_[0:NCH, 1:2]), gp.res() + sc_.res(), m5.res())
                P.op("act", lambda e, gp=gp, m5=m5, edge=edge: e.activation(m5[:, 4, :], gp[:, edge:edge + 1].to_broadcast([NCH, 128]), AF.Exp, scale=-1.0), gp.res(), m5.res())
                for q_ in range(5):
                    ps = psum(K)
                    P.op("pe", lambda e, ps=ps, m5=m5, q_=q_: e.transpose(ps[:, 0:NCH], m5[:, q_, :], K.cst[0:NCH, 0:NCH]), m5.res() + K.cst.res(), ps.res())
                    P.op("act", lambda e, ps=ps, colt=colt, q_=q_: e.copy(colt[:, q_, :], ps[:, 0:NCH]), ps.res(), colt.res())
                P.dma(K.gcol[hh][d][:, :], colt[:].rearrange("p a c -> p (a c)"), colt.res(), K.gcol[hh][d].res())
    with P.phase():
        streams = [gdn_solve_stream(K, l, hh, d) for hh in range(2) for d in range(2)]
        live = list(streams)
        while live:
            nxt = []
            for g in live:
                try:
                    next(g)
                    nxt.append(g)
                except StopIteration:
                    pass
            live = nxt


def gdn_solve_stream(K, l, hh, d):
    P, cfg = K.P, K.cfg
    S, NCH = cfg.S, cfg.NCH
    tg = "gs%d%d" % (hh, d)
    colt = P.sb(tg + "col", [128, 5, NCH], F32)
    P.dma(colt[:].rearrange("p a c -> p (a c)"), K.gcol[hh][d][:, :], K.gcol[hh][d].res(), colt.res())
    nb = 2
    kT = [P.sb(tg + "kT%d" % i, [128, 128], BF16) for i in range(nb)]
    qT = [P.sb(tg + "qT%d" % i, [128, 128], BF16) for i in range(nb)]
    ktm = [P.sb(tg + "ktm%d" % i, [128, 128], BF16) for i in range(nb)]
    rows = [P.sb(tg + "rw%d" % i, [1, 2, 128], F32) for i in range(nb)]
    EA = P.sb(tg + "EA", [128, 128], F32)
    EQ = P.sb(tg + "EQ", [128, 128], F32)
    EG = P.sb(tg + "EG", [128, 128], F32)
    Nm = P.sb(tg + "N", [128, 128], F32)
    Pm = [P.sb(tg + "P%d" % i, [128, 128], F32) for i in range(2)]
    Qm = [P.sb(tg + "Q%d" % i, [128, 128], F32) for i in range(2)]
    Xm = [P.sb(tg + "X%d" % i, [128, 128], F32) for i in range(2)]
    obuf = [P.sb(tg + "o%d" % i, [128, 4, 128], BF16) for i in range(nb)]
    ones_row = K.cst[0:1, 128:256]
    ident = K.cst[:, 0:128]
    neg_incl = K.cst[:, (2 + 2 * d) * 128:(3 + 2 * d) * 128]
    neg_strict = K.cst[:, (3 + 2 * d) * 128:(4 + 2 * d) * 128]
    n = 0
    for c in range(NCH):
        cs = c * 128
        i = n % nb
        n += 1
        P.dma(kT[i][:], K.gk[hh][:, cs:cs + 128], K.gk[hh].res(), kT[i].res())
        P.dma(qT[i][:], K.gq[hh][:, cs:cs + 128], K.gq[hh].res(), qT[i].res())
        P.dma(ktm[i][:], K.gktm[hh][cs:cs + 128, :], K.gktm[hh].res(), ktm[i].res())
        P.dma(rows[i][:], K.grow[hh][d][:, cs:cs + 128].rearrange("(o r) t -> o r t", o=1), K.grow[hh][d].res(), rows[i].res())
        gcol = colt[:, 0, c:c + 1]
        psA = psum(K)
        P.op("pe", lambda e, i=i, psA=psA: e.matmul(psA[:, 0:128], ones_row, rows[i][:, 1, :], start=True, stop=False), rows[i].res() + K.cst.res(), psA.res())
        P.op("pe", lambda e, psA=psA: e.matmul(psA[:, 0:128], ident, neg_strict, start=False, stop=True), K.cst.res(), psA.res())
        P.op("act", lambda e, psA=psA, gcol=gcol: e.activation(EA[:], psA[:, 0:128], AF.Exp, bias=gcol), psA.res() + colt.res(), EA.res())
        psK = psum(K)
        P.op("pe", lambda e, i=i, psK=psK: e.matmul(psK[:, 0:128], kT[i][:], kT[i][:], start=True, stop=True), kT[i].res(), psK.res())
        P.op("dve", lambda e, psK=psK: e.tensor_tensor(Nm[:], psK[:, 0:128], EA[:], ALU.mult), psK.res() + EA.res(), Nm.res())
        psQ = psum(K)
        P.op("pe", lambda e, i=i, psQ=psQ: e.matmul(psQ[:, 0:128], ones_row, rows[i][:, 0, :], start=True, stop=False), rows[i].res() + K.cst.res(), psQ.res())
        P.op("pe", lambda e, psQ=psQ: e.matmul(psQ[:, 0:128], ident, neg_incl, start=False, stop=True), K.cst.res(), psQ.res())
        P.op("act", lambda e, psQ=psQ, gcol=gcol: e.activation(EQ[:], psQ[:, 0:128], AF.Exp, bias=gcol), psQ.res() + colt.res(), EQ.res())
        psKQ = psum(K)
        P.op("pe", lambda e, i=i, psKQ=psKQ: e.matmul(psKQ[:, 0:128], kT[i][:], qT[i][:], start=True, stop=True), kT[i].res() + qT[i].res(), psKQ.res())
        P.op("dve", lambda e, i=i, psKQ=psKQ: e.tensor_tensor(obuf[i][:, 1, :], psKQ[:, 0:128], EQ[:], ALU.mult), psKQ.res() + EQ.res(), obuf[i].res())
        psG = psum(K)
        P.op("pe", lambda e, i=i, psG=psG: e.matmul(psG[:, 0:128], ones_row, rows[i][:, 0, :], start=True, stop=True), rows[i].res() + K.cst.res(), psG.res())
        P.op("act", lambda e, psG=psG: e.activation(EG[:], psG[:, 0:128], AF.Exp), psG.res(), EG.res())
        P.op("dve", lambda e, i=i: e.tensor_tensor(obuf[i][:, 2, :], qT[i][:], EG[:], ALU.mult), qT[i].res() + EG.res(), obuf[i].res())
        P.op("dve", lambda e, i=i, c=c: e.tensor_scalar(obuf[i][:, 3, :], ktm[i][:], colt[:, 3, c:c + 1], None, ALU.mult), ktm[i].res() + colt.res(), obuf[i].res())
        ps = psum(K)
        P.op("pe", lambda e, ps=ps: e.transpose(ps[:, 0:128], Nm[:], ident), Nm.res() + K.cst.res(), ps.res())
        P.op("act", lambda e, ps=ps: e.copy(Qm[0][:], ps[:, 0:128]), ps.res(), Qm[0].res())
        P.op("dve", lambda e: e.tensor_tensor(Xm[0][:], ident, Nm[:], ALU.subtract), Nm.res() + K.cst.res(), Xm[0].res())
        Pc, Qc, Xc = Nm, Qm[0], Xm[0]
        for k in range(1, 7):
            Qn = Qm[k % 2]
            psq = psum(K)
            P.op("pe", lambda e, psq=psq, Pc=Pc, Qc=Qc: e.matmul(psq[:, 0:128], Pc[:], Qc[:], start=True, stop=True), Pc.res() + Qc.res(), psq.res())
            if k < 6:
                Pn = Pm[k % 2]
                psp = psum(K)
                P.op("pe", lambda e, psp=psp, Pc=Pc, Qc=Qc: e.matmul(psp[:, 0:128], Qc[:], Pc[:], start=True, stop=True), Pc.res() + Qc.res(), psp.res())
            P.op("act", lambda e, psq=psq, Qn=Qn: e.copy(Qn[:], psq[:, 0:128]), psq.res(), Qn.res())
            if k < 6:
                P.op("act", lambda e, psp=psp, Pn=Pn: e.copy(Pn[:], psp[:, 0:128]), psp.res(), Pn.res())
            Xn = Xm[k % 2]
            psx = psum(K)
            P.op("pe", lambda e, psx=psx, Qn=Qn, Xc=Xc: e.matmul(psx[:, 0:128], Qn[:], Xc[:], start=True, stop=True), Qn.res() + Xc.res(), psx.res())
            P.op("dve", lambda e, psx=psx, Xn=Xn, Xc=Xc: e.tensor_tensor(Xn[:], psx[:, 0:128], Xc[:], ALU.add), psx.res() + Xc.res(), Xn.res())
            Qc, Xc = Qn, Xn
            if k < 6:
                Pc = Pn
        P.op("dve", lambda e, i=i, Xc=Xc, c=c: e.tensor_scalar(obuf[i][:, 0, :], Xc[:], colt[:, 2, c:c + 1], None, ALU.mult), Xc.res() + colt.res(), obuf[i].res())
        for q_, dst in enumerate((K.gTT, K.gAQ, K.gQE, K.gKD)):
            P.dma(dst[hh][d][cs:cs + 128, :], obuf[i][:, q_, :], obuf[i].res(), dst[hh][d].res(c))
        yield


def gdn_stream(K, l, hh, d):
    P, cfg = K.P, K.cfg
    S, NCH = cfg.S, cfg.NCH
    tg = "gr%d%d" % (hh, d)
    colt = P.sb(tg + "col", [128, 5, NCH], F32)
    P.dma(colt[:].rearrange("p a c -> p (a c)"), K.gcol[hh][d][:, :], K.gcol[hh][d].res(), colt.res())
    st = P.sb(tg + "st", [128, 128], F32)
    stb = P.sb(tg + "stb", [128, 128], BF16)
    P.op("dve", lambda e: e.memset(st[:], 0.0), (), st.res())
    P.op("dve", lambda e: e.memset(stb[:], 0.0), (), stb.res())
    nb = 2
    kT = [P.sb(tg + "kT%d" % i, [128, 128], BF16) for i in range(nb)]
    mats = [P.sb(tg + "m%d" % i, [128, 4, 128], BF16) for i in range(nb)]
    v = [P.sb(tg + "v%d" % i, [128, 128], F32) for i in range(nb)]
    Rb = [P.sb(tg + "R%d" % i, [128, 128], BF16) for i in range(nb)]
    Ub = [P.sb(tg + "U%d" % i, [128, 128], BF16) for i in range(nb)]
    o = [P.sb(tg + "o%d" % i, [128, 128], F32) for i in range(nb)]
    order = range(NCH) if d == 0 else range(NCH - 1, -1, -1)
    hc0 = 256 + hh * 128
    n = 0
    for c in order:
        cs = c * 128
        i = n % nb
        n += 1
        P.dma(kT[i][:], K.gk[hh][:, cs:cs + 128], K.gk[hh].res(), kT[i].res())
        for q_, src in enumerate((K.gTT, K.gAQ, K.gQE, K.gKD)):
            P.dma(mats[i][:, q_, :], src[hh][d][cs:cs + 128, :], src[hh][d].res(c), mats[i].res())
        P.dma(v[i][:], K.gvtm[hh][cs:cs + 128, :], K.gvtm[hh].res(), v[i].res())
        ps1 = psum(K)
        P.op("pe", lambda e, i=i, ps1=ps1: e.matmul(ps1[:, 0:128], kT[i][:], stb[:], start=True, stop=True), kT[i].res() + stb.res(), ps1.res())
        P.op("dve", lambda e, i=i, ps1=ps1, c=c: e.scalar_tensor_tensor(Rb[i][:], ps1[:, 0:128], colt[:, 1, c:c + 1], v[i][:], ALU.mult, ALU.add), ps1.res() + colt.res() + v[i].res(), Rb[i].res())
        ps2 = psum(K)
        P.op("pe", lambda e, i=i, ps2=ps2: e.matmul(ps2[:, 0:128], mats[i][:, 0, :], Rb[i][:], start=True, stop=True), mats[i].res() + Rb[i].res(), ps2.res())
        P.op("act", lambda e, i=i, ps2=ps2: e.copy(Ub[i][:], ps2[:, 0:128]), ps2.res(), Ub[i].res())
        ps3 = psum(K)
        P.op("pe", lambda e, i=i, ps3=ps3: e.matmul(ps3[:, 0:128], mats[i][:, 1, :], Ub[i][:], start=True, stop=False), mats[i].res() + Ub[i].res(), ps3.res())
        P.op("pe", lambda e, i=i, ps3=ps3: e.matmul(ps3[:, 0:128], mats[i][:, 2, :], stb[:], start=False, stop=True), mats[i].res() + stb.res(), ps3.res())
        P.op("act", lambda e, i=i, ps3=ps3: e.copy(o[i][:], ps3[:, 0:128]), ps3.res(), o[i].res())
        P.dma(K.H[d][cs:cs + 128, hc0:hc0 + 128], o[i][:], o[i].res(), K.H[d].res(1 + hh))
        ps4 = psum(K)
        P.op("pe", lambda e, i=i, ps4=ps4: e.matmul(ps4[:, 0:128], mats[i][:, 3, :], Ub[i][:], start=True, stop=True), mats[i].res() + Ub[i].res(), ps4.res())
        P.op("dve", lambda e, ps4=ps4, c=c: e.scalar_tensor_tensor(st[:], st[:], colt[:, 4, c:c + 1], ps4[:, 0:128], ALU.mult, ALU.add), st.res() + colt.res() + ps4.res(), st.res())
        P.op("act", lambda e: e.copy(stb[:], st[:]), st.res(), stb.res())
        yield
```

```python
import numpy as np
from contextlib import ExitStack
import concourse.bass as bass
import concourse.mybir as mybir
from concourse.bass_utils import run_bass_kernel_spmd

F32 = mybir.dt.float32
BF16 = mybir.dt.bfloat16
ALU = mybir.AluOpType
AF = mybir.ActivationFunctionType

ENGS = ("pe", "dve", "act", "pool", "sp")
NDSEM = 6
NEG = -30000.0
EPS = 1e-6
NCORES = 8


class Res:
    __slots__ = ("w", "r")

    def __init__(self):
        self.w = None
        self.r = {}


class T:
    def __init__(self, h, nparts=1):
        self.h = h
        self.parts = [Res() for _ in range(nparts)]

    def __getitem__(self, idx):
        return self.h[idx]

    def ap(self):
        return self.h.ap() if hasattr(self.h, "ap") else self.h[:]

    def res(self, i=None):
        if i is None:
            return self.parts
        if isinstance(i, (list, tuple, range)):
            return [self.parts[j] for j in i]
        return [self.parts[i]]


class Prog:
    def __init__(self, nc, es):
        self.nc = nc
        self.es0 = es
        self.es = es
        self.ops = {e: [] for e in ENGS}
        self.cnt = {e: 0 for e in ENGS}
        self.sem = {e: es.enter_context(nc.semaphore("s_" + e)) for e in ENGS}
        self.dsem = {}
        self.dcnt = {}
        self.drr = {}
        for q in ("sp", "pool", "cc", "act"):
            self.dsem[q] = [es.enter_context(nc.semaphore("d_%s%d" % (q, i))) for i in range(NDSEM)]
            self.dcnt[q] = [0] * NDSEM
            self.drr[q] = 0
        self.seen = {e: {} for e in ENGS}
        self.psum_rr = 0
        self.nins = 0

    def sb(self, name, shape, dtype, nparts=1):
        self.nins += 0
        self._uid = getattr(self, "_uid", 0) + 1
        return T(self.es.enter_context(self.nc.sbuf_tensor("%s_u%d" % (name, self._uid), list(shape), dtype)), nparts)

    def dram(self, name, shape, dtype, nparts=1):
        return T(self.nc.dram_tensor(name, list(shape), dtype, kind="Internal"), nparts)

    def _semh(self, key):
        if isinstance(key, str):
            return self.sem[key]
        return self.dsem[key[0]][key[1]]

    def _deps(self, eng, reads, writes):
        need = {}
        for r in reads:
            if r.w is not None and need.get(r.w[0], 0) < r.w[1]:
                need[r.w[0]] = r.w[1]
        for w in writes:
            if w.w is not None and need.get(w.w[0], 0) < w.w[1]:
                need[w.w[0]] = w.w[1]
            for k, v in w.r.items():
                if need.get(k, 0) < v:
                    need[k] = v
        waits = []
        seen = self.seen[eng]
        for k, v in need.items():
            if seen.get(k, 0) < v:
                seen[k] = v
                waits.append((k, v))
        return waits

    def _mark(self, key, v, reads, writes):
        for r in reads:
            r.r[key] = v
        for w in writes:
            w.w = (key, v)
            w.r = {}

    def op(self, eng, fn, reads=(), writes=()):
        waits = self._deps(eng, reads, writes)
        if eng == "pe":
            waits = [(k, v) for (k, v) in waits if k != "pe"]
        self.cnt[eng] += 1
        v = self.cnt[eng]
        self.ops[eng].append((waits, fn, (eng, 1)))
        self._mark(eng, v, reads, writes)
        self.nins += 1

    def _async(self, q, cls, fn, reads, writes, inc):
        i = self.drr[cls]
        self.drr[cls] = (i + 1) % NDSEM
        key = (cls, i)
        waits = self._deps(q, reads, writes)
        prev = self.dcnt[cls][i]
        if prev and self.seen[q].get(key, 0) < prev:
            self.seen[q][key] = prev
            waits.append((key, prev))
        self.dcnt[cls][i] += inc
        v = self.dcnt[cls][i]
        self.ops[q].append((waits, fn, (key, inc)))
        self._mark(key, v, reads, writes)
        self.nins += 1

    def dma(self, out_ap, in_ap, reads=(), writes=(), q="sp", slow=False):
        cls = q if q in ("sp", "act") else "pool"
        if slow:
            self._async(q, cls, lambda e, o=out_ap, a=in_ap: e.dma_start(out=o, in_=a, allow_slow_non_contiguous=True), reads, writes, 16)
        else:
            self._async(q, cls, lambda e, o=out_ap, a=in_ap: e.dma_start(out=o, in_=a), reads, writes, 16)

    def coll(self, kind, out_ap, in_ap, groups, reads=(), writes=()):
        self._async("pool", "cc",
                    lambda e, o=out_ap, a=in_ap: e.collective_compute(kind, ALU.bypass, replica_groups=groups,
                                                                      ins=[a], outs=[o]),
                    reads, writes, 1)

    def barrier(self, full=False):
        tgt = {e: self.cnt[e] for e in ENGS if self.cnt[e]}
        for cls in (("sp", "act", "pool", "cc") if full else ("sp", "act")):
            for i in range(NDSEM):
                if self.dcnt[cls][i]:
                    tgt[(cls, i)] = self.dcnt[cls][i]
        for e in ENGS:
            waits = []
            for k, v in tgt.items():
                if self.seen[e].get(k, 0) < v:
                    self.seen[e][k] = v
                    waits.append((k, v))
            self.ops[e].append((waits, None, None))

    def flush(self):
        nc = self.nc
        ops = self.ops
        self.ops = {e: [] for e in ENGS}
        with nc.Block() as block:
            def run(e, h):
                for waits, fn, inc in ops[e]:
                    for k, v in waits:
                        h.wait_ge(self._semh(k), v)
                    if fn is not None:
                        fn(h).then_inc(self._semh(inc[0]), inc[1])

            block.tensor(lambda h: run("pe", h))
            block.vector(lambda h: run("dve", h))
            block.scalar(lambda h: run("act", h))
            block.gpsimd(lambda h: run("pool", h))
            block.sync(lambda h: run("sp", h))

    class _Phase:
        def __init__(self, P):
            self.P = P

        def __enter__(self):
            self.es = ExitStack()
            self.es.__enter__()
            self.P.es = self.es
            return self.P

        def __exit__(self, *a):
            self.P.barrier()
            self.P.flush()
            self.P.es = self.P.es0
            return self.es.__exit__(*a)

    def phase(self):
        return Prog._Phase(self)


A_HEADS, A_DK, A_DV = 4, 128, 256
B_HEADS, B_DK, B_DV = 8, 128, 128
C_HEADS, C_DK, C_DV = 4, 128, 256
GLA_RANK, GLA_TAU, GATE_RANK, CONV_K = 16, 16.0, 256, 5
A_QK, A_V = 512, 1024
B_QK, B_V, B_QKV = 1024, 1024, 3072
C_QK, C_V = 512, 1024
PROJ_SIZES = (A_QK, A_QK, A_V, A_V, 16, B_QKV, B_V, 32, C_QK, C_QK, C_V, C_V, 32, GATE_RANK)
OFF = np.concatenate([[0], np.cumsum(PROJ_SIZES)]).tolist()
(O_AQ, O_AK, O_AV, O_AO, O_AGT, O_BQKV, O_BZ, O_BGT, O_CQ, O_CK, O_CV, O_CR, O_CLR, O_GH) = OFF[:14]
NFM = 11
NTM = 5
NCOLS = NFM * 128 + NTM * 256 + 256


def my_cols(j):
    r = lambda a, n: list(range(a, a + n))
    cols = []
    cols += r(O_AQ + j * 128, 128) + r(O_AK + j * 128, 128)
    for part in range(3):
        for hh in (2 * j, 2 * j + 1):
            cols += r(O_BQKV + part * 1024 + hh * 128, 128)
    cols += r(O_CQ + j * 128, 128) + r(O_CK + j * 128, 128)
    small = [-1] * 128
    for g in range(4):
        small[g] = O_AGT + g * 4 + j
    k = 4
    for g in range(4):
        for hh in (2 * j, 2 * j + 1):
            small[k] = O_BGT + g * 8 + hh
            k += 1
    for i in range(16):
        small[32 + i] = O_CLR + i
        small[64 + i] = O_CLR + 16 + i
    cols += small
    cols += r(O_AV + j * 256, 256) + r(O_AO + j * 256, 256)
    cols += r(O_BZ + 2 * j * 128, 256)
    cols += r(O_CV + j * 256, 256) + r(O_CR + j * 256, 256)
    cols += r(O_GH, 256)
    assert len(cols) == NCOLS
    return cols


def block_widths():
    return [128] * NFM + [256] * NTM + [128, 128]


def tile_major(W):
    K, N = W.shape
    return np.ascontiguousarray(W.reshape(K // 128, 128, N // 128, 128).transpose(2, 1, 0, 3)).reshape(-1)


def piece_plan(E):
    P = max(1, -(-E // (8 * 131072)))
    while E % (8 * P) != 0:
        P += 1
    pe = E // (8 * P)
    b = 1
    for cand in (2048, 1024, 512, 256, 128, 64, 661, 1):
        if pe % cand == 0:
            b = cand
            break
    return P, pe, pe // b, b


class Cfg:
    def __init__(self, D, FF, S, L):
        self.D, self.FF, self.S, self.L = D, FF, S, L
        self.KT = D // 128
        self.FT = FF // 128
        self.TL = S // 4
        self.NCH = S // 128
        self.TT = min(256, self.TL)
        self.PT = 256
        self.PTP = min(512, self.TL)
        self.YP = min(512, S)
        self.big = [("wa", 1024, D), ("wb", 1024, D), ("wc", 1024, D), ("wg", 768, D),
                    ("wo", D, D), ("w1", D, FF), ("w2", FF, D)]


def sp_layout(cfg):
    L, KT = cfg.L, cfg.KT
    o = {}
    n = 0
    for name, w in [("g1", L * KT), ("g2", L * KT), ("gf", KT), ("bm", L * 3 * KT), ("convw", L * 6 * 5),
                    ("convb", L * 6), ("agb", L * 4), ("ang", L * 256), ("gdn_alog", L * 4), ("gdn_dtb", L * 4),
                    ("bng", L * 256), ("glaw", L * 2 * 128), ("glab", L * 2), ("cng", L * 256)]:
        o[name] = n
        n += w
    o["_n"] = n
    return o


def make_consts():
    c = np.zeros((128, 8, 128), np.float32)
    s = np.arange(128)[:, None]
    t = np.arange(128)[None, :]
    c[:, 0] = np.eye(128)
    c[:, 1] = 1.0
    c[:, 2] = np.where(s <= t, 0, NEG)
    c[:, 3] = np.where(s < t, 0, NEG)
    c[:, 4] = np.where(s >= t, 0, NEG)
    c[:, 5] = np.where(s > t, 0, NEG)
    c[:, 6] = (s <= t)
    c[:, 7] = (s >= t)
    return c.reshape(128, 1024)


def prep_inputs(inputs, cfg):
    D, FF, S, L, KT = cfg.D, cfg.FF, cfg.S, cfg.L, cfg.KT
    f = lambda k: np.asarray(inputs[k], dtype=np.float32)
    x = f("x")
    spo = sp_layout(cfg)
    bigsrc = {"wa": f("w_branch_a"), "wb": f("w_branch_b"), "wc": f("w_branch_c"),
              "wg": f("w_merge_gate").reshape(L, 768, D), "wo": f("w_out"), "w1": f("w_ff1"), "w2": f("w_ff2")}
    w_in = f("w_in")
    consts = make_consts()
    shards = {}
    for name, K_, N_ in cfg.big:
        P_, pe, a, b = piece_plan(K_ * N_)
        for l in range(L):
            if name == "wg":
                flat = np.concatenate([tile_major(bigsrc[name][l][jb * 256:(jb + 1) * 256]) for jb in range(3)])
            else:
                flat = tile_major(bigsrc[name][l])
            shards[(name, l)] = flat.reshape(P_, 8, pe)
    in_maps = []
    bw = block_widths()
    for c in range(NCORES):
        b_, j = c // 4, c % 4
        m = {}
        m["x_own"] = np.ascontiguousarray(x[b_, j * cfg.TL:(j + 1) * cfg.TL, :])
        m["consts"] = consts
        rm = np.zeros((128, 4), np.float32)
        rm[:, j] = 1.0
        m["rmask"] = rm
        cols = np.array(my_cols(j))
        for l in range(L):
            wsel = np.where(cols[None, :] >= 0, w_in[l][:, np.maximum(cols, 0)], 0.0).astype(np.float32)
            blocks = []
            c0 = 0
            for w_ in bw:
                blk = wsel[:, c0:c0 + w_]
                blocks.append(np.ascontiguousarray(blk.reshape(KT, 128, w_).transpose(1, 0, 2)).reshape(-1))
                c0 += w_
            m["win_%d" % l] = np.concatenate(blocks).reshape(D, NCOLS)
            for name, K_, N_ in cfg.big:
                P_, pe, a, bb = piece_plan(K_ * N_)
                m["%s_%d" % (name, l)] = np.ascontiguousarray(shards[(name, l)][:, c, :]).reshape(P_ * a, bb)
        sp = np.zeros((128, spo["_n"]), np.float32)
        tm = lambda v: v.reshape(KT, 128).T
        for l in range(L):
            sp[:, spo["g1"] + l * KT: spo["g1"] + (l + 1) * KT] = tm(f("norm1_g")[l])
            sp[:, spo["g2"] + l * KT: spo["g2"] + (l + 1) * KT] = tm(f("norm2_g")[l])
            for jb in range(3):
                o = spo["bm"] + (l * 3 + jb) * KT
                sp[:, o:o + KT] = tm(f("b_merge_gate")[l, jb])
            for blk in range(6):
                part, hh = blk // 2, 2 * j + blk % 2
                ch = part * 1024 + hh * 128
                o = spo["convw"] + (l * 6 + blk) * 5
                sp[:, o:o + 5] = f("conv_w")[l][:, ch:ch + 128].T
                sp[:, spo["convb"] + l * 6 + blk] = f("conv_b")[l][ch:ch + 128]
            sp[:, spo["agb"] + l * 4: spo["agb"] + l * 4 + 4] = f("mlstm_gate_b")[l][:, j][None, :]
            sp[:, spo["ang"] + l * 256: spo["ang"] + (l + 1) * 256] = f("mlstm_norm_g")[l][j * 256:(j + 1) * 256][None, :]
            for d_ in range(2):
                for hh in range(2):
                    sp[:, spo["gdn_alog"] + l * 4 + d_ * 2 + hh] = f("gdn_a_log")[l, d_, 2 * j + hh]
                    sp[:, spo["gdn_dtb"] + l * 4 + d_ * 2 + hh] = f("gdn_dt_bias")[l, d_, 2 * j + hh]
            sp[:, spo["bng"] + l * 256: spo["bng"] + (l + 1) * 256] = f("gdn_norm_g")[l][2 * j * 128:(2 * j + 2) * 128][None, :]
            for d_ in range(2):
                o = spo["glaw"] + (l * 2 + d_) * 128
                sp[0:16, o:o + 128] = f("gla_w_gate")[l, d_][:, j * 128:(j + 1) * 128]
                sp[:, spo["glab"] + l * 2 + d_] = f("gla_b_gate")[l, d_][j * 128:(j + 1) * 128]
            sp[:, spo["cng"] + l * 256: spo["cng"] + (l + 1) * 256] = f("gla_norm_g")[l][j * 256:(j + 1) * 256][None, :]
        sp[:, spo["gf"]: spo["gf"] + KT] = tm(f("final_g"))
        m["sp"] = sp
        in_maps.append(m)
    return in_maps


class Ctx:
    pass


def flat_blocks(t, nblk_elems):
    a = t.h.ap()
    fl = a.rearrange("r b -> (r b)")
    return fl.rearrange("(n p f) -> n p f", p=128, f=nblk_elems // 128)


def build(cfg, debug=()):
    D, FF, S, L, KT, FT, TL, NCH, TT, PT = cfg.D, cfg.FF, cfg.S, cfg.L, cfg.KT, cfg.FT, cfg.TL, cfg.NCH, cfg.TT, cfg.PT
    nc = bass.Bass("TRN2", target_bir_lowering=False)
    K = Ctx()
    K.cfg, K.nc = cfg, nc
    spo = sp_layout(cfg)
    K.spo = spo
    ext = lambda name, shape, dt=F32: T(nc.dram_tensor(name, list(shape), dt, kind="ExternalInput"))
    K.x_own = ext("x_own", [TL, D])
    K.consts_d = ext("consts", [128, 1024])
    K.rmask_d = ext("rmask", [128, 4])
    K.sp_d = ext("sp", [128, spo["_n"]])
    K.win_d = [ext("win_%d" % l, [D, NCOLS]) for l in range(L)]
    K.big_d = {}
    for name, K_, N_ in cfg.big:
        P_, pe, a, b = piece_plan(K_ * N_)
        for l in range(L):
            K.big_d[(name, l)] = ext("%s_%d" % (name, l), [P_ * a, b])
    K.out = T(nc.dram_tensor("out", [TL, D], F32, kind="ExternalOutput"))
    K.stub_y = ext("stub_y", [S // cfg.YP * 768, cfg.YP], BF16) if "stub_y" in debug else None
    K.debug = debug
    K.which = "abc"
    K.post = True
    K.post_lvl = 9
    for d_ in debug:
        if d_.startswith("postlvl="):
            K.post_lvl = int(d_[8:])
    for d_ in debug:
        if d_.startswith("which="):
            K.which = d_[6:]
        if d_ == "nopost":
            K.post = False
    K.dbg_out = []
    with ExitStack() as es:
        P = Prog(nc, es)
        K.P = P
        K.ps = [T(es.enter_context(nc.psum_tensor("ps%d" % i, [128, 512], F32))) for i in range(7)]
        K.psb = T(es.enter_context(nc.psum_tensor("psb", [128, 1024], BF16)), nparts=4)
        K.ps_rr = 0
        K.psb_rr = 0
        K.cst = P.sb("cst", [128, 1024], F32)
        K.spt = P.sb("spt", [128, spo["_n"]], F32)
        K.rmask = P.sb("rmaskt", [128, 4], F32)
        K.identb = P.sb("identb", [128, 128], BF16)
        P.dma(K.cst[:], K.consts_d[:], K.consts_d.res(), K.cst.res())
        P.dma(K.spt[:], K.sp_d[:], K.sp_d.res(), K.spt.res())
        P.dma(K.rmask[:], K.rmask_d[:], K.rmask_d.res(), K.rmask.res())
        P.op("dve", lambda e: e.tensor_copy(K.identb[:], K.cst[:, 0:128]), K.cst.res(), K.identb.res())
        K.xres = P.dram("xres", [D, TL], F32, nparts=TL // TT)
        K.xnp = P.dram("xnp", [TL // 128 * 128, KT * 128], BF16, nparts=TL // 128)
        K.xng = P.dram("xng", [TL // 128 * 4 * 128, KT * 128], BF16, nparts=TL // 128)
        K.ghT = P.dram("ghT", [256, TL], BF16)
        K.wfull = {}
        K.winb = []
        for l in range(L):
            K.winb.append(P.dram("winb_%d" % l, [D, NCOLS], BF16))
            for name, K_, N_ in cfg.big:
                P_, pe, a, b = piece_plan(K_ * N_)
                K.wfull[(name, l)] = P.dram("wf_%s_%d" % (name, l), [P_ * 8 * a, b], BF16)
                K.wfull[(name, l)].tmp = P.dram("wsb_%s_%d" % (name, l), [P_ * a, b], BF16)
                K.wfull[(name, l)].half = P.dram("wsh_%s_%d" % (name, l), [P_ * 4 * a, b], BF16, nparts=P_)
        alloc_mixer_dram(K)
        if "dumpH" in debug:
            K.dbg_out += [("H0", K.H[0]), ("H1", K.H[1]), ("fm32", K.fm32), ("fm16", K.fm16)]
        if "dumpY" in debug:
            K.dbg_out += [("yp", K.yp)]
        cast_win(K, 0)
        phase_x0(K)
        for l in range(L):
            phase_a(K, l)
            if l == 0:
                phase_weights(K, 0)
            phase_b(K, l)
            if l + 1 < L:
                cast_win(K, l + 1)
                phase_weights(K, l + 1)
            phase_dense(K, l, final=(l == L - 1))
        for name, t in K.dbg_out:
            o = T(nc.dram_tensor("dbg_" + name, list(t.h.shape), t.h.dtype, kind="ExternalOutput"))
            P.dma(o.h.ap(), t.h.ap(), t.res(), o.res(), q="pool")
        P.barrier(full=True)
        P.flush()
    return nc


def psum(K):
    i = K.ps_rr
    K.ps_rr = (i + 1) % len(K.ps)
    return K.ps[i]


def cast_win(K, l):
    P, cfg = K.P, K.cfg
    rows = cfg.D
    step = max(1, rows // 8)
    for r0 in range(0, rows, step):
        P.dma(K.winb[l][r0:r0 + step, :], K.win_d[l][r0:r0 + step, :], K.win_d[l].res(), K.winb[l].res(), q="pool")


def phase_weights(K, l):
    P, cfg = K.P, K.cfg
    g4 = [[0, 1, 2, 3], [4, 5, 6, 7]]
    g2 = [[0, 4], [1, 5], [2, 6], [3, 7]]
    for name, K_, N_ in cfg.big:
        P_, pe, a, b = piece_plan(K_ * N_)
        src, full = K.big_d[(name, l)], K.wfull[(name, l)]
        tmp, half = full.tmp, full.half
        nrow = P_ * a
        step = max(a, (nrow // 8 // a) * a) if nrow >= 8 * a else nrow
        for r0 in range(0, nrow, step):
            r1 = min(nrow, r0 + step)
            P.dma(tmp[r0:r1, :], src[r0:r1, :], src.res(), tmp.res(), q="pool")
        for p in range(P_):
            P.coll("AllGather", half[p * 4 * a:(p + 1) * 4 * a, :], tmp[p * a:(p + 1) * a, :], g4, tmp.res(), half.res(p))
        for p in range(P_):
            P.coll("AllGather", full[p * 8 * a:(p + 1) * 8 * a, :], half[p * 4 * a:(p + 1) * 4 * a, :], g2, half.res(p), full.res())


def phase_x0(K):
    P, cfg = K.P, K.cfg
    D, KT, TL = cfg.D, cfg.KT, cfg.TL
    xr = K.xres.h.ap().rearrange("(k p) t -> p k t", p=128)
    with P.phase():
        xt = [P.sb("x0_in%d" % i, [128, D], F32) for i in range(2)]
        xo = [P.sb("x0_out%d" % i, [128, KT, 128], F32) for i in range(2)]
        ident = K.cst[:, 0:128]
        for tb in range(TL // 128):
            a, o = xt[tb % 2], xo[tb % 2]
            P.dma(a[:], K.x_own[tb * 128:(tb + 1) * 128, :], K.x_own.res(), a.res())
            for g in range(KT // 4):
                ps = psum(K)
                for i in range(4):
                    kt = g * 4 + i
                    P.op("pe", lambda e, ps=ps, i=i, kt=kt, a=a: e.transpose(ps[:, i * 128:(i + 1) * 128], a[:, kt * 128:(kt + 1) * 128], ident),
                         a.res() + K.cst.res(), ps.res())
                P.op("act", lambda e, ps=ps, g=g, o=o: e.copy(o[:, g * 4:(g + 1) * 4, :], ps[:, :]), ps.res(), o.res())
            P.dma(xr[:, :, tb * 128:(tb + 1) * 128], o[:], o.res(), K.xres.res((tb * 128) // cfg.TT))


def rms_stats(K, xt, nkt, ntok, sq, rstd, tagres):
    P = K.P
    ones = K.cst[:, 128:256]
    ps = psum(K)
    G = 4
    for g in range(nkt // G):
        s = sq[g % 2]
        P.op("act", lambda e, s=s, g=g: e.activation(s[:], xt[:, g * G:(g + 1) * G, :], AF.Square), xt.res(), s.res())
        for i in range(G):
            kt = g * G + i
            P.op("pe", lambda e, s=s, i=i, kt=kt: e.matmul(ps[:, 0:ntok], ones, s[:, i, :], start=(kt == 0), stop=(kt == nkt - 1)),
                 s.res() + K.cst.res(), ps.res())
    P.op("act", lambda e: e.activation(rstd[:], ps[:, 0:ntok], AF.Sqrt, bias=K.epsD[:, 0:1], scale=1.0 / (nkt * 128)),
         ps.res() + K.epst.res(), rstd.res())
    P.op("dve", lambda e: e.reciprocal(rstd[:], rstd[:]), rstd.res(), rstd.res())


def phase_a(K, l):
    P, cfg, spo = K.P, K.cfg, K.spo
    D, KT, TL = cfg.D, cfg.KT, cfg.TL
    xr = K.xres.h.ap().rearrange("(k p) t -> p k t", p=128)
    groups4 = [[0, 1, 2, 3], [4, 5, 6, 7]]
    gho = (NFM * 128 + NTM * 256) * D
    wfl = K.winb[l].h.ap().rearrange("r b -> (r b)")
    with P.phase():
        make_eps(K)
        xt = [P.sb("a_x%d" % i, [128, KT, 128], F32) for i in range(2)]
        xn = [P.sb("a_xn%d" % i, [128, KT, 128], BF16) for i in range(2)]
        sq = [P.sb("a_sq%d" % i, [128, 4, 128], F32) for i in range(2)]
        rstd = [P.sb("a_rstd%d" % i, [128, 128], F32) for i in range(2)]
        wgh = P.sb("a_wgh", [128, 2, KT, 128], BF16)
        gho_sb = [P.sb("a_gho%d" % i, [128, 2, 128], BF16) for i in range(2)]
        for m in range(2):
            src = wfl[gho + m * D * 128: gho + (m + 1) * D * 128].rearrange("(p f) -> p f", p=128)
            P.dma(wgh[:, m, :, :], src, K.winb[l].res(), wgh.res())
        g1 = K.spt
        ght = K.ghT.h.ap().rearrange("(m p) t -> p m t", p=128)
        for pc in range(TL // 128):
            a, n, r, go = xt[pc % 2], xn[pc % 2], rstd[pc % 2], gho_sb[pc % 2]
            P.dma(a[:], xr[:, :, pc * 128:(pc + 1) * 128], K.xres.res((pc * 128) // cfg.TT), a.res())
            rms_stats(K, a, KT, 128, sq, r, None)
            for kt in range(KT):
                c = spo["g1"] + l * KT + kt
                P.op("dve", lambda e, kt=kt, c=c, a=a, n=n, r=r: e.scalar_tensor_tensor(n[:, kt, :], a[:, kt, :], g1[:, c:c + 1], r[:], ALU.mult, ALU.mult),
                     a.res() + r.res() + K.spt.res(), n.res())
            P.dma(K.xnp[pc * 128:(pc + 1) * 128, :], n[:].rearrange("p k t -> p (k t)"), n.res(), K.xnp.res(pc))
            for m in range(2):
                ps = psum(K)
                for kt in range(KT):
                    P.op("pe", lambda e, ps=ps, m=m, kt=kt, n=n: e.matmul(ps[:, 0:128], wgh[:, m, kt, :], n[:, kt, :], start=(kt == 0), stop=(kt == KT - 1)),
                         wgh.res() + n.res(), ps.res())
                P.op("act", lambda e, ps=ps, m=m, go=go: e.copy(go[:, m, :], ps[:, 0:128]), ps.res(), go.res())
            P.dma(ght[:, :, pc * 128:(pc + 1) * 128], go[:], go.res(), K.ghT.res())
            P.coll("AllGather", K.xng[pc * 512:(pc + 1) * 512, :], K.xnp[pc * 128:(pc + 1) * 128, :], groups4,
                   K.xnp.res(pc), K.xng.res(pc))


def make_eps(K):
    P = K.P
    K.epst = P.sb("epst", [128, 2], F32)
    K.epsD = K.epst
    P.op("dve", lambda e: e.memset(K.epst[:], EPS), (), K.epst.res())


def phase_dense(K, l, final):
    P, cfg, spo = K.P, K.cfg, K.spo
    D, FF, KT, FT, TL, TT, YP = cfg.D, cfg.FF, cfg.KT, cfg.FT, cfg.TL, cfg.TT, cfg.YP
    xr = K.xres.h.ap().rearrange("(k p) t -> p k t", p=128)
    ght = K.ghT.h.ap().rearrange("(m p) t -> p m t", p=128)
    ygv = K.yg.h.ap().rearrange("(q c p) t -> q p c t", c=6, p=128)
    wbr = [flat_blocks(K.wfull[(n, l)], 8 * 128 * 128) for n in ("wa", "wb", "wc")]
    wgfl = K.wfull[("wg", l)].h.ap().rearrange("r b -> (r b)")
    wo = flat_blocks(K.wfull[("wo", l)], D * 128)
    w1 = flat_blocks(K.wfull[("w1", l)], D * 128)
    w2 = flat_blocks(K.wfull[("w2", l)], FF * 128)
    FSUB = min(FT, 32)
    with P.phase():
        make_eps(K)
        xT = P.sb("d_xT", [128, KT, TT], F32, nparts=KT)
        act = P.sb("d_act", [128, KT, TT], BF16, nparts=KT)
        hT = P.sb("d_hT", [128, FT, TT], BF16, nparts=FT)
        yT = [P.sb("d_yT%d" % j, [128, 6, TT], BF16) for j in range(4)]
        ycand = [P.sb("d_yc%d" % i, [128, 6, TT], BF16) for i in range(2)]
        ghs = P.sb("d_gh", [128, 2, TT], BF16)
        NW = 4
        wp = [P.sb("d_w%d" % i, [128, 4096], BF16) for i in range(NW)]
        K.wrr = 0
        sq = [P.sb("d_sq%d" % i, [128, 4, TT], F32) for i in range(2)]
        rstd = P.sb("d_rstd", [128, TT], F32)
        gs = [P.sb("d_gs%d" % i, [128, TT], F32) for i in range(2)]
        tmp = [P.sb("d_tmp%d" % i, [128, TT], F32) for i in range(2)]
        acc = P.sb("d_acc", [128, TT], F32)
        if final:
            otv = hT.h[:].rearrange("p f t -> p (f t)").bitcast(F32)

        def wnext():
            w = wp[K.wrr]
            K.wrr = (K.wrr + 1) % NW
            return w

        for tt in range(TL // TT):
            t0 = tt * TT
            P.dma(xT[:], xr[:, :, t0:t0 + TT], K.xres.res(tt), xT.res())
            P.dma(ghs[:], ght[:, :, t0:t0 + TT], K.ghT.res(), ghs.res())
            ci = 0
            for j in range(4):
                for r in range(4):
                    g0 = r * TL + t0
                    tb, off = g0 // YP, g0 % YP
                    yc = ycand[ci % 2]
                    ci += 1
                    P.dma(yc[:], ygv[tb * 4 + j][:, :, off:off + TT], K.yg.res(tb), yc.res())
                    if r == 0:
                        P.op("dve", lambda e, j=j, yc=yc, r=r: e.tensor_scalar(yT[j][:], yc[:], K.rmask[:, r:r + 1], None, ALU.mult),
                             yc.res() + K.rmask.res(), yT[j].res())
                    else:
                        P.op("dve", lambda e, j=j, yc=yc, r=r: e.scalar_tensor_tensor(yT[j][:], yc[:], K.rmask[:, r:r + 1], yT[j][:], ALU.mult, ALU.add),
                             yc.res() + K.rmask.res() + yT[j].res(), yT[j].res())
            for dt in range(KT):
                w = wnext()
                for jb in range(3):
                    o = jb * 256 * D + dt * 256 * 128
                    P.dma(w[:, jb * 256:(jb + 1) * 256], wgfl[o:o + 256 * 128].rearrange("(p f) -> p f", p=128),
                          K.wfull[("wg", l)].res(), w.res())
                    P.dma(w[:, 768 + jb * 1024: 768 + (jb + 1) * 1024], wbr[jb][dt], K.wfull[(("wa", "wb", "wc")[jb], l)].res(), w.res())
                for jb in range(3):
                    psg = psum(K)
                    for rt in range(2):
                        c0 = jb * 256 + rt * 128
                        P.op("pe", lambda e, psg=psg, w=w, c0=c0, rt=rt: e.matmul(psg[:, 0:TT], w[:, c0:c0 + 128], ghs[:, rt, :], start=(rt == 0), stop=(rt == 1)),
                             w.res() + ghs.res(), psg.res())
                    g = gs[jb % 2]
                    bc = spo["bm"] + (l * 3 + jb) * KT + dt
                    P.op("act", lambda e, psg=psg, g=g, bc=bc: e.activation(g[:], psg[:, 0:TT], AF.Sigmoid, bias=K.spt[:, bc:bc + 1]),
                         psg.res() + K.spt.res(), g.res())
                    psb = psum(K)
                    for ct in range(8):
                        c0 = 768 + jb * 1024 + ct * 128
                        P.op("pe", lambda e, psb=psb, w=w, c0=c0, ct=ct, jb=jb: e.matmul(psb[:, 0:TT], w[:, c0:c0 + 128], yT[ct // 2][:, 2 * jb + ct % 2, :], start=(ct == 0), stop=(ct == 7)),
                             w.res() + yT[ct // 2].res(), psb.res())
                    if jb == 0:
                        P.op("dve", lambda e, psb=psb, g=g: e.tensor_tensor(acc[:], psb[:, 0:TT], g[:], ALU.mult), psb.res() + g.res(), acc.res())
                    else:
                        tm_ = tmp[jb % 2]
                        P.op("dve", lambda e, psb=psb, g=g, tm_=tm_: e.tensor_tensor(tm_[:], psb[:, 0:TT], g[:], ALU.mult), psb.res() + g.res(), tm_.res())
                        if jb == 1:
                            P.op("dve", lambda e, tm_=tm_: e.tensor_tensor(acc[:], acc[:], tm_[:], ALU.add), acc.res() + tm_.res(), acc.res())
                        else:
                            P.op("dve", lambda e, tm_=tm_, dt=dt: e.tensor_tensor(act[:, dt, :], acc[:], tm_[:], ALU.add), acc.res() + tm_.res(), act.res(dt))
            for et in range(KT):
                w = wnext()
                P.dma(w[:, 0:KT * 128], wo[et], K.wfull[("wo", l)].res(), w.res())
                ps = psum(K)
                for dt in range(KT):
                    P.op("pe", lambda e, ps=ps, w=w, dt=dt: e.matmul(ps[:, 0:TT], w[:, dt * 128:(dt + 1) * 128], act[:, dt, :], start=(dt == 0), stop=(dt == KT - 1)),
                         w.res() + act.res(dt), ps.res())
                P.op("dve", lambda e, ps=ps, et=et: e.tensor_tensor(xT[:, et, :], xT[:, et, :], ps[:, 0:TT], ALU.add), ps.res() + xT.res(et), xT.res(et))
            rms_stats(K, xT, KT, TT, sq, rstd, None)
            for kt in range(KT):
                c = spo["g2"] + l * KT + kt
                P.op("dve", lambda e, kt=kt, c=c: e.scalar_tensor_tensor(act[:, kt, :], xT[:, kt, :], K.spt[:, c:c + 1], rstd[:], ALU.mult, ALU.mult),
                     xT.res(kt) + rstd.res() + K.spt.res(), act.res(kt))
            for ft in range(FT):
                w = wnext()
                P.dma(w[:, 0:KT * 128], w1[ft], K.wfull[("w1", l)].res(), w.res())
                ps = psum(K)
                for kt in range(KT):
                    P.op("pe", lambda e, ps=ps, w=w, kt=kt: e.matmul(ps[:, 0:TT], w[:, kt * 128:(kt + 1) * 128], act[:, kt, :], start=(kt == 0), stop=(kt == KT - 1)),
                         w.res() + act.res(kt), ps.res())
                sv = tmp[ft % 2]
                P.op("act", lambda e, ps=ps, sv=sv: e.activation(sv[:], ps[:, 0:TT], AF.Square), ps.res(), sv.res())
                P.op("dve", lambda e, ps=ps, sv=sv, ft=ft: e.scalar_tensor_tensor(hT[:, ft, :], ps[:, 0:TT], 0.0, sv[:], ALU.is_gt, ALU.mult),
                     ps.res() + sv.res(), hT.res(ft))
            for dt in range(KT):
                ps = psum(K)
                for sub in range(FT // FSUB):
                    w = wnext()
                    P.dma(w[:, 0:FSUB * 128], w2[dt][:, sub * FSUB * 128:(sub + 1) * FSUB * 128], K.wfull[("w2", l)].res(), w.res())
                    for fi in range(FSUB):
                        ft = sub * FSUB + fi
                        P.op("pe", lambda e, ps=ps, w=w, fi=fi, ft=ft: e.matmul(ps[:, 0:TT], w[:, fi * 128:(fi + 1) * 128], hT[:, ft, :], start=(ft == 0), stop=(ft == FT - 1)),
                             w.res() + hT.res(ft), ps.res())
                P.op("dve", lambda e, ps=ps, dt=dt: e.tensor_tensor(xT[:, dt, :], xT[:, dt, :], ps[:, 0:TT], ALU.add), ps.res() + xT.res(dt), xT.res(dt))
            if not final:
                P.dma(xr[:, :, t0:t0 + TT], xT[:], xT.res(), K.xres.res(tt))
            else:
                rms_stats(K, xT, KT, TT, sq, rstd, None)
                for kt in range(KT):
                    c = spo["gf"] + kt
                    P.op("dve", lambda e, kt=kt, c=c: e.scalar_tensor_tensor(xT[:, kt, :], xT[:, kt, :], K.spt[:, c:c + 1], rstd[:], ALU.mult, ALU.mult),
                         xT.res(kt) + rstd.res() + K.spt.res(), xT.res(kt))
                ident = K.cst[:, 0:128]
                for s_ in range(TT // 128):
                    o_ = otv[:, (s_ % 2) * D:(s_ % 2 + 1) * D]
                    for g in range(KT // 4):
                        ps = psum(K)
                        for i in range(4):
                            kt = g * 4 + i
                            P.op("pe", lambda e, ps=ps, i=i, kt=kt, s_=s_: e.transpose(ps[:, i * 128:(i + 1) * 128], xT[:, kt, s_ * 128:(s_ + 1) * 128], ident),
                                 xT.res(kt) + K.cst.res(), ps.res())
                        P.op("act", lambda e, ps=ps, g=g, o_=o_: e.copy(o_[:, g * 512:(g + 1) * 512], ps[:, :]), ps.res(), hT.res())
                    P.dma(K.out[t0 + s_ * 128: t0 + (s_ + 1) * 128, :], o_, hT.res(), K.out.res())


def alloc_mixer_dram(K):
    P, cfg = K.P, K.cfg
    S, YP = cfg.S, cfg.YP
    npc = S // YP
    K.fm32 = P.dram("fm32", [9 * 128, S], F32, nparts=9)
    K.fm16 = P.dram("fm16", [2 * 128, S], BF16, nparts=2)
    K.tm = {"av": P.dram("tm_av", [S, 256], BF16), "ao": P.dram("tm_ao", [S, 256], F32),
            "bz": P.dram("tm_bz", [S, 256], F32), "cv": P.dram("tm_cv", [S, 256], BF16),
            "cr": P.dram("tm_cr", [S, 256], F32)}
    K.yp = P.dram("yp", [npc * 6 * 128, YP], BF16, nparts=npc)
    K.yg = P.dram("yg", [npc * 4 * 6 * 128, YP], BF16, nparts=npc)
    K.H = [P.dram("H%d" % d, [S, 768], F32, nparts=4) for d in range(2)]
    K.gl = {}


def b_proj(K, l):
    P, cfg = K.P, K.cfg
    D, KT, S, TL, PT = cfg.D, cfg.KT, cfg.S, cfg.TL, cfg.PTP
    wfl = K.winb[l].h.ap().rearrange("r b -> (r b)")
    bw = block_widths()
    boff = np.concatenate([[0], np.cumsum(bw)]).tolist()
    tmnames = ["av", "ao", "bz", "cv", "cr"]
    NB = NFM + NTM
    with P.phase():
        xn = [P.sb("p_xn%d" % i, [128, KT, PT], BF16) for i in range(2)]
        NWQ = 4
        wq = [P.sb("p_w%d" % i, [128, KT * 256], BF16) for i in range(NWQ)]
        st32 = [P.sb("p_s32_%d" % i, [128, 512], F32) for i in range(3)]
        st16 = [P.sb("p_s16_%d" % i, [128, 512], BF16) for i in range(3)]
        ntile = S // PT
        jobs = [(ti, bi) for ti in range(ntile) for bi in range(NB)]

        def load_w(k):
            ti, bi = jobs[k]
            w = wq[k % NWQ]
            wd = 128 if bi < NFM else 256
            o = boff[bi] * D
            P.dma(w[:, 0:KT * wd], wfl[o:o + D * wd].rearrange("(p f) -> p f", p=128), K.winb[l].res(), w.res())

        def load_x(ti):
            g0 = ti * PT
            r, w0 = g0 // TL, g0 % TL
            x = xn[ti % 2]
            for i in range(PT // 128):
                pc = (w0 + i * 128) // 128
                P.dma(x[:, :, i * 128:(i + 1) * 128], K.xng[pc * 512 + r * 128: pc * 512 + (r + 1) * 128, :].rearrange("p (k t) -> p k t", t=128),
                      K.xng.res(pc), x.res())

        load_x(0)
        for k in range(min(NWQ - 1, len(jobs))):
            load_w(k)
        sr = 0
        for k, (ti, bi) in enumerate(jobs):
            g0 = ti * PT
            x = xn[ti % 2]
            w = wq[k % NWQ]
            if bi == 0 and ti + 1 < ntile:
                load_x(ti + 1)
            if k + NWQ - 1 < len(jobs):
                load_w(k + NWQ - 1)
            if bi < NFM:
                ps = psum(K)
                for kt in range(KT):
                    P.op("pe", lambda e, ps=ps, w=w, kt=kt, x=x: e.matmul(ps[:, 0:PT], w[:, kt * 128:(kt + 1) * 128], x[:, kt, :], start=(kt == 0), stop=(kt == KT - 1)),
                         w.res() + x.res(), ps.res())
                if bi < 2:
                    st = st16[sr % 3]
                    sr += 1
                    P.op("act", lambda e, ps=ps, st=st, bi=bi: e.activation(st[:, 0:PT], ps[:, 0:PT], AF.Copy, scale=(1.0 if bi == 0 else A_DK ** -0.5)), ps.res(), st.res())
                    P.dma(K.fm16[bi * 128:(bi + 1) * 128, g0:g0 + PT], st[:, 0:PT], st.res(), K.fm16.res(bi), q="act")
                else:
                    st = st32[sr % 3]
                    sr += 1
                    P.op("act", lambda e, ps=ps, st=st: e.copy(st[:, 0:PT], ps[:, 0:PT]), ps.res(), st.res())
                    P.dma(K.fm32[(bi - 2) * 128:(bi - 1) * 128, g0:g0 + PT], st[:, 0:PT], st.res(), K.fm32.res(bi - 2), q="act")
            else:
                nm = tmnames[bi - NFM]
                dst = K.tm[nm]
                is16 = nm in ("av", "cv")
                for sub in range(PT // 128):
                    ps = psum(K)
                    for kt in range(KT):
                        P.op("pe", lambda e, ps=ps, w=w, kt=kt, x=x, sub=sub: e.matmul(ps[:, 0:256], x[:, kt, sub * 128:(sub + 1) * 128], w[:, kt * 256:(kt + 1) * 256], start=(kt == 0), stop=(kt == KT - 1)),
                             w.res() + x.res(), ps.res())
                    st = (st16 if is16 else st32)[sr % 3]
                    sr += 1
                    P.op("dve", lambda e, ps=ps, st=st: e.tensor_copy(st[:, 0:256], ps[:, 0:256]), ps.res(), st.res())
                    P.dma(dst[g0 + sub * 128: g0 + (sub + 1) * 128, :], st[:, 0:256], st.res(), dst.res(), q="act")


def b_ygather(K, l):
    P, cfg = K.P, K.cfg
    npc = cfg.S // cfg.YP
    groups4 = [[0, 1, 2, 3], [4, 5, 6, 7]]
    for tb in range(npc):
        P.coll("AllGather", K.yg[tb * 4 * 768:(tb + 1) * 4 * 768, :], K.yp[tb * 768:(tb + 1) * 768, :], groups4,
               K.yp.res(tb), K.yg.res(tb))


def phase_b(K, l):
    b_proj(K, l)
    if K.stub_y is not None:
        P = K.P
        P.dma(K.yp[:, :], K.stub_y[:, :], K.stub_y.res(), K.yp.res(), q="pool")
    else:
        b_mixers(K, l)
    b_ygather(K, l)


def run_cfg(inputs, cfg, debug=(), extra=None):
    in_maps = prep_inputs(inputs, cfg)
    if extra is not None:
        for c in range(NCORES):
            in_maps[c].update(extra[c])
    nc = build(cfg, debug=debug)
    res = run_bass_kernel_spmd(nc, in_maps, core_ids=list(range(NCORES)))
    out = np.zeros((2, cfg.S, cfg.D), np.float32)
    for c in range(NCORES):
        b_, j = c // 4, c % 4
        out[b_, j * cfg.TL:(j + 1) * cfg.TL, :] = res.results[c]["out"]
    return out, res


def kernel(**inputs):
    x = np.asarray(inputs["x"])
    L = int(np.asarray(inputs["w_in"]).shape[0])
    cfg = Cfg(int(x.shape[2]), int(np.asarray(inputs["w_ff1"]).shape[2]), int(x.shape[1]), L)
    out, _ = run_cfg(inputs, cfg)
    return out.astype(np.float32)


def tr_bf(K, dst_ap, dst_res, src_ap, src_res, eng="act", scale_ap=None):
    P = K.P
    i = K.psb_rr
    K.psb_rr = (i + 1) % 4
    pv = K.psb[:, i * 256:i * 256 + 128]
    P.op("pe", lambda e: e.transpose(pv, src_ap, K.identb[:]), list(src_res) + K.identb.res(), K.psb.res(i))
    if scale_ap is not None:
        P.op("dve", lambda e: e.tensor_scalar(dst_ap, pv, scale_ap[0], None, ALU.mult), K.psb.res(i) + list(scale_ap[1]), dst_res)
    elif eng == "act":
        P.op("act", lambda e: e.copy(dst_ap, pv), K.psb.res(i), dst_res)
    else:
        P.op("dve", lambda e: e.tensor_copy(dst_ap, pv), K.psb.res(i), dst_res)


def gla_prep(K, l):
    P, cfg, spo = K.P, K.cfg, K.spo
    S, PT = cfg.S, cfg.PT
    K.glaB = [P.dram("glaB%d_%d" % (d, l), [128, S + 1], F32, nparts=S // PT) for d in range(2)]
    if "dumpG" in K.debug and l == 0:
        K.dbg_out += [("glaB0", K.glaB[0]), ("glaB1", K.glaB[1])]
    with P.phase():
        negb = P.sb("gp_negb", [128, 2], F32)
        zc = P.sb("gp_z", [128, 1], F32)
        P.op("dve", lambda e: e.memset(zc[:], 0.0), (), zc.res())
        P.op("dve", lambda e: e.tensor_scalar(negb[:], K.spt[:, spo["glab"] + l * 2: spo["glab"] + l * 2 + 2], -1.0, None, ALU.mult), K.spt.res(), negb.res())
        clr = [P.sb("gp_clr%d" % i, [16, PT], F32) for i in range(2)]
        ex = [P.sb("gp_e%d" % i, [128, PT], F32) for i in range(2)]
        bt = [P.sb("gp_b%d" % i, [128, PT], F32) for i in range(3)]
        n = 0
        for d in range(2):
            P.dma(K.glaB[d][:, (0 if d == 0 else S):(1 if d == 0 else S + 1)], zc[:], zc.res(), K.glaB[d].res(0 if d == 0 else S // PT - 1), slow=True)
            prev = None
            order = range(S // PT) if d == 0 else range(S // PT - 1, -1, -1)
            wg = K.spt[0:16, spo["glaw"] + (l * 2 + d) * 128: spo["glaw"] + (l * 2 + d + 1) * 128]
            for ti in order:
                g0 = ti * PT
                c_, e_, b_ = clr[n % 2], ex[n % 2], bt[n % 3]
                n += 1
                r0 = 8 * 128 + 32 + 32 * d
                P.dma(c_[:], K.fm32[r0:r0 + 16, g0:g0 + PT], K.fm32.res(8), c_.res())
                ps = psum(K)
                P.op("pe", lambda e, ps=ps, c_=c_, wg=wg: e.matmul(ps[:, 0:PT], wg, c_[:], start=True, stop=True), c_.res() + K.spt.res(), ps.res())
                P.op("act", lambda e, ps=ps, e_=e_, d=d: e.activation(e_[:], ps[:, 0:PT], AF.Exp, bias=negb[:, d:d + 1], scale=-1.0), ps.res() + negb.res(), e_.res())
                P.op("act", lambda e, e_=e_: e.activation(e_[:], e_[:], AF.Ln, bias=K.cst[:, 128:129], scale=1.0), e_.res() + K.cst.res(), e_.res())
                P.op("dve", lambda e, e_=e_: e.tensor_scalar(e_[:], e_[:], 1.0 / GLA_TAU, None, ALU.mult), e_.res(), e_.res())
                if d == 0:
                    init = 0.0 if prev is None else prev[:, PT - 1:PT]
                    P.op("dve", lambda e, b_=b_, e_=e_, init=init: e.tensor_tensor_scan(b_[:], e_[:], e_[:], init, ALU.add, ALU.bypass),
                         e_.res() + (prev.res() if prev is not None else []), b_.res())
                    P.dma(K.glaB[d][:, 1 + g0:1 + g0 + PT], b_[:], b_.res(), K.glaB[d].res(ti))
                else:
                    init = 0.0 if prev is None else prev[:, 0:1]
                    P.op("dve", lambda e, b_=b_, e_=e_, init=init: e.tensor_tensor_scan(b_[:, ::-1], e_[:, ::-1], e_[:, ::-1], init, ALU.add, ALU.bypass),
                         e_.res() + (prev.res() if prev is not None else []), b_.res())
                    P.dma(K.glaB[d][:, g0:g0 + PT], b_[:], b_.res(), K.glaB[d].res(ti))
                prev = b_


def gla_stream(K, l, d):
    P, cfg = K.P, K.cfg
    S, NCH, PT = cfg.S, cfg.NCH, cfg.PT
    tg = "gl%d" % d
    st = P.sb(tg + "_st", [128, 256], F32)
    stb = P.sb(tg + "_stb", [128, 256], BF16)
    P.op("dve", lambda e: e.memset(st[:], 0.0), (), st.res())
    P.op("dve", lambda e: e.memset(stb[:], 0.0), (), stb.res())
    nb = 2
    bs = [P.sb(tg + "_bs%d" % i, [128, 129], F32) for i in range(nb)]
    qk = [P.sb(tg + "_qk%d" % i, [128, 2, 128], F32) for i in range(nb)]
    v = [P.sb(tg + "_v%d" % i, [128, 256], BF16) for i in range(nb)]
    E1 = [P.sb(tg + "_E1%d" % i, [128, 128], F32) for i in range(nb)]
    E2 = [P.sb(tg + "_E2%d" % i, [128, 128], F32) for i in range(nb)]
    qd = [P.sb(tg + "_qd%d" % i, [128, 128], BF16) for i in range(nb)]
    kd = [P.sb(tg + "_kd%d" % i, [128, 128], BF16) for i in range(nb)]
    ktm = [P.sb(tg + "_kt%d" % i, [128, 128], BF16) for i in range(nb)]
    stm = [P.sb(tg + "_sm%d" % i, [128, 128], BF16) for i in range(nb)]
    o = [P.sb(tg + "_o%d" % i, [128, 256], F32) for i in range(nb)]
    tS = P.sb(tg + "_tS", [128, 256], F32)
    mask = K.cst[:, (6 + d) * 128:(7 + d) * 128]
    order = range(NCH) if d == 0 else range(NCH - 1, -1, -1)
    n = 0
    for c in order:
        cs = c * 128
        i = n % nb
        n += 1
        P.dma(qk[i][:, 0, :], K.fm32[6 * 128:7 * 128, cs:cs + 128], K.fm32.res(6), qk[i].res())
        P.dma(qk[i][:, 1, :], K.fm32[7 * 128:8 * 128, cs:cs + 128], K.fm32.res(7), qk[i].res())
        P.dma(bs[i][:], K.glaB[d][:, cs:cs + 129], K.glaB[d].res(), bs[i].res())
        P.dma(v[i][:], K.tm["cv"][cs:cs + 128, :], K.tm["cv"].res(), v[i].res())
        if d == 0:
            bcur, bref, edge = bs[i][:, 1:129], bs[i][:, 0:1], 127
        else:
            bcur, bref, edge = bs[i][:, 0:128], bs[i][:, 128:129], 0
        P.op("act", lambda e, i=i, bcur=bcur, bref=bref: e.activation(E1[i][:], bcur, AF.Exp, bias=bref, scale=-1.0), bs[i].res(), E1[i].res())
        P.op("dve", lambda e, i=i: e.reciprocal(E2[i][:], E1[i][:]), E1[i].res(), E2[i].res())
        P.op("dve", lambda e, i=i: e.scalar_tensor_tensor(qd[i][:], qk[i][:, 0, :], C_DK ** -0.5, E1[i][:], ALU.mult, ALU.mult), qk[i].res() + E1[i].res(), qd[i].res())
        P.op("dve", lambda e, i=i: e.tensor_tensor(kd[i][:], qk[i][:, 1, :], E2[i][:], ALU.mult), qk[i].res() + E2[i].res(), kd[i].res())
        ps1 = psum(K)
        P.op("pe", lambda e, i=i, ps1=ps1: e.matmul(ps1[:, 0:128], kd[i][:], qd[i][:], start=True, stop=True), kd[i].res() + qd[i].res(), ps1.res())
        P.op("dve", lambda e, i=i, ps1=ps1: e.tensor_tensor(stm[i][:], ps1[:, 0:128], mask, ALU.mult), ps1.res() + K.cst.res(), stm[i].res())
        tr_bf(K, ktm[i][:], ktm[i].res(), kd[i][:], kd[i].res())
        ps2 = psum(K)
        P.op("pe", lambda e, i=i, ps2=ps2: e.matmul(ps2[:, 0:256], stm[i][:], v[i][:], start=True, stop=False), stm[i].res() + v[i].res(), ps2.res())
        P.op("pe", lambda e, i=i, ps2=ps2: e.matmul(ps2[:, 0:256], qd[i][:], stb[:], start=False, stop=True), qd[i].res() + stb.res(), ps2.res())
        P.op("act", lambda e, i=i, ps2=ps2: e.copy(o[i][:], ps2[:, 0:256]), ps2.res(), o[i].res())
        P.dma(K.H[d][cs:cs + 128, 512:768], o[i][:], o[i].res(), K.H[d].res(3))
        ps3 = psum(K)
        P.op("pe", lambda e, i=i, ps3=ps3: e.matmul(ps3[:, 0:256], ktm[i][:], v[i][:], start=True, stop=True), ktm[i].res() + v[i].res(), ps3.res())
        P.op("dve", lambda e, ps3=ps3: e.tensor_tensor(tS[:], st[:], ps3[:, 0:256], ALU.add), st.res() + ps3.res(), tS.res())
        P.op("dve", lambda e, i=i, edge=edge: e.tensor_scalar(st[:], tS[:], E1[i][:, edge:edge + 1], None, ALU.mult), tS.res() + E1[i].res(), st.res())
        P.op("act", lambda e: e.copy(stb[:], st[:]), st.res(), stb.res())
        yield


def mlstm_prep(K, l):
    P, cfg, spo = K.P, K.cfg, K.spo
    S, NCH = cfg.S, cfg.NCH
    RS = min(S, 1024)
    K.mlG = [P.dram("mlG%d_%d" % (d, l), [3, S], F32) for d in range(2)]
    with P.phase():
        nbias = P.sb("mp_nb", [1, 4], F32)
        P.op("dve", lambda e: e.tensor_scalar(nbias[:], K.spt[0:1, spo["agb"] + l * 4: spo["agb"] + l * 4 + 4], -1.0, None, ALU.mult), K.spt.res(), nbias.res())
        zr = P.sb("mp_z", [1, RS], F32)
        P.op("dve", lambda e: e.memset(zr[:], 0.0), (), zr.res())
        names = ["fr", "ir", "lf", "F", "m", "a", "u", "em"]
        tsets = [{nm: P.sb("mp_%s_%d" % (nm, i_), [1, RS], F32) for nm in names} for i_ in range(2)]
        for d in range(2):
            prevF = prevm = None
            segs = range(S // RS) if d == 0 else range(S // RS - 1, -1, -1)
            for si, sg in enumerate(segs):
                t = tsets[si % 2]
                g0 = sg * RS
                rb = 8 * 128
                P.dma(t["ir"][:], K.fm32[rb + d:rb + d + 1, g0:g0 + RS], K.fm32.res(8), t["ir"].res())
                P.dma(t["fr"][:], K.fm32[rb + 2 + d:rb + 3 + d, g0:g0 + RS], K.fm32.res(8), t["fr"].res())
                P.op("act", lambda e, t=t, d=d: e.activation(t["fr"][:], t["fr"][:], AF.Exp, bias=nbias[:, 2 + d:3 + d], scale=-1.0), t["fr"].res() + nbias.res(), t["fr"].res())
                P.op("act", lambda e, t=t: e.activation(t["fr"][:], t["fr"][:], AF.Ln, bias=K.cst[0:1, 128:129], scale=1.0), t["fr"].res() + K.cst.res(), t["fr"].res())
                P.op("dve", lambda e, t=t: e.tensor_scalar(t["lf"][:], t["fr"][:], -1.0, None, ALU.mult), t["fr"].res(), t["lf"].res())
                P.op("dve", lambda e, t=t, d=d: e.tensor_scalar(t["ir"][:], t["ir"][:], K.spt[0:1, spo["agb"] + l * 4 + d: spo["agb"] + l * 4 + d + 1], None, ALU.add), t["ir"].res() + K.spt.res(), t["ir"].res())
                if d == 0:
                    iF = 0.0 if prevF is None else prevF[:, RS - 1:RS]
                    im = 0.0 if prevm is None else prevm[:, RS - 1:RS]
                    vw = lambda ap: ap[:]
                else:
                    iF = 0.0 if prevF is None else prevF[:, 0:1]
                    im = 0.0 if prevm is None else prevm[:, 0:1]
                    vw = lambda ap: ap[:, ::-1]
                dep = (prevF.res() if prevF is not None else []) + (prevm.res() if prevm is not None else [])
                P.op("dve", lambda e, t=t, iF=iF, vw=vw: e.tensor_tensor_scan(vw(t["F"]), vw(t["lf"]), vw(zr), iF, ALU.add, ALU.add), t["lf"].res() + zr.res() + dep, t["F"].res())
                P.op("dve", lambda e, t=t, im=im, vw=vw: e.tensor_tensor_scan(vw(t["m"]), vw(t["lf"]), vw(t["ir"]), im, ALU.add, ALU.max), t["lf"].res() + t["ir"].res() + dep, t["m"].res())
                P.op("dve", lambda e, t=t: e.tensor_tensor(t["a"][:], t["F"][:], t["m"][:], ALU.subtract), t["F"].res() + t["m"].res(), t["a"].res())
                P.op("dve", lambda e, t=t: e.tensor_tensor(t["u"][:], t["ir"][:], t["F"][:], ALU.subtract), t["F"].res() + t["ir"].res(), t["u"].res())
                P.op("act", lambda e, t=t: e.activation(t["em"][:], t["m"][:], AF.Exp, scale=-1.0), t["m"].res(), t["em"].res())
                for ri, nm in enumerate(("a", "u", "em")):
                    P.dma(K.mlG[d][ri:ri + 1, g0:g0 + RS], t[nm][:], t[nm].res(), K.mlG[d].res())
                prevF, prevm = t["F"], t["m"]


def col_from_rows(K, dst, dram_row_ap, dram_res, nch, tmp):
    P = K.P
    P.dma(tmp[0:nch, :], dram_row_ap.rearrange("o (c t) -> (o c) t", t=128), dram_res, tmp.res())
    ps = psum(K)
    P.op("pe", lambda e: e.transpose(ps[:, 0:nch], tmp[0:nch, :], K.cst[0:nch, 0:nch]), tmp.res() + K.cst.res(), ps.res())
    P.op("act", lambda e: e.copy(dst, ps[:, 0:nch]), ps.res(), [])


def mlstm_stream(K, l, d):
    P, cfg = K.P, K.cfg
    S, NCH = cfg.S, cfg.NCH
    tg = "ml%d" % d
    st = P.sb(tg + "_st", [128, 257], F32)
    stb = P.sb(tg + "_stb", [128, 257], BF16)
    P.op("dve", lambda e: e.memset(st[:], 0.0), (), st.res())
    P.op("dve", lambda e: e.memset(stb[:], 0.0), (), stb.res())
    cols = P.sb(tg + "_cols", [128, 2, NCH], F32)
    cmt = P.sb(tg + "_cmt", [128, 128], F32)
    for ri in range(2):
        P.dma(cmt[0:NCH, :], K.mlG[d][1 + ri:2 + ri, :].rearrange("o (c t) -> (o c) t", t=128), K.mlG[d].res(), cmt.res())
        ps = psum(K)
        P.op("pe", lambda e, ps=ps: e.transpose(ps[:, 0:NCH], cmt[0:NCH, :], K.cst[0:NCH, 0:NCH]), cmt.res() + K.cst.res(), ps.res())
        P.op("act", lambda e, ps=ps, ri=ri: e.copy(cols[:, ri, :], ps[:, 0:NCH]), ps.res(), cols.res())
    nb = 2
    q = [P.sb(tg + "_q%d" % i, [128, 128], BF16) for i in range(nb)]
    k = [P.sb(tg + "_k%d" % i, [128, 128], BF16) for i in range(nb)]
    va = [P.sb(tg + "_va%d" % i, [128, 257], BF16) for i in range(nb)]
    ar = [P.sb(tg + "_ar%d" % i, [1, 128], F32) for i in range(nb)]
    W = [P.sb(tg + "_W%d" % i, [128, 128], F32) for i in range(nb)]
    Wi = [P.sb(tg + "_Wi%d" % i, [128, 128], F32) for i in range(nb)]
    Dm = [P.sb(tg + "_Dm%d" % i, [128, 128], BF16) for i in range(nb)]
    qd = [P.sb(tg + "_qd%d" % i, [128, 128], BF16) for i in range(nb)]
    kw = [P.sb(tg + "_kw%d" % i, [128, 128], BF16) for i in range(nb)]
    h = [P.sb(tg + "_h%d" % i, [128, 256], F32) for i in range(nb)]
    dn = [P.sb(tg + "_dn%d" % i, [128, 2], F32) for i in range(nb)]
    negab = [P.sb(tg + "_na%d" % i, [128, 1], F32) for i in range(2)]
    for i in range(nb):
        P.op("dve", lambda e, i=i: e.memset(va[i][:, 256:257], 1.0), (), va[i].res())
    P.op("dve", lambda e: e.memset(negab[0][:], 0.0), (), negab[0].res())
    ones_row = K.cst[0:1, 128:256]
    ident = K.cst[:, 0:128]
    negm = K.cst[:, (2 + 2 * d) * 128:(3 + 2 * d) * 128]
    edge = 127 if d == 0 else 0
    order = range(NCH) if d == 0 else range(NCH - 1, -1, -1)
    n = 0
    for c in order:
        cs = c * 128
        i = n % nb
        na_in, na_out = negab[n % 2], negab[(n + 1) % 2]
        n += 1
        P.dma(q[i][:], K.fm16[0:128, cs:cs + 128], K.fm16.res(0), q[i].res())
        P.dma(k[i][:], K.fm16[128:256, cs:cs + 128], K.fm16.res(1), k[i].res())
        P.dma(va[i][:, 0:256], K.tm["av"][cs:cs + 128, :], K.tm["av"].res(), va[i].res())
        P.dma(ar[i][:], K.mlG[d][0:1, cs:cs + 128], K.mlG[d].res(), ar[i].res())
        ps1 = psum(K)
        P.op("pe", lambda e, i=i, ps1=ps1: e.matmul(ps1[:, 0:128], ones_row, ar[i][:], start=True, stop=False), ar[i].res() + K.cst.res(), ps1.res())
        P.op("pe", lambda e, ps1=ps1: e.matmul(ps1[:, 0:128], ident, negm, start=False, stop=True), K.cst.res(), ps1.res())
        P.op("act", lambda e, i=i, ps1=ps1, c=c: e.activation(W[i][:], ps1[:, 0:128], AF.Exp, bias=cols[:, 0, c:c + 1]), ps1.res() + cols.res(), W[i].res())
        ps1b = psum(K)
        P.op("pe", lambda e, i=i, ps1b=ps1b: e.matmul(ps1b[:, 0:128], ones_row, ar[i][:], start=True, stop=True), ar[i].res() + K.cst.res(), ps1b.res())
        P.op("act", lambda e, i=i, ps1b=ps1b, na_in=na_in: e.activation(Wi[i][:], ps1b[:, 0:128], AF.Exp, bias=na_in[:, 0:1]), ps1b.res() + na_in.res(), Wi[i].res())
        P.op("dve", lambda e, ps1b=ps1b, na_out=na_out: e.tensor_scalar(na_out[:], ps1b[:, edge:edge + 1], -1.0, None, ALU.mult), ps1b.res(), na_out.res())
        ps2 = psum(K)
        P.op("pe", lambda e, i=i, ps2=ps2: e.matmul(ps2[:, 0:128], k[i][:], q[i][:], start=True, stop=True), k[i].res() + q[i].res(), ps2.res())
        P.op("dve", lambda e, i=i, ps2=ps2: e.tensor_tensor(Dm[i][:], ps2[:, 0:128], W[i][:], ALU.mult), ps2.res() + W[i].res(), Dm[i].res())
        P.op("dve", lambda e, i=i: e.tensor_tensor(qd[i][:], q[i][:], Wi[i][:], ALU.mult), q[i].res() + Wi[i].res(), qd[i].res())
        tr_bf(K, kw[i][:], kw[i].res(), k[i][:], k[i].res(), scale_ap=(W[i][:, edge:edge + 1], W[i].res()))
        ps3 = psum(K)
        P.op("pe", lambda e, i=i, ps3=ps3: e.matmul(ps3[:, 0:257], Dm[i][:], va[i][:], start=True, stop=False), Dm[i].res() + va[i].res(), ps3.res())
        P.op("pe", lambda e, i=i, ps3=ps3: e.matmul(ps3[:, 0:257], qd[i][:], stb[:], start=False, stop=True), qd[i].res() + stb.res(), ps3.res())
        P.op("act", lambda e, i=i, ps3=ps3: e.activation(dn[i][:, 0:1], ps3[:, 256:257], AF.Abs), ps3.res(), dn[i].res())
        P.op("dve", lambda e, i=i, c=c: e.tensor_tensor(dn[i][:, 0:1], dn[i][:, 0:1], cols[:, 1, c:c + 1], ALU.max), dn[i].res() + cols.res(), dn[i].res())
        P.op("dve", lambda e, i=i: e.reciprocal(dn[i][:, 1:2], dn[i][:, 0:1]), dn[i].res(), dn[i].res())
        P.op("act", lambda e, i=i, ps3=ps3: e.activation(h[i][:], ps3[:, 0:256], AF.Copy, scale=dn[i][:, 1:2]), ps3.res() + dn[i].res(), h[i].res())
        P.dma(K.H[d][cs:cs + 128, 0:256], h[i][:], h[i].res(), K.H[d].res(0))
        ps4 = psum(K)
        P.op("pe", lambda e, i=i, ps4=ps4: e.matmul(ps4[:, 0:257], kw[i][:], va[i][:], start=True, stop=True), kw[i].res() + va[i].res(), ps4.res())
        P.op("dve", lambda e, i=i, ps4=ps4: e.scalar_tensor_tensor(st[:], st[:], Wi[i][:, edge:edge + 1], ps4[:, 0:257], ALU.mult, ALU.add), st.res() + Wi[i].res() + ps4.res(), st.res())
        P.op("act", lambda e: e.copy(stb[:], st[:]), st.res(), stb.res())
        yield


def b_post(K, l):
    P, cfg, spo = K.P, K.cfg, K.spo
    S, NCH, YP = cfg.S, cfg.NCH, cfg.YP
    ypv = K.yp.h.ap().rearrange("(q c p) t -> q p c t", c=6, p=128)
    segs = [(0, 256, 0), (256, 128, 1), (384, 128, 2), (512, 256, 3)]
    with P.phase():
        make_eps(K)
        nb = 2
        hf = [P.sb("po_hf%d" % i, [128, 768], F32) for i in range(nb)]
        hb = [P.sb("po_hb%d" % i, [128, 768], F32) for i in range(nb)]
        gt = [P.sb("po_g%d" % i, [128, 768], F32) for i in range(nb)]
        junk = P.sb("po_junk", [128, 768], F32)
        ss = [P.sb("po_ss%d" % i, [128, 4], F32) for i in range(nb)]
        t1 = [P.sb("po_t1%d" % i, [128, 768], F32) for i in range(nb)]
        yb = [P.sb("po_y%d" % i, [128, 768], F32) for i in range(nb)]
        yt = [P.sb("po_yt%d" % i, [128, 6, 128], BF16) for i in range(nb)]
        ng = P.sb("po_ng", [128, 768], F32)
        P.op("dve", lambda e: e.tensor_copy(ng[:, 0:256], K.spt[:, spo["ang"] + l * 256: spo["ang"] + (l + 1) * 256]), K.spt.res(), ng.res())
        P.op("dve", lambda e: e.tensor_copy(ng[:, 256:512], K.spt[:, spo["bng"] + l * 256: spo["bng"] + (l + 1) * 256]), K.spt.res(), ng.res())
        P.op("dve", lambda e: e.tensor_copy(ng[:, 512:768], K.spt[:, spo["cng"] + l * 256: spo["cng"] + (l + 1) * 256]), K.spt.res(), ng.res())
        for c in range(NCH):
            cs = c * 128
            i = c % nb
            P.dma(hf[i][:], K.H[0][cs:cs + 128, :], K.H[0].res(), hf[i].res())
            P.dma(hb[i][:], K.H[1][cs:cs + 128, :], K.H[1].res(), hb[i].res())
            P.dma(gt[i][:, 0:256], K.tm["ao"][cs:cs + 128, :], K.tm["ao"].res(), gt[i].res())
            P.dma(gt[i][:, 256:512], K.tm["bz"][cs:cs + 128, :], K.tm["bz"].res(), gt[i].res())
            P.dma(gt[i][:, 512:768], K.tm["cr"][cs:cs + 128, :], K.tm["cr"].res(), gt[i].res())
            P.op("dve", lambda e, i=i: e.tensor_tensor(hf[i][:], hf[i][:], hb[i][:], ALU.add), hf[i].res() + hb[i].res(), hf[i].res())
            if K.post_lvl < 2:
                continue
            for si, (c0, w, _) in enumerate(segs):
                P.op("act", lambda e, i=i, c0=c0, w=w: e.activation(junk[:, c0:c0 + w], hf[i][:, c0:c0 + w], AF.Square), hf[i].res(), junk.res())
                P.op("dve", lambda e, i=i, c0=c0, w=w, si=si: e.reduce_sum(ss[i][:, si:si + 1], junk[:, c0:c0 + w], mybir.AxisListType.X), junk.res(), ss[i].res())
            for si, (c0, w, _) in enumerate(segs):
                P.op("act", lambda e, i=i, si=si, w=w: e.activation(ss[i][:, si:si + 1], ss[i][:, si:si + 1], AF.Sqrt, bias=K.epst[:, 0:1], scale=1.0 / w),
                     ss[i].res() + K.epst.res(), ss[i].res())
            P.op("dve", lambda e, i=i: e.reciprocal(ss[i][:], ss[i][:]), ss[i].res(), ss[i].res())
            for si, (c0, w, _) in enumerate(segs):
                P.op("dve", lambda e, i=i, c0=c0, w=w, si=si: e.scalar_tensor_tensor(t1[i][:, c0:c0 + w], hf[i][:, c0:c0 + w], ss[i][:, si:si + 1], ng[:, c0:c0 + w], ALU.mult, ALU.mult),
                     hf[i].res() + ss[i].res() + ng.res(), t1[i].res())
            if K.post_lvl < 3:
                continue
            P.op("act", lambda e, i=i: e.activation(gt[i][:, 0:256], gt[i][:, 0:256], AF.Sigmoid), gt[i].res(), gt[i].res())
            P.op("act", lambda e, i=i: e.activation(gt[i][:, 256:768], gt[i][:, 256:768], AF.Silu), gt[i].res(), gt[i].res())
            P.op("dve", lambda e, i=i: e.tensor_tensor(yb[i][:], t1[i][:], gt[i][:], ALU.mult), t1[i].res() + gt[i].res(), yb[i].res())
            if K.post_lvl < 4:
                continue
            for g in range(2):
                ps = psum(K)
                for k_ in range(3):
                    ct = g * 3 + k_
                    P.op("pe", lambda e, ps=ps, k_=k_, ct=ct, i=i: e.transpose(ps[:, k_ * 128:(k_ + 1) * 128], yb[i][:, ct * 128:(ct + 1) * 128], K.cst[:, 0:128]),
                         yb[i].res() + K.cst.res(), ps.res())
                if g == 0:
                    P.op("act", lambda e, ps=ps, i=i, g=g: e.copy(yt[i][:, g * 3:(g + 1) * 3, :], ps[:, 0:384]), ps.res(), yt[i].res())
                else:
                    P.op("dve", lambda e, ps=ps, i=i, g=g: e.tensor_copy(yt[i][:, g * 3:(g + 1) * 3, :], ps[:, 0:384]), ps.res(), yt[i].res())
            tb, off = cs // YP, cs % YP
            if "post_nodma" not in K.debug:
                P.dma(ypv[tb][:, :, off:off + 128], yt[i][:], yt[i].res(), K.yp.res(tb))


def b_mixers(K, l):
    P = K.P
    which = K.which
    if "c" in which:
        gla_prep(K, l)
    if "a" in which:
        mlstm_prep(K, l)
    if "b" in which:
        gdn_prep(K, l)
    with P.phase():
        streams = []
        if "c" in which:
            streams += [gla_stream(K, l, 0), gla_stream(K, l, 1)]
        if "a" in which:
            streams += [mlstm_stream(K, l, 0), mlstm_stream(K, l, 1)]
        if "b" in which:
            streams += [gdn_stream(K, l, hh, d) for hh in range(2) for d in range(2)]
        live = list(streams)
        while live:
            nxt = []
            for g in live:
                try:
                    next(g)
                    nxt.append(g)
                except StopIteration:
                    pass
            live = nxt
    if K.post:
        b_post(K, l)


def gdn_prep(K, l):
    P, cfg, spo = K.P, K.cfg, K.spo
    S, NCH, PT = cfg.S, cfg.NCH, cfg.PT
    if not hasattr(K, "gq"):
        K.gq = [P.dram("gdn_q%d" % h, [128, S], BF16) for h in range(2)]
        K.gk = [P.dram("gdn_k%d" % h, [128, S], BF16) for h in range(2)]
        K.gktm = [P.dram("gdn_ktm%d" % h, [S, 128], BF16) for h in range(2)]
        K.gvtm = [P.dram("gdn_vtm%d" % h, [S, 128], F32) for h in range(2)]
        K.grow = [[P.dram("gdn_row%d%d" % (h, d), [2, S], F32) for d in range(2)] for h in range(2)]
        K.gcol = [[P.dram("gdn_col%d%d" % (h, d), [128, 5 * NCH], F32) for d in range(2)] for h in range(2)]
        mk = lambda nm: [[P.dram("gdn_%s%d%d" % (nm, h, d), [S, 128], BF16, nparts=NCH) for d in range(2)] for h in range(2)]
        K.gTT, K.gAQ, K.gQE, K.gKD = mk("tt"), mk("aq"), mk("qe"), mk("kd")
    ones = K.cst[:, 128:256]
    with P.phase():
        make_eps(K)
        xh = [P.sb("g1_xh%d" % i, [128, PT + 4], F32) for i in range(2)]
        acc = [P.sb("g1_acc%d" % i, [128, PT], F32) for i in range(2)]
        sq = [P.sb("g1_sq%d" % i, [128, PT], F32) for i in range(2)]
        rn = [P.sb("g1_rn%d" % i, [128, PT], F32) for i in range(2)]
        ob = [P.sb("g1_ob%d" % i, [128, PT], BF16) for i in range(2)]
        tmo = [P.sb("g1_tm%d" % i, [128, 128], BF16) for i in range(2)]
        tvo = [P.sb("g1_tv%d" % i, [128, 128], F32) for i in range(2)]
        n = 0
        for ti in range(S // PT):
            g0 = ti * PT
            lo, hi = max(0, g0 - 2), min(S, g0 + PT + 2)
            for hh in range(2):
                for part in range(3):
                    blk = part * 2 + hh
                    x_, a_, s_, r_, o_ = xh[n % 2], acc[n % 2], sq[n % 2], rn[n % 2], ob[n % 2]
                    n += 1
                    if g0 == 0:
                        P.op("dve", lambda e, x_=x_: e.memset(x_[:, 0:2], 0.0), (), x_.res())
                    if g0 + PT == S:
                        P.op("dve", lambda e, x_=x_: e.memset(x_[:, PT + 2:PT + 4], 0.0), (), x_.res())
                    P.dma(x_[:, lo - (g0 - 2): hi - (g0 - 2)], K.fm32[blk * 128:(blk + 1) * 128, lo:hi], K.fm32.res(blk), x_.res())
                    cw = spo["convw"] + (l * 6 + blk) * 5
                    P.op("dve", lambda e, x_=x_, a_=a_, cw=cw: e.tensor_scalar(a_[:], x_[:, 0:PT], K.spt[:, cw:cw + 1], None, ALU.mult), x_.res() + K.spt.res(), a_.res())
                    for tap in range(1, 5):
                        P.op("dve", lambda e, x_=x_, a_=a_, cw=cw, tap=tap: e.scalar_tensor_tensor(a_[:], x_[:, tap:tap + PT], K.spt[:, cw + tap:cw + tap + 1], a_[:], ALU.mult, ALU.add),
                             x_.res() + K.spt.res() + a_.res(), a_.res())
                    cb = spo["convb"] + l * 6 + blk
                    P.op("act", lambda e, a_=a_, cb=cb: e.activation(a_[:], a_[:], AF.Silu, bias=K.spt[:, cb:cb + 1]), a_.res() + K.spt.res(), a_.res())
                    if part < 2:
                        P.op("act", lambda e, a_=a_, s_=s_: e.activation(s_[:], a_[:], AF.Square), a_.res(), s_.res())
                        ps = psum(K)
                        P.op("pe", lambda e, ps=ps, s_=s_: e.matmul(ps[:, 0:PT], ones, s_[:], start=True, stop=True), s_.res() + K.cst.res(), ps.res())
                        P.op("act", lambda e, ps=ps, r_=r_: e.activation(r_[:], ps[:, 0:PT], AF.Sqrt, bias=K.epst[:, 0:1], scale=1.0), ps.res() + K.epst.res(), r_.res())
                        P.op("dve", lambda e, r_=r_: e.reciprocal(r_[:], r_[:]), r_.res(), r_.res())
                        sc = B_DK ** -0.5 if part == 0 else 1.0
                        P.op("dve", lambda e, a_=a_, r_=r_, o_=o_, sc=sc: e.scalar_tensor_tensor(o_[:], a_[:], sc, r_[:], ALU.mult, ALU.mult), a_.res() + r_.res(), o_.res())
                        dst = (K.gq if part == 0 else K.gk)[hh]
                        P.dma(dst[:, g0:g0 + PT], o_[:], o_.res(), dst.res())
                        if part == 1:
                            for sub in range(PT // 128):
                                t_ = tmo[sub % 2]
                                tr_bf(K, t_[:], t_.res(), o_[:, sub * 128:(sub + 1) * 128], o_.res())
                                P.dma(K.gktm[hh][g0 + sub * 128: g0 + (sub + 1) * 128, :], t_[:], t_.res(), K.gktm[hh].res())
                    else:
                        for sub in range(PT // 128):
                            t_ = tvo[sub % 2]
                            ps = psum(K)
                            P.op("pe", lambda e, ps=ps, a_=a_, sub=sub: e.transpose(ps[:, 0:128], a_[:, sub * 128:(sub + 1) * 128], K.cst[:, 0:128]), a_.res() + K.cst.res(), ps.res())
                            P.op("act", lambda e, ps=ps, t_=t_: e.copy(t_[:], ps[:, 0:128]), ps.res(), t_.res())
                            P.dma(K.gvtm[hh][g0 + sub * 128: g0 + (sub + 1) * 128, :], t_[:], t_.res(), K.gvtm[hh].res())
    with P.phase():
        for hh in range(2):
            for d in range(2):
                tg = "g2_%d%d" % (hh, d)
                xg = P.sb(tg + "xg", [NCH, 128], F32)
                xb = P.sb(tg + "xb", [NCH, 128], F32)
                gp = P.sb(tg + "gp", [NCH, 128], F32)
                m5 = P.sb(tg + "m5", [NCH, 5, 128], F32)
                row = P.sb(tg + "row", [NCH, 2, 128], F32)
                sc_ = P.sb(tg + "sc", [128, 4], F32)
                colt = P.sb(tg + "col", [128, 5, NCH], F32)
                rb = 8 * 128 + 4
                P.dma(xg[:], K.fm32[rb + d * 2 + hh: rb + d * 2 + hh + 1, :].rearrange("o (c t) -> (o c) t", t=128), K.fm32.res(8), xg.res())
                P.dma(xb[:], K.fm32[rb + 4 + d * 2 + hh: rb + 4 + d * 2 + hh + 1, :].rearrange("o (c t) -> (o c) t", t=128), K.fm32.res(8), xb.res())
                ca = spo["gdn_alog"] + l * 4 + d * 2 + hh
                cd = spo["gdn_dtb"] + l * 4 + d * 2 + hh
                P.op("act", lambda e, sc_=sc_, ca=ca: e.activation(sc_[:, 0:1], K.spt[:, ca:ca + 1], AF.Exp), K.spt.res(), sc_.res())
                P.op("act", lambda e, xg=xg, cd=cd: e.activation(xg[:], xg[:], AF.Exp, bias=K.spt[0:NCH, cd:cd + 1]), xg.res() + K.spt.res(), xg.res())
                P.op("act", lambda e, xg=xg: e.activation(xg[:], xg[:], AF.Ln, bias=K.cst[0:NCH, 128:129]), xg.res() + K.cst.res(), xg.res())
                P.op("dve", lambda e, xg=xg, sc_=sc_: e.tensor_scalar(xg[:], xg[:], sc_[0:NCH, 0:1], None, ALU.mult), xg.res() + sc_.res(), xg.res())
                vw = (lambda ap: ap) if d == 0 else (lambda ap: ap[:, ::-1])
                P.op("dve", lambda e, xg=xg, gp=gp, vw=vw: e.tensor_tensor_scan(vw(gp[:, :]), vw(xg[:, :]), vw(xg[:, :]), 0.0, ALU.add, ALU.bypass), xg.res(), gp.res())
                P.op("act", lambda e, xb=xb: e.activation(xb[:], xb[:], AF.Exp, scale=-1.0), xb.res(), xb.res())
                P.op("act", lambda e, xb=xb: e.activation(xb[:], xb[:], AF.Ln, bias=K.cst[0:NCH, 128:129]), xb.res() + K.cst.res(), xb.res())
                P.op("dve", lambda e, gp=gp, row=row: e.tensor_scalar(row[:, 0, :], gp[:], -1.0, None, ALU.mult), gp.res(), row.res())
                P.op("dve", lambda e, gp=gp, xb=xb, row=row: e.scalar_tensor_tensor(row[:, 1, :], gp[:], -1.0, xb[:], ALU.mult, ALU.subtract), gp.res() + xb.res(), row.res())
                for r_ in range(2):
                    P.dma(K.grow[hh][d][r_:r_ + 1, :].rearrange("o (c t) -> (o c) t", t=128), row[:, r_, :], row.res(), K.grow[hh][d].res())
                edge = 127 if d == 0 else 0
                P.op("dve", lambda e, gp=gp, m5=m5: e.tensor_copy(m5[:, 0, :], gp[:]), gp.res(), m5.res())
                P.op("act", lambda e, gp=gp, m5=m5: e.activation(m5[:, 1, :], gp[:], AF.Exp, scale=-1.0), gp.res(), m5.res())
                P.op("dve", lambda e, m5=m5: e.tensor_scalar(m5[:, 1, :], m5[:, 1, :], -1.0, None, ALU.mult), m5.res(), m5.res())
                P.op("act", lambda e, xb=xb, m5=m5: e.activation(m5[:, 2, :], xb[:], AF.Exp, scale=-1.0), xb.res(), m5.res())
                P.op("dve", lambda e, gp=gp, sc_=sc_, edge=edge: e.tensor_scalar(sc_[0:NCH, 1:2], gp[:, edge:edge + 1], -1.0, None, ALU.mult), gp.res(), sc_.res())
                P.op("act", lambda e, gp=gp, m5=m5, sc_=sc_: e.activation(m5[:, 3, :], gp[:], AF.Exp, bias=sc_[0:NCH, 1:2]), gp.res() + sc_.res(), m5.res())
                P.op("act", lambda e, gp=gp, m5=m5, edge=edge: e.activation(m5[:, 4, :], gp[:, edge:edge + 1].to_broadcast([NCH, 128]), AF.Exp, scale=-1.0), gp.res(), m5.res())
                for q_ in range(5):
                    ps = psum(K)
                    P.op("pe", lambda e, ps=ps, m5=m5, q_=q_: e.transpose(ps[:, 0:NCH], m5[:, q_, :], K.cst[0:NCH, 0:NCH]), m5.res() + K.cst.res(), ps.res())
                    P.op("act", lambda e, ps=ps, colt=colt, q_=q_: e.copy(colt[:, q_, :], ps[:, 0:NCH]), ps.res(), colt.res())
                P.dma(K.gcol[hh][d][:, :], colt[:].rearrange("p a c -> p (a c)"), colt.res(), K.gcol[hh][d].res())
    with P.phase():
        streams = [gdn_solve_stream(K, l, hh, d, pt, 2) for hh in range(2) for d in range(2) for pt in range(2)]
        live = list(streams)
        while live:
            nxt = []
            for g in live:
                try:
                    next(g)
                    nxt.append(g)
                except StopIteration:
                    pass
            live = nxt


def gdn_solve_stream(K, l, hh, d, part=0, nparts=1):
    P, cfg = K.P, K.cfg
    S, NCH = cfg.S, cfg.NCH
    tg = "gs%d%d%d" % (hh, d, part)
    colt = P.sb(tg + "col", [128, 5, NCH], F32)
    P.dma(colt[:].rearrange("p a c -> p (a c)"), K.gcol[hh][d][:, :], K.gcol[hh][d].res(), colt.res())
    nb = 2
    kT = [P.sb(tg + "kT%d" % i, [128, 128], BF16) for i in range(nb)]
    qT = [P.sb(tg + "qT%d" % i, [128, 128], BF16) for i in range(nb)]
    ktm = [P.sb(tg + "ktm%d" % i, [128, 128], BF16) for i in range(nb)]
    rows = [P.sb(tg + "rw%d" % i, [1, 2, 128], F32) for i in range(nb)]
    EA = P.sb(tg + "EA", [128, 128], F32)
    EQ = P.sb(tg + "EQ", [128, 128], F32)
    EG = P.sb(tg + "EG", [128, 128], F32)
    Nm = P.sb(tg + "N", [128, 128], F32)
    Pm = [P.sb(tg + "P%d" % i, [128, 128], F32) for i in range(2)]
    Qm = [P.sb(tg + "Q%d" % i, [128, 128], F32) for i in range(2)]
    Xm = [P.sb(tg + "X%d" % i, [128, 128], F32) for i in range(2)]
    obuf = [P.sb(tg + "o%d" % i, [128, 4, 128], BF16) for i in range(nb)]
    ones_row = K.cst[0:1, 128:256]
    ident = K.cst[:, 0:128]
    neg_incl = K.cst[:, (2 + 2 * d) * 128:(3 + 2 * d) * 128]
    neg_strict = K.cst[:, (3 + 2 * d) * 128:(4 + 2 * d) * 128]
    n = 0
    for c in range(part, NCH, nparts):
        cs = c * 128
        i = n % nb
        n += 1
        P.dma(kT[i][:], K.gk[hh][:, cs:cs + 128], K.gk[hh].res(), kT[i].res())
        P.dma(qT[i][:], K.gq[hh][:, cs:cs + 128], K.gq[hh].res(), qT[i].res())
        P.dma(ktm[i][:], K.gktm[hh][cs:cs + 128, :], K.gktm[hh].res(), ktm[i].res())
        P.dma(rows[i][:], K.grow[hh][d][:, cs:cs + 128].rearrange("(o r) t -> o r t", o=1), K.grow[hh][d].res(), rows[i].res())
        gcol = colt[:, 0, c:c + 1]
        psA = psum(K)
        P.op("pe", lambda e, i=i, psA=psA: e.matmul(psA[:, 0:128], ones_row, rows[i][:, 1, :], start=True, stop=False), rows[i].res() + K.cst.res(), psA.res())
        P.op("pe", lambda e, psA=psA: e.matmul(psA[:, 0:128], ident, neg_strict, start=False, stop=True), K.cst.res(), psA.res())
        P.op("act", lambda e, psA=psA, gcol=gcol: e.activation(EA[:], psA[:, 0:128], AF.Exp, bias=gcol), psA.res() + colt.res(), EA.res())
        psK = psum(K)
        P.op("pe", lambda e, i=i, psK=psK: e.matmul(psK[:, 0:128], kT[i][:], kT[i][:], start=True, stop=True), kT[i].res(), psK.res())
        P.op("dve", lambda e, psK=psK: e.tensor_tensor(Nm[:], psK[:, 0:128], EA[:], ALU.mult), psK.res() + EA.res(), Nm.res())
        psQ = psum(K)
        P.op("pe", lambda e, i=i, psQ=psQ: e.matmul(psQ[:, 0:128], ones_row, rows[i][:, 0, :], start=True, stop=False), rows[i].res() + K.cst.res(), psQ.res())
        P.op("pe", lambda e, psQ=psQ: e.matmul(psQ[:, 0:128], ident, neg_incl, start=False, stop=True), K.cst.res(), psQ.res())
        P.op("act", lambda e, psQ=psQ, gcol=gcol: e.activation(EQ[:], psQ[:, 0:128], AF.Exp, bias=gcol), psQ.res() + colt.res(), EQ.res())
        psKQ = psum(K)
        P.op("pe", lambda e, i=i, psKQ=psKQ: e.matmul(psKQ[:, 0:128], kT[i][:], qT[i][:], start=True, stop=True), kT[i].res() + qT[i].res(), psKQ.res())
        P.op("dve", lambda e, i=i, psKQ=psKQ: e.tensor_tensor(obuf[i][:, 1, :], psKQ[:, 0:128], EQ[:], ALU.mult), psKQ.res() + EQ.res(), obuf[i].res())
        psG = psum(K)
        P.op("pe", lambda e, i=i, psG=psG: e.matmul(psG[:, 0:128], ones_row, rows[i][:, 0, :], start=True, stop=True), rows[i].res() + K.cst.res(), psG.res())
        P.op("act", lambda e, psG=psG: e.activation(EG[:], psG[:, 0:128], AF.Exp), psG.res(), EG.res())
        P.op("dve", lambda e, i=i: e.tensor_tensor(obuf[i][:, 2, :], qT[i][:], EG[:], ALU.mult), qT[i].res() + EG.res(), obuf[i].res())
        P.op("dve", lambda e, i=i, c=c: e.tensor_scalar(obuf[i][:, 3, :], ktm[i][:], colt[:, 3, c:c + 1], None, ALU.mult), ktm[i].res() + colt.res(), obuf[i].res())
        ps = psum(K)
        P.op("pe", lambda e, ps=ps: e.transpose(ps[:, 0:128], Nm[:], ident), Nm.res() + K.cst.res(), ps.res())
        P.op("act", lambda e, ps=ps: e.copy(Qm[0][:], ps[:, 0:128]), ps.res(), Qm[0].res())
        P.op("dve", lambda e: e.tensor_tensor(Xm[0][:], ident, Nm[:], ALU.subtract), Nm.res() + K.cst.res(), Xm[0].res())
        Pc, Qc, Xc = Nm, Qm[0], Xm[0]
        for k in range(1, 7):
            Qn = Qm[k % 2]
            psq = psum(K)
            P.op("pe", lambda e, psq=psq, Pc=Pc, Qc=Qc: e.matmul(psq[:, 0:128], Pc[:], Qc[:], start=True, stop=True), Pc.res() + Qc.res(), psq.res())
            if k < 6:
                Pn = Pm[k % 2]
                psp = psum(K)
                P.op("pe", lambda e, psp=psp, Pc=Pc, Qc=Qc: e.matmul(psp[:, 0:128], Qc[:], Pc[:], start=True, stop=True), Pc.res() + Qc.res(), psp.res())
            P.op("act", lambda e, psq=psq, Qn=Qn: e.copy(Qn[:], psq[:, 0:128]), psq.res(), Qn.res())
            if k < 6:
                P.op("act", lambda e, psp=psp, Pn=Pn: e.copy(Pn[:], psp[:, 0:128]), psp.res(), Pn.res())
            Xn = Xm[k % 2]
            psx = psum(K)
            P.op("pe", lambda e, psx=psx, Qn=Qn, Xc=Xc: e.matmul(psx[:, 0:128], Qn[:], Xc[:], start=True, stop=True), Qn.res() + Xc.res(), psx.res())
            P.op("dve", lambda e, psx=psx, Xn=Xn, Xc=Xc: e.tensor_tensor(Xn[:], psx[:, 0:128], Xc[:], ALU.add), psx.res() + Xc.res(), Xn.res())
            Qc, Xc = Qn, Xn
            if k < 6:
                Pc = Pn
        P.op("dve", lambda e, i=i, Xc=Xc, c=c: e.tensor_scalar(obuf[i][:, 0, :], Xc[:], colt[:, 2, c:c + 1], None, ALU.mult), Xc.res() + colt.res(), obuf[i].res())
        for q_, dst in enumerate((K.gTT, K.gAQ, K.gQE, K.gKD)):
            P.dma(dst[hh][d][cs:cs + 128, :], obuf[i][:, q_, :], obuf[i].res(), dst[hh][d].res(c))
        yield


def gdn_stream(K, l, hh, d):
    P, cfg = K.P, K.cfg
    S, NCH = cfg.S, cfg.NCH
    tg = "gr%d%d" % (hh, d)
    colt = P.sb(tg + "col", [128, 5, NCH], F32)
    P.dma(colt[:].rearrange("p a c -> p (a c)"), K.gcol[hh][d][:, :], K.gcol[hh][d].res(), colt.res())
    st = P.sb(tg + "st", [128, 128], F32)
    stb = P.sb(tg + "stb", [128, 128], BF16)
    P.op("dve", lambda e: e.memset(st[:], 0.0), (), st.res())
    P.op("dve", lambda e: e.memset(stb[:], 0.0), (), stb.res())
    nb = 2
    kT = [P.sb(tg + "kT%d" % i, [128, 128], BF16) for i in range(nb)]
    mats = [P.sb(tg + "m%d" % i, [128, 4, 128], BF16) for i in range(nb)]
    v = [P.sb(tg + "v%d" % i, [128, 128], F32) for i in range(nb)]
    Rb = [P.sb(tg + "R%d" % i, [128, 128], BF16) for i in range(nb)]
    Ub = [P.sb(tg + "U%d" % i, [128, 128], BF16) for i in range(nb)]
    o = [P.sb(tg + "o%d" % i, [128, 128], F32) for i in range(nb)]
    order = range(NCH) if d == 0 else range(NCH - 1, -1, -1)
    hc0 = 256 + hh * 128
    n = 0
    for c in order:
        cs = c * 128
        i = n % nb
        n += 1
        P.dma(kT[i][:], K.gk[hh][:, cs:cs + 128], K.gk[hh].res(), kT[i].res())
        for q_, src in enumerate((K.gTT, K.gAQ, K.gQE, K.gKD)):
            P.dma(mats[i][:, q_, :], src[hh][d][cs:cs + 128, :], src[hh][d].res(c), mats[i].res())
        P.dma(v[i][:], K.gvtm[hh][cs:cs + 128, :], K.gvtm[hh].res(), v[i].res())
        ps1 = psum(K)
        P.op("pe", lambda e, i=i, ps1=ps1: e.matmul(ps1[:, 0:128], kT[i][:], stb[:], start=True, stop=True), kT[i].res() + stb.res(), ps1.res())
        P.op("dve", lambda e, i=i, ps1=ps1, c=c: e.scalar_tensor_tensor(Rb[i][:], ps1[:, 0:128], colt[:, 1, c:c + 1], v[i][:], ALU.mult, ALU.add), ps1.res() + colt.res() + v[i].res(), Rb[i].res())
        ps2 = psum(K)
        P.op("pe", lambda e, i=i, ps2=ps2: e.matmul(ps2[:, 0:128], mats[i][:, 0, :], Rb[i][:], start=True, stop=True), mats[i].res() + Rb[i].res(), ps2.res())
        P.op("act", lambda e, i=i, ps2=ps2: e.copy(Ub[i][:], ps2[:, 0:128]), ps2.res(), Ub[i].res())
        ps3 = psum(K)
        P.op("pe", lambda e, i=i, ps3=ps3: e.matmul(ps3[:, 0:128], mats[i][:, 1, :], Ub[i][:], start=True, stop=False), mats[i].res() + Ub[i].res(), ps3.res())
        P.op("pe", lambda e, i=i, ps3=ps3: e.matmul(ps3[:, 0:128], mats[i][:, 2, :], stb[:], start=False, stop=True), mats[i].res() + stb.res(), ps3.res())
        P.op("act", lambda e, i=i, ps3=ps3: e.copy(o[i][:], ps3[:, 0:128]), ps3.res(), o[i].res())
        P.dma(K.H[d][cs:cs + 128, hc0:hc0 + 128], o[i][:], o[i].res(), K.H[d].res(1 + hh))
        ps4 = psum(K)
        P.op("pe", lambda e, i=i, ps4=ps4: e.matmul(ps4[:, 0:128], mats[i][:, 3, :], Ub[i][:], start=True, stop=True), mats[i].res() + Ub[i].res(), ps4.res())
        P.op("dve", lambda e, ps4=ps4, c=c: e.scalar_tensor_tensor(st[:], st[:], colt[:, 4, c:c + 1], ps4[:, 0:128], ALU.mult, ALU.add), st.res() + colt.res() + ps4.res(), st.res())
        P.op("act", lambda e: e.copy(stb[:], st[:]), st.res(), stb.res())
        yield
```
